# Optimizing a Trainium2 kernel written in Bass

```python
import jax
import jax.numpy as jnp
from jax import lax
import numpy as np

D_MODEL = 4096
BATCH = 4
SEQ = 2048
DEPTH = 1
DEC_BATCH = 128
DEC_SEQ = 4
PAST_LEN = 16384
PAGE_SIZE = 128

GLA_HEADS = 4
GLA_VAL = D_MODEL // 2
GLA_KEY = GLA_VAL // 2
GLA_DK = GLA_KEY // GLA_HEADS
GLA_DV = GLA_VAL // GLA_HEADS
GLA_RANK = 16
GLA_TAU = 16.0
GLA_CHUNK = 64
CONV_DIM = D_MODEL // 2
CONV_WIDTH = 31
N_MEM = 256
CA_HEADS = 4
CA_HEAD_DIM = D_MODEL // 16
CA_DIM = CA_HEADS * CA_HEAD_DIM
N_GROUPS = 4
EXPERTS_PER_GROUP = 8
N_EXPERTS = N_GROUPS * EXPERTS_PER_GROUP
TOP_K_IN_GROUP = 2
D_EXPERT = D_MODEL // 8
EPS = 1e-6

OFF_Q = 0
OFF_K = OFF_Q + GLA_KEY
OFF_V = OFF_K + GLA_KEY
OFF_R = OFF_V + GLA_VAL
OFF_A = OFF_R + GLA_VAL
OFF_U = OFF_A + GLA_RANK
OFF_G = OFF_U + 2 * CONV_DIM
IN_COLS = OFF_G + 2 * D_MODEL

kernel_name = 'gla_conformer_parallel_hmoe_decoder_step'

F32 = jnp.float32


def _rmsnorm(x, g):
    xf = x.astype(F32)
    y = xf * lax.rsqrt(jnp.mean(xf * xf, axis=-1, keepdims=True) + EPS)
    return (y * g.astype(F32)).astype(x.dtype)


def _layernorm(xf, g, b):
    mu = jnp.mean(xf, axis=-1, keepdims=True)
    xc = xf - mu
    var = jnp.mean(xc * xc, axis=-1, keepdims=True)
    return xc * lax.rsqrt(var + EPS) * g.astype(F32) + b.astype(F32)


def _gla(q, k, v, log_a, h0):
    B, T, H, _ = q.shape
    C = min(GLA_CHUNK, T)
    pad = (-T) % C
    n = (T + pad) // C

    def blocks(t):
        t = jnp.pad(t.astype(F32), ((0, 0), (0, pad), (0, 0), (0, 0)))
        return t.reshape(B, n, C, H, t.shape[-1]).transpose(0, 3, 1, 2, 4)

    q, k, v, log_a = blocks(q), blocks(k), blocks(v), blocks(log_a)
    b = jnp.cumsum(log_a, axis=3)
    b_last = b[:, :, :, -1:, :]
    q_e = q * jnp.exp(b)
    k_e = k * jnp.exp(-b)
    causal = jnp.tril(jnp.ones((C, C), dtype=bool))
    scores = jnp.where(causal, jnp.einsum('bhncd,bhnsd->bhncs', q_e, k_e), 0.0)
    o_intra = jnp.einsum('bhncs,bhnsv->bhncv', scores, v)
    d_state = jnp.einsum('bhnsd,bhnsv->bhndv', k * jnp.exp(b_last - b), v)
    decay = jnp.exp(b_last[:, :, :, 0, :])

    def step(S, inp):
        dec, ds = inp
        return dec[..., None] * S + ds, S

    S_T, S_prev = lax.scan(step, h0.astype(F32),
                           (jnp.moveaxis(decay, 2, 0), jnp.moveaxis(d_state, 2, 0)))
    S_prev = jnp.moveaxis(S_prev, 0, 2)
    o = o_intra + jnp.einsum('bhncd,bhndv->bhncv', q_e, S_prev)
    o = o.transpose(0, 2, 3, 1, 4).reshape(B, n * C, H, -1)[:, :T]
    return o, S_T


def _mixer(h, h0, buf, w_in, w_alpha_up, b_alpha, gla_norm_g, w_branch_a,
           conv_dw_w, conv_dw_b, conv_ln_g, conv_ln_b, w_branch_b, w_out):
    B, T, _ = h.shape
    z = h @ w_in
    q = z[..., OFF_Q:OFF_K].reshape(B, T, GLA_HEADS, GLA_DK) * (GLA_DK ** -0.5)
    k = z[..., OFF_K:OFF_V].reshape(B, T, GLA_HEADS, GLA_DK)
    v = z[..., OFF_V:OFF_R].reshape(B, T, GLA_HEADS, GLA_DV)
    r = z[..., OFF_R:OFF_A]
    a_low = z[..., OFF_A:OFF_U]
    u = z[..., OFF_U:OFF_G]
    gates = jax.nn.sigmoid(z[..., OFF_G:].astype(F32))
    log_a = jax.nn.log_sigmoid((a_low @ w_alpha_up + b_alpha).astype(F32)) / GLA_TAU
    log_a = log_a.reshape(B, T, GLA_HEADS, GLA_DK)
    o, h_T = _gla(q, k, v, log_a, h0)
    mu = jnp.mean(o, axis=-1, keepdims=True)
    oc = o - mu
    o_n = oc * lax.rsqrt(jnp.mean(oc * oc, axis=-1, keepdims=True) + EPS)
    o_n = o_n * gla_norm_g.astype(F32).reshape(GLA_HEADS, GLA_DV)
    o_g = o_n.reshape(B, T, GLA_VAL) * jax.nn.silu(r.astype(F32))
    branch_a = o_g.astype(h.dtype) @ w_branch_a
    ug = u[..., :CONV_DIM] * jax.nn.sigmoid(u[..., CONV_DIM:])
    ext = jnp.concatenate([buf.astype(ug.dtype), ug], axis=1)
    conv = lax.conv_general_dilated(ext, conv_dw_w[:, None, :].astype(ext.dtype),
                                    window_strides=(1,), padding='VALID',
                                    dimension_numbers=('NWC', 'WIO', 'NWC'),
                                    feature_group_count=CONV_DIM) + conv_dw_b
    c = jax.nn.silu(_layernorm(conv.astype(F32), conv_ln_g, conv_ln_b))
    branch_b = c.astype(h.dtype) @ w_branch_b
    merged = gates[..., :D_MODEL] * branch_a + gates[..., D_MODEL:] * branch_b
    y = merged.astype(h.dtype) @ w_out
    new_buf = ext[:, -(CONV_WIDTH - 1):]
    return y, h_T, new_buf


def _mem_kv(mem, norm_mem_g, w_ca_k, w_ca_v):
    B, M, _ = mem.shape
    m = _rmsnorm(mem, norm_mem_g)
    k = (m @ w_ca_k).reshape(B, M, CA_HEADS, CA_HEAD_DIM)
    v = (m @ w_ca_v).reshape(B, M, CA_HEADS, CA_HEAD_DIM)
    return k, v


def _cross_attn(h, mem_k, mem_v, w_ca_q, w_ca_o):
    B, T, _ = h.shape
    q = (h @ w_ca_q).reshape(B, T, CA_HEADS, CA_HEAD_DIM)
    s = jnp.einsum('bthd,bshd->bhts', q.astype(F32), mem_k.astype(F32)) * (CA_HEAD_DIM ** -0.5)
    p = jax.nn.softmax(s, axis=-1)
    o = jnp.einsum('bhts,bshd->bthd', p, mem_v.astype(F32)).reshape(B, T, CA_DIM)
    return o.astype(h.dtype) @ w_ca_o


def _hmoe(h, w_router_group, b_router_group, w_router_expert, b_router_expert,
          w_exp_gate, w_exp_up, w_exp_down):
    B, T, _ = h.shape
    hf = h.astype(F32)
    pg = jax.nn.softmax(hf @ w_router_group.astype(F32) + b_router_group.astype(F32), axis=-1)
    gsel = jnp.argmax(pg, axis=-1)
    pg_top = jnp.max(pg, axis=-1, keepdims=True)
    le = (hf @ w_router_expert.astype(F32) + b_router_expert.astype(F32)).reshape(
        B, T, N_GROUPS, EXPERTS_PER_GROUP)
    le_sel = jnp.einsum('btge,btg->bte', le, jax.nn.one_hot(gsel, N_GROUPS, dtype=F32))
    pe = jax.nn.softmax(le_sel, axis=-1)
    top_v, top_i = lax.top_k(pe, TOP_K_IN_GROUP)
    top_v = top_v / jnp.sum(top_v, axis=-1, keepdims=True)
    eid = gsel[..., None] * EXPERTS_PER_GROUP + top_i
    gate = jnp.sum(jax.nn.one_hot(eid, N_EXPERTS, dtype=F32) * (pg_top * top_v)[..., None],
                   axis=-2)
    out = jnp.zeros((B, T, D_MODEL), F32)
    for grp in range(N_GROUPS):
        sl = slice(grp * EXPERTS_PER_GROUP, (grp + 1) * EXPERTS_PER_GROUP)
        a = jnp.einsum('btd,edf->btef', h, w_exp_gate[sl])
        u = jnp.einsum('btd,edf->btef', h, w_exp_up[sl])
        hid = (jax.nn.silu(a) * u * gate[..., sl, None].astype(h.dtype)).astype(h.dtype)
        out = out + jnp.einsum('btef,efd->btd', hid, w_exp_down[sl])
    return out.astype(h.dtype)


def _block(x, mem_k, mem_v, h0, buf, norm_mix_g, w_in, w_alpha_up, b_alpha, gla_norm_g,
           w_branch_a, conv_dw_w, conv_dw_b, conv_ln_g, conv_ln_b, w_branch_b, w_out,
           norm_ca_g, w_ca_q, w_ca_o, norm_ffn_g, w_router_group, b_router_group,
           w_router_expert, b_router_expert, w_exp_gate, w_exp_up, w_exp_down):
    y, h_T, new_buf = _mixer(_rmsnorm(x, norm_mix_g), h0, buf, w_in, w_alpha_up, b_alpha,
                             gla_norm_g, w_branch_a, conv_dw_w, conv_dw_b, conv_ln_g,
                             conv_ln_b, w_branch_b, w_out)
    x = x + y
    x = x + _cross_attn(_rmsnorm(x, norm_ca_g), mem_k, mem_v, w_ca_q, w_ca_o)
    x = x + _hmoe(_rmsnorm(x, norm_ffn_g), w_router_group, b_router_group, w_router_expert,
                  b_router_expert, w_exp_gate, w_exp_up, w_exp_down)
    return x, h_T, new_buf


def setup_inputs(seed: int = 0) -> dict:
    key = jax.random.key(seed)
    ks = iter(jax.random.split(key, 40))

    def nrm(shape, scale):
        return jax.random.normal(next(ks), shape, F32) * scale

    def gain(shape):
        return 1.0 + nrm(shape, 0.02)

    L = DEPTH
    return {
        'x_prompt': nrm((BATCH, SEQ, D_MODEL), 1.0),
        'x_sample': nrm((DEC_BATCH, DEC_SEQ, D_MODEL), 1.0),
        'mem_prompt': nrm((BATCH, N_MEM, D_MODEL), 1.0),
        'state_gla': nrm((L, DEC_BATCH, GLA_HEADS, GLA_DK, GLA_DV), 0.5),
        'state_conv': nrm((L, DEC_BATCH, CONV_WIDTH - 1, CONV_DIM), 0.5),
        'cache_mem_k': nrm((L, DEC_BATCH, N_MEM, CA_HEADS, CA_HEAD_DIM), 1.0),
        'cache_mem_v': nrm((L, DEC_BATCH, N_MEM, CA_HEADS, CA_HEAD_DIM), 1.0),
        'norm_mix_g': gain((L, D_MODEL)),
        'w_in': nrm((L, D_MODEL, IN_COLS), D_MODEL ** -0.5),
        'w_alpha_up': nrm((L, GLA_RANK, GLA_KEY), GLA_RANK ** -0.5),
        'b_alpha': nrm((L, GLA_KEY), 0.1),
        'gla_norm_g': gain((L, GLA_VAL)),
        'w_branch_a': nrm((L, GLA_VAL, D_MODEL), GLA_VAL ** -0.5),
        'conv_dw_w': nrm((L, CONV_WIDTH, CONV_DIM), CONV_WIDTH ** -0.5),
        'conv_dw_b': nrm((L, CONV_DIM), 0.02),
        'conv_ln_g': gain((L, CONV_DIM)),
        'conv_ln_b': nrm((L, CONV_DIM), 0.02),
        'w_branch_b': nrm((L, CONV_DIM, D_MODEL), CONV_DIM ** -0.5),
        'w_out': nrm((L, D_MODEL, D_MODEL), D_MODEL ** -0.5),
        'norm_ca_g': gain((L, D_MODEL)),
        'norm_mem_g': gain((L, D_MODEL)),
        'w_ca_q': nrm((L, D_MODEL, CA_DIM), D_MODEL ** -0.5),
        'w_ca_k': nrm((L, D_MODEL, CA_DIM), D_MODEL ** -0.5),
        'w_ca_v': nrm((L, D_MODEL, CA_DIM), D_MODEL ** -0.5),
        'w_ca_o': nrm((L, CA_DIM, D_MODEL), CA_DIM ** -0.5),
        'norm_ffn_g': gain((L, D_MODEL)),
        'w_router_group': nrm((L, D_MODEL, N_GROUPS), D_MODEL ** -0.5),
        'b_router_group': nrm((L, N_GROUPS), 0.01),
        'w_router_expert': nrm((L, D_MODEL, N_EXPERTS), D_MODEL ** -0.5),
        'b_router_expert': nrm((L, N_EXPERTS), 0.01),
        'w_exp_gate': nrm((L, N_EXPERTS, D_MODEL, D_EXPERT), D_MODEL ** -0.5),
        'w_exp_up': nrm((L, N_EXPERTS, D_MODEL, D_EXPERT), D_MODEL ** -0.5),
        'w_exp_down': nrm((L, N_EXPERTS, D_EXPERT, D_MODEL), D_EXPERT ** -0.5),
        'norm_final_g': gain((D_MODEL,)),
    }


def reference(x_prompt, x_sample, mem_prompt, state_gla, state_conv, cache_mem_k, cache_mem_v,
              norm_mix_g, w_in, w_alpha_up, b_alpha, gla_norm_g, w_branch_a, conv_dw_w,
              conv_dw_b, conv_ln_g, conv_ln_b, w_branch_b, w_out, norm_ca_g, norm_mem_g,
              w_ca_q, w_ca_k, w_ca_v, w_ca_o, norm_ffn_g, w_router_group, b_router_group,
              w_router_expert, b_router_expert, w_exp_gate, w_exp_up, w_exp_down,
              norm_final_g):
    bp = x_prompt.shape[0]
    xp, xs = x_prompt, x_sample
    gla_p, conv_p, mk_p, mv_p, gla_s, conv_s = [], [], [], [], [], []
    for l in range(DEPTH):
        lw = dict(norm_mix_g=norm_mix_g[l], w_in=w_in[l], w_alpha_up=w_alpha_up[l],
                  b_alpha=b_alpha[l], gla_norm_g=gla_norm_g[l], w_branch_a=w_branch_a[l],
                  conv_dw_w=conv_dw_w[l], conv_dw_b=conv_dw_b[l], conv_ln_g=conv_ln_g[l],
                  conv_ln_b=conv_ln_b[l], w_branch_b=w_branch_b[l], w_out=w_out[l],
                  norm_ca_g=norm_ca_g[l], w_ca_q=w_ca_q[l], w_ca_o=w_ca_o[l],
                  norm_ffn_g=norm_ffn_g[l], w_router_group=w_router_group[l],
                  b_router_group=b_router_group[l], w_router_expert=w_router_expert[l],
                  b_router_expert=b_router_expert[l], w_exp_gate=w_exp_gate[l],
                  w_exp_up=w_exp_up[l], w_exp_down=w_exp_down[l])
        mk, mv = _mem_kv(mem_prompt, norm_mem_g[l], w_ca_k[l], w_ca_v[l])
        h0_p = jnp.zeros((bp, GLA_HEADS, GLA_DK, GLA_DV), F32)
        buf_p = jnp.zeros((bp, CONV_WIDTH - 1, CONV_DIM), x_prompt.dtype)
        xp, hp, bfp = _block(xp, mk, mv, h0_p, buf_p, **lw)
        xs, hs, bfs = _block(xs, cache_mem_k[l], cache_mem_v[l], state_gla[l], state_conv[l], **lw)
        gla_p.append(hp.astype(state_gla.dtype))
        conv_p.append(bfp.astype(state_conv.dtype))
        mk_p.append(mk.astype(cache_mem_k.dtype))
        mv_p.append(mv.astype(cache_mem_v.dtype))
        gla_s.append(hs.astype(state_gla.dtype))
        conv_s.append(bfs.astype(state_conv.dtype))
    y_prompt = _rmsnorm(xp, norm_final_g)
    y_sample = _rmsnorm(xs, norm_final_g)
    return (y_prompt, y_sample, jnp.stack(gla_p), jnp.stack(conv_p), jnp.stack(mk_p),
            jnp.stack(mv_p), jnp.stack(gla_s), jnp.stack(conv_s))
```

```python
import contextlib
import numpy as np
import concourse.bass as bass
import concourse.mybir as mybir
from concourse.bass_utils import run_bass_kernel_spmd

F32 = mybir.dt.float32
BF16 = mybir.dt.bfloat16
U32 = mybir.dt.uint32
I32 = mybir.dt.int32
AF = mybir.ActivationFunctionType
ALU = mybir.AluOpType
AX = mybir.AxisListType

D = 4096
NCORE = 8
NP = 1024
NS = 64
NT = NP + NS
NSEQ = 16
IN_COLS = 18448
OFF_Q, OFF_K, OFF_V, OFF_R, OFF_A, OFF_U, OFF_G = 0, 1024, 2048, 4096, 6144, 6160, 10256
EPS = 1e-6
CAP = 256
NE = 32
ZROW = NT
DUMP = 2 * NT
NGRP = [(0, 512), (512, 512), (1024, 64)]
TILES = [(i * 128, 128) for i in range(8)] + [(1024, 64)]

C_ID, C_MU, C_MS, C_RM, C_IO, C_TK = 0, 128, 256, 320, 336, 592
CWP = 619
C_BM, C_RS, C_LS, C_ON = 619, 1643, 2731, 2859
CW = 2987


def make_consts():
    c = np.zeros((128, CW), np.float32)
    p = np.arange(128)
    c[:, C_ID:C_ID + 128] = np.eye(128)
    c[:, C_MU:C_MU + 128] = (p[:, None] <= p[None, :])
    q = np.arange(64)
    ms = ((q[:, None] // 4) == (q[None, :] // 4)) & (q[:, None] <= q[None, :])
    c[:64, C_MS:C_MS + 64] = ms
    bm = (np.arange(16)[:, None] == (q[None, :] // 4)).astype(np.float32)
    c[:, C_BM:C_BM + 1024] = bm.reshape(1, 1024)
    c[:64, C_RM:C_RM + 16] = ((q[:, None] // 4) == np.arange(16)[None, :])
    t = np.arange(NT)
    rs = np.ones(NT, np.float32)
    rs[:NP][t[:NP] % 128 == 0] = 0.0
    rs[NP:][(t[NP:] - NP) % 4 == 0] = 0.0
    c[:, C_RS:C_RS + NT] = rs[None, :]
    c[:, C_IO:C_IO + 256] = np.arange(256)[None, :]
    for i in range(9):
        tok = i * 128 + p
        c[:, C_TK + 3 * i + 0] = tok // 32
        c[:, C_TK + 3 * i + 1] = tok % 32
        c[:, C_TK + 3 * i + 2] = 1.0
    c[:, C_LS:C_LS + 128] = (p[:, None] < p[None, :])
    c[:, C_ON:C_ON + 128] = 1.0
    return c


class Sched:
    def __init__(self, nc, es, ndma=28):
        self.nc = nc
        self.E = dict(pe=nc.tensor, act=nc.scalar, dve=nc.vector, pool=nc.gpsimd, sp=nc.sync)
        self.sem = {k: es.enter_context(nc.semaphore("c_" + k)) for k in self.E}
        self.cnt = {k: 0 for k in self.E}
        self.seen = {k: {} for k in self.E}
        self.dsem = [es.enter_context(nc.semaphore("dm%d" % i)) for i in range(ndma)]
        self.dval = [0] * ndma
        self.dnext = {"sp": 0, "pool": 0, "act": 0}
        self.dhalf = ndma // 2
        self.res = {}

    def _semobj(self, key):
        return self.sem[key] if isinstance(key, str) else self.dsem[key]

    def _wait(self, eng, tok):
        key, val, src = tok
        if self.seen[eng].get(key, 0) >= val:
            return
        self.E[eng].wait_ge(self._semobj(key), val)
        self.seen[eng][key] = val

    def _deps(self, eng, reads, writes):
        for r in reads:
            st = self.res.get(r)
            if st and st["w"]:
                tok = st["w"]
                if not (tok[2] == eng and eng == "pe"):
                    self._wait(eng, tok)
        for w in writes:
            st = self.res.get(w)
            if st:
                if st["w"] and st["w"][2] != eng:
                    self._wait(eng, st["w"])
                for tok in st["r"]:
                    if tok[2] != eng:
                        self._wait(eng, tok)

    def _commit(self, tok, reads, writes):
        for r in reads:
            st = self.res.setdefault(r, {"w": None, "r": []})
            if tok[2] != "dma":
                st["r"] = [x for x in st["r"] if x[2] != tok[2]]
            st["r"].append(tok)
        for w in writes:
            self.res[w] = {"w": tok, "r": []}

    mute = False

    def op(self, eng, fn, reads=(), writes=()):
        if self.mute:
            return
        self._deps(eng, reads, writes)
        ins = fn(self.E[eng])
        self.cnt[eng] += 1
        ins.then_inc(self.sem[eng], 1)
        self._commit((eng, self.cnt[eng], eng), reads, writes)

    def dma(self, q, out, in_, reads=(), writes=(), fn=None):
        if self.mute:
            return
        base = 0 if q == "pool" else self.dhalf
        i = base + self.dnext[q]
        self.dnext[q] = (self.dnext[q] + 1) % self.dhalf
        if self.dval[i] > 0:
            self._wait(q, (i, self.dval[i], "dma"))
        self._deps(q, reads, writes)
        if fn is None:
            ins = self.E[q].dma_start(out=out, in_=in_)
        else:
            ins = fn(self.E[q])
        ins.then_inc(self.dsem[i], 16)
        self.dval[i] += 16
        self._commit((i, self.dval[i], "dma"), reads, writes)

    def finish(self):
        for i, v in enumerate(self.dval):
            if v > 0:
                self._wait("sp", (i, v, "dma"))
        for k in ("pe", "act", "dve", "pool"):
            if self.cnt[k] > 0:
                self._wait("sp", (k, self.cnt[k], k))


class _Stop(Exception):
    pass


def build(stage=99, mute_until=None, lite=None):
    nc = bass.Bass("TRN2", target_bir_lowering=False)

    def stage_end(k):
        if mute_until is not None and k >= mute_until:
            S.mute = False
        if stage <= k:
            raise _Stop()

    def din(name, shape, dt=F32):
        kind = "ExternalInput" if (lite is None or name in lite) else "Internal"
        return nc.dram_tensor(name, list(shape), dt, kind=kind).ap()

    def dout(name, shape, dt=F32):
        return nc.dram_tensor(name, list(shape), dt, kind="ExternalOutput").ap()

    def dscr(name, shape, dt):
        return nc.dram_tensor(name, list(shape), dt, kind="Internal").ap()

    x_main = din("x_main", [NP, D])
    x_pre = din("x_pre", [NP, D])
    x_s = din("x_s", [NS, D])
    mem = din("mem", [256, D])
    st_gla = din("st_gla", [NSEQ, 4, 256, 512])
    st_conv = din("st_conv", [NSEQ, 30, 2048])
    ck = din("ck", [NSEQ, 256, 1024])
    cv = din("cv", [NSEQ, 256, 1024])
    consts = din("consts", [128, CW])
    norm_mix_g = din("norm_mix_g", [D])
    w_in = din("w_in", [D, IN_COLS])
    w_alpha_up = din("w_alpha_up", [16, 1024])
    b_alpha = din("b_alpha", [1, 1024])
    gla_norm_g = din("gla_norm_g", [2048])
    w_branch_a = din("w_branch_a", [2048, D])
    convp_in = din("convp_in", [34, 2048])
    w_branch_b = din("w_branch_b", [2048, D])
    w_out = din("w_out", [D, D])
    norm_ca_g = din("norm_ca_g", [D])
    norm_mem_g = din("norm_mem_g", [D])
    w_ca_q = din("w_ca_q", [D, 1024])
    w_ca_k = din("w_ca_k", [D, 1024])
    w_ca_v = din("w_ca_v", [D, 1024])
    w_ca_o = din("w_ca_o", [1024, D])
    norm_ffn_g = din("norm_ffn_g", [D])
    w_router = din("w_router", [D, 36])
    b_router = din("b_router", [36])
    w_exp_gate = din("w_exp_gate", [NE, D, 512])
    w_exp_up = din("w_exp_up", [NE, D, 512])
    w_exp_down = din("w_exp_down", [NE, 512, D])
    norm_final_g = din("norm_final_g", [D])

    y_main = dout("y_main", [NP, D])
    y_s = dout("y_s", [NS, D])
    gla_p = dout("gla_p", [4, 256, 512])
    conv_p = dout("conv_p", [30, 2048])
    mk_o = dout("mk_o", [256, 1024])
    mv_o = dout("mv_o", [256, 1024])
    gla_s = dout("gla_s", [NSEQ, 4, 256, 512])
    conv_s = dout("conv_s", [NSEQ, 30, 2048])

    s_state = dscr("s_state", [4, 256, 512], F32)
    s_ogT = dscr("s_ogT", [2048, NT], BF16)
    s_mT = dscr("s_mT", [D, NT], BF16)
    s_x1 = dscr("s_x1", [NT, D], F32)
    s_x2 = dscr("s_x2", [NT, D], F32)
    s_h3 = dscr("s_h3", [NT + 1, D], BF16)
    s_o12 = dscr("s_o12", [2 * NT + 1, D], F32)

    es = contextlib.ExitStack()
    with es:
      S = Sched(nc, es)
      try:
        pass
        XTK = ["xt", "xt_E1", "xt_E2", "xt_E3"]
        HBK = ["hb", "hb_kdT", "hb_sT", "hb_og", "hb_ogT"]
        op, dma = S.op, S.dma

        def sb(name, shape, dt, stack=es):
            return stack.enter_context(nc.sbuf_tensor(name, list(shape), dt))

        ps = [es.enter_context(nc.psum_tensor("ps%d" % i, [128, 512], F32)) for i in range(8)]

        def PS(i):
            return "ps%d" % i

        cst = sb("cst", [128, CWP], F32)
        ident_b = sb("ident_b", [128, 128], BF16)
        bm_b = sb("bm_b", [128, 16, 64], BF16)
        rs_b = sb("rs_b", [128, NT], BF16)
        ls_b = sb("ls_b", [128, 128], BF16)
        ones_b = sb("ones_b", [128, 128], BF16)
        NSLOT = 2
        wsl = [sb("wsl%d" % i, [128, 8192], BF16) for i in range(NSLOT)]
        gbc = sb("gbc", [128, D], BF16)
        xt = sb("xt", [128, D], F32)
        hb = sb("hb", [128, D], BF16)
        sm = sb("sm", [128, 64], F32)

        dma("sp", cst[:], consts[:, 0:CWP], writes=["cst"])
        dma("sp", xt[:, 0:CW - CWP], consts[:, CWP:CW], writes=[*XTK])
        ident_f = cst[:, C_ID:C_ID + 128]
        op("dve", lambda e: e.tensor_copy(out=ident_b[:], in_=cst[:, C_ID:C_ID + 128]), ["cst"], ["ident_b"])
        op("dve", lambda e: e.tensor_copy(out=bm_b[:].rearrange("p a b -> p (a b)"), in_=xt[:, C_BM - CWP:C_BM - CWP + 1024]), [*XTK], ["bm_b"])
        op("dve", lambda e: e.tensor_copy(out=rs_b[:], in_=xt[:, C_RS - CWP:C_RS - CWP + NT]), [*XTK], ["rs_b"])
        op("dve", lambda e: e.tensor_copy(out=ls_b[:], in_=xt[:, C_LS - CWP:C_LS - CWP + 128]), [*XTK], ["ls_b"])
        op("dve", lambda e: e.tensor_copy(out=ones_b[:], in_=xt[:, C_ON - CWP:C_ON - CWP + 128]), [*XTK], ["ones_b"])
        hscope = contextlib.ExitStack()
        hT = sb("hT", [128, 32, NT], BF16, hscope)
        if mute_until is not None:
            S.mute = True

        wstate = {"n": 0, "plan": [], "issued": 0}

        def wplan(ap2d):
            wstate["plan"].append(ap2d)

        def _wissue(i):
            ap2d = wstate["plan"][i]
            K, ncols = ap2d.shape
            kc = K // 128
            slot = i % NSLOT
            view = wsl[slot][:, 0:kc * ncols].rearrange("p (k n) -> p k n", k=kc)
            dma("pool", view, ap2d.rearrange("(k p) n -> p k n", p=128), writes=["wsl%d" % slot])

        def wnext(ap2d_check=None):
            i = wstate["n"]
            wstate["n"] += 1
            if S.mute:
                wstate["issued"] = max(wstate["issued"], wstate["n"])
            while not S.mute and wstate["issued"] < min(len(wstate["plan"]), i + NSLOT):
                _wissue(wstate["issued"])
                wstate["issued"] += 1
            ap2d = wstate["plan"][i]
            if ap2d_check is not None:
                assert ap2d.shape == ap2d_check.shape and ap2d.offset == ap2d_check.offset, (i, ap2d, ap2d_check)
            K, ncols = ap2d.shape
            kc = K // 128
            slot = i % NSLOT
            view = wsl[slot][:, 0:kc * ncols].rearrange("p (k n) -> p k n", k=kc)
            return view, "wsl%d" % slot

        def load_gain(g_ap):
            dma("pool", gbc[:], g_ap.partition_broadcast(128), writes=["gbc"])

        def norm_tile(src_ap, rows, col, dstT, dst_key, scratch_out=None, fp32_T=None, src_reads=()):
            dma("sp", xt[0:rows, :], src_ap, reads=list(src_reads), writes=[*XTK])
            op("act", lambda e: e.activation(out=hb[0:rows, :], in_=xt[0:rows, :], func=AF.Square,
                                             accum_out=sm[0:rows, 0:1]), [*XTK], [*HBK, "sm"])
            op("act", lambda e: e.activation(out=sm[0:rows, 1:2], in_=sm[0:rows, 0:1], func=AF.Sqrt,
                                             scale=1.0 / D, bias=EPS), ["sm"], ["sm"])
            op("dve", lambda e: e.reciprocal(out=sm[0:rows, 2:3], in_=sm[0:rows, 1:2]), ["sm"], ["sm"])
            if fp32_T is None:
                op("dve", lambda e: e.scalar_tensor_tensor(out=hb[0:rows, :], in0=xt[0:rows, :], scalar=sm[0:rows, 2:3],
                                                           in1=gbc[0:rows, :], op0=ALU.mult, op1=ALU.mult),
                   [*XTK, "sm", "gbc"], [*HBK])
                if scratch_out is not None:
                    dma("sp", scratch_out, hb[0:rows, :], reads=[*HBK])
                for g in range(4):
                    pv = ps[4 + g][:].bitcast(BF16).rearrange("p (a b) -> p a b", a=8)

                    def tr(e, g=g, pv=pv):
                        for j in range(8):
                            c = g * 8 + j
                            ins = e.transpose(out=pv[:, j, 0:rows], in_=hb[0:rows, c * 128:(c + 1) * 128],
                                              identity=ident_b[0:rows, 0:rows])
                        return ins
                    op("pe", tr, [*HBK, "ident_b"], [PS(4 + g)])
                    eng = "act" if g % 2 == 0 else "dve"
                    if eng == "act":
                        op("act", lambda e, g=g, pv=pv: e.activation(out=dstT[:, g * 8:(g + 1) * 8, col:col + rows],
                                                                      in_=pv[:, :, 0:rows], func=AF.Copy),
                           [PS(4 + g)], [dst_key])
                    else:
                        op("dve", lambda e, g=g, pv=pv: e.tensor_copy(out=dstT[:, g * 8:(g + 1) * 8, col:col + rows],
                                                                       in_=pv[:, :, 0:rows]),
                           [PS(4 + g)], [dst_key])
            else:
                fp32_T(rows)

        def gemm_ws(wview, wkey, kc, mcols, acts, act_key, groups, evac, psbanks, extra_reads=()):
            nb = 0
            for m in range(mcols // 128):
                for gi, (t0, n) in enumerate(groups):
                    b = psbanks[nb % len(psbanks)]
                    nb += 1

                    def mm(e, m=m, t0=t0, n=n, b=b):
                        for k in range(kc):
                            ins = e.matmul(ps[b][:, 0:n], lhsT=wview[:, k, m * 128:(m + 1) * 128],
                                           rhs=acts[:, k, t0:t0 + n], start=(k == 0), stop=(k == kc - 1))
                        return ins
                    op("pe", mm, [wkey, act_key] + list(extra_reads), [PS(b)])
                    evac(m, gi, t0, n, ps[b][:, 0:n], PS(b))

        def gemm_as(wviews, wkeys, actT, act_key, tiles, ncols, evac, psbanks):
            nb = 0
            for ti, (t0, rows) in enumerate(tiles):
                b = psbanks[nb % len(psbanks)]
                nb += 1
                ktot = sum(v.shape[1] for v, _ in wviews)

                def mm(e, t0=t0, rows=rows, b=b):
                    kk = 0
                    for v, koff in wviews:
                        for k in range(v.shape[1]):
                            ins = e.matmul(ps[b][0:rows, 0:ncols], lhsT=actT[:, koff + k, t0:t0 + rows],
                                           rhs=v[:, k, :], start=(kk == 0), stop=(kk == ktot - 1))
                            kk += 1
                    return ins
                op("pe", mm, list(wkeys) + [act_key], [PS(b)])
                evac(ti, t0, rows, ps[b][0:rows, 0:ncols], PS(b))

        def cols(a, n):
            return w_in[:, a:a + n]

        def plan_gla(with_q):
            wplan(cols(OFF_A, 16))
            for h in range(4):
                if with_q:
                    wplan(cols(OFF_Q + 256 * h, 256))
                wplan(cols(OFF_K + 256 * h, 256))
                wplan(cols(OFF_V + 512 * h, 256))
                wplan(cols(OFF_V + 512 * h + 256, 256))
                if with_q:
                    wplan(cols(OFF_R + 512 * h, 256))
                    wplan(cols(OFF_R + 512 * h + 256, 256))
        plan_gla(False)
        plan_gla(True)
        for m in range(16):
            wplan(cols(OFF_G + 256 * m, 256))
            wplan(w_branch_a[:, 256 * m:256 * m + 256])
        for m in range(8):
            wplan(cols(OFF_U + 256 * m, 256))
            wplan(cols(OFF_U + 2048 + 256 * m, 256))
        for m in range(16):
            wplan(cols(OFF_G + D + 256 * m, 256))
            wplan(w_branch_b[:, 256 * m:256 * m + 256])
        for cb in range(16):
            wplan(w_out[:, 256 * cb:256 * cb + 256])
        for wm in (w_ca_k, w_ca_v):
            for cb in range(4):
                wplan(wm[:, 256 * cb:256 * cb + 256])
        for m in range(4):
            wplan(w_ca_k[:, 256 * m:256 * m + 256])
        for m in range(4):
            wplan(w_ca_q[:, 256 * m:256 * m + 256])
        for cb in range(4):
            wplan(w_ca_o[:, 1024 * cb:1024 * cb + 1024])
        for ex in range(NE):
            wplan(w_exp_gate[ex][:, 0:256])
            wplan(w_exp_up[ex][:, 0:256])
            wplan(w_exp_gate[ex][:, 256:512])
            wplan(w_exp_up[ex][:, 256:512])
            wplan(w_exp_down[ex][:, 0:2048])
            wplan(w_exp_down[ex][:, 2048:4096])

        mix = contextlib.ExitStack()
        with mix:
            alT = sb("alT", [17, NT], BF16, mix)
            walb = sb("walb", [17, 1024], BF16, mix)
            ggl = sb("ggl", [128, 512], F32, mix)
            hT_halo = sb("hT_halo", [128, 32, 32], BF16, mix)
            dma("pool", walb[0:16, :], w_alpha_up, writes=["walb"])
            dma("pool", walb[16:17, :], b_alpha, writes=["walb"])
            load_gain(norm_mix_g)

            gl = contextlib.ExitStack()
            with gl:
                U1 = sb("U1", [128, 4608], F32, gl)
                bT = U1[:, 0:2 * NT].rearrange("p (c t) -> p c t", c=2)
                dfT = U1[:, 2 * NT:4 * NT].rearrange("p (c t) -> p c t", c=2)
                U1b = U1[:].bitcast(BF16)
                vh = U1b[:, 0:4608].rearrange("p (i v) -> p i v", i=9)
                gr = U1b[:, 4608:9216].rearrange("p (i v) -> p i v", i=9)
                xtb = xt[:].bitcast(BF16)
                E1 = xtb[:, 0:2 * NT].rearrange("p (c t) -> p c t", c=2)
                E2 = xtb[:, 2 * NT:4 * NT].rearrange("p (c t) -> p c t", c=2)
                E3 = xtb[:, 4 * NT:6 * NT].rearrange("p (c t) -> p c t", c=2)
                kdT = hb[:, 0:2 * NT].rearrange("p (c t) -> p c t", c=2)
                sT = hb[:, 2 * NT:2 * NT + 128]
                og = hb[:, 2304:2816]
                ogT = hb[:, 2816:3328].rearrange("p (a b) -> p a b", a=4)
                dec = sb("dec", [128, 2, 24], F32, gl)
                qeT = sb("qeT", [128, 2, NT], BF16, gl)
                keT = sb("keT", [128, 2, NT], BF16, gl)
                kd = sb("kd", [128, 9, 256], BF16, gl)
                of = sb("of", [128, 512], F32, gl)
                grf = of
                QMs = sb("QMs", [128, 2, 64], BF16, gl)
                Sf = sb("Sf", [128, 2, 512], F32, gl)
                Sb = sb("Sb", [128, 2, 512], BF16, gl)
                st6 = sb("st6", [128, 8], F32, gl)
                s0f = [sb("s0f%d" % i, [128, 512], F32, gl) for i in range(2)]
                s0b = [sb("s0b%d" % i, [128, 512], BF16, gl) for i in range(2)]
                sout = [sb("sout%d" % i, [128, 512], F32, gl) for i in range(2)]
                kdm = sb("kdm", [64, 256], BF16, gl)

                def gla_pass(main):
                    ntok = NT if main else NP
                    groups = NGRP if main else NGRP[:2]
                    tiles = TILES if main else TILES[:8]
                    for ti, (t0, rows) in enumerate(tiles):
                        if main:
                            src = x_main[t0:t0 + rows, :] if ti < 8 else x_s[:, :]
                        else:
                            src = x_pre[t0:t0 + rows, :]
                        norm_tile(src, rows, t0, hT, "hT")
                    if not main:
                        op("dve", lambda e: e.tensor_copy(out=hT_halo[:], in_=hT[:, :, NP - 32:NP]), ["hT"], ["hT_halo"])
                    wv, wk = wnext()
                    op("pool", lambda e: e.memset(alT[:], 1.0), [], ["alT"])

                    def ev_a(m, gi, t0, n, pap, pkey):
                        op("act", lambda e: e.activation(out=alT[0:16, t0:t0 + n], in_=pap[0:16, :], func=AF.Copy),
                           [pkey], ["alT"])
                    for gi, (t0, n) in enumerate(groups):
                        b = gi % 4

                        def mm(e, t0=t0, n=n, b=b):
                            for k in range(32):
                                ins = e.matmul(ps[b][0:16, 0:n], lhsT=wv[:, k, 0:16], rhs=hT[:, k, t0:t0 + n],
                                               start=(k == 0), stop=(k == 31))
                            return ins
                        op("pe", mm, [wk, "hT"], [PS(b)])
                        ev_a(0, gi, t0, n, ps[b][:, 0:n], PS(b))

                    for h in range(4):
                        for dc in range(2):
                            for gi, (t0, n) in enumerate(groups):
                                b = 4 + (dc * 3 + gi) % 4
                                c0 = (2 * h + dc) * 128
                                op("pe", lambda e, b=b, c0=c0, t0=t0, n=n: e.matmul(
                                    ps[b][:, 0:n], lhsT=walb[:, c0:c0 + 128], rhs=alT[:, t0:t0 + n], start=True, stop=True),
                                   ["walb", "alT"], [PS(b)])
                                op("act", lambda e, b=b, dc=dc, t0=t0, n=n: e.activation(
                                    out=dfT[:, dc, t0:t0 + n], in_=ps[b][:, 0:n], func=AF.Exp, scale=-1.0),
                                   [PS(b)], ["U1"])
                            op("act", lambda e, dc=dc: e.activation(out=dfT[:, dc, 0:ntok], in_=dfT[:, dc, 0:ntok],
                                                                     func=AF.Ln, bias=1.0), ["U1"], ["U1"])
                            op("dve", lambda e, dc=dc: e.tensor_scalar(out=dfT[:, dc, 0:ntok], in0=dfT[:, dc, 0:ntok],
                                                                        scalar1=-1.0 / 16.0, scalar2=None, op0=ALU.mult),
                               ["U1"], ["U1"])
                            op("dve", lambda e, dc=dc: e.tensor_tensor_scan(
                                out=bT[:, dc, 0:ntok], data0=rs_b[:, 0:ntok], data1=dfT[:, dc, 0:ntok],
                                initial=0.0, op0=ALU.mult, op1=ALU.add), ["U1", "rs_b"], ["U1"])
                        bTp = bT[:, :, 0:NP].rearrange("p c (n t) -> p c n t", t=128)
                        dfp = dfT[:, :, 0:NP].rearrange("p c (n t) -> p c n t", t=128)
                        for dc in range(2):
                            op("dve", lambda e, dc=dc: e.tensor_tensor(
                                out=dfp[:, dc], in0=bTp[:, dc, :, 127:128].to_broadcast([128, 8, 128]), in1=bTp[:, dc],
                                op=ALU.subtract), ["U1"], ["U1"])
                            op("act", lambda e, dc=dc: e.activation(out=dec[:, dc, 0:8], in_=bTp[:, dc, :, 127], func=AF.Exp),
                               ["U1"], ["dec"])
                        if main:
                            bTs = bT[:, :, NP:NT].rearrange("p c (n t) -> p c n t", t=4)
                            dfs = dfT[:, :, NP:NT].rearrange("p c (n t) -> p c n t", t=4)
                            for dc in range(2):
                                op("dve", lambda e, dc=dc: e.tensor_tensor(
                                    out=dfs[:, dc], in0=bTs[:, dc, :, 3:4].to_broadcast([128, 16, 4]), in1=bTs[:, dc],
                                    op=ALU.subtract), ["U1"], ["U1"])
                                op("act", lambda e, dc=dc: e.activation(out=dec[:, dc, 8:24], in_=bTs[:, dc, :, 3], func=AF.Exp),
                                   ["U1"], ["dec"])
                        op("act", lambda e: e.activation(out=E3[:, :, 0:ntok], in_=dfT[:, :, 0:ntok], func=AF.Exp),
                           ["U1"], ["xt_E3"])
                        if main:
                            op("act", lambda e: e.activation(out=E1[:, :, 0:ntok], in_=bT[:, :, 0:ntok], func=AF.Exp),
                               ["U1"], ["xt_E1"])
                            op("act", lambda e: e.activation(out=E2[:, :, 0:ntok], in_=bT[:, :, 0:ntok], func=AF.Exp,
                                                             scale=-1.0), ["U1"], ["xt_E2"])
                            wv, wk = wnext()

                            def ev_q(m, gi, t0, n, pap, pkey):
                                op("dve", lambda e: e.scalar_tensor_tensor(
                                    out=qeT[:, m, t0:t0 + n], in0=pap, scalar=1.0 / 16.0, in1=E1[:, m, t0:t0 + n],
                                    op0=ALU.mult, op1=ALU.mult), [pkey, "xt_E1"], ["qeT"])
                            gemm_ws(wv, wk, 32, 256, hT, "hT", groups, ev_q, [0, 1, 2, 3])
                        wv, wk = wnext()

                        def ev_k(m, gi, t0, n, pap, pkey):
                            if main:
                                op("dve", lambda e: e.tensor_tensor(out=keT[:, m, t0:t0 + n], in0=pap,
                                                                    in1=E2[:, m, t0:t0 + n], op=ALU.mult),
                                   [pkey, "xt_E2"], ["keT"])
                            op("dve", lambda e: e.tensor_tensor(out=kdT[:, m, t0:t0 + n], in0=pap,
                                                                in1=E3[:, m, t0:t0 + n], op=ALU.mult),
                               [pkey, "xt_E3"], ["hb_kdT"])
                        gemm_ws(wv, wk, 32, 256, hT, "hT", groups, ev_k, [0, 1, 2, 3])
                        for ti, (t0, rows) in enumerate(tiles):
                            b = 4 + ti % 2
                            pv = ps[b][:].bitcast(BF16)

                            def trk(e, t0=t0, rows=rows, pv=pv):
                                for dc in range(2):
                                    ins = e.transpose(out=pv[0:rows, dc * 128:(dc + 1) * 128], in_=kdT[:, dc, t0:t0 + rows],
                                                      identity=ident_b[:])
                                return ins
                            op("pe", trk, ["hb_kdT", "ident_b"], [PS(b)])
                            op("act", lambda e, ti=ti, rows=rows, pv=pv: e.activation(
                                out=kd[0:rows, ti, :], in_=pv[0:rows, 0:256], func=AF.Copy), [PS(b)], ["kd"])
                        for hf in range(2):
                            wv0, wk0 = wnext()

                            def ev_v(ti, t0, rows, pap, pkey, hf=hf):
                                op("act", lambda e: e.activation(out=vh[0:rows, ti, hf * 256:(hf + 1) * 256], in_=pap, func=AF.Copy),
                                   [pkey], ["U1"])
                            gemm_as([(wv0, 0)], [wk0], hT, "hT", tiles, 256, ev_v, [0, 1, 2, 3])
                        if main:
                            dma("sp", ggl[:], gla_norm_g[h * 512:(h + 1) * 512].partition_broadcast(128), writes=["ggl"])
                            for hf in range(2):
                                wv0, wk0 = wnext()

                                def ev_r(ti, t0, rows, pap, pkey, hf=hf):
                                    op("act", lambda e: e.activation(out=grf[0:rows, 0:256], in_=pap, func=AF.Silu), [pkey], ["of"])
                                    op("dve", lambda e: e.tensor_tensor(out=gr[0:rows, ti, hf * 256:(hf + 1) * 256], in0=grf[0:rows, 0:256],
                                                                        in1=ggl[0:rows, hf * 256:(hf + 1) * 256], op=ALU.mult),
                                       ["of", "ggl"], ["U1"])
                                gemm_as([(wv0, 0)], [wk0], hT, "hT", tiles, 256, ev_r, [0, 1, 2, 3])
                        if main:
                            dma("sp", Sf[:], s_state[h].rearrange("(c p) v -> p c v", p=128), reads=["s_state%d" % h], writes=["Sf"])
                        else:
                            op("pool", lambda e: e.memset(Sf[:], 0.0), [], ["Sf"])
                        op("act", lambda e: e.activation(out=Sb[:], in_=Sf[:], func=AF.Copy), ["Sf"], ["Sb"])
                        for n_ in range(8):
                            t0 = n_ * 128
                            if main:
                                def mm_s(e, t0=t0):
                                    for dc in range(2):
                                        ins = e.matmul(ps[4][:, 0:128], lhsT=keT[:, dc, t0:t0 + 128], rhs=qeT[:, dc, t0:t0 + 128],
                                                       start=(dc == 0), stop=(dc == 1))
                                    return ins
                                op("pe", mm_s, ["keT", "qeT"], [PS(4)])
                                op("dve", lambda e: e.tensor_tensor(out=sT[:], in0=ps[4][:, 0:128], in1=cst[:, C_MU:C_MU + 128],
                                                                    op=ALU.mult), [PS(4), "cst"], ["hb_sT"])

                                def mm_o(e, t0=t0, n_=n_):
                                    e.matmul(ps[5][:, :], lhsT=sT[:], rhs=vh[:, n_, :], start=True, stop=False)
                                    for dc in range(2):
                                        ins = e.matmul(ps[5][:, :], lhsT=qeT[:, dc, t0:t0 + 128], rhs=Sb[:, dc, :],
                                                       start=False, stop=(dc == 1))
                                    return ins
                                op("pe", mm_o, ["hb_sT", "U1", "qeT", "Sb"], [PS(5)])
                                finish_o(h, n_, 128, t0)
                            for dc in range(2):
                                b = 6 + dc
                                op("pe", lambda e, dc=dc, b=b, n_=n_: e.matmul(
                                    ps[b][:, :], lhsT=kd[:, n_, dc * 128:(dc + 1) * 128], rhs=vh[:, n_, :], start=True, stop=True),
                                   ["kd", "U1"], [PS(b)])
                                op("dve", lambda e, dc=dc, b=b, n_=n_: e.scalar_tensor_tensor(
                                    out=Sf[:, dc, :], in0=Sf[:, dc, :], scalar=dec[:, dc, n_:n_ + 1], in1=ps[b][:, :],
                                    op0=ALU.mult, op1=ALU.add), ["Sf", "dec", PS(b)], ["Sf"])
                            if n_ < 7:
                                op("act", lambda e: e.activation(out=Sb[:], in_=Sf[:], func=AF.Copy), ["Sf"], ["Sb"])
                        if main:
                            dma("sp", gla_p[h].rearrange("(c p) v -> p c v", p=128), Sf[:], reads=["Sf"])
                            sample_gla(h)
                        else:
                            dma("sp", s_state[h].rearrange("(c p) v -> p c v", p=128), Sf[:], reads=["Sf"],
                                writes=["s_state%d" % h])

                def finish_o(h, ti, rows, t0):
                    op("dve", lambda e: e.bn_stats(out=st6[0:rows, 0:6], in_=ps[5][0:rows, :]), [PS(5)], ["st6"])
                    op("dve", lambda e: e.bn_aggr(out=st6[0:rows, 6:8], in_=st6[0:rows, 0:6]), ["st6"], ["st6"])
                    op("act", lambda e: e.activation(out=st6[0:rows, 0:1], in_=st6[0:rows, 7:8], func=AF.Sqrt, bias=EPS),
                       ["st6"], ["st6"])
                    op("dve", lambda e: e.reciprocal(out=st6[0:rows, 1:2], in_=st6[0:rows, 0:1]), ["st6"], ["st6"])
                    op("dve", lambda e: e.tensor_scalar(out=of[0:rows, :], in0=ps[5][0:rows, :], scalar1=st6[0:rows, 6:7],
                                                        scalar2=st6[0:rows, 1:2], op0=ALU.subtract, op1=ALU.mult),
                       [PS(5), "st6"], ["of"])
                    op("dve", lambda e: e.tensor_tensor(out=og[0:rows, :], in0=of[0:rows, :], in1=gr[0:rows, ti, :], op=ALU.mult),
                       ["of", "U1"], ["hb_og"])
                    pv = ps[4][:].bitcast(BF16).rearrange("p (a b) -> p a b", a=8)

                    def tr(e):
                        for j in range(4):
                            ins = e.transpose(out=pv[:, j, 0:rows], in_=og[0:rows, j * 128:(j + 1) * 128],
                                              identity=ident_b[0:rows, 0:rows])
                        return ins
                    op("pe", tr, ["hb_og", "ident_b"], [PS(4)])
                    op("act", lambda e: e.activation(out=ogT[:, :, 0:rows], in_=pv[:, 0:4, 0:rows], func=AF.Copy), [PS(4)], ["hb_ogT"])
                    dma("sp", s_ogT[h * 512:(h + 1) * 512, t0:t0 + rows].rearrange("(c p) t -> p c t", p=128),
                        ogT[:, :, 0:rows], reads=["hb_ogT"], writes=["s_ogT"])

                def sample_gla(h):
                    def mm_s(e):
                        for dc in range(2):
                            ins = e.matmul(ps[4][0:64, 0:64], lhsT=keT[:, dc, NP:NT], rhs=qeT[:, dc, NP:NT],
                                           start=(dc == 0), stop=(dc == 1))
                        return ins
                    op("pe", mm_s, ["keT", "qeT"], [PS(4)])
                    op("dve", lambda e: e.tensor_tensor(out=sT[0:64, 0:64], in0=ps[4][0:64, 0:64], in1=cst[0:64, C_MS:C_MS + 64],
                                                        op=ALU.mult), [PS(4), "cst"], ["hb_sT"])
                    for sq in range(NSEQ):
                        op("dve", lambda e, sq=sq: e.tensor_tensor(
                            out=QMs[:], in0=qeT[:, :, NP:NT], in1=bm_b[:, sq, :].unsqueeze(1).to_broadcast([128, 2, 64]),
                            op=ALU.mult), ["qeT", "bm_b"], ["QMs"])
                        op("dve", lambda e, sq=sq: e.tensor_scalar(out=kdm[:], in0=kd[0:64, 8, :],
                                                                    scalar1=cst[0:64, C_RM + sq:C_RM + sq + 1], scalar2=None,
                                                                    op0=ALU.mult), ["kd", "cst"], ["kdm"])
                        for dc in range(2):
                            bi = dc
                            src = st_gla[sq, h, dc * 128:(dc + 1) * 128, :]
                            dma("sp", s0f[bi][:], src, writes=["s0f%d" % bi])
                            dma("pool", s0b[bi][:], src, writes=["s0b%d" % bi])

                            def mm_o(e, sq=sq, dc=dc, bi=bi):
                                if sq == 0 and dc == 0:
                                    e.matmul(ps[5][0:64, :], lhsT=sT[0:64, 0:64], rhs=vh[0:64, 8, :], start=True, stop=False)
                                return e.matmul(ps[5][0:64, :], lhsT=QMs[:, dc, :], rhs=s0b[bi][:, :],
                                                start=False, stop=(sq == NSEQ - 1 and dc == 1))
                            op("pe", mm_o, ["hb_sT", "U1", "QMs", "s0b%d" % bi], [PS(5)])
                            b = 6 + dc
                            op("pe", lambda e, dc=dc, b=b: e.matmul(ps[b][:, :], lhsT=kdm[:, dc * 128:(dc + 1) * 128],
                                                                     rhs=vh[0:64, 8, :], start=True, stop=True),
                               ["kdm", "U1"], [PS(b)])
                            op("dve", lambda e, dc=dc, b=b, sq=sq, bi=bi: e.scalar_tensor_tensor(
                                out=sout[bi][:, :], in0=s0f[bi][:, :], scalar=dec[:, dc, 8 + sq:9 + sq], in1=ps[b][:, :],
                                op0=ALU.mult, op1=ALU.add), ["s0f%d" % bi, "dec", PS(b)], ["sout%d" % bi])
                            dma("sp", gla_s[sq, h, dc * 128:(dc + 1) * 128, :], sout[bi][:], reads=["sout%d" % bi])
                    finish_o(h, 8, 64, NP)

                stage_end(0)
                gla_pass(False)
                stage_end(1)
                gla_pass(True)
                stage_end(2)

            with contextlib.ExitStack() as mg_es:
                ogTa = sb("ogTa", [128, 16, NT], BF16, mg_es)
                sga = sb("sga", [128, 512], F32, mg_es)
                mo = sb("mo", [128, 2, NT], BF16, mg_es)
                dma("sp", ogTa[:], s_ogT.rearrange("(c p) t -> p c t", p=128), reads=["s_ogT"], writes=["ogTa"])
                sgs = sb("sgs", [128, 2, NT], BF16, mg_es)
                for m in range(16):
                    wga, kga = wnext()

                    def ev_ga(mm_, gi, t0, n, pap, pkey):
                        op("act", lambda e: e.activation(out=sgs[:, mm_, t0:t0 + n], in_=pap, func=AF.Sigmoid), [pkey], ["sgs"])
                    gemm_ws(wga, kga, 32, 256, hT, "hT", NGRP, ev_ga, [0, 1, 2, 3])
                    wba, kba = wnext()

                    def ev_ba(mm_, gi, t0, n, pap, pkey):
                        op("dve", lambda e: e.tensor_tensor(out=mo[:, mm_, t0:t0 + n], in0=pap, in1=sgs[:, mm_, t0:t0 + n], op=ALU.mult),
                           [pkey, "sgs"], ["mo"])
                    gemm_ws(wba, kba, 16, 256, ogTa, "ogTa", NGRP, ev_ba, [4, 5, 6, 7])
                    dma("sp", s_mT[m * 256:(m + 1) * 256, :].rearrange("(c p) t -> p c t", p=128), mo[:], reads=["mo"],
                        writes=["s_mT"])

            stage_end(3)
            cv_es = contextlib.ExitStack()
            with cv_es:
                cT = sb("cT", [128, 16, NT], BF16, cv_es)
                cpar = sb("cpar", [128, 16, 34], F32, cv_es)
                extP = sb("extP", [128, 30 + NP], F32, cv_es)
                extS = sb("extS", [128, 16, 34], F32, cv_es)
                sig = xt[:, 3136:3648]
                acc = xt[:, 2048:2048 + NT]
                stc = sb("stc", [120, 4, 128], F32, cv_es)
                cvp_tm = sb("cvp_tm", [30, 128], F32, cv_es)
                cvs_tm = sb("cvs_tm", [64, 128], F32, cv_es)
                cpl = xt[0:34, 0:2048]
                dma("sp", cpl, convp_in, writes=[*XTK])
                for j in range(16):
                    b = 4 + j % 2
                    op("pe", lambda e, j=j, b=b: e.transpose(out=ps[b][:, 0:34], in_=xt[0:34, j * 128:(j + 1) * 128],
                                                             identity=ident_f[0:34, 0:34]), [*XTK, "cst"], [PS(b)])
                    op("dve", lambda e, j=j, b=b: e.tensor_copy(out=cpar[:, j, :], in_=ps[b][:, 0:34]), [PS(b)], ["cpar"])
                dma("sp", conv_s[:, 0:26, :], st_conv[:, 4:30, :])
                ugroups = [(0, 512), (512, 512), (1024, 64)]
                uscope = contextlib.ExitStack()
                u1s = sb("u1s", [128, 2, 32 + NT], F32, uscope)
                for m in range(8):
                    wu1, k1 = wnext()
                    for jj in range(2):
                        for gi, (t0, n) in enumerate([(-32, 32)] + ugroups):
                            src = hT_halo if t0 < 0 else hT
                            skey = "hT_halo" if t0 < 0 else "hT"
                            a0 = 0 if t0 < 0 else t0
                            b1 = (jj * 4 + gi) % 4

                            def mmu1(e, b=b1, a0=a0, n=n, src=src, jj=jj):
                                for k in range(32):
                                    ins = e.matmul(ps[b][:, 0:n], lhsT=wu1[:, k, jj * 128:(jj + 1) * 128], rhs=src[:, k, a0:a0 + n],
                                                   start=(k == 0), stop=(k == 31))
                                return ins
                            op("pe", mmu1, [k1, skey], [PS(b1)])
                            op("act", lambda e, b1=b1, jj=jj, t0=t0, n=n: e.activation(out=u1s[:, jj, 32 + t0:32 + t0 + n], in_=ps[b1][:, 0:n],
                                                                                        func=AF.Copy), [PS(b1)], ["u1s"])
                    wu2, k2 = wnext()
                    for jj in range(2):
                        j = 2 * m + jj
                        for g4 in range(4):
                            dma("sp", stc[:, g4, :], st_conv[4 * g4:4 * g4 + 4, :, j * 128:(j + 1) * 128].rearrange("s r c -> (s r) c"),
                                writes=["stc"])
                        for g4 in range(4):
                            op("pe", lambda e, g4=g4: e.transpose(out=ps[6][:, g4 * 120:(g4 + 1) * 120], in_=stc[:, g4, :],
                                                                  identity=ident_f[0:120, 0:120]), ["stc", "cst"], [PS(6)])
                        op("dve", lambda e: e.tensor_copy(out=extS[:, :, 0:30],
                                                          in_=ps[6][:, 0:480].rearrange("p (s r) -> p s r", r=30)),
                           [PS(6)], ["extS"])
                        for gi, (t0, n) in enumerate([(-32, 32)] + ugroups):
                            src = hT_halo if t0 < 0 else hT
                            skey = "hT_halo" if t0 < 0 else "hT"
                            a0 = 0 if t0 < 0 else t0
                            b2 = gi % 4

                            def mmu(e, wv, b, a0=a0, n=n, src=src, jj=jj):
                                for k in range(32):
                                    ins = e.matmul(ps[b][:, 0:n], lhsT=wv[:, k, jj * 128:(jj + 1) * 128], rhs=src[:, k, a0:a0 + n],
                                                   start=(k == 0), stop=(k == 31))
                                return ins
                            op("pe", lambda e, b2=b2, f=mmu: f(e, wu2, b2), [k2, skey], [PS(b2)])
                            op("act", lambda e, b2=b2, n=n: e.activation(out=sig[:, 0:n], in_=ps[b2][:, 0:n], func=AF.Sigmoid),
                               [PS(b2)], ["xt_E3"])
                            if t0 < 0:
                                dst = extP[:, 0:30]
                                i0 = u1s[:, jj, 2:32]
                                i1 = sig[:, 2:32]
                                wk_ = "extP"
                            elif t0 < NP:
                                dst = extP[:, 30 + t0:30 + t0 + n]
                                i0 = u1s[:, jj, 32 + t0:32 + t0 + n]
                                i1 = sig[:, 0:n]
                                wk_ = "extP"
                            else:
                                dst = extS[:, :, 30:34]
                                i0 = u1s[:, jj, 32 + NP:32 + NT].rearrange("p (s r) -> p s r", r=4)
                                i1 = sig[:, 0:64].rearrange("p (s r) -> p s r", r=4)
                                wk_ = "extS"
                            op("dve", lambda e, dst=dst, i0=i0, i1=i1: e.tensor_tensor(out=dst, in0=i0, in1=i1, op=ALU.mult),
                               ["u1s", "xt_E3"], [wk_])
                        accS = acc[:, NP:NT].rearrange("p (s r) -> p s r", r=4)
                        op("dve", lambda e, j=j: e.tensor_scalar(out=acc[:, 0:NP], in0=extP[:, 0:NP], scalar1=cpar[:, j, 0:1],
                                                                  scalar2=cpar[:, j, 31:32], op0=ALU.mult, op1=ALU.add),
                           ["extP", "cpar"], ["xt_E2"])
                        op("dve", lambda e, j=j: e.tensor_scalar(out=accS, in0=extS[:, :, 0:4], scalar1=cpar[:, j, 0:1],
                                                                  scalar2=cpar[:, j, 31:32], op0=ALU.mult, op1=ALU.add),
                           ["extS", "cpar"], ["xt_E2"])
                        for tp in range(1, 31):
                            op("dve", lambda e, j=j, tp=tp: e.scalar_tensor_tensor(
                                out=acc[:, 0:NP], in0=extP[:, tp:tp + NP], scalar=cpar[:, j, tp:tp + 1], in1=acc[:, 0:NP],
                                op0=ALU.mult, op1=ALU.add), ["extP", "cpar", "xt_E2"], ["xt_E2"])
                            op("dve", lambda e, j=j, tp=tp: e.scalar_tensor_tensor(
                                out=accS, in0=extS[:, :, tp:tp + 4], scalar=cpar[:, j, tp:tp + 1], in1=accS,
                                op0=ALU.mult, op1=ALU.add), ["extS", "cpar", "xt_E2"], ["xt_E2"])
                        op("act", lambda e, j=j: e.activation(out=cT[:, j, :], in_=acc[:, :], func=AF.Copy), ["xt_E2"], ["cT"])
                        op("pe", lambda e: e.transpose(out=ps[7][0:30, 0:128], in_=extP[:, NP:NP + 30], identity=ident_f),
                           ["extP", "cst"], [PS(7)])
                        op("act", lambda e, j=j: e.activation(out=cvp_tm[:, :], in_=ps[7][0:30, 0:128],
                                                               func=AF.Copy), [PS(7)], ["cvp_tm"])
                        dma("sp", conv_p[:, j * 128:(j + 1) * 128], cvp_tm[:, :], reads=["cvp_tm"])
                        op("pool", lambda e: e.tensor_copy(out=sig[:, 0:64].rearrange("p (s r) -> p s r", r=4), in_=extS[:, :, 30:34]),
                           ["extS"], ["xt_E3"])
                        op("pe", lambda e: e.transpose(out=ps[7][0:64, 128:256], in_=sig[:, 0:64], identity=ident_f),
                           ["xt_E3", "cst"], [PS(7)])
                        op("act", lambda e, j=j: e.activation(out=cvs_tm[:, :], in_=ps[7][0:64, 128:256],
                                                               func=AF.Copy), [PS(7)], ["cvs_tm"])
                        for sq in range(NSEQ):
                            dma("sp", conv_s[sq, 26:30, j * 128:(j + 1) * 128], cvs_tm[4 * sq:4 * sq + 4, :], reads=["cvs_tm"])
                uscope.close()
                with contextlib.ExitStack() as ln_es:
                    sq_t = sb("sq_t", [128, 512], BF16, ln_es)
                    mu = sb("mu", [128, 512], F32, ln_es)
                    rs = sb("rs", [128, 512], F32, ln_es)
                    tmpf = sb("tmpf", [128, 512], F32, ln_es)
                    for gi, (t0, n) in enumerate(NGRP):
                        def mm1(e, t0=t0, n=n):
                            for j in range(16):
                                ins = e.matmul(ps[0][:, 0:n], lhsT=ones_b[:], rhs=cT[:, j, t0:t0 + n], start=(j == 0), stop=(j == 15))
                            return ins
                        op("pe", mm1, ["ones_b", "cT"], [PS(0)])
                        for j in range(16):
                            op("dve", lambda e, j=j, t0=t0, n=n: e.tensor_tensor(out=sq_t[:, 0:n], in0=cT[:, j, t0:t0 + n],
                                                                                  in1=cT[:, j, t0:t0 + n], op=ALU.mult),
                               ["cT"], ["sq_t"])
                            op("pe", lambda e, j=j, n=n: e.matmul(ps[1][:, 0:n], lhsT=ones_b[:], rhs=sq_t[:, 0:n],
                                                                  start=(j == 0), stop=(j == 15)), ["ones_b", "sq_t"], [PS(1)])
                        op("act", lambda e, n=n: e.activation(out=mu[:, 0:n], in_=ps[0][:, 0:n], func=AF.Copy, scale=1.0 / 2048),
                           [PS(0)], ["mu"])
                        op("dve", lambda e, n=n: e.tensor_tensor(out=tmpf[:, 0:n], in0=mu[:, 0:n], in1=mu[:, 0:n], op=ALU.mult),
                           ["mu"], ["tmpf"])
                        op("dve", lambda e, n=n: e.scalar_tensor_tensor(out=rs[:, 0:n], in0=ps[1][:, 0:n], scalar=1.0 / 2048,
                                                                         in1=tmpf[:, 0:n], op0=ALU.mult, op1=ALU.subtract),
                           [PS(1), "tmpf"], ["rs"])
                        op("act", lambda e, n=n: e.activation(out=rs[:, 0:n], in_=rs[:, 0:n], func=AF.Sqrt, bias=EPS), ["rs"], ["rs"])
                        op("dve", lambda e, n=n: e.reciprocal(out=rs[:, 0:n], in_=rs[:, 0:n]), ["rs"], ["rs"])
                        for j in range(16):
                            op("dve", lambda e, j=j, t0=t0, n=n: e.tensor_tensor(out=tmpf[:, 0:n], in0=cT[:, j, t0:t0 + n],
                                                                                  in1=mu[:, 0:n], op=ALU.subtract),
                               ["cT", "mu"], ["tmpf"])
                            op("dve", lambda e, n=n: e.tensor_tensor(out=tmpf[:, 0:n], in0=tmpf[:, 0:n], in1=rs[:, 0:n], op=ALU.mult),
                               ["tmpf", "rs"], ["tmpf"])
                            op("act", lambda e, j=j, t0=t0, n=n: e.activation(out=cT[:, j, t0:t0 + n], in_=tmpf[:, 0:n], func=AF.Silu,
                                                                               scale=cpar[:, j, 32:33], bias=cpar[:, j, 33:34]),
                               ["tmpf", "cpar"], ["cT"])
                stage_end(4)
                with contextlib.ExitStack() as mg_es:
                    xtb2 = xt[:].bitcast(BF16)
                    mab = xtb2[:, 0:2 * NT].rearrange("p (c t) -> p c t", c=2)
                    mo = xtb2[:, 2 * NT:4 * NT].rearrange("p (c t) -> p c t", c=2)
                    sgs2 = xt[:, 2304:2304 + NT].bitcast(BF16).rearrange("p (c t) -> p c t", c=2)
                    for m in range(16):
                        wgb, kgb = wnext()
                        dma("sp", mab[:], s_mT[m * 256:(m + 1) * 256, :].rearrange("(c p) t -> p c t", p=128), reads=["s_mT"],
                            writes=["xt_E1"])

                        def ev_gb(mm_, gi, t0, n, pap, pkey):
                            op("act", lambda e: e.activation(out=sgs2[:, mm_, t0:t0 + n], in_=pap, func=AF.Sigmoid), [pkey], ["xt_E3"])
                        gemm_ws(wgb, kgb, 32, 256, hT, "hT", NGRP, ev_gb, [0, 1, 2, 3])
                        wbb, kbb = wnext()

                        def ev_bb(mm_, gi, t0, n, pap, pkey):
                            op("dve", lambda e: e.tensor_tensor(out=sgs2[:, mm_, t0:t0 + n], in0=pap, in1=sgs2[:, mm_, t0:t0 + n], op=ALU.mult),
                               [pkey, "xt_E3"], ["xt_E3"])
                            op("dve", lambda e: e.tensor_tensor(out=mo[:, mm_, t0:t0 + n], in0=sgs2[:, mm_, t0:t0 + n], in1=mab[:, mm_, t0:t0 + n],
                                                                op=ALU.add), ["xt_E3", "xt_E1"], ["xt_E2"])
                        gemm_ws(wbb, kbb, 16, 256, cT, "cT", NGRP, ev_bb, [4, 5, 6, 7])
                        dma("sp", s_mT[m * 256:(m + 1) * 256, :].rearrange("(c p) t -> p c t", p=128), mo[:], reads=["xt_E2", "xt_E1"],
                            writes=["s_mT"])

        stage_end(5)
        late = contextlib.ExitStack()
        with late:
            xres = [sb("xres%d" % i, [128, 512], F32, late) for i in range(3)]
            dma("sp", hT[:], s_mT.rearrange("(c p) t -> p c t", p=128), reads=["s_mT"], writes=["hT"])
            rcount = [0]

            def resid_gemm(wview, wkey, actT, act_key, src_fn, src_key, dst, c0, ncols, dst_key):
                def ev(ti, t0, rows, pap, pkey):
                    bi = rcount[0] % 3
                    rcount[0] += 1
                    dma("sp", xres[bi][0:rows, 0:ncols], src_fn(t0, rows, c0, ncols), reads=[src_key], writes=["xres%d" % bi])
                    op("dve", lambda e: e.tensor_tensor(out=xres[bi][0:rows, 0:ncols], in0=pap, in1=xres[bi][0:rows, 0:ncols], op=ALU.add),
                       [pkey, "xres%d" % bi], ["xres%d" % bi])
                    dma("sp", dst[t0:t0 + rows, c0:c0 + ncols], xres[bi][0:rows, 0:ncols], reads=["xres%d" % bi],
                        writes=[dst_key])
                gemm_as([(wview, 0)], [wkey], actT, act_key, TILES, ncols, ev, [0, 1, 2, 3])

            def xsrc(t0, rows, c0, ncols):
                if t0 < NP:
                    return x_main[t0:t0 + rows, c0:c0 + ncols]
                return x_s[:, c0:c0 + ncols]
            for cb in range(16):
                w0, k0 = wnext()
                resid_gemm(w0, k0, hT, "hT", xsrc, "x_in", s_x1, cb * 256, 256, "s_x1")

            stage_end(6)
            ca = contextlib.ExitStack()
            with ca:
                mvb = sb("mvb", [128, 2, 1024], BF16, ca)
                mkT = sb("mkT", [128, 8, 256], BF16, ca)
                q2T = sb("q2T", [128, 8, NT], BF16, ca)
                oT = q2T
                kvf = sb("kvf", [128, 512], F32, ca)
                mscope = contextlib.ExitStack()
                mT = sb("mT", [128, 32, 256], BF16, mscope)
                load_gain(norm_mem_g)
                for i in range(2):
                    norm_tile(mem[i * 128:(i + 1) * 128, :], 128, i * 128, mT, "mT")
                stage_end(6.05)
                for which, dst_o in ((0, mk_o), (1, mv_o)):
                    for cb in range(4):
                        w0, k0 = wnext()

                        def ev_kv(ti, t0, rows, pap, pkey, which=which, cb=cb, dst_o=dst_o):
                            kb = "kvf%d" % (ti % 2)
                            kv_ = kvf[:, (ti % 2) * 256:(ti % 2) * 256 + 256]
                            op("act", lambda e: e.activation(out=kv_, in_=pap, func=AF.Copy), [pkey], [kb])
                            if which == 1:
                                op("dve", lambda e: e.tensor_copy(out=mvb[:, ti, cb * 256:(cb + 1) * 256], in_=kv_), [kb], ["mvb"])
                            dma("sp", dst_o[t0:t0 + 128, cb * 256:(cb + 1) * 256], kv_, reads=[kb])
                        gemm_as([(w0, 0)], [k0], mT, "mT", [(0, 128), (128, 128)], 256, ev_kv, [0, 1, 2, 3])
                stage_end(6.1)
                for m in range(4):
                    wv, wk = wnext()

                    def ev_mk(mm_, gi, t0, n, pap, pkey, m=m):
                        op("act", lambda e: e.activation(out=mkT[:, 2 * m + mm_, :], in_=pap, func=AF.Copy), [pkey], ["mkT"])
                    gemm_ws(wv, wk, 32, 256, mT, "mT", [(0, 256)], ev_mk, [0, 1, 2, 3])
                stage_end(6.2)
                mscope.close()
                load_gain(norm_ca_g)
                for ti, (t0, rows) in enumerate(TILES):
                    norm_tile(s_x1[t0:t0 + rows, :], rows, t0, hT, "hT", src_reads=["s_x1"])
                for m in range(4):
                    wv, wk = wnext()

                    def ev_q2(mm_, gi, t0, n, pap, pkey, m=m):
                        op("act", lambda e: e.activation(out=q2T[:, 2 * m + mm_, t0:t0 + n], in_=pap, func=AF.Copy), [pkey], ["q2T"])
                    gemm_ws(wv, wk, 32, 256, hT, "hT", NGRP, ev_q2, [0, 1, 2, 3])

                stage_end(6.4)
                at = contextlib.ExitStack()
                with at:
                    pex = sb("pex", [128, 4, 256], F32, at)
                    pbf = sb("pbf", [128, 4, 256], BF16, at)
                    pT = hb[:].rearrange("p (s h t) -> p s h t", s=2, h=4)
                    mx = sb("mx", [128, 16], F32, at)
                    xtb3 = xt[:].bitcast(BF16)
                    kc = [xtb3[:, i * 2048:(i + 1) * 2048].rearrange("p (c d) -> p c d", c=2) for i in range(2)]
                    vc = [xtb3[:, 4096 + i * 2048:4096 + (i + 1) * 2048].rearrange("p (c d) -> p c d", c=2) for i in range(2)]
                    kTs = sb("kTs", [128, 8, 256], BF16, at)
                    Q2M = sb("Q2M", [128, 8, 64], BF16, at)
                    pTm = sb("pTm", [128, 2, 4, 64], BF16, at)
                    osb = sb("osb", [64, 1024], BF16, at)

                    def softmax_rows(rows, banks):
                        for h in range(4):
                            sc = ps[banks[h // 2]][0:rows, (h % 2) * 256:(h % 2) * 256 + 256]
                            op("dve", lambda e, h=h, sc=sc: e.reduce_max(out=mx[0:rows, h:h + 1], in_=sc, axis=AX.X),
                               [PS(banks[h // 2])], ["mx"])
                        op("dve", lambda e: e.tensor_scalar(out=mx[0:rows, 4:8], in0=mx[0:rows, 0:4], scalar1=-1.0 / 16.0,
                                                            scalar2=None, op0=ALU.mult), ["mx"], ["mx"])
                        for h in range(4):
                            sc = ps[banks[h // 2]][0:rows, (h % 2) * 256:(h % 2) * 256 + 256]
                            op("act", lambda e, h=h, sc=sc: e.activation(out=pex[0:rows, h, :], in_=sc, func=AF.Exp, scale=1.0 / 16.0,
                                                                          bias=mx[0:rows, 4 + h:5 + h], accum_out=mx[0:rows, 8 + h:9 + h]),
                               [PS(banks[h // 2]), "mx"], ["pex", "mx"])
                        op("dve", lambda e: e.reciprocal(out=mx[0:rows, 12:16], in_=mx[0:rows, 8:12]), ["mx"], ["mx"])
                        for h in range(4):
                            op("dve", lambda e, h=h: e.tensor_scalar(out=pbf[0:rows, h, :], in0=pex[0:rows, h, :],
                                                                      scalar1=mx[0:rows, 12 + h:13 + h], scalar2=None, op0=ALU.mult),
                               ["pex", "mx"], ["pbf"])

                    for gq in range(2):
                        for tl in range(4):
                            t0 = gq * 512 + tl * 128

                            def mm_sc(e, t0=t0):
                                for h in range(4):
                                    for dc in range(2):
                                        ins = e.matmul(ps[h // 2][:, (h % 2) * 256:(h % 2) * 256 + 256], lhsT=q2T[:, 2 * h + dc, t0:t0 + 128],
                                                       rhs=mkT[:, 2 * h + dc, :], start=(dc == 0), stop=(dc == 1))
                                return ins
                            op("pe", mm_sc, ["q2T", "mkT"], [PS(0), PS(1)])
                            softmax_rows(128, [0, 1])
                            pv = ps[2][:].bitcast(BF16).rearrange("p (a b) -> p a b", a=8)

                            def trp(e):
                                for h in range(4):
                                    for sc_ in range(2):
                                        ins = e.transpose(out=pv[:, sc_ * 4 + h, :], in_=pbf[:, h, sc_ * 128:(sc_ + 1) * 128], identity=ident_b[:])
                                return ins
                            op("pe", trp, ["pbf", "ident_b"], [PS(2)])
                            op("act", lambda e, tl=tl: e.activation(out=pT[:, :, :, tl * 128:(tl + 1) * 128],
                                                                     in_=pv.rearrange("p (s h) t -> p s h t", s=2), func=AF.Copy),
                               [PS(2)], ["hb_kdT"])
                        for h in range(4):
                            for dc in range(2):
                                b = 4 + (h * 2 + dc) % 4

                                def mm_ov(e, h=h, dc=dc, b=b):
                                    for sc_ in range(2):
                                        ins = e.matmul(ps[b][:, :], lhsT=mvb[:, sc_, h * 256 + dc * 128:h * 256 + dc * 128 + 128],
                                                       rhs=pT[:, sc_, h, :], start=(sc_ == 0), stop=(sc_ == 1))
                                    return ins
                                op("pe", mm_ov, ["mvb", "hb_kdT"], [PS(b)])
                                op("act", lambda e, h=h, dc=dc, b=b, gq=gq: e.activation(
                                    out=oT[:, 2 * h + dc, gq * 512:(gq + 1) * 512], in_=ps[b][:, :], func=AF.Copy), [PS(b)], ["q2T"])
                    stage_end(6.6)
                    for c8 in range(8):
                        op("dve", lambda e, c8=c8: e.tensor_copy(out=Q2M[:, c8, :], in_=q2T[:, c8, NP:NT]), ["q2T"], ["Q2M"])
                    bmv = bm_b[:]
                    for sq in range(NSEQ):
                        bi = sq % 2
                        dma("pool", kc[bi][:], ck[sq].rearrange("(c p) d -> p c d", p=128), writes=[("xt_E1", "xt_E2")[bi]])
                        for half in range(2):
                            pv = ps[4 + half][:].bitcast(BF16).rearrange("p (a b) -> p a b", a=8)

                            def trk(e, half=half, pv=pv, bi=bi):
                                for c4 in range(4):
                                    c8 = half * 4 + c4
                                    for sc_ in range(2):
                                        ins = e.transpose(out=pv[:, c4 * 2 + sc_, :], in_=kc[bi][:, sc_, c8 * 128:(c8 + 1) * 128],
                                                          identity=ident_b[:])
                                return ins
                            op("pe", trk, [("xt_E1", "xt_E2")[bi], "ident_b"], [PS(4 + half)])
                            op("act", lambda e, half=half, pv=pv: e.activation(
                                out=kTs[:, half * 4:(half + 1) * 4, :].rearrange("p c (s t) -> p c s t", s=2),
                                in_=pv.rearrange("p (c s) t -> p c s t", s=2), func=AF.Copy), [PS(4 + half)], ["kTs"])
                        qm = sb if False else None

                        def mm_ss(e, sq=sq):
                            for h in range(4):
                                for dc in range(2):
                                    ins = e.matmul(ps[h][0:64, 0:256], lhsT=QMs[:, 2 * h + dc, :], rhs=kTs[:, 2 * h + dc, :],
                                                   start=(sq == 0 and dc == 0), stop=(sq == NSEQ - 1 and dc == 1))
                            return ins
                        QMs = sb("QMs%d" % sq, [128, 8, 64], BF16, at) if sq < 2 else QMs_l[sq % 2]
                        if sq < 2:
                            if sq == 0:
                                QMs_l = [QMs, None]
                            else:
                                QMs_l[1] = QMs
                        op("dve", lambda e, sq=sq, QMs=QMs: e.tensor_tensor(
                            out=QMs[:], in0=Q2M[:], in1=bmv[:, sq, :].unsqueeze(1).to_broadcast([128, 8, 64]), op=ALU.mult),
                           ["Q2M", "bm_b"], ["QMs%d" % (sq % 2)])
                        op("pe", mm_ss, ["QMs%d" % (sq % 2), "kTs"], [PS(0), PS(1), PS(2), PS(3)])
                    for h in range(4):
                        op("dve", lambda e, h=h: e.reduce_max(out=mx[0:64, h:h + 1], in_=ps[h][0:64, 0:256], axis=AX.X), [PS(h)], ["mx"])
                    op("dve", lambda e: e.tensor_scalar(out=mx[0:64, 4:8], in0=mx[0:64, 0:4], scalar1=-1.0 / 16.0, scalar2=None,
                                                        op0=ALU.mult), ["mx"], ["mx"])
                    for h in range(4):
                        op("act", lambda e, h=h: e.activation(out=pex[0:64, h, :], in_=ps[h][0:64, 0:256], func=AF.Exp, scale=1.0 / 16.0,
                                                               bias=mx[0:64, 4 + h:5 + h], accum_out=mx[0:64, 8 + h:9 + h]),
                           [PS(h), "mx"], ["pex", "mx"])
                    op("dve", lambda e: e.reciprocal(out=mx[0:64, 12:16], in_=mx[0:64, 8:12]), ["mx"], ["mx"])
                    for h in range(4):
                        op("dve", lambda e, h=h: e.tensor_scalar(out=pbf[0:64, h, :], in0=pex[0:64, h, :], scalar1=mx[0:64, 12 + h:13 + h],
                                                                  scalar2=None, op0=ALU.mult), ["pex", "mx"], ["pbf"])
                    pv = ps[4][:].bitcast(BF16).rearrange("p (a b) -> p a b", a=8)

                    def trps(e):
                        for h in range(4):
                            for sc_ in range(2):
                                ins = e.transpose(out=pv[:, sc_ * 4 + h, 0:64], in_=pbf[0:64, h, sc_ * 128:(sc_ + 1) * 128],
                                                  identity=ident_b[0:64, 0:64])
                        return ins
                    op("pe", trps, ["pbf", "ident_b"], [PS(4)])
                    pTs = sb("pTs", [128, 8, 64], BF16, at)
                    op("act", lambda e: e.activation(out=pTs[:], in_=pv[:, :, 0:64], func=AF.Copy), [PS(4)], ["pTs"])
                    for sq in range(NSEQ):
                        bi = sq % 2
                        dma("pool", vc[bi][:], cv[sq].rearrange("(c p) d -> p c d", p=128), writes=[("xt_E3", "xt")[bi]])
                        op("dve", lambda e, sq=sq: e.tensor_tensor(
                            out=pTm[:].rearrange("p s h t -> p (s h) t"), in0=pTs[:],
                            in1=bmv[:, sq, :].unsqueeze(1).to_broadcast([128, 8, 64]), op=ALU.mult), ["pTs", "bm_b"], ["pTm"])

                        def mm_os(e, sq=sq, bi=bi):
                            for h in range(4):
                                for sc_ in range(2):
                                    ins = e.matmul(ps[h][0:64, 0:256], lhsT=pTm[:, sc_, h, :], rhs=vc[bi][:, sc_, h * 256:(h + 1) * 256],
                                                   start=(sq == 0 and sc_ == 0), stop=(sq == NSEQ - 1 and sc_ == 1))
                            return ins
                        op("pe", mm_os, ["pTm", ("xt_E3", "xt")[bi]], [PS(0), PS(1), PS(2), PS(3)])
                    for h in range(4):
                        op("act", lambda e, h=h: e.activation(out=osb[:, h * 256:(h + 1) * 256], in_=ps[h][0:64, 0:256], func=AF.Copy),
                           [PS(h)], ["osb"])
                    pv = ps[5][:].bitcast(BF16).rearrange("p (a b) -> p a b", a=8)

                    def tro(e):
                        for c8 in range(8):
                            ins = e.transpose(out=pv[:, c8, 0:64], in_=osb[:, c8 * 128:(c8 + 1) * 128], identity=ident_b[0:64, 0:64])
                        return ins
                    op("pe", tro, ["osb", "ident_b"], [PS(5)])
                    op("act", lambda e: e.activation(out=oT[:, :, NP:NT], in_=pv[:, :, 0:64], func=AF.Copy), [PS(5)], ["q2T"])

                stage_end(6.8)
                def x1src(t0, rows, c0, ncols):
                    return s_x1[t0:t0 + rows, c0:c0 + ncols]
                for cb in range(4):
                    w0, k0 = wnext()
                    for sub in range(2):
                        resid_gemm(w0[:, :, sub * 512:(sub + 1) * 512], k0, oT, "q2T", x1src, "s_x1", s_x2, cb * 1024 + sub * 512, 512, "s_x2")

            stage_end(7)
            late.close()
            hscope.close()
            moe = contextlib.ExitStack()
            with moe:
                wr = sb("wr", [128, 32, 36], F32, moe)
                brt = sb("brt", [128, 36], F32, moe)
                xgTbuf = sb("xgTbuf", [128, D], F32, moe)
                hTf = xgTbuf[:].rearrange("p (k t) -> p k t", k=32)
                xgT = xgTbuf[:].bitcast(BF16).rearrange("p (k c) -> p k c", k=32)
                hbf = sb("hbf", [128, D], F32, moe)
                gbcf = sb("gbcf", [128, D], F32, moe)
                lg = sb("lg", [128, 9, 36], F32, moe)
                gate = sb("gate", [128, 9, 32], F32, moe)
                msk = sb("msk", [128, 9, 32], BF16, moe)
                m2f = sb("m2f", [128, 9, 32], F32, moe)
                rank = sb("rank", [128, 9, 32], F32, moe)
                tmp = sb("tmp", [128, 9, 40], F32, moe)
                rhs6 = sb("rhs6", [128, 9, NE, 6], BF16, moe)
                sel = sb("sel", [128, 9, CAP], BF16, moe)
                slot = sb("slot", [128, 2, 8], F32, moe)
                idxg = sb("idxg", [128, 2], U32, moe)
                idxs = sb("idxs", [128, 2], U32, moe)
                xg = sb("xg", [128, 2, D], BF16, moe)
                hidT = sb("hidT", [128, 4, CAP], BF16, moe)
                sgl = sb("sgl", [128, 2, CAP], F32, moe)
                ye = [sb("ye0", [128, D], F32, moe), hbf]
                yek = ["ye0", "hbf"]
                dma("sp", wr[:], w_router.rearrange("(k p) n -> p k n", p=128), writes=["wr"])
                dma("sp", brt[:], b_router.partition_broadcast(128), writes=["brt"])
                op("pool", lambda e: e.memset(hb[0:1, :], 0.0), [], [*HBK])
                dma("sp", s_h3[ZROW:ZROW + 1, :], hb[0:1, :], reads=[*HBK], writes=["s_h3"])
                dma("sp", gbcf[:], norm_ffn_g.partition_broadcast(128), writes=["gbcf"])

                for ti, (t0, rows) in enumerate(TILES):
                    def f32T(rows, ti=ti, t0=t0):
                        op("dve", lambda e: e.scalar_tensor_tensor(out=hbf[0:rows, :], in0=xt[0:rows, :], scalar=sm[0:rows, 2:3],
                                                                   in1=gbcf[0:rows, :], op0=ALU.mult, op1=ALU.mult),
                           [*XTK, "sm", "gbcf"], ["hbf"])
                        op("act", lambda e: e.activation(out=hb[0:rows, :], in_=hbf[0:rows, :], func=AF.Copy), ["hbf"], [*HBK])
                        dma("sp", s_h3[t0:t0 + rows, :], hb[0:rows, :], reads=[*HBK], writes=["s_h3"])
                        for g in range(8):
                            b = 4 + g % 4

                            def tr(e, g=g, b=b):
                                for j in range(4):
                                    c = g * 4 + j
                                    ins = e.transpose(out=ps[b][:, j * 128:j * 128 + rows], in_=hbf[0:rows, c * 128:(c + 1) * 128],
                                                      identity=ident_f[0:rows, 0:rows])
                                return ins
                            op("pe", tr, ["hbf", "cst"], [PS(b)])
                            op("act" if g % 2 == 0 else "dve",
                               (lambda e, g=g, b=b: e.activation(out=hTf[:, g * 4:(g + 1) * 4, 0:rows],
                                                                 in_=ps[b][:].rearrange("p (a t) -> p a t", a=4)[:, :, 0:rows], func=AF.Copy))
                               if g % 2 == 0 else
                               (lambda e, g=g, b=b: e.tensor_copy(out=hTf[:, g * 4:(g + 1) * 4, 0:rows],
                                                                  in_=ps[b][:].rearrange("p (a t) -> p a t", a=4)[:, :, 0:rows])),
                               [PS(b)], ["xgT"])

                        def mmr(e):
                            for k in range(32):
                                ins = e.matmul(ps[3][0:rows, 0:36], lhsT=hTf[:, k, 0:rows], rhs=wr[:, k, :], start=(k == 0), stop=(k == 31))
                            return ins
                        op("pe", mmr, ["xgT", "wr"], [PS(3)])
                        op("dve", lambda e: e.tensor_tensor(out=lg[0:rows, ti, :], in0=ps[3][0:rows, 0:36], in1=brt[0:rows, :], op=ALU.add),
                           [PS(3), "brt"], ["lg"])
                    norm_tile(s_x2[t0:t0 + rows, :], rows, t0, None, None, fp32_T=f32T, src_reads=["s_x2"])
                op("pool", lambda e: e.memset(lg[64:128, 8, :], -30000.0), [], ["lg"]) if False else None
                R_ = ["lg", "tmp", "gate", "msk", "m2f"]
                lgg = lg[:, :, 0:4]
                lge = lg[:, :, 4:36].rearrange("p t (g e) -> p t g e", g=4)
                T0, T1, T2, T3, T4, T5 = (tmp[:, :, i:i + 1] for i in range(6))
                gm = tmp[:, :, 8:12]
                op("dve", lambda e: e.tensor_reduce(out=tmp[:, :, 0:1], in_=lgg, axis=AX.X, op=ALU.max), ["lg"], ["tmp"])
                op("dve", lambda e: e.tensor_tensor(out=tmp[:, :, 12:16], in0=lgg, in1=T0.to_broadcast([128, 9, 4]), op=ALU.subtract),
                   ["lg", "tmp"], ["tmp"])
                op("dve", lambda e: e.tensor_single_scalar(out=gm, in_=tmp[:, :, 12:16], scalar=0.0, op=ALU.is_ge), ["tmp"], ["tmp"])
                op("act", lambda e: e.activation(out=tmp[:, :, 16:20], in_=tmp[:, :, 12:16], func=AF.Exp), ["tmp"], ["tmp"])
                op("dve", lambda e: e.tensor_reduce(out=tmp[:, :, 1:2], in_=tmp[:, :, 16:20], axis=AX.X, op=ALU.add), ["tmp"], ["tmp"])
                op("dve", lambda e: e.reciprocal(out=tmp[:, :, 2:3], in_=tmp[:, :, 1:2]), ["tmp"], ["tmp"])
                op("dve", lambda e: e.tensor_scalar(out=tmp[:, :, 20:24], in0=gm, scalar1=-1.0, scalar2=10000.0, op0=ALU.add, op1=ALU.mult),
                   ["tmp"], ["tmp"])
                lem = m2f[:].rearrange("p t (g e) -> p t g e", g=4)
                op("dve", lambda e: e.tensor_tensor(out=lem, in0=lge, in1=tmp[:, :, 20:24].unsqueeze(3).to_broadcast([128, 9, 4, 8]),
                                                    op=ALU.add), ["lg", "tmp"], ["m2f"])
                op("dve", lambda e: e.tensor_reduce(out=tmp[:, :, 3:4], in_=m2f[:], axis=AX.X, op=ALU.max), ["m2f"], ["tmp"])
                op("dve", lambda e: e.tensor_tensor(out=gate[:], in0=m2f[:], in1=T3.to_broadcast([128, 9, 32]), op=ALU.is_ge),
                   ["m2f", "tmp"], ["gate"])
                op("dve", lambda e: e.scalar_tensor_tensor(out=rank[:].rearrange("p t e -> p (t e)"), in0=gate[:].rearrange("p t e -> p (t e)"),
                                                           scalar=-20000.0, in1=m2f[:].rearrange("p t e -> p (t e)"), op0=ALU.mult, op1=ALU.add),
                   ["gate", "m2f"], ["rank"])
                op("dve", lambda e: e.tensor_reduce(out=tmp[:, :, 4:5], in_=rank[:], axis=AX.X, op=ALU.max), ["rank"], ["tmp"])
                op("dve", lambda e: e.tensor_tensor(out=m2f[:], in0=rank[:], in1=T4.to_broadcast([128, 9, 32]), op=ALU.is_ge),
                   ["rank", "tmp"], ["m2f"])
                op("dve", lambda e: e.tensor_tensor(out=tmp[:, :, 5:6], in0=T3, in1=T4, op=ALU.subtract), ["tmp"], ["tmp"])
                op("act", lambda e: e.activation(out=tmp[:, :, 6:7], in_=tmp[:, :, 5:6], func=AF.Sigmoid), ["tmp"], ["tmp"])
                op("dve", lambda e: e.tensor_tensor(out=tmp[:, :, 24:25], in0=tmp[:, :, 6:7], in1=tmp[:, :, 2:3], op=ALU.mult), ["tmp"], ["tmp"])
                op("dve", lambda e: e.tensor_tensor(out=tmp[:, :, 25:26], in0=tmp[:, :, 2:3], in1=tmp[:, :, 24:25], op=ALU.subtract),
                   ["tmp"], ["tmp"])
                op("dve", lambda e: e.tensor_tensor(out=msk[:], in0=gate[:], in1=m2f[:], op=ALU.add), ["gate", "m2f"], ["msk"])
                op("dve", lambda e: e.tensor_tensor(out=gate[:], in0=gate[:], in1=tmp[:, :, 24:25].to_broadcast([128, 9, 32]), op=ALU.mult),
                   ["gate", "tmp"], ["gate"])
                op("dve", lambda e: e.tensor_tensor(out=rank[:], in0=m2f[:], in1=tmp[:, :, 25:26].to_broadcast([128, 9, 32]), op=ALU.mult),
                   ["m2f", "tmp"], ["rank"])
                op("dve", lambda e: e.tensor_tensor(out=gate[:], in0=gate[:], in1=rank[:], op=ALU.add), ["gate", "rank"], ["gate"])
                op("pool", lambda e: e.memset(msk[64:128, 8, :], 0.0), [], ["msk"])
                tk = cst[:, C_TK:C_TK + 27].rearrange("p (t c) -> p t c", c=3)
                for c3 in range(3):
                    op("dve", lambda e, c3=c3: e.tensor_tensor(out=rhs6[:, :, :, c3], in0=msk[:],
                                                                in1=tk[:, :, c3:c3 + 1].to_broadcast([128, 9, 32]), op=ALU.mult),
                       ["msk", "cst"], ["rhs6"])
                op("dve", lambda e: e.tensor_tensor(out=rhs6[:, :, :, 3], in0=gate[:], in1=msk[:], op=ALU.mult), ["gate", "msk"], ["rhs6"])
                op("dve", lambda e: e.tensor_tensor(out=rank[:], in0=gate[:], in1=rhs6[:, :, :, 3], op=ALU.subtract), ["gate", "rhs6"], ["rank"])
                op("dve", lambda e: e.tensor_tensor(out=rhs6[:, :, :, 4], in0=rank[:], in1=msk[:], op=ALU.mult), ["rank", "msk"], ["rhs6"])
                op("dve", lambda e: e.tensor_tensor(out=rhs6[:, :, :, 5], in0=m2f[:], in1=msk[:], op=ALU.mult), ["m2f", "msk"], ["rhs6"])
                for ti in range(9):
                    def mmrk(e, ti=ti):
                        for tj in range(ti):
                            e.matmul(ps[4][:, 0:32], lhsT=ones_b[:], rhs=msk[:, tj, :], start=(tj == 0), stop=False)
                        return e.matmul(ps[4][:, 0:32], lhsT=ls_b[:], rhs=msk[:, ti, :], start=(ti == 0), stop=True)
                    op("pe", mmrk, ["ones_b", "ls_b", "msk"], [PS(4)])
                    op("dve", lambda e, ti=ti: e.tensor_copy(out=rank[:, ti, :], in_=ps[4][:, 0:32]), [PS(4)], ["rank"])
                op("dve", lambda e: e.tensor_copy(out=m2f[:], in_=msk[:]), ["msk"], ["m2f"])

                stage_end(8)
                for ex in range(NE):
                    for ti in range(9):
                        op("dve", lambda e, ti=ti, ex=ex: e.tensor_scalar(
                            out=sel[:, ti, :], in0=cst[:, C_IO:C_IO + CAP], scalar1=rank[:, ti, ex:ex + 1], scalar2=m2f[:, ti, ex:ex + 1],
                            op0=ALU.is_equal, op1=ALU.mult), ["cst", "rank", "m2f"], ["sel"])
                    for sg in range(CAP // 128):
                        def mmsl(e, sg=sg, ex=ex):
                            for ti in range(9):
                                ins = e.matmul(ps[4][:, sg * 8:sg * 8 + 6], lhsT=sel[:, ti, sg * 128:(sg + 1) * 128], rhs=rhs6[:, ti, ex, :],
                                               start=(ti == 0), stop=(ti == 8))
                            return ins
                        op("pe", mmsl, ["sel", "rhs6"], [PS(4)])
                    op("dve", lambda e: e.tensor_copy(out=slot[:].rearrange("p a b -> p (a b)"), in_=ps[4][:, 0:16]), [PS(4)], ["slot"])
                    op("dve", lambda e: e.scalar_tensor_tensor(out=slot[:, :, 6], in0=slot[:, :, 0], scalar=32.0, in1=slot[:, :, 1],
                                                               op0=ALU.mult, op1=ALU.add), ["slot"], ["slot"])
                    op("dve", lambda e: e.tensor_scalar(out=slot[:, :, 7], in0=slot[:, :, 2], scalar1=-1.0, scalar2=-float(ZROW),
                                                        op0=ALU.add, op1=ALU.mult), ["slot"], ["slot"])
                    op("dve", lambda e: e.tensor_tensor(out=slot[:, :, 0], in0=slot[:, :, 6], in1=slot[:, :, 7], op=ALU.add), ["slot"], ["slot"])
                    op("dve", lambda e: e.tensor_copy(out=idxg[:], in_=slot[:, :, 0]), ["slot"], ["idxg"])
                    op("dve", lambda e: e.scalar_tensor_tensor(out=slot[:, :, 1], in0=slot[:, :, 5], scalar=float(NT), in1=slot[:, :, 6],
                                                               op0=ALU.mult, op1=ALU.add), ["slot"], ["slot"])
                    op("dve", lambda e: e.tensor_scalar(out=slot[:, :, 7], in0=slot[:, :, 2], scalar1=-1.0, scalar2=-float(DUMP),
                                                        op0=ALU.add, op1=ALU.mult), ["slot"], ["slot"])
                    op("dve", lambda e: e.tensor_tensor(out=slot[:, :, 1], in0=slot[:, :, 1], in1=slot[:, :, 7], op=ALU.add), ["slot"], ["slot"])
                    op("dve", lambda e: e.tensor_copy(out=idxs[:], in_=slot[:, :, 1]), ["slot"], ["idxs"])
                    op("dve", lambda e: e.tensor_tensor(out=slot[:, :, 3], in0=slot[:, :, 3], in1=slot[:, :, 4], op=ALU.add), ["slot"], ["slot"])
                    for sg in range(CAP // 128):
                        dma("pool", None, None, reads=["idxg", "s_h3"], writes=["xg"],
                            fn=lambda e, sg=sg: e.indirect_dma_start(
                                out=xg[:, sg, :], out_offset=None, in_=s_h3[:, :],
                                in_offset=bass.IndirectOffsetOnAxis(ap=idxg[:, sg:sg + 1], axis=0)))
                    for sg in range(CAP // 128):
                        for g in range(4):
                            b = 4 + g % 4
                            pv = ps[b][:].bitcast(BF16).rearrange("p (a b) -> p a b", a=8)

                            def trx(e, sg=sg, g=g, pv=pv):
                                for j in range(8):
                                    c = g * 8 + j
                                    ins = e.transpose(out=pv[:, j, :], in_=xg[:, sg, c * 128:(c + 1) * 128], identity=ident_b[:])
                                return ins
                            op("pe", trx, ["xg", "ident_b"], [PS(b)])
                            if g % 2 == 0:
                                op("act", lambda e, sg=sg, g=g, pv=pv: e.activation(
                                    out=xgT[:, g * 8:(g + 1) * 8, sg * 128:(sg + 1) * 128], in_=pv, func=AF.Copy), [PS(b)], ["xgT"])
                            else:
                                op("dve", lambda e, sg=sg, g=g, pv=pv: e.tensor_copy(
                                    out=xgT[:, g * 8:(g + 1) * 8, sg * 128:(sg + 1) * 128], in_=pv), [PS(b)], ["xgT"])
                    for half in range(2):
                        for which in range(2):
                            wv_, kv_ = wnext()
                            for mm_ in range(2):
                                fcn = half * 2 + mm_
                                bq = (which * 2 + mm_) % 4

                                def mmg(e, b=bq, mm_=mm_, wv_=wv_):
                                    for k in range(32):
                                        ins = e.matmul(ps[b][:, 0:CAP], lhsT=wv_[:, k, mm_ * 128:(mm_ + 1) * 128], rhs=xgT[:, k, :],
                                                       start=(k == 0), stop=(k == 31))
                                    return ins
                                op("pe", mmg, [kv_, "xgT"], [PS(bq)])
                                if which == 0:
                                    op("act", lambda e, bq=bq, mm_=mm_: e.activation(out=sgl[:, mm_, :], in_=ps[bq][:, 0:CAP], func=AF.Silu),
                                       [PS(bq)], ["sgl"])
                                else:
                                    op("dve", lambda e, bq=bq, fcn=fcn, mm_=mm_: e.tensor_tensor(out=hidT[:, fcn, :], in0=ps[bq][:, 0:CAP],
                                                                                                 in1=sgl[:, mm_, :], op=ALU.mult),
                                       [PS(bq), "sgl"], ["hidT"])
                    for dh in range(2):
                      wdv, kdv = wnext()
                      for sg in range(CAP // 128):
                        yb = ye[sg % 2]
                        for cb in range(dh * 4, dh * 4 + 4):
                            wv, wk_ = wdv, kdv
                            b = 4 + cb % 4

                            def mmd(e, sg=sg, cb=cb, wv=wv, b=b):
                                for k in range(4):
                                    ins = e.matmul(ps[b][:, :], lhsT=hidT[:, k, sg * 128:(sg + 1) * 128],
                                                   rhs=wv[:, k, (cb % 4) * 512:(cb % 4) * 512 + 512], start=(k == 0), stop=(k == 3))
                                return ins
                            op("pe", mmd, ["hidT", wk_], [PS(b)])
                            if cb % 2 == 0:
                                op("act", lambda e, sg=sg, cb=cb, b=b, yb=yb: e.activation(
                                    out=yb[:, cb * 512:(cb + 1) * 512], in_=ps[b][:, :], func=AF.Copy, scale=slot[:, sg, 3:4]),
                                   [PS(b), "slot"], [yek[sg % 2]])
                            else:
                                op("dve", lambda e, sg=sg, cb=cb, b=b, yb=yb: e.tensor_scalar(
                                    out=yb[:, cb * 512:(cb + 1) * 512], in0=ps[b][:, :], scalar1=slot[:, sg, 3:4], scalar2=None, op0=ALU.mult),
                                   [PS(b), "slot"], [yek[sg % 2]])
                        if dh == 1:
                            dma("pool", None, None, reads=["idxs", yek[sg % 2]], writes=["s_o12"],
                                fn=lambda e, sg=sg, yb=yb: e.indirect_dma_start(
                                    out=s_o12[:, :], out_offset=bass.IndirectOffsetOnAxis(ap=idxs[:, sg:sg + 1], axis=0),
                                    in_=yb[:, :], in_offset=None))

                stage_end(9)
                dma("sp", gbcf[:], norm_final_g.partition_broadcast(128), writes=["gbcf"])
                o1 = hbf
                for ti, (t0, rows) in enumerate(TILES):
                    dma("sp", xt[0:rows, :], s_x2[t0:t0 + rows, :], reads=["s_x2"], writes=[*XTK])
                    dma("sp", o1[0:rows, :], s_o12[t0:t0 + rows, :], reads=["s_o12"], writes=["hbf"])
                    op("dve", lambda e, rows=rows: e.tensor_tensor(out=xt[0:rows, :], in0=xt[0:rows, :], in1=o1[0:rows, :], op=ALU.add),
                       [*XTK, "hbf"], [*XTK])
                    dma("sp", o1[0:rows, :], s_o12[NT + t0:NT + t0 + rows, :], reads=["s_o12"], writes=["hbf"])
                    op("dve", lambda e, rows=rows: e.tensor_tensor(out=xt[0:rows, :], in0=xt[0:rows, :], in1=o1[0:rows, :], op=ALU.add),
                       [*XTK, "hbf"], [*XTK])
                    op("act", lambda e, rows=rows: e.activation(out=hb[0:rows, :], in_=xt[0:rows, :], func=AF.Square,
                                                                 accum_out=sm[0:rows, 0:1]), [*XTK], [*HBK, "sm"])
                    op("act", lambda e, rows=rows: e.activation(out=sm[0:rows, 1:2], in_=sm[0:rows, 0:1], func=AF.Sqrt,
                                                                 scale=1.0 / D, bias=EPS), ["sm"], ["sm"])
                    op("dve", lambda e, rows=rows: e.reciprocal(out=sm[0:rows, 2:3], in_=sm[0:rows, 1:2]), ["sm"], ["sm"])
                    op("dve", lambda e, rows=rows: e.scalar_tensor_tensor(out=o1[0:rows, :], in0=xt[0:rows, :], scalar=sm[0:rows, 2:3],
                                                                           in1=gbcf[0:rows, :], op0=ALU.mult, op1=ALU.mult),
                       [*XTK, "sm", "gbcf"], ["hbf"])
                    dst = y_main[t0:t0 + rows, :] if ti < 8 else y_s[:, :]
                    dma("sp", dst, o1[0:rows, :], reads=["hbf"])
      except _Stop:
        S.finish()
        es.pop_all()
        return nc
      S.finish()
    return nc


_NC_CACHE = {}
_RET_MAPS = [False]


def kernel(**inp):
    f = lambda k: np.ascontiguousarray(np.asarray(inp[k], dtype=np.float32))
    x_prompt = f("x_prompt")
    x_sample = f("x_sample")
    consts = make_consts()
    convp = np.ascontiguousarray(np.concatenate(
        [f("conv_dw_w")[0], f("conv_dw_b")[0][None], f("conv_ln_g")[0][None], f("conv_ln_b")[0][None]], axis=0))
    w_router = np.ascontiguousarray(np.concatenate([f("w_router_group")[0], f("w_router_expert")[0]], axis=1))
    b_router = np.ascontiguousarray(np.concatenate([f("b_router_group")[0], f("b_router_expert")[0]], axis=0))
    shared = {
        "consts": consts,
        "norm_mix_g": f("norm_mix_g")[0], "w_in": f("w_in")[0], "w_alpha_up": f("w_alpha_up")[0],
        "b_alpha": f("b_alpha"), "gla_norm_g": f("gla_norm_g")[0], "w_branch_a": f("w_branch_a")[0],
        "convp_in": convp, "w_branch_b": f("w_branch_b")[0], "w_out": f("w_out")[0],
        "norm_ca_g": f("norm_ca_g")[0], "norm_mem_g": f("norm_mem_g")[0], "w_ca_q": f("w_ca_q")[0],
        "w_ca_k": f("w_ca_k")[0], "w_ca_v": f("w_ca_v")[0], "w_ca_o": f("w_ca_o")[0],
        "norm_ffn_g": f("norm_ffn_g")[0], "w_router": w_router, "b_router": b_router,
        "w_exp_gate": f("w_exp_gate")[0], "w_exp_up": f("w_exp_up")[0], "w_exp_down": f("w_exp_down")[0],
        "norm_final_g": f("norm_final_g"),
    }
    st_gla = f("state_gla")[0]
    st_conv = f("state_conv")[0]
    ck = f("cache_mem_k")[0].reshape(128, 256, 1024)
    cv = f("cache_mem_v")[0].reshape(128, 256, 1024)
    mem = f("mem_prompt")
    zeros_pre = np.zeros((NP, D), np.float32)
    in_maps = []
    for c in range(NCORE):
        b, half = c // 2, c % 2
        m = dict(shared)
        m["x_main"] = x_prompt[b, half * NP:(half + 1) * NP]
        m["x_pre"] = x_prompt[b, 0:NP] if half == 1 else zeros_pre
        m["x_s"] = x_sample[c * NSEQ:(c + 1) * NSEQ].reshape(NS, D)
        m["mem"] = mem[b]
        m["st_gla"] = st_gla[c * NSEQ:(c + 1) * NSEQ]
        m["st_conv"] = st_conv[c * NSEQ:(c + 1) * NSEQ]
        m["ck"] = ck[c * NSEQ:(c + 1) * NSEQ]
        m["cv"] = cv[c * NSEQ:(c + 1) * NSEQ]
        in_maps.append(m)
    if _RET_MAPS[0]:
        return in_maps
    if "nc" not in _NC_CACHE:
        _NC_CACHE["nc"] = build()
    nc = _NC_CACHE["nc"]
    res = run_bass_kernel_spmd(nc, in_maps, core_ids=list(range(NCORE)))
    r = res.results
    y_prompt = np.stack([np.concatenate([r[2 * b]["y_main"], r[2 * b + 1]["y_main"]], axis=0) for b in range(4)])
    y_sample = np.concatenate([r[c]["y_s"].reshape(NSEQ, 4, D) for c in range(NCORE)], axis=0)
    gla_prompt = np.stack([r[2 * b + 1]["gla_p"] for b in range(4)])[None]
    conv_prompt = np.stack([r[2 * b + 1]["conv_p"] for b in range(4)])[None]
    mk = np.stack([r[2 * b]["mk_o"].reshape(256, 4, 256) for b in range(4)])[None]
    mv = np.stack([r[2 * b]["mv_o"].reshape(256, 4, 256) for b in range(4)])[None]
    gla_sample = np.concatenate([r[c]["gla_s"] for c in range(NCORE)], axis=0)[None]
    conv_sample = np.concatenate([r[c]["conv_s"] for c in range(NCORE)], axis=0)[None]
    return (y_prompt.astype(np.float32), y_sample.astype(np.float32), gla_prompt.astype(np.float32),
            conv_prompt.astype(np.float32), mk.astype(np.float32), mv.astype(np.float32),
            gla_sample.astype(np.float32), conv_sample.astype(np.float32))
```

```python
import contextlib
import numpy as np
import concourse.bass as bass
import concourse.mybir as mybir
from concourse.bass_utils import run_bass_kernel_spmd

F32 = mybir.dt.float32
BF16 = mybir.dt.bfloat16
U32 = mybir.dt.uint32
I32 = mybir.dt.int32
AF = mybir.ActivationFunctionType
ALU = mybir.AluOpType
AX = mybir.AxisListType

D = 4096
NCORE = 8
NP = 1024
NS = 64
NT = NP + NS
NSEQ = 16
IN_COLS = 18448
OFF_Q, OFF_K, OFF_V, OFF_R, OFF_A, OFF_U, OFF_G = 0, 1024, 2048, 4096, 6144, 6160, 10256
EPS = 1e-6
CAP = 256
NE = 32
ZROW = NT
DUMP = 2 * NT
NGRP = [(0, 512), (512, 512), (1024, 64)]
TILES = [(i * 128, 128) for i in range(8)] + [(1024, 64)]

C_ID, C_MU, C_MS, C_RM, C_IO, C_TK = 0, 128, 256, 320, 336, 592
CWP = 619
C_BM, C_RS, C_LS, C_ON = 619, 1643, 2731, 2859
CW = 2987


def make_consts():
    c = np.zeros((128, CW), np.float32)
    p = np.arange(128)
    c[:, C_ID:C_ID + 128] = np.eye(128)
    c[:, C_MU:C_MU + 128] = (p[:, None] <= p[None, :])
    q = np.arange(64)
    ms = ((q[:, None] // 4) == (q[None, :] // 4)) & (q[:, None] <= q[None, :])
    c[:64, C_MS:C_MS + 64] = ms
    bm = (np.arange(16)[:, None] == (q[None, :] // 4)).astype(np.float32)
    c[:, C_BM:C_BM + 1024] = bm.reshape(1, 1024)
    c[:64, C_RM:C_RM + 16] = ((q[:, None] // 4) == np.arange(16)[None, :])
    t = np.arange(NT)
    rs = np.ones(NT, np.float32)
    rs[:NP][t[:NP] % 128 == 0] = 0.0
    rs[NP:][(t[NP:] - NP) % 4 == 0] = 0.0
    c[:, C_RS:C_RS + NT] = rs[None, :]
    c[:, C_IO:C_IO + 256] = np.arange(256)[None, :]
    for i in range(9):
        tok = i * 128 + p
        c[:, C_TK + 3 * i + 0] = tok // 32
        c[:, C_TK + 3 * i + 1] = tok % 32
        c[:, C_TK + 3 * i + 2] = 1.0
    c[:, C_LS:C_LS + 128] = (p[:, None] < p[None, :])
    c[:, C_ON:C_ON + 128] = 1.0
    return c


class Sched:
    def __init__(self, nc, es, ndma=28):
        self.nc = nc
        self.E = dict(pe=nc.tensor, act=nc.scalar, dve=nc.vector, pool=nc.gpsimd, sp=nc.sync)
        self.sem = {k: es.enter_context(nc.semaphore("c_" + k)) for k in self.E}
        self.cnt = {k: 0 for k in self.E}
        self.seen = {k: {} for k in self.E}
        self.dsem = [es.enter_context(nc.semaphore("dm%d" % i)) for i in range(ndma)]
        self.dval = [0] * ndma
        self.dnext = {"sp": 0, "pool": 0, "act": 0}
        self.dhalf = ndma // 2
        self.res = {}

    def _semobj(self, key):
        return self.sem[key] if isinstance(key, str) else self.dsem[key]

    def _wait(self, eng, tok):
        key, val, src = tok
        if self.seen[eng].get(key, 0) >= val:
            return
        self.E[eng].wait_ge(self._semobj(key), val)
        self.seen[eng][key] = val

    def _deps(self, eng, reads, writes):
        for r in reads:
            st = self.res.get(r)
            if st and st["w"]:
                tok = st["w"]
                if not (tok[2] == eng and eng == "pe"):
                    self._wait(eng, tok)
        for w in writes:
            st = self.res.get(w)
            if st:
                if st["w"] and st["w"][2] != eng:
                    self._wait(eng, st["w"])
                for tok in st["r"]:
                    if tok[2] != eng:
                        self._wait(eng, tok)

    def _commit(self, tok, reads, writes):
        for r in reads:
            st = self.res.setdefault(r, {"w": None, "r": []})
            if tok[2] != "dma":
                st["r"] = [x for x in st["r"] if x[2] != tok[2]]
            st["r"].append(tok)
        for w in writes:
            self.res[w] = {"w": tok, "r": []}

    mute = False

    def op(self, eng, fn, reads=(), writes=()):
        if self.mute:
            return
        self._deps(eng, reads, writes)
        ins = fn(self.E[eng])
        self.cnt[eng] += 1
        ins.then_inc(self.sem[eng], 1)
        self._commit((eng, self.cnt[eng], eng), reads, writes)

    def dma(self, q, out, in_, reads=(), writes=(), fn=None):
        if self.mute:
            return
        base = 0 if q == "pool" else self.dhalf
        i = base + self.dnext[q]
        self.dnext[q] = (self.dnext[q] + 1) % self.dhalf
        if self.dval[i] > 0:
            self._wait(q, (i, self.dval[i], "dma"))
        self._deps(q, reads, writes)
        if fn is None:
            ins = self.E[q].dma_start(out=out, in_=in_)
        else:
            ins = fn(self.E[q])
        ins.then_inc(self.dsem[i], 16)
        self.dval[i] += 16
        self._commit((i, self.dval[i], "dma"), reads, writes)

    def finish(self):
        for i, v in enumerate(self.dval):
            if v > 0:
                self._wait("sp", (i, v, "dma"))
        for k in ("pe", "act", "dve", "pool"):
            if self.cnt[k] > 0:
                self._wait("sp", (k, self.cnt[k], k))


class _Stop(Exception):
    pass


def build(stage=99, mute_until=None, lite=None):
    nc = bass.Bass("TRN2", target_bir_lowering=False)

    def stage_end(k):
        if mute_until is not None and k >= mute_until:
            S.mute = False
        if stage <= k:
            raise _Stop()

    def din(name, shape, dt=F32):
        kind = "ExternalInput" if (lite is None or name in lite) else "Internal"
        return nc.dram_tensor(name, list(shape), dt, kind=kind).ap()

    def dout(name, shape, dt=F32):
        return nc.dram_tensor(name, list(shape), dt, kind="ExternalOutput").ap()

    def dscr(name, shape, dt):
        return nc.dram_tensor(name, list(shape), dt, kind="Internal").ap()

    x_main = din("x_main", [NP, D])
    x_pre = din("x_pre", [NP, D])
    x_s = din("x_s", [NS, D])
    mem = din("mem", [256, D])
    st_gla = din("st_gla", [NSEQ, 4, 256, 512])
    st_conv = din("st_conv", [NSEQ, 30, 2048])
    ck = din("ck", [NSEQ, 256, 1024])
    cv = din("cv", [NSEQ, 256, 1024])
    consts = din("consts", [128, CW])
    norm_mix_g = din("norm_mix_g", [D])
    w_in = din("w_in", [D, IN_COLS])
    w_alpha_up = din("w_alpha_up", [16, 1024])
    b_alpha = din("b_alpha", [1, 1024])
    gla_norm_g = din("gla_norm_g", [2048])
    w_branch_a = din("w_branch_a", [2048, D])
    convp_in = din("convp_in", [34, 2048])
    w_branch_b = din("w_branch_b", [2048, D])
    w_out = din("w_out", [D, D])
    norm_ca_g = din("norm_ca_g", [D])
    norm_mem_g = din("norm_mem_g", [D])
    w_ca_q = din("w_ca_q", [D, 1024])
    w_ca_k = din("w_ca_k", [D, 1024])
    w_ca_v = din("w_ca_v", [D, 1024])
    w_ca_o = din("w_ca_o", [1024, D])
    norm_ffn_g = din("norm_ffn_g", [D])
    w_router = din("w_router", [D, 36])
    b_router = din("b_router", [36])
    w_exp_gate = din("w_exp_gate", [NE, D, 512])
    w_exp_up = din("w_exp_up", [NE, D, 512])
    w_exp_down = din("w_exp_down", [NE, 512, D])
    norm_final_g = din("norm_final_g", [D])

    y_main = dout("y_main", [NP, D])
    y_s = dout("y_s", [NS, D])
    gla_p = dout("gla_p", [4, 256, 512])
    conv_p = dout("conv_p", [30, 2048])
    mk_o = dout("mk_o", [256, 1024])
    mv_o = dout("mv_o", [256, 1024])
    gla_s = dout("gla_s", [NSEQ, 4, 256, 512])
    conv_s = dout("conv_s", [NSEQ, 30, 2048])

    s_state = dscr("s_state", [4, 256, 512], F32)
    s_ogT = dscr("s_ogT", [2048, NT], BF16)
    s_mT = dscr("s_mT", [D, NT], BF16)
    s_x1 = dscr("s_x1", [NT, D], F32)
    s_x2 = dscr("s_x2", [NT, D], F32)
    s_h3 = dscr("s_h3", [NT + 1, D], BF16)
    s_o12 = dscr("s_o12", [2 * NT + 1, D], F32)

    es = contextlib.ExitStack()
    with es:
      S = Sched(nc, es)
      try:
        pass
        XTK = ["xt", "xt_E1", "xt_E2", "xt_E3"]
        HBK = ["hb", "hb_kdT", "hb_sT", "hb_og", "hb_ogT"]
        op, dma = S.op, S.dma

        def sb(name, shape, dt, stack=es):
            return stack.enter_context(nc.sbuf_tensor(name, list(shape), dt))

        ps = [es.enter_context(nc.psum_tensor("ps%d" % i, [128, 512], F32)) for i in range(8)]

        def PS(i):
            return "ps%d" % i

        cst = sb("cst", [128, CWP], F32)
        ident_b = sb("ident_b", [128, 128], BF16)
        bm_b = sb("bm_b", [128, 16, 64], BF16)
        rs_b = sb("rs_b", [128, NT], BF16)
        ls_b = sb("ls_b", [128, 128], BF16)
        ones_b = sb("ones_b", [128, 128], BF16)
        NSLOT = 2
        wsl = [sb("wsl%d" % i, [128, 8192], BF16) for i in range(NSLOT)]
        gbc = sb("gbc", [128, D], BF16)
        xt = sb("xt", [128, D], F32)
        hb = sb("hb", [128, D], BF16)
        sm = sb("sm", [128, 64], F32)

        dma("sp", cst[:], consts[:, 0:CWP], writes=["cst"])
        dma("sp", xt[:, 0:CW - CWP], consts[:, CWP:CW], writes=[*XTK])
        ident_f = cst[:, C_ID:C_ID + 128]
        op("dve", lambda e: e.tensor_copy(out=ident_b[:], in_=cst[:, C_ID:C_ID + 128]), ["cst"], ["ident_b"])
        op("dve", lambda e: e.tensor_copy(out=bm_b[:].rearrange("p a b -> p (a b)"), in_=xt[:, C_BM - CWP:C_BM - CWP + 1024]), [*XTK], ["bm_b"])
        op("dve", lambda e: e.tensor_copy(out=rs_b[:], in_=xt[:, C_RS - CWP:C_RS - CWP + NT]), [*XTK], ["rs_b"])
        op("dve", lambda e: e.tensor_copy(out=ls_b[:], in_=xt[:, C_LS - CWP:C_LS - CWP + 128]), [*XTK], ["ls_b"])
        op("dve", lambda e: e.tensor_copy(out=ones_b[:], in_=xt[:, C_ON - CWP:C_ON - CWP + 128]), [*XTK], ["ones_b"])
        hscope = contextlib.ExitStack()
        hT = sb("hT", [128, 32, NT], BF16, hscope)
        if mute_until is not None:
            S.mute = True

        wstate = {"n": 0, "plan": [], "issued": 0, "moe0": 10 ** 9}

        def wplan(ap2d):
            wstate["plan"].append(ap2d)

        def slot_of(i):
            m0 = wstate["moe0"]
            if i < m0:
                return i % 2
            return [m0 % 2, 1 - m0 % 2, 2][(i - m0) % 3]

        def look(i):
            return 2 if i < wstate["moe0"] else 3

        def _wissue(i):
            ap2d = wstate["plan"][i]
            K, ncols = ap2d.shape
            kc = K // 128
            slot = slot_of(i)
            view = wsl[slot][:, 0:kc * ncols].rearrange("p (k n) -> p k n", k=kc)
            dma("pool", view, ap2d.rearrange("(k p) n -> p k n", p=128), writes=["wsl%d" % slot])

        def wnext(ap2d_check=None):
            i = wstate["n"]
            wstate["n"] += 1
            if S.mute:
                wstate["issued"] = max(wstate["issued"], wstate["n"])
            while not S.mute and wstate["issued"] < min(len(wstate["plan"]), i + look(i)):
                _wissue(wstate["issued"])
                wstate["issued"] += 1
            ap2d = wstate["plan"][i]
            if ap2d_check is not None:
                assert ap2d.shape == ap2d_check.shape and ap2d.offset == ap2d_check.offset, (i, ap2d, ap2d_check)
            K, ncols = ap2d.shape
            kc = K // 128
            slot = slot_of(i)
            view = wsl[slot][:, 0:kc * ncols].rearrange("p (k n) -> p k n", k=kc)
            return view, "wsl%d" % slot

        def load_gain(g_ap):
            dma("pool", gbc[:], g_ap.partition_broadcast(128), writes=["gbc"])

        def norm_tile(src_ap, rows, col, dstT, dst_key, scratch_out=None, fp32_T=None, src_reads=()):
            dma("sp", xt[0:rows, :], src_ap, reads=list(src_reads), writes=[*XTK])
            op("act", lambda e: e.activation(out=hb[0:rows, :], in_=xt[0:rows, :], func=AF.Square,
                                             accum_out=sm[0:rows, 0:1]), [*XTK], [*HBK, "sm"])
            op("act", lambda e: e.activation(out=sm[0:rows, 1:2], in_=sm[0:rows, 0:1], func=AF.Sqrt,
                                             scale=1.0 / D, bias=EPS), ["sm"], ["sm"])
            op("dve", lambda e: e.reciprocal(out=sm[0:rows, 2:3], in_=sm[0:rows, 1:2]), ["sm"], ["sm"])
            if fp32_T is None:
                op("dve", lambda e: e.scalar_tensor_tensor(out=hb[0:rows, :], in0=xt[0:rows, :], scalar=sm[0:rows, 2:3],
                                                           in1=gbc[0:rows, :], op0=ALU.mult, op1=ALU.mult),
                   [*XTK, "sm", "gbc"], [*HBK])
                if scratch_out is not None:
                    dma("sp", scratch_out, hb[0:rows, :], reads=[*HBK])
                for g in range(4):
                    pv = ps[4 + g][:].bitcast(BF16).rearrange("p (a b) -> p a b", a=8)

                    def tr(e, g=g, pv=pv):
                        for j in range(8):
                            c = g * 8 + j
                            ins = e.transpose(out=pv[:, j, 0:rows], in_=hb[0:rows, c * 128:(c + 1) * 128],
                                              identity=ident_b[0:rows, 0:rows])
                        return ins
                    op("pe", tr, [*HBK, "ident_b"], [PS(4 + g)])
                    eng = "act" if g % 2 == 0 else "dve"
                    if eng == "act":
                        op("act", lambda e, g=g, pv=pv: e.activation(out=dstT[:, g * 8:(g + 1) * 8, col:col + rows],
                                                                      in_=pv[:, :, 0:rows], func=AF.Copy),
                           [PS(4 + g)], [dst_key])
                    else:
                        op("dve", lambda e, g=g, pv=pv: e.tensor_copy(out=dstT[:, g * 8:(g + 1) * 8, col:col + rows],
                                                                       in_=pv[:, :, 0:rows]),
                           [PS(4 + g)], [dst_key])
            else:
                fp32_T(rows)

        def gemm_ws(wview, wkey, kc, mcols, acts, act_key, groups, evac, psbanks, extra_reads=()):
            nb = 0
            for m in range(mcols // 128):
                for gi, (t0, n) in enumerate(groups):
                    b = psbanks[nb % len(psbanks)]
                    nb += 1

                    def mm(e, m=m, t0=t0, n=n, b=b):
                        for k in range(kc):
                            ins = e.matmul(ps[b][:, 0:n], lhsT=wview[:, k, m * 128:(m + 1) * 128],
                                           rhs=acts[:, k, t0:t0 + n], start=(k == 0), stop=(k == kc - 1))
                        return ins
                    op("pe", mm, [wkey, act_key] + list(extra_reads), [PS(b)])
                    evac(m, gi, t0, n, ps[b][:, 0:n], PS(b))

        def gemm_as(wviews, wkeys, actT, act_key, tiles, ncols, evac, psbanks):
            nb = 0
            for ti, (t0, rows) in enumerate(tiles):
                b = psbanks[nb % len(psbanks)]
                nb += 1
                ktot = sum(v.shape[1] for v, _ in wviews)

                def mm(e, t0=t0, rows=rows, b=b):
                    kk = 0
                    for v, koff in wviews:
                        for k in range(v.shape[1]):
                            ins = e.matmul(ps[b][0:rows, 0:ncols], lhsT=actT[:, koff + k, t0:t0 + rows],
                                           rhs=v[:, k, :], start=(kk == 0), stop=(kk == ktot - 1))
                            kk += 1
                    return ins
                op("pe", mm, list(wkeys) + [act_key], [PS(b)])
                evac(ti, t0, rows, ps[b][0:rows, 0:ncols], PS(b))

        def cols(a, n):
            return w_in[:, a:a + n]

        def plan_gla(with_q):
            wplan(cols(OFF_A, 16))
            for h in range(4):
                if with_q:
                    wplan(cols(OFF_Q + 256 * h, 256))
                wplan(cols(OFF_K + 256 * h, 256))
                wplan(cols(OFF_V + 512 * h, 256))
                wplan(cols(OFF_V + 512 * h + 256, 256))
                if with_q:
                    wplan(cols(OFF_R + 512 * h, 256))
                    wplan(cols(OFF_R + 512 * h + 256, 256))
        plan_gla(False)
        plan_gla(True)
        for m in range(16):
            wplan(cols(OFF_G + 256 * m, 256))
            wplan(w_branch_a[:, 256 * m:256 * m + 256])
        for m in range(8):
            wplan(cols(OFF_U + 256 * m, 256))
            wplan(cols(OFF_U + 2048 + 256 * m, 256))
        for m in range(16):
            wplan(cols(OFF_G + D + 256 * m, 256))
            wplan(w_branch_b[:, 256 * m:256 * m + 256])
        for cb in range(16):
            wplan(w_out[:, 256 * cb:256 * cb + 256])
        for wm in (w_ca_k, w_ca_v):
            for cb in range(4):
                wplan(wm[:, 256 * cb:256 * cb + 256])
        for m in range(4):
            wplan(w_ca_k[:, 256 * m:256 * m + 256])
        for m in range(4):
            wplan(w_ca_q[:, 256 * m:256 * m + 256])
        for cb in range(4):
            wplan(w_ca_o[:, 1024 * cb:1024 * cb + 1024])
        wstate["moe0"] = len(wstate["plan"])
        for ex in range(NE):
            wplan(w_exp_gate[ex][:, 0:256])
            wplan(w_exp_up[ex][:, 0:256])
            wplan(w_exp_gate[ex][:, 256:512])
            wplan(w_exp_up[ex][:, 256:512])
            wplan(w_exp_down[ex][:, 0:2048])
            wplan(w_exp_down[ex][:, 2048:4096])

        mix = contextlib.ExitStack()
        with mix:
            alT = sb("alT", [17, NT], BF16, mix)
            walb = sb("walb", [17, 1024], BF16, mix)
            ggl = sb("ggl", [128, 512], F32, mix)
            hT_halo = sb("hT_halo", [128, 32, 32], BF16, mix)
            dma("pool", walb[0:16, :], w_alpha_up, writes=["walb"])
            dma("pool", walb[16:17, :], b_alpha, writes=["walb"])
            load_gain(norm_mix_g)

            gl = contextlib.ExitStack()
            with gl:
                U1 = sb("U1", [128, 4608], F32, gl)
                bT = U1[:, 0:2 * NT].rearrange("p (c t) -> p c t", c=2)
                dfT = U1[:, 2 * NT:4 * NT].rearrange("p (c t) -> p c t", c=2)
                U1b = U1[:].bitcast(BF16)
                vh = U1b[:, 0:4608].rearrange("p (i v) -> p i v", i=9)
                gr = U1b[:, 4608:9216].rearrange("p (i v) -> p i v", i=9)
                xtb = xt[:].bitcast(BF16)
                E1 = xtb[:, 0:2 * NT].rearrange("p (c t) -> p c t", c=2)
                E2 = xtb[:, 2 * NT:4 * NT].rearrange("p (c t) -> p c t", c=2)
                E3 = xtb[:, 4 * NT:6 * NT].rearrange("p (c t) -> p c t", c=2)
                kdT = hb[:, 0:2 * NT].rearrange("p (c t) -> p c t", c=2)
                sT = hb[:, 2 * NT:2 * NT + 128]
                og = hb[:, 2304:2816]
                ogT = hb[:, 2816:3328].rearrange("p (a b) -> p a b", a=4)
                dec = sb("dec", [128, 2, 24], F32, gl)
                qeT = sb("qeT", [128, 2, NT], BF16, gl)
                keT = sb("keT", [128, 2, NT], BF16, gl)
                kd = sb("kd", [128, 9, 256], BF16, gl)
                of = sb("of", [128, 512], F32, gl)
                grf = of
                QMs = sb("QMs", [128, 2, 64], BF16, gl)
                Sf = sb("Sf", [128, 2, 512], F32, gl)
                Sb = sb("Sb", [128, 2, 512], BF16, gl)
                st6 = sb("st6", [128, 8], F32, gl)
                s0f = [sb("s0f%d" % i, [128, 512], F32, gl) for i in range(2)]
                s0b = [sb("s0b%d" % i, [128, 512], BF16, gl) for i in range(2)]
                sout = [sb("sout%d" % i, [128, 512], F32, gl) for i in range(2)]
                kdm = sb("kdm", [64, 256], BF16, gl)

                def gla_pass(main):
                    ntok = NT if main else NP
                    groups = NGRP if main else NGRP[:2]
                    tiles = TILES if main else TILES[:8]
                    for ti, (t0, rows) in enumerate(tiles):
                        if main:
                            src = x_main[t0:t0 + rows, :] if ti < 8 else x_s[:, :]
                        else:
                            src = x_pre[t0:t0 + rows, :]
                        norm_tile(src, rows, t0, hT, "hT")
                    if not main:
                        op("dve", lambda e: e.tensor_copy(out=hT_halo[:], in_=hT[:, :, NP - 32:NP]), ["hT"], ["hT_halo"])
                    wv, wk = wnext()
                    op("pool", lambda e: e.memset(alT[:], 1.0), [], ["alT"])

                    def ev_a(m, gi, t0, n, pap, pkey):
                        op("act", lambda e: e.activation(out=alT[0:16, t0:t0 + n], in_=pap[0:16, :], func=AF.Copy),
                           [pkey], ["alT"])
                    for gi, (t0, n) in enumerate(groups):
                        b = gi % 4

                        def mm(e, t0=t0, n=n, b=b):
                            for k in range(32):
                                ins = e.matmul(ps[b][0:16, 0:n], lhsT=wv[:, k, 0:16], rhs=hT[:, k, t0:t0 + n],
                                               start=(k == 0), stop=(k == 31))
                            return ins
                        op("pe", mm, [wk, "hT"], [PS(b)])
                        ev_a(0, gi, t0, n, ps[b][:, 0:n], PS(b))

                    for h in range(4):
                        for dc in range(2):
                            for gi, (t0, n) in enumerate(groups):
                                b = 4 + (dc * 3 + gi) % 4
                                c0 = (2 * h + dc) * 128
                                op("pe", lambda e, b=b, c0=c0, t0=t0, n=n: e.matmul(
                                    ps[b][:, 0:n], lhsT=walb[:, c0:c0 + 128], rhs=alT[:, t0:t0 + n], start=True, stop=True),
                                   ["walb", "alT"], [PS(b)])
                                op("act", lambda e, b=b, dc=dc, t0=t0, n=n: e.activation(
                                    out=dfT[:, dc, t0:t0 + n], in_=ps[b][:, 0:n], func=AF.Exp, scale=-1.0),
                                   [PS(b)], ["U1"])
                            op("act", lambda e, dc=dc: e.activation(out=dfT[:, dc, 0:ntok], in_=dfT[:, dc, 0:ntok],
                                                                     func=AF.Ln, bias=1.0), ["U1"], ["U1"])
                            op("dve", lambda e, dc=dc: e.tensor_scalar(out=dfT[:, dc, 0:ntok], in0=dfT[:, dc, 0:ntok],
                                                                        scalar1=-1.0 / 16.0, scalar2=None, op0=ALU.mult),
                               ["U1"], ["U1"])
                            op("dve", lambda e, dc=dc: e.tensor_tensor_scan(
                                out=bT[:, dc, 0:ntok], data0=rs_b[:, 0:ntok], data1=dfT[:, dc, 0:ntok],
                                initial=0.0, op0=ALU.mult, op1=ALU.add), ["U1", "rs_b"], ["U1"])
                        bTp = bT[:, :, 0:NP].rearrange("p c (n t) -> p c n t", t=128)
                        dfp = dfT[:, :, 0:NP].rearrange("p c (n t) -> p c n t", t=128)
                        for dc in range(2):
                            op("dve", lambda e, dc=dc: e.tensor_tensor(
                                out=dfp[:, dc], in0=bTp[:, dc, :, 127:128].to_broadcast([128, 8, 128]), in1=bTp[:, dc],
                                op=ALU.subtract), ["U1"], ["U1"])
                            op("act", lambda e, dc=dc: e.activation(out=dec[:, dc, 0:8], in_=bTp[:, dc, :, 127], func=AF.Exp),
                               ["U1"], ["dec"])
                        if main:
                            bTs = bT[:, :, NP:NT].rearrange("p c (n t) -> p c n t", t=4)
                            dfs = dfT[:, :, NP:NT].rearrange("p c (n t) -> p c n t", t=4)
                            for dc in range(2):
                                op("dve", lambda e, dc=dc: e.tensor_tensor(
                                    out=dfs[:, dc], in0=bTs[:, dc, :, 3:4].to_broadcast([128, 16, 4]), in1=bTs[:, dc],
                                    op=ALU.subtract), ["U1"], ["U1"])
                                op("act", lambda e, dc=dc: e.activation(out=dec[:, dc, 8:24], in_=bTs[:, dc, :, 3], func=AF.Exp),
                                   ["U1"], ["dec"])
                        op("act", lambda e: e.activation(out=E3[:, :, 0:ntok], in_=dfT[:, :, 0:ntok], func=AF.Exp),
                           ["U1"], ["xt_E3"])
                        if main:
                            op("act", lambda e: e.activation(out=E1[:, :, 0:ntok], in_=bT[:, :, 0:ntok], func=AF.Exp),
                               ["U1"], ["xt_E1"])
                            op("act", lambda e: e.activation(out=E2[:, :, 0:ntok], in_=bT[:, :, 0:ntok], func=AF.Exp,
                                                             scale=-1.0), ["U1"], ["xt_E2"])
                            wv, wk = wnext()

                            def ev_q(m, gi, t0, n, pap, pkey):
                                op("dve", lambda e: e.scalar_tensor_tensor(
                                    out=qeT[:, m, t0:t0 + n], in0=pap, scalar=1.0 / 16.0, in1=E1[:, m, t0:t0 + n],
                                    op0=ALU.mult, op1=ALU.mult), [pkey, "xt_E1"], ["qeT"])
                            gemm_ws(wv, wk, 32, 256, hT, "hT", groups, ev_q, [0, 1, 2, 3])
                        wv, wk = wnext()

                        def ev_k(m, gi, t0, n, pap, pkey):
                            if main:
                                op("dve", lambda e: e.tensor_tensor(out=keT[:, m, t0:t0 + n], in0=pap,
                                                                    in1=E2[:, m, t0:t0 + n], op=ALU.mult),
                                   [pkey, "xt_E2"], ["keT"])
                            op("dve", lambda e: e.tensor_tensor(out=kdT[:, m, t0:t0 + n], in0=pap,
                                                                in1=E3[:, m, t0:t0 + n], op=ALU.mult),
                               [pkey, "xt_E3"], ["hb_kdT"])
                        gemm_ws(wv, wk, 32, 256, hT, "hT", groups, ev_k, [0, 1, 2, 3])
                        for ti, (t0, rows) in enumerate(tiles):
                            b = 4 + ti % 2
                            pv = ps[b][:].bitcast(BF16)

                            def trk(e, t0=t0, rows=rows, pv=pv):
                                for dc in range(2):
                                    ins = e.transpose(out=pv[0:rows, dc * 128:(dc + 1) * 128], in_=kdT[:, dc, t0:t0 + rows],
                                                      identity=ident_b[:])
                                return ins
                            op("pe", trk, ["hb_kdT", "ident_b"], [PS(b)])
                            op("act", lambda e, ti=ti, rows=rows, pv=pv: e.activation(
                                out=kd[0:rows, ti, :], in_=pv[0:rows, 0:256], func=AF.Copy), [PS(b)], ["kd"])
                        for hf in range(2):
                            wv0, wk0 = wnext()

                            def ev_v(ti, t0, rows, pap, pkey, hf=hf):
                                op("act", lambda e: e.activation(out=vh[0:rows, ti, hf * 256:(hf + 1) * 256], in_=pap, func=AF.Copy),
                                   [pkey], ["U1"])
                            gemm_as([(wv0, 0)], [wk0], hT, "hT", tiles, 256, ev_v, [0, 1, 2, 3])
                        if main:
                            dma("sp", ggl[:], gla_norm_g[h * 512:(h + 1) * 512].partition_broadcast(128), writes=["ggl"])
                            for hf in range(2):
                                wv0, wk0 = wnext()

                                def ev_r(ti, t0, rows, pap, pkey, hf=hf):
                                    op("act", lambda e: e.activation(out=grf[0:rows, 0:256], in_=pap, func=AF.Silu), [pkey], ["of"])
                                    op("dve", lambda e: e.tensor_tensor(out=gr[0:rows, ti, hf * 256:(hf + 1) * 256], in0=grf[0:rows, 0:256],
                                                                        in1=ggl[0:rows, hf * 256:(hf + 1) * 256], op=ALU.mult),
                                       ["of", "ggl"], ["U1"])
                                gemm_as([(wv0, 0)], [wk0], hT, "hT", tiles, 256, ev_r, [0, 1, 2, 3])
                        if main:
                            dma("sp", Sf[:], s_state[h].rearrange("(c p) v -> p c v", p=128), reads=["s_state%d" % h], writes=["Sf"])
                        else:
                            op("pool", lambda e: e.memset(Sf[:], 0.0), [], ["Sf"])
                        op("act", lambda e: e.activation(out=Sb[:], in_=Sf[:], func=AF.Copy), ["Sf"], ["Sb"])
                        for n_ in range(8):
                            t0 = n_ * 128
                            if main:
                                def mm_s(e, t0=t0):
                                    for dc in range(2):
                                        ins = e.matmul(ps[4][:, 0:128], lhsT=keT[:, dc, t0:t0 + 128], rhs=qeT[:, dc, t0:t0 + 128],
                                                       start=(dc == 0), stop=(dc == 1))
                                    return ins
                                op("pe", mm_s, ["keT", "qeT"], [PS(4)])
                                op("dve", lambda e: e.tensor_tensor(out=sT[:], in0=ps[4][:, 0:128], in1=cst[:, C_MU:C_MU + 128],
                                                                    op=ALU.mult), [PS(4), "cst"], ["hb_sT"])

                                def mm_o(e, t0=t0, n_=n_):
                                    e.matmul(ps[5][:, :], lhsT=sT[:], rhs=vh[:, n_, :], start=True, stop=False)
                                    for dc in range(2):
                                        ins = e.matmul(ps[5][:, :], lhsT=qeT[:, dc, t0:t0 + 128], rhs=Sb[:, dc, :],
                                                       start=False, stop=(dc == 1))
                                    return ins
                                op("pe", mm_o, ["hb_sT", "U1", "qeT", "Sb"], [PS(5)])
                                finish_o(h, n_, 128, t0)
                            for dc in range(2):
                                b = 6 + dc
                                op("pe", lambda e, dc=dc, b=b, n_=n_: e.matmul(
                                    ps[b][:, :], lhsT=kd[:, n_, dc * 128:(dc + 1) * 128], rhs=vh[:, n_, :], start=True, stop=True),
                                   ["kd", "U1"], [PS(b)])
                                op("dve", lambda e, dc=dc, b=b, n_=n_: e.scalar_tensor_tensor(
                                    out=Sf[:, dc, :], in0=Sf[:, dc, :], scalar=dec[:, dc, n_:n_ + 1], in1=ps[b][:, :],
                                    op0=ALU.mult, op1=ALU.add), ["Sf", "dec", PS(b)], ["Sf"])
                            if n_ < 7:
                                op("act", lambda e: e.activation(out=Sb[:], in_=Sf[:], func=AF.Copy), ["Sf"], ["Sb"])
                        if main:
                            dma("sp", gla_p[h].rearrange("(c p) v -> p c v", p=128), Sf[:], reads=["Sf"])
                            sample_gla(h)
                        else:
                            dma("sp", s_state[h].rearrange("(c p) v -> p c v", p=128), Sf[:], reads=["Sf"],
                                writes=["s_state%d" % h])

                def finish_o(h, ti, rows, t0):
                    op("dve", lambda e: e.bn_stats(out=st6[0:rows, 0:6], in_=ps[5][0:rows, :]), [PS(5)], ["st6"])
                    op("dve", lambda e: e.bn_aggr(out=st6[0:rows, 6:8], in_=st6[0:rows, 0:6]), ["st6"], ["st6"])
                    op("act", lambda e: e.activation(out=st6[0:rows, 0:1], in_=st6[0:rows, 7:8], func=AF.Sqrt, bias=EPS),
                       ["st6"], ["st6"])
                    op("dve", lambda e: e.reciprocal(out=st6[0:rows, 1:2], in_=st6[0:rows, 0:1]), ["st6"], ["st6"])
                    op("dve", lambda e: e.tensor_scalar(out=of[0:rows, :], in0=ps[5][0:rows, :], scalar1=st6[0:rows, 6:7],
                                                        scalar2=st6[0:rows, 1:2], op0=ALU.subtract, op1=ALU.mult),
                       [PS(5), "st6"], ["of"])
                    op("dve", lambda e: e.tensor_tensor(out=og[0:rows, :], in0=of[0:rows, :], in1=gr[0:rows, ti, :], op=ALU.mult),
                       ["of", "U1"], ["hb_og"])
                    pv = ps[4][:].bitcast(BF16).rearrange("p (a b) -> p a b", a=8)

                    def tr(e):
                        for j in range(4):
                            ins = e.transpose(out=pv[:, j, 0:rows], in_=og[0:rows, j * 128:(j + 1) * 128],
                                              identity=ident_b[0:rows, 0:rows])
                        return ins
                    op("pe", tr, ["hb_og", "ident_b"], [PS(4)])
                    op("act", lambda e: e.activation(out=ogT[:, :, 0:rows], in_=pv[:, 0:4, 0:rows], func=AF.Copy), [PS(4)], ["hb_ogT"])
                    dma("sp", s_ogT[h * 512:(h + 1) * 512, t0:t0 + rows].rearrange("(c p) t -> p c t", p=128),
                        ogT[:, :, 0:rows], reads=["hb_ogT"], writes=["s_ogT"])

                def sample_gla(h):
                    def mm_s(e):
                        for dc in range(2):
                            ins = e.matmul(ps[4][0:64, 0:64], lhsT=keT[:, dc, NP:NT], rhs=qeT[:, dc, NP:NT],
                                           start=(dc == 0), stop=(dc == 1))
                        return ins
                    op("pe", mm_s, ["keT", "qeT"], [PS(4)])
                    op("dve", lambda e: e.tensor_tensor(out=sT[0:64, 0:64], in0=ps[4][0:64, 0:64], in1=cst[0:64, C_MS:C_MS + 64],
                                                        op=ALU.mult), [PS(4), "cst"], ["hb_sT"])
                    for sq in range(NSEQ):
                        op("dve", lambda e, sq=sq: e.tensor_tensor(
                            out=QMs[:], in0=qeT[:, :, NP:NT], in1=bm_b[:, sq, :].unsqueeze(1).to_broadcast([128, 2, 64]),
                            op=ALU.mult), ["qeT", "bm_b"], ["QMs"])
                        op("dve", lambda e, sq=sq: e.tensor_scalar(out=kdm[:], in0=kd[0:64, 8, :],
                                                                    scalar1=cst[0:64, C_RM + sq:C_RM + sq + 1], scalar2=None,
                                                                    op0=ALU.mult), ["kd", "cst"], ["kdm"])
                        for dc in range(2):
                            bi = dc
                            src = st_gla[sq, h, dc * 128:(dc + 1) * 128, :]
                            dma("sp", s0f[bi][:], src, writes=["s0f%d" % bi])
                            dma("pool", s0b[bi][:], src, writes=["s0b%d" % bi])

                            def mm_o(e, sq=sq, dc=dc, bi=bi):
                                if sq == 0 and dc == 0:
                                    e.matmul(ps[5][0:64, :], lhsT=sT[0:64, 0:64], rhs=vh[0:64, 8, :], start=True, stop=False)
                                return e.matmul(ps[5][0:64, :], lhsT=QMs[:, dc, :], rhs=s0b[bi][:, :],
                                                start=False, stop=(sq == NSEQ - 1 and dc == 1))
                            op("pe", mm_o, ["hb_sT", "U1", "QMs", "s0b%d" % bi], [PS(5)])
                            b = 6 + dc
                            op("pe", lambda e, dc=dc, b=b: e.matmul(ps[b][:, :], lhsT=kdm[:, dc * 128:(dc + 1) * 128],
                                                                     rhs=vh[0:64, 8, :], start=True, stop=True),
                               ["kdm", "U1"], [PS(b)])
                            op("dve", lambda e, dc=dc, b=b, sq=sq, bi=bi: e.scalar_tensor_tensor(
                                out=sout[bi][:, :], in0=s0f[bi][:, :], scalar=dec[:, dc, 8 + sq:9 + sq], in1=ps[b][:, :],
                                op0=ALU.mult, op1=ALU.add), ["s0f%d" % bi, "dec", PS(b)], ["sout%d" % bi])
                            dma("sp", gla_s[sq, h, dc * 128:(dc + 1) * 128, :], sout[bi][:], reads=["sout%d" % bi])
                    finish_o(h, 8, 64, NP)

                stage_end(0)
                gla_pass(False)
                stage_end(1)
                gla_pass(True)
                stage_end(2)

            with contextlib.ExitStack() as mg_es:
                ogTa = sb("ogTa", [128, 16, NT], BF16, mg_es)
                sga = sb("sga", [128, 512], F32, mg_es)
                mo = sb("mo", [128, 2, NT], BF16, mg_es)
                dma("sp", ogTa[:], s_ogT.rearrange("(c p) t -> p c t", p=128), reads=["s_ogT"], writes=["ogTa"])
                sgs = sb("sgs", [128, 2, NT], BF16, mg_es)
                for m in range(16):
                    wga, kga = wnext()

                    def ev_ga(mm_, gi, t0, n, pap, pkey):
                        op("act", lambda e: e.activation(out=sgs[:, mm_, t0:t0 + n], in_=pap, func=AF.Sigmoid), [pkey], ["sgs"])
                    gemm_ws(wga, kga, 32, 256, hT, "hT", NGRP, ev_ga, [0, 1, 2, 3])
                    wba, kba = wnext()

                    def ev_ba(mm_, gi, t0, n, pap, pkey):
                        op("dve", lambda e: e.tensor_tensor(out=mo[:, mm_, t0:t0 + n], in0=pap, in1=sgs[:, mm_, t0:t0 + n], op=ALU.mult),
                           [pkey, "sgs"], ["mo"])
                    gemm_ws(wba, kba, 16, 256, ogTa, "ogTa", NGRP, ev_ba, [4, 5, 6, 7])
                    dma("sp", s_mT[m * 256:(m + 1) * 256, :].rearrange("(c p) t -> p c t", p=128), mo[:], reads=["mo"],
                        writes=["s_mT"])

            stage_end(3)
            cv_es = contextlib.ExitStack()
            with cv_es:
                cT = sb("cT", [128, 16, NT], BF16, cv_es)
                cpar = sb("cpar", [128, 16, 34], F32, cv_es)
                extP = sb("extP", [128, 30 + NP], F32, cv_es)
                extS = sb("extS", [128, 16, 34], F32, cv_es)
                sig = xt[:, 3136:3648]
                acc = xt[:, 2048:2048 + NT]
                stc = sb("stc", [120, 4, 128], F32, cv_es)
                cvp_tm = sb("cvp_tm", [30, 128], F32, cv_es)
                cvs_tm = sb("cvs_tm", [64, 128], F32, cv_es)
                cpl = xt[0:34, 0:2048]
                dma("sp", cpl, convp_in, writes=[*XTK])
                for j in range(16):
                    b = 4 + j % 2
                    op("pe", lambda e, j=j, b=b: e.transpose(out=ps[b][:, 0:34], in_=xt[0:34, j * 128:(j + 1) * 128],
                                                             identity=ident_f[0:34, 0:34]), [*XTK, "cst"], [PS(b)])
                    op("dve", lambda e, j=j, b=b: e.tensor_copy(out=cpar[:, j, :], in_=ps[b][:, 0:34]), [PS(b)], ["cpar"])
                dma("sp", conv_s[:, 0:26, :], st_conv[:, 4:30, :])
                ugroups = [(0, 512), (512, 512), (1024, 64)]
                uscope = contextlib.ExitStack()
                u1s = sb("u1s", [128, 2, 32 + NT], F32, uscope)
                hbf32 = hb[:].bitcast(F32)
                extPs = [extP[:, :], hbf32[:, 0:30 + NP]]
                extSs = [extS[:, :, :], hbf32[:, 1056:1056 + 544].rearrange("p (s r) -> p s r", r=34)]
                extPk = ["extP", "extP1"]
                extSk = ["extS", "extS1"]
                sigs = [xt[:, 3136:3648], xt[:, 1088:1600]]
                sigk = ["sig0", "sig1"]
                accs = [xt[:, 2048:2048 + NT], xt[:, 0:NT]]
                acck = ["acc0", "acc1"]
                CBAR = [*XTK, *HBK, "extP1", "extS1", "sig0", "sig1", "acc0", "acc1"]
                op("pool", lambda e: e.memset(sm[:, 63:64], 0.0), [], CBAR)
                pending_tail = [None]

                def make_tail(j, par):
                    extPc, extSc, sigc = extPs[par], extSs[par], sigs[par]

                    def tail():
                        op("pe", lambda e: e.transpose(out=ps[7][0:30, 0:128], in_=extPc[:, NP:NP + 30], identity=ident_f),
                           [extPk[par], "cst"], [PS(7)])
                        op("act", lambda e: e.activation(out=cvp_tm[:, :], in_=ps[7][0:30, 0:128], func=AF.Copy), [PS(7)], ["cvp_tm"])
                        dma("sp", conv_p[:, j * 128:(j + 1) * 128], cvp_tm[:, :], reads=["cvp_tm"])
                        op("pool", lambda e: e.tensor_copy(out=sigc[:, 0:64].rearrange("p (s r) -> p s r", r=4), in_=extSc[:, :, 30:34]),
                           [extSk[par]], [sigk[par]])
                        op("pe", lambda e: e.transpose(out=ps[7][0:64, 128:256], in_=sigc[:, 0:64], identity=ident_f),
                           [sigk[par], "cst"], [PS(7)])
                        op("act", lambda e: e.activation(out=cvs_tm[:, :], in_=ps[7][0:64, 128:256], func=AF.Copy), [PS(7)], ["cvs_tm"])
                        for sq in range(NSEQ):
                            dma("sp", conv_s[sq, 26:30, j * 128:(j + 1) * 128], cvs_tm[4 * sq:4 * sq + 4, :], reads=["cvs_tm"])
                    return tail

                for m in range(8):
                    wu1, k1 = wnext()
                    for jj in range(2):
                        for gi, (t0, n) in enumerate([(-32, 32)] + ugroups):
                            src = hT_halo if t0 < 0 else hT
                            skey = "hT_halo" if t0 < 0 else "hT"
                            a0 = 0 if t0 < 0 else t0
                            b1 = (jj * 4 + gi) % 4

                            def mmu1(e, b=b1, a0=a0, n=n, src=src, jj=jj):
                                for k in range(32):
                                    ins = e.matmul(ps[b][:, 0:n], lhsT=wu1[:, k, jj * 128:(jj + 1) * 128], rhs=src[:, k, a0:a0 + n],
                                                   start=(k == 0), stop=(k == 31))
                                return ins
                            op("pe", mmu1, [k1, skey], [PS(b1)])
                            op("act", lambda e, b1=b1, jj=jj, t0=t0, n=n: e.activation(out=u1s[:, jj, 32 + t0:32 + t0 + n], in_=ps[b1][:, 0:n],
                                                                                        func=AF.Copy), [PS(b1)], ["u1s"])
                    wu2, k2 = wnext()
                    for jj in range(2):
                        j = 2 * m + jj
                        par = jj
                        extPc, extSc, sigc, accc = extPs[par], extSs[par], sigs[par], accs[par]
                        kP, kS, kG, kA = extPk[par], extSk[par], sigk[par], acck[par]
                        for g4 in range(4):
                            dma("sp", stc[:, g4, :], st_conv[4 * g4:4 * g4 + 4, :, j * 128:(j + 1) * 128].rearrange("s r c -> (s r) c"),
                                writes=["stc"])
                        for g4 in range(4):
                            op("pe", lambda e, g4=g4: e.transpose(out=ps[6][:, g4 * 120:(g4 + 1) * 120], in_=stc[:, g4, :],
                                                                  identity=ident_f[0:120, 0:120]), ["stc", "cst"], [PS(6)])
                        op("dve", lambda e: e.tensor_copy(out=extSc[:, :, 0:30],
                                                          in_=ps[6][:, 0:480].rearrange("p (s r) -> p s r", r=30)),
                           [PS(6)], [kS])
                        for gi, (t0, n) in enumerate([(-32, 32)] + ugroups):
                            src = hT_halo if t0 < 0 else hT
                            skey = "hT_halo" if t0 < 0 else "hT"
                            a0 = 0 if t0 < 0 else t0
                            b2 = gi % 4

                            def mmu(e, wv, b, a0=a0, n=n, src=src, jj=jj):
                                for k in range(32):
                                    ins = e.matmul(ps[b][:, 0:n], lhsT=wv[:, k, jj * 128:(jj + 1) * 128], rhs=src[:, k, a0:a0 + n],
                                                   start=(k == 0), stop=(k == 31))
                                return ins
                            op("pe", lambda e, b2=b2, f=mmu: f(e, wu2, b2), [k2, skey], [PS(b2)])
                            op("act", lambda e, b2=b2, n=n: e.activation(out=sigc[:, 0:n], in_=ps[b2][:, 0:n], func=AF.Sigmoid),
                               [PS(b2)], [kG])
                            if t0 < 0:
                                dst = extPc[:, 0:30]
                                i0 = u1s[:, jj, 2:32]
                                i1 = sigc[:, 2:32]
                                wk_ = kP
                            elif t0 < NP:
                                dst = extPc[:, 30 + t0:30 + t0 + n]
                                i0 = u1s[:, jj, 32 + t0:32 + t0 + n]
                                i1 = sigc[:, 0:n]
                                wk_ = kP
                            else:
                                dst = extSc[:, :, 30:34]
                                i0 = u1s[:, jj, 32 + NP:32 + NT].rearrange("p (s r) -> p s r", r=4)
                                i1 = sigc[:, 0:64].rearrange("p (s r) -> p s r", r=4)
                                wk_ = kS
                            op("dve", lambda e, dst=dst, i0=i0, i1=i1: e.tensor_tensor(out=dst, in0=i0, in1=i1, op=ALU.mult),
                               ["u1s", kG], [wk_])
                        if pending_tail[0] is not None:
                            pending_tail[0]()
                        pending_tail[0] = make_tail(j, par)
                        accS = accc[:, NP:NT].rearrange("p (s r) -> p s r", r=4)
                        op("dve", lambda e, j=j: e.tensor_scalar(out=accc[:, 0:NP], in0=extPc[:, 0:NP], scalar1=cpar[:, j, 0:1],
                                                                  scalar2=cpar[:, j, 31:32], op0=ALU.mult, op1=ALU.add),
                           [kP, "cpar"], [kA])
                        op("dve", lambda e, j=j: e.tensor_scalar(out=accS, in0=extSc[:, :, 0:4], scalar1=cpar[:, j, 0:1],
                                                                  scalar2=cpar[:, j, 31:32], op0=ALU.mult, op1=ALU.add),
                           [kS, "cpar"], [kA])
                        for tp in range(1, 31):
                            op("dve", lambda e, j=j, tp=tp: e.scalar_tensor_tensor(
                                out=accc[:, 0:NP], in0=extPc[:, tp:tp + NP], scalar=cpar[:, j, tp:tp + 1], in1=accc[:, 0:NP],
                                op0=ALU.mult, op1=ALU.add), [kP, "cpar", kA], [kA])
                            op("dve", lambda e, j=j, tp=tp: e.scalar_tensor_tensor(
                                out=accS, in0=extSc[:, :, tp:tp + 4], scalar=cpar[:, j, tp:tp + 1], in1=accS,
                                op0=ALU.mult, op1=ALU.add), [kS, "cpar", kA], [kA])
                        op("act", lambda e, j=j: e.activation(out=cT[:, j, :], in_=accc[:, :], func=AF.Copy), [kA], ["cT"])
                pending_tail[0]()
                op("pool", lambda e: e.memset(sm[:, 63:64], 0.0), [], CBAR)
                uscope.close()
                with contextlib.ExitStack() as ln_es:
                    sq_t = sb("sq_t", [128, 512], BF16, ln_es)
                    mu = sb("mu", [128, 512], F32, ln_es)
                    rs = sb("rs", [128, 512], F32, ln_es)
                    tmpf = sb("tmpf", [128, 512], F32, ln_es)
                    for gi, (t0, n) in enumerate(NGRP):
                        def mm1(e, t0=t0, n=n):
                            for j in range(16):
                                ins = e.matmul(ps[0][:, 0:n], lhsT=ones_b[:], rhs=cT[:, j, t0:t0 + n], start=(j == 0), stop=(j == 15))
                            return ins
                        op("pe", mm1, ["ones_b", "cT"], [PS(0)])
                        for j in range(16):
                            op("dve", lambda e, j=j, t0=t0, n=n: e.tensor_tensor(out=sq_t[:, 0:n], in0=cT[:, j, t0:t0 + n],
                                                                                  in1=cT[:, j, t0:t0 + n], op=ALU.mult),
                               ["cT"], ["sq_t"])
                            op("pe", lambda e, j=j, n=n: e.matmul(ps[1][:, 0:n], lhsT=ones_b[:], rhs=sq_t[:, 0:n],
                                                                  start=(j == 0), stop=(j == 15)), ["ones_b", "sq_t"], [PS(1)])
                        op("act", lambda e, n=n: e.activation(out=mu[:, 0:n], in_=ps[0][:, 0:n], func=AF.Copy, scale=1.0 / 2048),
                           [PS(0)], ["mu"])
                        op("dve", lambda e, n=n: e.tensor_tensor(out=tmpf[:, 0:n], in0=mu[:, 0:n], in1=mu[:, 0:n], op=ALU.mult),
                           ["mu"], ["tmpf"])
                        op("dve", lambda e, n=n: e.scalar_tensor_tensor(out=rs[:, 0:n], in0=ps[1][:, 0:n], scalar=1.0 / 2048,
                                                                         in1=tmpf[:, 0:n], op0=ALU.mult, op1=ALU.subtract),
                           [PS(1), "tmpf"], ["rs"])
                        op("act", lambda e, n=n: e.activation(out=rs[:, 0:n], in_=rs[:, 0:n], func=AF.Sqrt, bias=EPS), ["rs"], ["rs"])
                        op("dve", lambda e, n=n: e.reciprocal(out=rs[:, 0:n], in_=rs[:, 0:n]), ["rs"], ["rs"])
                        for j in range(16):
                            op("dve", lambda e, j=j, t0=t0, n=n: e.tensor_tensor(out=tmpf[:, 0:n], in0=cT[:, j, t0:t0 + n],
                                                                                  in1=mu[:, 0:n], op=ALU.subtract),
                               ["cT", "mu"], ["tmpf"])
                            op("dve", lambda e, n=n: e.tensor_tensor(out=tmpf[:, 0:n], in0=tmpf[:, 0:n], in1=rs[:, 0:n], op=ALU.mult),
                               ["tmpf", "rs"], ["tmpf"])
                            op("act", lambda e, j=j, t0=t0, n=n: e.activation(out=cT[:, j, t0:t0 + n], in_=tmpf[:, 0:n], func=AF.Silu,
                                                                               scale=cpar[:, j, 32:33], bias=cpar[:, j, 33:34]),
                               ["tmpf", "cpar"], ["cT"])
                stage_end(4)
                with contextlib.ExitStack() as mg_es:
                    xtb2 = xt[:].bitcast(BF16)
                    mab = xtb2[:, 0:2 * NT].rearrange("p (c t) -> p c t", c=2)
                    mo = xtb2[:, 2 * NT:4 * NT].rearrange("p (c t) -> p c t", c=2)
                    sgs2 = xt[:, 2304:2304 + NT].bitcast(BF16).rearrange("p (c t) -> p c t", c=2)
                    for m in range(16):
                        wgb, kgb = wnext()
                        dma("sp", mab[:], s_mT[m * 256:(m + 1) * 256, :].rearrange("(c p) t -> p c t", p=128), reads=["s_mT"],
                            writes=["xt_E1"])

                        def ev_gb(mm_, gi, t0, n, pap, pkey):
                            op("act", lambda e: e.activation(out=sgs2[:, mm_, t0:t0 + n], in_=pap, func=AF.Sigmoid), [pkey], ["xt_E3"])
                        gemm_ws(wgb, kgb, 32, 256, hT, "hT", NGRP, ev_gb, [0, 1, 2, 3])
                        wbb, kbb = wnext()

                        def ev_bb(mm_, gi, t0, n, pap, pkey):
                            op("dve", lambda e: e.tensor_tensor(out=sgs2[:, mm_, t0:t0 + n], in0=pap, in1=sgs2[:, mm_, t0:t0 + n], op=ALU.mult),
                               [pkey, "xt_E3"], ["xt_E3"])
                            op("dve", lambda e: e.tensor_tensor(out=mo[:, mm_, t0:t0 + n], in0=sgs2[:, mm_, t0:t0 + n], in1=mab[:, mm_, t0:t0 + n],
                                                                op=ALU.add), ["xt_E3", "xt_E1"], ["xt_E2"])
                        gemm_ws(wbb, kbb, 16, 256, cT, "cT", NGRP, ev_bb, [4, 5, 6, 7])
                        dma("sp", s_mT[m * 256:(m + 1) * 256, :].rearrange("(c p) t -> p c t", p=128), mo[:], reads=["xt_E2", "xt_E1"],
                            writes=["s_mT"])

        stage_end(5)
        late = contextlib.ExitStack()
        with late:
            xres = [sb("xres%d" % i, [128, 512], F32, late) for i in range(3)]
            dma("sp", hT[:], s_mT.rearrange("(c p) t -> p c t", p=128), reads=["s_mT"], writes=["hT"])
            rcount = [0]

            def resid_gemm(wview, wkey, actT, act_key, src_fn, src_key, dst, c0, ncols, dst_key):
                def ev(ti, t0, rows, pap, pkey):
                    bi = rcount[0] % 3
                    rcount[0] += 1
                    dma("sp", xres[bi][0:rows, 0:ncols], src_fn(t0, rows, c0, ncols), reads=[src_key], writes=["xres%d" % bi])
                    op("dve", lambda e: e.tensor_tensor(out=xres[bi][0:rows, 0:ncols], in0=pap, in1=xres[bi][0:rows, 0:ncols], op=ALU.add),
                       [pkey, "xres%d" % bi], ["xres%d" % bi])
                    dma("sp", dst[t0:t0 + rows, c0:c0 + ncols], xres[bi][0:rows, 0:ncols], reads=["xres%d" % bi],
                        writes=[dst_key])
                gemm_as([(wview, 0)], [wkey], actT, act_key, TILES, ncols, ev, [0, 1, 2, 3])

            def xsrc(t0, rows, c0, ncols):
                if t0 < NP:
                    return x_main[t0:t0 + rows, c0:c0 + ncols]
                return x_s[:, c0:c0 + ncols]
            for cb in range(16):
                w0, k0 = wnext()
                resid_gemm(w0, k0, hT, "hT", xsrc, "x_in", s_x1, cb * 256, 256, "s_x1")

            stage_end(6)
            ca = contextlib.ExitStack()
            with ca:
                mvb = sb("mvb", [128, 2, 1024], BF16, ca)
                mkT = sb("mkT", [128, 8, 256], BF16, ca)
                q2T = sb("q2T", [128, 8, NT], BF16, ca)
                oT = q2T
                kvf = sb("kvf", [128, 512], F32, ca)
                mscope = contextlib.ExitStack()
                mT = sb("mT", [128, 32, 256], BF16, mscope)
                load_gain(norm_mem_g)
                for i in range(2):
                    norm_tile(mem[i * 128:(i + 1) * 128, :], 128, i * 128, mT, "mT")
                stage_end(6.05)
                for which, dst_o in ((0, mk_o), (1, mv_o)):
                    for cb in range(4):
                        w0, k0 = wnext()

                        def ev_kv(ti, t0, rows, pap, pkey, which=which, cb=cb, dst_o=dst_o):
                            kb = "kvf%d" % (ti % 2)
                            kv_ = kvf[:, (ti % 2) * 256:(ti % 2) * 256 + 256]
                            op("act", lambda e: e.activation(out=kv_, in_=pap, func=AF.Copy), [pkey], [kb])
                            if which == 1:
                                op("dve", lambda e: e.tensor_copy(out=mvb[:, ti, cb * 256:(cb + 1) * 256], in_=kv_), [kb], ["mvb"])
                            dma("sp", dst_o[t0:t0 + 128, cb * 256:(cb + 1) * 256], kv_, reads=[kb])
                        gemm_as([(w0, 0)], [k0], mT, "mT", [(0, 128), (128, 128)], 256, ev_kv, [0, 1, 2, 3])
                stage_end(6.1)
                for m in range(4):
                    wv, wk = wnext()

                    def ev_mk(mm_, gi, t0, n, pap, pkey, m=m):
                        op("act", lambda e: e.activation(out=mkT[:, 2 * m + mm_, :], in_=pap, func=AF.Copy), [pkey], ["mkT"])
                    gemm_ws(wv, wk, 32, 256, mT, "mT", [(0, 256)], ev_mk, [0, 1, 2, 3])
                stage_end(6.2)
                mscope.close()
                load_gain(norm_ca_g)
                for ti, (t0, rows) in enumerate(TILES):
                    norm_tile(s_x1[t0:t0 + rows, :], rows, t0, hT, "hT", src_reads=["s_x1"])
                for m in range(4):
                    wv, wk = wnext()

                    def ev_q2(mm_, gi, t0, n, pap, pkey, m=m):
                        op("act", lambda e: e.activation(out=q2T[:, 2 * m + mm_, t0:t0 + n], in_=pap, func=AF.Copy), [pkey], ["q2T"])
                    gemm_ws(wv, wk, 32, 256, hT, "hT", NGRP, ev_q2, [0, 1, 2, 3])

                stage_end(6.4)
                at = contextlib.ExitStack()
                with at:
                    pex = sb("pex", [128, 4, 256], F32, at)
                    pbf = sb("pbf", [128, 4, 256], BF16, at)
                    pT = hb[:].rearrange("p (s h t) -> p s h t", s=2, h=4)
                    mx = sb("mx", [128, 16], F32, at)
                    xtb3 = xt[:].bitcast(BF16)
                    kc = [xtb3[:, i * 2048:(i + 1) * 2048].rearrange("p (c d) -> p c d", c=2) for i in range(2)]
                    vc = [xtb3[:, 4096 + i * 2048:4096 + (i + 1) * 2048].rearrange("p (c d) -> p c d", c=2) for i in range(2)]
                    kTs = sb("kTs", [128, 8, 256], BF16, at)
                    Q2M = sb("Q2M", [128, 8, 64], BF16, at)
                    pTm = sb("pTm", [128, 2, 4, 64], BF16, at)
                    osb = sb("osb", [64, 1024], BF16, at)

                    def softmax_rows(rows, banks):
                        for h in range(4):
                            sc = ps[banks[h // 2]][0:rows, (h % 2) * 256:(h % 2) * 256 + 256]
                            op("dve", lambda e, h=h, sc=sc: e.reduce_max(out=mx[0:rows, h:h + 1], in_=sc, axis=AX.X),
                               [PS(banks[h // 2])], ["mx"])
                        op("dve", lambda e: e.tensor_scalar(out=mx[0:rows, 4:8], in0=mx[0:rows, 0:4], scalar1=-1.0 / 16.0,
                                                            scalar2=None, op0=ALU.mult), ["mx"], ["mx"])
                        for h in range(4):
                            sc = ps[banks[h // 2]][0:rows, (h % 2) * 256:(h % 2) * 256 + 256]
                            op("act", lambda e, h=h, sc=sc: e.activation(out=pex[0:rows, h, :], in_=sc, func=AF.Exp, scale=1.0 / 16.0,
                                                                          bias=mx[0:rows, 4 + h:5 + h], accum_out=mx[0:rows, 8 + h:9 + h]),
                               [PS(banks[h // 2]), "mx"], ["pex", "mx"])
                        op("dve", lambda e: e.reciprocal(out=mx[0:rows, 12:16], in_=mx[0:rows, 8:12]), ["mx"], ["mx"])
                        for h in range(4):
                            op("dve", lambda e, h=h: e.tensor_scalar(out=pbf[0:rows, h, :], in0=pex[0:rows, h, :],
                                                                      scalar1=mx[0:rows, 12 + h:13 + h], scalar2=None, op0=ALU.mult),
                               ["pex", "mx"], ["pbf"])

                    for gq in range(2):
                        for tl in range(4):
                            t0 = gq * 512 + tl * 128

                            def mm_sc(e, t0=t0):
                                for h in range(4):
                                    for dc in range(2):
                                        ins = e.matmul(ps[h // 2][:, (h % 2) * 256:(h % 2) * 256 + 256], lhsT=q2T[:, 2 * h + dc, t0:t0 + 128],
                                                       rhs=mkT[:, 2 * h + dc, :], start=(dc == 0), stop=(dc == 1))
                                return ins
                            op("pe", mm_sc, ["q2T", "mkT"], [PS(0), PS(1)])
                            softmax_rows(128, [0, 1])
                            pv = ps[2][:].bitcast(BF16).rearrange("p (a b) -> p a b", a=8)

                            def trp(e):
                                for h in range(4):
                                    for sc_ in range(2):
                                        ins = e.transpose(out=pv[:, sc_ * 4 + h, :], in_=pbf[:, h, sc_ * 128:(sc_ + 1) * 128], identity=ident_b[:])
                                return ins
                            op("pe", trp, ["pbf", "ident_b"], [PS(2)])
                            op("act", lambda e, tl=tl: e.activation(out=pT[:, :, :, tl * 128:(tl + 1) * 128],
                                                                     in_=pv.rearrange("p (s h) t -> p s h t", s=2), func=AF.Copy),
                               [PS(2)], ["hb_kdT"])
                        for h in range(4):
                            for dc in range(2):
                                b = 4 + (h * 2 + dc) % 4

                                def mm_ov(e, h=h, dc=dc, b=b):
                                    for sc_ in range(2):
                                        ins = e.matmul(ps[b][:, :], lhsT=mvb[:, sc_, h * 256 + dc * 128:h * 256 + dc * 128 + 128],
                                                       rhs=pT[:, sc_, h, :], start=(sc_ == 0), stop=(sc_ == 1))
                                    return ins
                                op("pe", mm_ov, ["mvb", "hb_kdT"], [PS(b)])
                                op("act", lambda e, h=h, dc=dc, b=b, gq=gq: e.activation(
                                    out=oT[:, 2 * h + dc, gq * 512:(gq + 1) * 512], in_=ps[b][:, :], func=AF.Copy), [PS(b)], ["q2T"])
                    stage_end(6.6)
                    for c8 in range(8):
                        op("dve", lambda e, c8=c8: e.tensor_copy(out=Q2M[:, c8, :], in_=q2T[:, c8, NP:NT]), ["q2T"], ["Q2M"])
                    bmv = bm_b[:]
                    for sq in range(NSEQ):
                        bi = sq % 2
                        dma("pool", kc[bi][:], ck[sq].rearrange("(c p) d -> p c d", p=128), writes=[("xt_E1", "xt_E2")[bi]])
                        for half in range(2):
                            pv = ps[4 + half][:].bitcast(BF16).rearrange("p (a b) -> p a b", a=8)

                            def trk(e, half=half, pv=pv, bi=bi):
                                for c4 in range(4):
                                    c8 = half * 4 + c4
                                    for sc_ in range(2):
                                        ins = e.transpose(out=pv[:, c4 * 2 + sc_, :], in_=kc[bi][:, sc_, c8 * 128:(c8 + 1) * 128],
                                                          identity=ident_b[:])
                                return ins
                            op("pe", trk, [("xt_E1", "xt_E2")[bi], "ident_b"], [PS(4 + half)])
                            op("act", lambda e, half=half, pv=pv: e.activation(
                                out=kTs[:, half * 4:(half + 1) * 4, :].rearrange("p c (s t) -> p c s t", s=2),
                                in_=pv.rearrange("p (c s) t -> p c s t", s=2), func=AF.Copy), [PS(4 + half)], ["kTs"])
                        qm = sb if False else None

                        def mm_ss(e, sq=sq):
                            for h in range(4):
                                for dc in range(2):
                                    ins = e.matmul(ps[h][0:64, 0:256], lhsT=QMs[:, 2 * h + dc, :], rhs=kTs[:, 2 * h + dc, :],
                                                   start=(sq == 0 and dc == 0), stop=(sq == NSEQ - 1 and dc == 1))
                            return ins
                        QMs = sb("QMs%d" % sq, [128, 8, 64], BF16, at) if sq < 2 else QMs_l[sq % 2]
                        if sq < 2:
                            if sq == 0:
                                QMs_l = [QMs, None]
                            else:
                                QMs_l[1] = QMs
                        op("dve", lambda e, sq=sq, QMs=QMs: e.tensor_tensor(
                            out=QMs[:], in0=Q2M[:], in1=bmv[:, sq, :].unsqueeze(1).to_broadcast([128, 8, 64]), op=ALU.mult),
                           ["Q2M", "bm_b"], ["QMs%d" % (sq % 2)])
                        op("pe", mm_ss, ["QMs%d" % (sq % 2), "kTs"], [PS(0), PS(1), PS(2), PS(3)])
                    for h in range(4):
                        op("dve", lambda e, h=h: e.reduce_max(out=mx[0:64, h:h + 1], in_=ps[h][0:64, 0:256], axis=AX.X), [PS(h)], ["mx"])
                    op("dve", lambda e: e.tensor_scalar(out=mx[0:64, 4:8], in0=mx[0:64, 0:4], scalar1=-1.0 / 16.0, scalar2=None,
                                                        op0=ALU.mult), ["mx"], ["mx"])
                    for h in range(4):
                        op("act", lambda e, h=h: e.activation(out=pex[0:64, h, :], in_=ps[h][0:64, 0:256], func=AF.Exp, scale=1.0 / 16.0,
                                                               bias=mx[0:64, 4 + h:5 + h], accum_out=mx[0:64, 8 + h:9 + h]),
                           [PS(h), "mx"], ["pex", "mx"])
                    op("dve", lambda e: e.reciprocal(out=mx[0:64, 12:16], in_=mx[0:64, 8:12]), ["mx"], ["mx"])
                    for h in range(4):
                        op("dve", lambda e, h=h: e.tensor_scalar(out=pbf[0:64, h, :], in0=pex[0:64, h, :], scalar1=mx[0:64, 12 + h:13 + h],
                                                                  scalar2=None, op0=ALU.mult), ["pex", "mx"], ["pbf"])
                    pv = ps[4][:].bitcast(BF16).rearrange("p (a b) -> p a b", a=8)

                    def trps(e):
                        for h in range(4):
                            for sc_ in range(2):
                                ins = e.transpose(out=pv[:, sc_ * 4 + h, 0:64], in_=pbf[0:64, h, sc_ * 128:(sc_ + 1) * 128],
                                                  identity=ident_b[0:64, 0:64])
                        return ins
                    op("pe", trps, ["pbf", "ident_b"], [PS(4)])
                    pTs = sb("pTs", [128, 8, 64], BF16, at)
                    op("act", lambda e: e.activation(out=pTs[:], in_=pv[:, :, 0:64], func=AF.Copy), [PS(4)], ["pTs"])
                    for sq in range(NSEQ):
                        bi = sq % 2
                        dma("pool", vc[bi][:], cv[sq].rearrange("(c p) d -> p c d", p=128), writes=[("xt_E3", "xt")[bi]])
                        op("dve", lambda e, sq=sq: e.tensor_tensor(
                            out=pTm[:].rearrange("p s h t -> p (s h) t"), in0=pTs[:],
                            in1=bmv[:, sq, :].unsqueeze(1).to_broadcast([128, 8, 64]), op=ALU.mult), ["pTs", "bm_b"], ["pTm"])

                        def mm_os(e, sq=sq, bi=bi):
                            for h in range(4):
                                for sc_ in range(2):
                                    ins = e.matmul(ps[h][0:64, 0:256], lhsT=pTm[:, sc_, h, :], rhs=vc[bi][:, sc_, h * 256:(h + 1) * 256],
                                                   start=(sq == 0 and sc_ == 0), stop=(sq == NSEQ - 1 and sc_ == 1))
                            return ins
                        op("pe", mm_os, ["pTm", ("xt_E3", "xt")[bi]], [PS(0), PS(1), PS(2), PS(3)])
                    for h in range(4):
                        op("act", lambda e, h=h: e.activation(out=osb[:, h * 256:(h + 1) * 256], in_=ps[h][0:64, 0:256], func=AF.Copy),
                           [PS(h)], ["osb"])
                    pv = ps[5][:].bitcast(BF16).rearrange("p (a b) -> p a b", a=8)

                    def tro(e):
                        for c8 in range(8):
                            ins = e.transpose(out=pv[:, c8, 0:64], in_=osb[:, c8 * 128:(c8 + 1) * 128], identity=ident_b[0:64, 0:64])
                        return ins
                    op("pe", tro, ["osb", "ident_b"], [PS(5)])
                    op("act", lambda e: e.activation(out=oT[:, :, NP:NT], in_=pv[:, :, 0:64], func=AF.Copy), [PS(5)], ["q2T"])

                stage_end(6.8)
                def x1src(t0, rows, c0, ncols):
                    return s_x1[t0:t0 + rows, c0:c0 + ncols]
                for cb in range(4):
                    w0, k0 = wnext()
                    for sub in range(2):
                        resid_gemm(w0[:, :, sub * 512:(sub + 1) * 512], k0, oT, "q2T", x1src, "s_x1", s_x2, cb * 1024 + sub * 512, 512, "s_x2")

            stage_end(7)
            late.close()
            hscope.close()
            moe = contextlib.ExitStack()
            with moe:
                wsl.append(sb("wsl2", [128, 8192], BF16, moe))
                wr = sb("wr", [128, 32, 36], F32, moe)
                brt = sb("brt", [128, 36], F32, moe)
                xgTbuf = sb("xgTbuf", [128, D], F32, moe)
                hTf = xgTbuf[:].rearrange("p (k t) -> p k t", k=32)
                xgT = xgTbuf[:].bitcast(BF16).rearrange("p (k c) -> p k c", k=32)
                hbf = sb("hbf", [128, D], F32, moe)
                gbcf = sb("gbcf", [128, D], F32, moe)
                lg = sb("lg", [128, 9, 36], F32, moe)
                gate = sb("gate", [128, 9, 32], F32, moe)
                msk = sb("msk", [128, 9, 32], BF16, moe)
                m2f = sb("m2f", [128, 9, 32], F32, moe)
                rank = sb("rank", [128, 9, 32], F32, moe)
                tmp = sb("tmp", [128, 9, 40], F32, moe)
                rhs6 = sb("rhs6", [128, 9, NE, 6], BF16, moe)
                sel = sb("sel", [128, 9, CAP], BF16, moe)
                slot = sb("slot", [128, 2, 8], F32, moe)
                idxg = sb("idxg", [128, 2], U32, moe)
                idxs = sb("idxs", [128, 2], U32, moe)
                xg = sb("xg", [128, 2, D], BF16, moe)
                hidT = sb("hidT", [128, 4, CAP], BF16, moe)
                sgl = sb("sgl", [128, 2, CAP], F32, moe)
                ye = [sb("ye0", [128, D], F32, moe), hbf]
                yek = ["ye0", "hbf"]
                dma("sp", wr[:], w_router.rearrange("(k p) n -> p k n", p=128), writes=["wr"])
                dma("sp", brt[:], b_router.partition_broadcast(128), writes=["brt"])
                op("pool", lambda e: e.memset(hb[0:1, :], 0.0), [], [*HBK])
                dma("sp", s_h3[ZROW:ZROW + 1, :], hb[0:1, :], reads=[*HBK], writes=["s_h3"])
                dma("sp", gbcf[:], norm_ffn_g.partition_broadcast(128), writes=["gbcf"])

                for ti, (t0, rows) in enumerate(TILES):
                    def f32T(rows, ti=ti, t0=t0):
                        op("dve", lambda e: e.scalar_tensor_tensor(out=hbf[0:rows, :], in0=xt[0:rows, :], scalar=sm[0:rows, 2:3],
                                                                   in1=gbcf[0:rows, :], op0=ALU.mult, op1=ALU.mult),
                           [*XTK, "sm", "gbcf"], ["hbf"])
                        op("act", lambda e: e.activation(out=hb[0:rows, :], in_=hbf[0:rows, :], func=AF.Copy), ["hbf"], [*HBK])
                        dma("sp", s_h3[t0:t0 + rows, :], hb[0:rows, :], reads=[*HBK], writes=["s_h3"])
                        for g in range(8):
                            b = 4 + g % 4

                            def tr(e, g=g, b=b):
                                for j in range(4):
                                    c = g * 4 + j
                                    ins = e.transpose(out=ps[b][:, j * 128:j * 128 + rows], in_=hbf[0:rows, c * 128:(c + 1) * 128],
                                                      identity=ident_f[0:rows, 0:rows])
                                return ins
                            op("pe", tr, ["hbf", "cst"], [PS(b)])
                            op("act" if g % 2 == 0 else "dve",
                               (lambda e, g=g, b=b: e.activation(out=hTf[:, g * 4:(g + 1) * 4, 0:rows],
                                                                 in_=ps[b][:].rearrange("p (a t) -> p a t", a=4)[:, :, 0:rows], func=AF.Copy))
                               if g % 2 == 0 else
                               (lambda e, g=g, b=b: e.tensor_copy(out=hTf[:, g * 4:(g + 1) * 4, 0:rows],
                                                                  in_=ps[b][:].rearrange("p (a t) -> p a t", a=4)[:, :, 0:rows])),
                               [PS(b)], ["xgT"])

                        def mmr(e):
                            for k in range(32):
                                ins = e.matmul(ps[3][0:rows, 0:36], lhsT=hTf[:, k, 0:rows], rhs=wr[:, k, :], start=(k == 0), stop=(k == 31))
                            return ins
                        op("pe", mmr, ["xgT", "wr"], [PS(3)])
                        op("dve", lambda e: e.tensor_tensor(out=lg[0:rows, ti, :], in0=ps[3][0:rows, 0:36], in1=brt[0:rows, :], op=ALU.add),
                           [PS(3), "brt"], ["lg"])
                    norm_tile(s_x2[t0:t0 + rows, :], rows, t0, None, None, fp32_T=f32T, src_reads=["s_x2"])
                op("pool", lambda e: e.memset(lg[64:128, 8, :], -30000.0), [], ["lg"]) if False else None
                R_ = ["lg", "tmp", "gate", "msk", "m2f"]
                lgg = lg[:, :, 0:4]
                lge = lg[:, :, 4:36].rearrange("p t (g e) -> p t g e", g=4)
                T0, T1, T2, T3, T4, T5 = (tmp[:, :, i:i + 1] for i in range(6))
                gm = tmp[:, :, 8:12]
                op("dve", lambda e: e.tensor_reduce(out=tmp[:, :, 0:1], in_=lgg, axis=AX.X, op=ALU.max), ["lg"], ["tmp"])
                op("dve", lambda e: e.tensor_tensor(out=tmp[:, :, 12:16], in0=lgg, in1=T0.to_broadcast([128, 9, 4]), op=ALU.subtract),
                   ["lg", "tmp"], ["tmp"])
                op("dve", lambda e: e.tensor_single_scalar(out=gm, in_=tmp[:, :, 12:16], scalar=0.0, op=ALU.is_ge), ["tmp"], ["tmp"])
                op("act", lambda e: e.activation(out=tmp[:, :, 16:20], in_=tmp[:, :, 12:16], func=AF.Exp), ["tmp"], ["tmp"])
                op("dve", lambda e: e.tensor_reduce(out=tmp[:, :, 1:2], in_=tmp[:, :, 16:20], axis=AX.X, op=ALU.add), ["tmp"], ["tmp"])
                op("dve", lambda e: e.reciprocal(out=tmp[:, :, 2:3], in_=tmp[:, :, 1:2]), ["tmp"], ["tmp"])
                op("dve", lambda e: e.tensor_scalar(out=tmp[:, :, 20:24], in0=gm, scalar1=-1.0, scalar2=10000.0, op0=ALU.add, op1=ALU.mult),
                   ["tmp"], ["tmp"])
                lem = m2f[:].rearrange("p t (g e) -> p t g e", g=4)
                op("dve", lambda e: e.tensor_tensor(out=lem, in0=lge, in1=tmp[:, :, 20:24].unsqueeze(3).to_broadcast([128, 9, 4, 8]),
                                                    op=ALU.add), ["lg", "tmp"], ["m2f"])
                op("dve", lambda e: e.tensor_reduce(out=tmp[:, :, 3:4], in_=m2f[:], axis=AX.X, op=ALU.max), ["m2f"], ["tmp"])
                op("dve", lambda e: e.tensor_tensor(out=gate[:], in0=m2f[:], in1=T3.to_broadcast([128, 9, 32]), op=ALU.is_ge),
                   ["m2f", "tmp"], ["gate"])
                op("dve", lambda e: e.scalar_tensor_tensor(out=rank[:].rearrange("p t e -> p (t e)"), in0=gate[:].rearrange("p t e -> p (t e)"),
                                                           scalar=-20000.0, in1=m2f[:].rearrange("p t e -> p (t e)"), op0=ALU.mult, op1=ALU.add),
                   ["gate", "m2f"], ["rank"])
                op("dve", lambda e: e.tensor_reduce(out=tmp[:, :, 4:5], in_=rank[:], axis=AX.X, op=ALU.max), ["rank"], ["tmp"])
                op("dve", lambda e: e.tensor_tensor(out=m2f[:], in0=rank[:], in1=T4.to_broadcast([128, 9, 32]), op=ALU.is_ge),
                   ["rank", "tmp"], ["m2f"])
                op("dve", lambda e: e.tensor_tensor(out=tmp[:, :, 5:6], in0=T3, in1=T4, op=ALU.subtract), ["tmp"], ["tmp"])
                op("act", lambda e: e.activation(out=tmp[:, :, 6:7], in_=tmp[:, :, 5:6], func=AF.Sigmoid), ["tmp"], ["tmp"])
                op("dve", lambda e: e.tensor_tensor(out=tmp[:, :, 24:25], in0=tmp[:, :, 6:7], in1=tmp[:, :, 2:3], op=ALU.mult), ["tmp"], ["tmp"])
                op("dve", lambda e: e.tensor_tensor(out=tmp[:, :, 25:26], in0=tmp[:, :, 2:3], in1=tmp[:, :, 24:25], op=ALU.subtract),
                   ["tmp"], ["tmp"])
                op("dve", lambda e: e.tensor_tensor(out=msk[:], in0=gate[:], in1=m2f[:], op=ALU.add), ["gate", "m2f"], ["msk"])
                op("dve", lambda e: e.tensor_tensor(out=gate[:], in0=gate[:], in1=tmp[:, :, 24:25].to_broadcast([128, 9, 32]), op=ALU.mult),
                   ["gate", "tmp"], ["gate"])
                op("dve", lambda e: e.tensor_tensor(out=rank[:], in0=m2f[:], in1=tmp[:, :, 25:26].to_broadcast([128, 9, 32]), op=ALU.mult),
                   ["m2f", "tmp"], ["rank"])
                op("dve", lambda e: e.tensor_tensor(out=gate[:], in0=gate[:], in1=rank[:], op=ALU.add), ["gate", "rank"], ["gate"])
                op("pool", lambda e: e.memset(msk[64:128, 8, :], 0.0), [], ["msk"])
                tk = cst[:, C_TK:C_TK + 27].rearrange("p (t c) -> p t c", c=3)
                for c3 in range(3):
                    op("dve", lambda e, c3=c3: e.tensor_tensor(out=rhs6[:, :, :, c3], in0=msk[:],
                                                                in1=tk[:, :, c3:c3 + 1].to_broadcast([128, 9, 32]), op=ALU.mult),
                       ["msk", "cst"], ["rhs6"])
                op("dve", lambda e: e.tensor_tensor(out=rhs6[:, :, :, 3], in0=gate[:], in1=msk[:], op=ALU.mult), ["gate", "msk"], ["rhs6"])
                op("dve", lambda e: e.tensor_tensor(out=rank[:], in0=gate[:], in1=rhs6[:, :, :, 3], op=ALU.subtract), ["gate", "rhs6"], ["rank"])
                op("dve", lambda e: e.tensor_tensor(out=rhs6[:, :, :, 4], in0=rank[:], in1=msk[:], op=ALU.mult), ["rank", "msk"], ["rhs6"])
                op("dve", lambda e: e.tensor_tensor(out=rhs6[:, :, :, 5], in0=m2f[:], in1=msk[:], op=ALU.mult), ["m2f", "msk"], ["rhs6"])
                for ti in range(9):
                    def mmrk(e, ti=ti):
                        for tj in range(ti):
                            e.matmul(ps[4][:, 0:32], lhsT=ones_b[:], rhs=msk[:, tj, :], start=(tj == 0), stop=False)
                        return e.matmul(ps[4][:, 0:32], lhsT=ls_b[:], rhs=msk[:, ti, :], start=(ti == 0), stop=True)
                    op("pe", mmrk, ["ones_b", "ls_b", "msk"], [PS(4)])
                    op("dve", lambda e, ti=ti: e.tensor_copy(out=rank[:, ti, :], in_=ps[4][:, 0:32]), [PS(4)], ["rank"])
                op("dve", lambda e: e.tensor_copy(out=m2f[:], in_=msk[:]), ["msk"], ["m2f"])

                stage_end(8)
                for ex in range(NE):
                    for ti in range(9):
                        op("dve", lambda e, ti=ti, ex=ex: e.tensor_scalar(
                            out=sel[:, ti, :], in0=cst[:, C_IO:C_IO + CAP], scalar1=rank[:, ti, ex:ex + 1], scalar2=m2f[:, ti, ex:ex + 1],
                            op0=ALU.is_equal, op1=ALU.mult), ["cst", "rank", "m2f"], ["sel"])
                    for sg in range(CAP // 128):
                        def mmsl(e, sg=sg, ex=ex):
                            for ti in range(9):
                                ins = e.matmul(ps[4][:, sg * 8:sg * 8 + 6], lhsT=sel[:, ti, sg * 128:(sg + 1) * 128], rhs=rhs6[:, ti, ex, :],
                                               start=(ti == 0), stop=(ti == 8))
                            return ins
                        op("pe", mmsl, ["sel", "rhs6"], [PS(4)])
                    op("dve", lambda e: e.tensor_copy(out=slot[:].rearrange("p a b -> p (a b)"), in_=ps[4][:, 0:16]), [PS(4)], ["slot"])
                    op("dve", lambda e: e.scalar_tensor_tensor(out=slot[:, :, 6], in0=slot[:, :, 0], scalar=32.0, in1=slot[:, :, 1],
                                                               op0=ALU.mult, op1=ALU.add), ["slot"], ["slot"])
                    op("dve", lambda e: e.tensor_scalar(out=slot[:, :, 7], in0=slot[:, :, 2], scalar1=-1.0, scalar2=-float(ZROW),
                                                        op0=ALU.add, op1=ALU.mult), ["slot"], ["slot"])
                    op("dve", lambda e: e.tensor_tensor(out=slot[:, :, 0], in0=slot[:, :, 6], in1=slot[:, :, 7], op=ALU.add), ["slot"], ["slot"])
                    op("dve", lambda e: e.tensor_copy(out=idxg[:], in_=slot[:, :, 0]), ["slot"], ["idxg"])
                    op("dve", lambda e: e.scalar_tensor_tensor(out=slot[:, :, 1], in0=slot[:, :, 5], scalar=float(NT), in1=slot[:, :, 6],
                                                               op0=ALU.mult, op1=ALU.add), ["slot"], ["slot"])
                    op("dve", lambda e: e.tensor_scalar(out=slot[:, :, 7], in0=slot[:, :, 2], scalar1=-1.0, scalar2=-float(DUMP),
                                                        op0=ALU.add, op1=ALU.mult), ["slot"], ["slot"])
                    op("dve", lambda e: e.tensor_tensor(out=slot[:, :, 1], in0=slot[:, :, 1], in1=slot[:, :, 7], op=ALU.add), ["slot"], ["slot"])
                    op("dve", lambda e: e.tensor_copy(out=idxs[:], in_=slot[:, :, 1]), ["slot"], ["idxs"])
                    op("dve", lambda e: e.tensor_tensor(out=slot[:, :, 3], in0=slot[:, :, 3], in1=slot[:, :, 4], op=ALU.add), ["slot"], ["slot"])
                    for sg in range(CAP // 128):
                        dma("pool", None, None, reads=["idxg", "s_h3"], writes=["xg"],
                            fn=lambda e, sg=sg: e.indirect_dma_start(
                                out=xg[:, sg, :], out_offset=None, in_=s_h3[:, :],
                                in_offset=bass.IndirectOffsetOnAxis(ap=idxg[:, sg:sg + 1], axis=0)))
                    for sg in range(CAP // 128):
                        for g in range(4):
                            b = 4 + g % 4
                            pv = ps[b][:].bitcast(BF16).rearrange("p (a b) -> p a b", a=8)

                            def trx(e, sg=sg, g=g, pv=pv):
                                for j in range(8):
                                    c = g * 8 + j
                                    ins = e.transpose(out=pv[:, j, :], in_=xg[:, sg, c * 128:(c + 1) * 128], identity=ident_b[:])
                                return ins
                            op("pe", trx, ["xg", "ident_b"], [PS(b)])
                            if g % 2 == 0:
                                op("act", lambda e, sg=sg, g=g, pv=pv: e.activation(
                                    out=xgT[:, g * 8:(g + 1) * 8, sg * 128:(sg + 1) * 128], in_=pv, func=AF.Copy), [PS(b)], ["xgT"])
                            else:
                                op("dve", lambda e, sg=sg, g=g, pv=pv: e.tensor_copy(
                                    out=xgT[:, g * 8:(g + 1) * 8, sg * 128:(sg + 1) * 128], in_=pv), [PS(b)], ["xgT"])
                    for half in range(2):
                        for which in range(2):
                            wv_, kv_ = wnext()
                            for mm_ in range(2):
                                fcn = half * 2 + mm_
                                bq = (which * 2 + mm_) % 4

                                def mmg(e, b=bq, mm_=mm_, wv_=wv_):
                                    for k in range(32):
                                        ins = e.matmul(ps[b][:, 0:CAP], lhsT=wv_[:, k, mm_ * 128:(mm_ + 1) * 128], rhs=xgT[:, k, :],
                                                       start=(k == 0), stop=(k == 31))
                                    return ins
                                op("pe", mmg, [kv_, "xgT"], [PS(bq)])
                                if which == 0:
                                    op("act", lambda e, bq=bq, mm_=mm_: e.activation(out=sgl[:, mm_, :], in_=ps[bq][:, 0:CAP], func=AF.Silu),
                                       [PS(bq)], ["sgl"])
                                else:
                                    op("dve", lambda e, bq=bq, fcn=fcn, mm_=mm_: e.tensor_tensor(out=hidT[:, fcn, :], in0=ps[bq][:, 0:CAP],
                                                                                                 in1=sgl[:, mm_, :], op=ALU.mult),
                                       [PS(bq), "sgl"], ["hidT"])
                    for dh in range(2):
                      wdv, kdv = wnext()
                      for sg in range(CAP // 128):
                        yb = ye[sg % 2]
                        for cb in range(dh * 4, dh * 4 + 4):
                            wv, wk_ = wdv, kdv
                            b = 4 + cb % 4

                            def mmd(e, sg=sg, cb=cb, wv=wv, b=b):
                                for k in range(4):
                                    ins = e.matmul(ps[b][:, :], lhsT=hidT[:, k, sg * 128:(sg + 1) * 128],
                                                   rhs=wv[:, k, (cb % 4) * 512:(cb % 4) * 512 + 512], start=(k == 0), stop=(k == 3))
                                return ins
                            op("pe", mmd, ["hidT", wk_], [PS(b)])
                            if cb % 2 == 0:
                                op("act", lambda e, sg=sg, cb=cb, b=b, yb=yb: e.activation(
                                    out=yb[:, cb * 512:(cb + 1) * 512], in_=ps[b][:, :], func=AF.Copy, scale=slot[:, sg, 3:4]),
                                   [PS(b), "slot"], [yek[sg % 2]])
                            else:
                                op("dve", lambda e, sg=sg, cb=cb, b=b, yb=yb: e.tensor_scalar(
                                    out=yb[:, cb * 512:(cb + 1) * 512], in0=ps[b][:, :], scalar1=slot[:, sg, 3:4], scalar2=None, op0=ALU.mult),
                                   [PS(b), "slot"], [yek[sg % 2]])
                        if dh == 1:
                            dma("pool", None, None, reads=["idxs", yek[sg % 2]], writes=["s_o12"],
                                fn=lambda e, sg=sg, yb=yb: e.indirect_dma_start(
                                    out=s_o12[:, :], out_offset=bass.IndirectOffsetOnAxis(ap=idxs[:, sg:sg + 1], axis=0),
                                    in_=yb[:, :], in_offset=None))

                stage_end(9)
                dma("sp", gbcf[:], norm_final_g.partition_broadcast(128), writes=["gbcf"])
                fsets = [
                    [(xt[:, :], [*XTK]), (hbf[:, :], ["hbf"]), (ye[0][:, :], ["ye0"])],
                    [(xgTbuf[:, :], ["xgT"]), (xg[:].rearrange("p a d -> p (a d)").bitcast(F32), ["xg"]),
                     (wsl[2][:].bitcast(F32), ["wsl2"])],
                ]
                for ti, (t0, rows) in enumerate(TILES):
                    (A, kA), (Bf, kB), (Cf, kC) = fsets[ti % 2]
                    dma("sp", A[0:rows, :], s_x2[t0:t0 + rows, :], reads=["s_x2"], writes=kA)
                    dma("sp", Bf[0:rows, :], s_o12[t0:t0 + rows, :], reads=["s_o12"], writes=kB)
                    dma("sp", Cf[0:rows, :], s_o12[NT + t0:NT + t0 + rows, :], reads=["s_o12"], writes=kC)
                    op("dve", lambda e, rows=rows, A=A, Bf=Bf: e.tensor_tensor(out=A[0:rows, :], in0=A[0:rows, :], in1=Bf[0:rows, :], op=ALU.add),
                       [*kA, *kB], kA)
                    op("dve", lambda e, rows=rows, A=A, Cf=Cf: e.tensor_tensor(out=A[0:rows, :], in0=A[0:rows, :], in1=Cf[0:rows, :], op=ALU.add),
                       [*kA, *kC], kA)
                    sc0 = 8 + 4 * (ti % 2)
                    ksm = "smf%d" % (ti % 2)
                    op("act", lambda e, rows=rows, A=A, sc0=sc0: e.activation(out=hb[0:rows, :], in_=A[0:rows, :], func=AF.Square,
                                                                               accum_out=sm[0:rows, sc0:sc0 + 1]), kA, [*HBK, ksm])
                    op("act", lambda e, rows=rows, sc0=sc0: e.activation(out=sm[0:rows, sc0 + 1:sc0 + 2], in_=sm[0:rows, sc0:sc0 + 1], func=AF.Sqrt,
                                                                          scale=1.0 / D, bias=EPS), [ksm], [ksm])
                    op("dve", lambda e, rows=rows, sc0=sc0: e.reciprocal(out=sm[0:rows, sc0 + 2:sc0 + 3], in_=sm[0:rows, sc0 + 1:sc0 + 2]), [ksm], [ksm])
                    op("dve", lambda e, rows=rows, A=A, Bf=Bf, sc0=sc0: e.scalar_tensor_tensor(
                        out=Bf[0:rows, :], in0=A[0:rows, :], scalar=sm[0:rows, sc0 + 2:sc0 + 3],
                        in1=gbcf[0:rows, :], op0=ALU.mult, op1=ALU.mult), [*kA, ksm, "gbcf"], kB)
                    dst = y_main[t0:t0 + rows, :] if ti < 8 else y_s[:, :]
                    dma("sp", dst, Bf[0:rows, :], reads=kB)
      except _Stop:
        S.finish()
        es.pop_all()
        return nc
      S.finish()
    return nc


_NC_CACHE = {}
_RET_MAPS = [False]


def kernel(**inp):
    f = lambda k: np.ascontiguousarray(np.asarray(inp[k], dtype=np.float32))
    x_prompt = f("x_prompt")
    x_sample = f("x_sample")
    consts = make_consts()
    convp = np.ascontiguousarray(np.concatenate(
        [f("conv_dw_w")[0], f("conv_dw_b")[0][None], f("conv_ln_g")[0][None], f("conv_ln_b")[0][None]], axis=0))
    w_router = np.ascontiguousarray(np.concatenate([f("w_router_group")[0], f("w_router_expert")[0]], axis=1))
    b_router = np.ascontiguousarray(np.concatenate([f("b_router_group")[0], f("b_router_expert")[0]], axis=0))
    shared = {
        "consts": consts,
        "norm_mix_g": f("norm_mix_g")[0], "w_in": f("w_in")[0], "w_alpha_up": f("w_alpha_up")[0],
        "b_alpha": f("b_alpha"), "gla_norm_g": f("gla_norm_g")[0], "w_branch_a": f("w_branch_a")[0],
        "convp_in": convp, "w_branch_b": f("w_branch_b")[0], "w_out": f("w_out")[0],
        "norm_ca_g": f("norm_ca_g")[0], "norm_mem_g": f("norm_mem_g")[0], "w_ca_q": f("w_ca_q")[0],
        "w_ca_k": f("w_ca_k")[0], "w_ca_v": f("w_ca_v")[0], "w_ca_o": f("w_ca_o")[0],
        "norm_ffn_g": f("norm_ffn_g")[0], "w_router": w_router, "b_router": b_router,
        "w_exp_gate": f("w_exp_gate")[0], "w_exp_up": f("w_exp_up")[0], "w_exp_down": f("w_exp_down")[0],
        "norm_final_g": f("norm_final_g"),
    }
    st_gla = f("state_gla")[0]
    st_conv = f("state_conv")[0]
    ck = f("cache_mem_k")[0].reshape(128, 256, 1024)
    cv = f("cache_mem_v")[0].reshape(128, 256, 1024)
    mem = f("mem_prompt")
    zeros_pre = np.zeros((NP, D), np.float32)
    in_maps = []
    for c in range(NCORE):
        b, half = c // 2, c % 2
        m = dict(shared)
        m["x_main"] = x_prompt[b, half * NP:(half + 1) * NP]
        m["x_pre"] = x_prompt[b, 0:NP] if half == 1 else zeros_pre
        m["x_s"] = x_sample[c * NSEQ:(c + 1) * NSEQ].reshape(NS, D)
        m["mem"] = mem[b]
        m["st_gla"] = st_gla[c * NSEQ:(c + 1) * NSEQ]
        m["st_conv"] = st_conv[c * NSEQ:(c + 1) * NSEQ]
        m["ck"] = ck[c * NSEQ:(c + 1) * NSEQ]
        m["cv"] = cv[c * NSEQ:(c + 1) * NSEQ]
        in_maps.append(m)
    if _RET_MAPS[0]:
        return in_maps
    if "nc" not in _NC_CACHE:
        _NC_CACHE["nc"] = build()
    nc = _NC_CACHE["nc"]
    res = run_bass_kernel_spmd(nc, in_maps, core_ids=list(range(NCORE)))
    r = res.results
    y_prompt = np.stack([np.concatenate([r[2 * b]["y_main"], r[2 * b + 1]["y_main"]], axis=0) for b in range(4)])
    y_sample = np.concatenate([r[c]["y_s"].reshape(NSEQ, 4, D) for c in range(NCORE)], axis=0)
    gla_prompt = np.stack([r[2 * b + 1]["gla_p"] for b in range(4)])[None]
    conv_prompt = np.stack([r[2 * b + 1]["conv_p"] for b in range(4)])[None]
    mk = np.stack([r[2 * b]["mk_o"].reshape(256, 4, 256) for b in range(4)])[None]
    mv = np.stack([r[2 * b]["mv_o"].reshape(256, 4, 256) for b in range(4)])[None]
    gla_sample = np.concatenate([r[c]["gla_s"] for c in range(NCORE)], axis=0)[None]
    conv_sample = np.concatenate([r[c]["conv_s"] for c in range(NCORE)], axis=0)[None]
    return (y_prompt.astype(np.float32), y_sample.astype(np.float32), gla_prompt.astype(np.float32),
            conv_prompt.astype(np.float32), mk.astype(np.float32), mv.astype(np.float32),
            gla_sample.astype(np.float32), conv_sample.astype(np.float32))
```

```python
import contextlib
import numpy as np
import concourse.bass as bass
import concourse.mybir as mybir
from concourse.bass_utils import run_bass_kernel_spmd

F32 = mybir.dt.float32
BF16 = mybir.dt.bfloat16
U32 = mybir.dt.uint32
I32 = mybir.dt.int32
AF = mybir.ActivationFunctionType
ALU = mybir.AluOpType
AX = mybir.AxisListType

D = 4096
NCORE = 8
NP = 1024
NS = 64
NT = NP + NS
NSEQ = 16
IN_COLS = 18448
OFF_Q, OFF_K, OFF_V, OFF_R, OFF_A, OFF_U, OFF_G = 0, 1024, 2048, 4096, 6144, 6160, 10256
EPS = 1e-6
CAP = 256
NE = 32
ZROW = NT
DUMP = 2 * NT
NGRP = [(0, 512), (512, 512), (1024, 64)]
TILES = [(i * 128, 128) for i in range(8)] + [(1024, 64)]

C_ID, C_MU, C_MS, C_RM, C_IO, C_TK = 0, 128, 256, 320, 336, 592
CWP = 619
C_BM, C_RS, C_LS, C_ON = 619, 1643, 2731, 2859
CW = 2987


def make_consts():
    c = np.zeros((128, CW), np.float32)
    p = np.arange(128)
    c[:, C_ID:C_ID + 128] = np.eye(128)
    c[:, C_MU:C_MU + 128] = (p[:, None] <= p[None, :])
    q = np.arange(64)
    ms = ((q[:, None] // 4) == (q[None, :] // 4)) & (q[:, None] <= q[None, :])
    c[:64, C_MS:C_MS + 64] = ms
    bm = (np.arange(16)[:, None] == (q[None, :] // 4)).astype(np.float32)
    c[:, C_BM:C_BM + 1024] = bm.reshape(1, 1024)
    c[:64, C_RM:C_RM + 16] = ((q[:, None] // 4) == np.arange(16)[None, :])
    t = np.arange(NT)
    rs = np.ones(NT, np.float32)
    rs[:NP][t[:NP] % 128 == 0] = 0.0
    rs[NP:][(t[NP:] - NP) % 4 == 0] = 0.0
    c[:, C_RS:C_RS + NT] = rs[None, :]
    c[:, C_IO:C_IO + 256] = np.arange(256)[None, :]
    for i in range(9):
        tok = i * 128 + p
        c[:, C_TK + 3 * i + 0] = tok // 32
        c[:, C_TK + 3 * i + 1] = tok % 32
        c[:, C_TK + 3 * i + 2] = 1.0
    c[:, C_LS:C_LS + 128] = (p[:, None] < p[None, :])
    c[:, C_ON:C_ON + 128] = 1.0
    return c


class Sched:
    def __init__(self, nc, es, ndma=28):
        self.nc = nc
        self.E = dict(pe=nc.tensor, act=nc.scalar, dve=nc.vector, pool=nc.gpsimd, sp=nc.sync)
        self.sem = {k: es.enter_context(nc.semaphore("c_" + k)) for k in self.E}
        self.cnt = {k: 0 for k in self.E}
        self.seen = {k: {} for k in self.E}
        self.dsem = [es.enter_context(nc.semaphore("dm%d" % i)) for i in range(ndma)]
        self.dval = [0] * ndma
        self.dnext = {"sp": 0, "pool": 0, "act": 0}
        self.dhalf = ndma // 2
        self.res = {}

    def _semobj(self, key):
        return self.sem[key] if isinstance(key, str) else self.dsem[key]

    def _wait(self, eng, tok):
        key, val, src = tok
        if self.seen[eng].get(key, 0) >= val:
            return
        self.E[eng].wait_ge(self._semobj(key), val)
        self.seen[eng][key] = val

    def _deps(self, eng, reads, writes):
        for r in reads:
            st = self.res.get(r)
            if st and st["w"]:
                tok = st["w"]
                if not (tok[2] == eng and eng == "pe"):
                    self._wait(eng, tok)
        for w in writes:
            st = self.res.get(w)
            if st:
                if st["w"] and st["w"][2] != eng:
                    self._wait(eng, st["w"])
                for tok in st["r"]:
                    if tok[2] != eng:
                        self._wait(eng, tok)

    def _commit(self, tok, reads, writes):
        for r in reads:
            st = self.res.setdefault(r, {"w": None, "r": []})
            if tok[2] != "dma":
                st["r"] = [x for x in st["r"] if x[2] != tok[2]]
            st["r"].append(tok)
        for w in writes:
            self.res[w] = {"w": tok, "r": []}

    mute = False

    def op(self, eng, fn, reads=(), writes=()):
        if self.mute:
            return
        self._deps(eng, reads, writes)
        ins = fn(self.E[eng])
        self.cnt[eng] += 1
        ins.then_inc(self.sem[eng], 1)
        self._commit((eng, self.cnt[eng], eng), reads, writes)

    def dma(self, q, out, in_, reads=(), writes=(), fn=None):
        if self.mute:
            return
        base = 0 if q == "pool" else self.dhalf
        i = base + self.dnext[q]
        self.dnext[q] = (self.dnext[q] + 1) % self.dhalf
        if self.dval[i] > 0:
            self._wait(q, (i, self.dval[i], "dma"))
        self._deps(q, reads, writes)
        if fn is None:
            ins = self.E[q].dma_start(out=out, in_=in_)
        else:
            ins = fn(self.E[q])
        ins.then_inc(self.dsem[i], 16)
        self.dval[i] += 16
        self._commit((i, self.dval[i], "dma"), reads, writes)

    def finish(self):
        for i, v in enumerate(self.dval):
            if v > 0:
                self._wait("sp", (i, v, "dma"))
        for k in ("pe", "act", "dve", "pool"):
            if self.cnt[k] > 0:
                self._wait("sp", (k, self.cnt[k], k))


class _Stop(Exception):
    pass


_SB_MIN = [1 << 30]


def build(stage=99, mute_until=None, lite=None):
    nc = bass.Bass("TRN2", target_bir_lowering=False)

    def stage_end(k):
        if mute_until is not None and k >= mute_until:
            S.mute = False
        if stage <= k:
            raise _Stop()

    def din(name, shape, dt=F32):
        kind = "ExternalInput" if (lite is None or name in lite) else "Internal"
        return nc.dram_tensor(name, list(shape), dt, kind=kind).ap()

    def dout(name, shape, dt=F32):
        return nc.dram_tensor(name, list(shape), dt, kind="ExternalOutput").ap()

    def dscr(name, shape, dt):
        return nc.dram_tensor(name, list(shape), dt, kind="Internal").ap()

    x_main = din("x_main", [NP, D])
    x_pre = din("x_pre", [NP, D])
    x_s = din("x_s", [NS, D])
    mem = din("mem", [256, D])
    st_gla = din("st_gla", [NSEQ, 4, 256, 512])
    st_conv = din("st_conv", [NSEQ, 30, 2048])
    ck = din("ck", [NSEQ, 256, 1024])
    cv = din("cv", [NSEQ, 256, 1024])
    consts = din("consts", [128, CW])
    norm_mix_g = din("norm_mix_g", [D])
    w_in = din("w_in", [D, IN_COLS])
    w_alpha_up = din("w_alpha_up", [16, 1024])
    b_alpha = din("b_alpha", [1, 1024])
    gla_norm_g = din("gla_norm_g", [2048])
    w_branch_a = din("w_branch_a", [2048, D])
    convp_in = din("convp_in", [34, 2048])
    w_branch_b = din("w_branch_b", [2048, D])
    w_out = din("w_out", [D, D])
    norm_ca_g = din("norm_ca_g", [D])
    norm_mem_g = din("norm_mem_g", [D])
    w_ca_q = din("w_ca_q", [D, 1024])
    w_ca_k = din("w_ca_k", [D, 1024])
    w_ca_v = din("w_ca_v", [D, 1024])
    w_ca_o = din("w_ca_o", [1024, D])
    norm_ffn_g = din("norm_ffn_g", [D])
    w_router = din("w_router", [D, 36])
    b_router = din("b_router", [36])
    w_exp_gate = din("w_exp_gate", [NE, D, 512])
    w_exp_up = din("w_exp_up", [NE, D, 512])
    w_exp_down = din("w_exp_down", [NE, 512, D])
    norm_final_g = din("norm_final_g", [D])

    y_main = dout("y_main", [NP, D])
    y_s = dout("y_s", [NS, D])
    gla_p = dout("gla_p", [4, 256, 512])
    conv_p = dout("conv_p", [30, 2048])
    mk_o = dout("mk_o", [256, 1024])
    mv_o = dout("mv_o", [256, 1024])
    gla_s = dout("gla_s", [NSEQ, 4, 256, 512])
    conv_s = dout("conv_s", [NSEQ, 30, 2048])

    s_state = dscr("s_state", [4, 256, 512], F32)
    s_ogT = dscr("s_ogT", [2048, NT], BF16)
    s_mT = dscr("s_mT", [D, NT], BF16)
    s_x1 = dscr("s_x1", [NT, D], F32)
    s_x2 = dscr("s_x2", [NT, D], F32)
    s_h3 = dscr("s_h3", [NT + 1, D], BF16)
    s_o12 = dscr("s_o12", [2 * NT + 1, D], F32)

    es = contextlib.ExitStack()
    with es:
      S = Sched(nc, es)
      try:
        pass
        XTK = ["xt", "xt_E1", "xt_E2", "xt_E3"]
        HBK = ["hb", "hb_kdT", "hb_sT", "hb_og", "hb_ogT", "hb_sTs"]
        op, dma = S.op, S.dma

        def sb(name, shape, dt, stack=es):
            t = stack.enter_context(nc.sbuf_tensor(name, list(shape), dt))
            _SB_MIN[0] = min(_SB_MIN[0], nc.sbuf_bytes_remaining)
            return t

        ps = [es.enter_context(nc.psum_tensor("ps%d" % i, [128, 512], F32)) for i in range(8)]

        def PS(i):
            return "ps%d" % i

        cst = sb("cst", [128, CWP], F32)
        ident_b = sb("ident_b", [128, 128], BF16)
        bm_b = sb("bm_b", [128, 16, 64], BF16)
        rs_b = sb("rs_b", [128, NT], BF16)
        ls_b = sb("ls_b", [128, 128], BF16)
        ones_b = sb("ones_b", [128, 128], BF16)
        NSLOT = 2
        wsl = [sb("wsl%d" % i, [128, 8192], BF16) for i in range(NSLOT)]
        gbc = sb("gbc", [128, D], BF16)
        xt = sb("xt", [128, D], F32)
        hb = sb("hb", [128, D], BF16)
        sm = sb("sm", [128, 64], F32)

        dma("sp", cst[:], consts[:, 0:CWP], writes=["cst"])
        dma("sp", xt[:, 0:CW - CWP], consts[:, CWP:CW], writes=[*XTK])
        ident_f = cst[:, C_ID:C_ID + 128]
        op("dve", lambda e: e.tensor_copy(out=ident_b[:], in_=cst[:, C_ID:C_ID + 128]), ["cst"], ["ident_b"])
        op("dve", lambda e: e.tensor_copy(out=bm_b[:].rearrange("p a b -> p (a b)"), in_=xt[:, C_BM - CWP:C_BM - CWP + 1024]), [*XTK], ["bm_b"])
        op("dve", lambda e: e.tensor_copy(out=rs_b[:], in_=xt[:, C_RS - CWP:C_RS - CWP + NT]), [*XTK], ["rs_b"])
        op("dve", lambda e: e.tensor_copy(out=ls_b[:], in_=xt[:, C_LS - CWP:C_LS - CWP + 128]), [*XTK], ["ls_b"])
        op("dve", lambda e: e.tensor_copy(out=ones_b[:], in_=xt[:, C_ON - CWP:C_ON - CWP + 128]), [*XTK], ["ones_b"])
        hscope = contextlib.ExitStack()
        hT = sb("hT", [128, 32, NT], BF16, hscope)
        if mute_until is not None:
            S.mute = True

        wstate = {"n": 0, "plan": [], "issued": 0, "moe0": 10 ** 9}

        def wplan(ap2d):
            wstate["plan"].append(ap2d)

        def slot_of(i):
            m0 = wstate["moe0"]
            if i < m0:
                return i % 2
            return [m0 % 2, 1 - m0 % 2, 2, 3][(i - m0) % 4]

        def look(i):
            return 2 if i < wstate["moe0"] else 4

        def _wissue(i):
            ap2d = wstate["plan"][i]
            K, ncols = ap2d.shape
            kc = K // 128
            slot = slot_of(i)
            view = wsl[slot][:, 0:kc * ncols].rearrange("p (k n) -> p k n", k=kc)
            dma("pool", view, ap2d.rearrange("(k p) n -> p k n", p=128), writes=["wsl%d" % slot])

        def wnext(ap2d_check=None):
            i = wstate["n"]
            wstate["n"] += 1
            if S.mute:
                wstate["issued"] = max(wstate["issued"], wstate["n"])
            while not S.mute and wstate["issued"] < min(len(wstate["plan"]), i + look(i)):
                _wissue(wstate["issued"])
                wstate["issued"] += 1
            ap2d = wstate["plan"][i]
            if ap2d_check is not None:
                assert ap2d.shape == ap2d_check.shape and ap2d.offset == ap2d_check.offset, (i, ap2d, ap2d_check)
            K, ncols = ap2d.shape
            kc = K // 128
            slot = slot_of(i)
            view = wsl[slot][:, 0:kc * ncols].rearrange("p (k n) -> p k n", k=kc)
            return view, "wsl%d" % slot

        def load_gain(g_ap):
            dma("pool", gbc[:], g_ap.partition_broadcast(128), writes=["gbc"])

        def norm_tile(src_ap, rows, col, dstT, dst_key, scratch_out=None, fp32_T=None, src_reads=()):
            dma("sp", xt[0:rows, :], src_ap, reads=list(src_reads), writes=[*XTK])
            op("act", lambda e: e.activation(out=hb[0:rows, :], in_=xt[0:rows, :], func=AF.Square,
                                             accum_out=sm[0:rows, 0:1]), [*XTK], [*HBK, "sm"])
            op("act", lambda e: e.activation(out=sm[0:rows, 1:2], in_=sm[0:rows, 0:1], func=AF.Sqrt,
                                             scale=1.0 / D, bias=EPS), ["sm"], ["sm"])
            op("dve", lambda e: e.reciprocal(out=sm[0:rows, 2:3], in_=sm[0:rows, 1:2]), ["sm"], ["sm"])
            if fp32_T is None:
                op("dve", lambda e: e.scalar_tensor_tensor(out=hb[0:rows, :], in0=xt[0:rows, :], scalar=sm[0:rows, 2:3],
                                                           in1=gbc[0:rows, :], op0=ALU.mult, op1=ALU.mult),
                   [*XTK, "sm", "gbc"], [*HBK])
                if scratch_out is not None:
                    dma("sp", scratch_out, hb[0:rows, :], reads=[*HBK])
                for g in range(4):
                    pv = ps[4 + g][:].bitcast(BF16).rearrange("p (a b) -> p a b", a=8)

                    def tr(e, g=g, pv=pv):
                        for j in range(8):
                            c = g * 8 + j
                            ins = e.transpose(out=pv[:, j, 0:rows], in_=hb[0:rows, c * 128:(c + 1) * 128],
                                              identity=ident_b[0:rows, 0:rows])
                        return ins
                    op("pe", tr, [*HBK, "ident_b"], [PS(4 + g)])
                    eng = "act" if g % 2 == 0 else "dve"
                    if eng == "act":
                        op("act", lambda e, g=g, pv=pv: e.activation(out=dstT[:, g * 8:(g + 1) * 8, col:col + rows],
                                                                      in_=pv[:, :, 0:rows], func=AF.Copy),
                           [PS(4 + g)], [dst_key])
                    else:
                        op("dve", lambda e, g=g, pv=pv: e.tensor_copy(out=dstT[:, g * 8:(g + 1) * 8, col:col + rows],
                                                                       in_=pv[:, :, 0:rows]),
                           [PS(4 + g)], [dst_key])
            else:
                fp32_T(rows)

        def gemm_ws(wview, wkey, kc, mcols, acts, act_key, groups, evac, psbanks, extra_reads=()):
            nb = 0
            for m in range(mcols // 128):
                for gi, (t0, n) in enumerate(groups):
                    b = psbanks[nb % len(psbanks)]
                    nb += 1

                    def mm(e, m=m, t0=t0, n=n, b=b):
                        for k in range(kc):
                            ins = e.matmul(ps[b][:, 0:n], lhsT=wview[:, k, m * 128:(m + 1) * 128],
                                           rhs=acts[:, k, t0:t0 + n], start=(k == 0), stop=(k == kc - 1))
                        return ins
                    op("pe", mm, [wkey, act_key] + list(extra_reads), [PS(b)])
                    evac(m, gi, t0, n, ps[b][:, 0:n], PS(b))

        def gemm_as(wviews, wkeys, actT, act_key, tiles, ncols, evac, psbanks):
            nb = 0
            for ti, (t0, rows) in enumerate(tiles):
                b = psbanks[nb % len(psbanks)]
                nb += 1
                ktot = sum(v.shape[1] for v, _ in wviews)

                def mm(e, t0=t0, rows=rows, b=b):
                    kk = 0
                    for v, koff in wviews:
                        for k in range(v.shape[1]):
                            ins = e.matmul(ps[b][0:rows, 0:ncols], lhsT=actT[:, koff + k, t0:t0 + rows],
                                           rhs=v[:, k, :], start=(kk == 0), stop=(kk == ktot - 1))
                            kk += 1
                    return ins
                op("pe", mm, list(wkeys) + [act_key], [PS(b)])
                evac(ti, t0, rows, ps[b][0:rows, 0:ncols], PS(b))

        def cols(a, n):
            return w_in[:, a:a + n]

        def plan_gla(with_q):
            wplan(cols(OFF_A, 16))
            for h in range(4):
                if with_q:
                    wplan(cols(OFF_Q + 256 * h, 256))
                wplan(cols(OFF_K + 256 * h, 256))
                wplan(cols(OFF_V + 512 * h, 256))
                wplan(cols(OFF_V + 512 * h + 256, 256))
                if with_q:
                    wplan(cols(OFF_R + 512 * h, 256))
                    wplan(cols(OFF_R + 512 * h + 256, 256))
        plan_gla(False)
        plan_gla(True)
        for m in range(16):
            wplan(cols(OFF_G + 256 * m, 256))
            wplan(w_branch_a[:, 256 * m:256 * m + 256])
        for m in range(8):
            wplan(cols(OFF_U + 256 * m, 256))
            wplan(cols(OFF_U + 2048 + 256 * m, 256))
        for m in range(16):
            wplan(cols(OFF_G + D + 256 * m, 256))
            wplan(w_branch_b[:, 256 * m:256 * m + 256])
        for cb in range(16):
            wplan(w_out[:, 256 * cb:256 * cb + 256])
        for wm in (w_ca_k, w_ca_v):
            for cb in range(4):
                wplan(wm[:, 256 * cb:256 * cb + 256])
        for m in range(4):
            wplan(w_ca_k[:, 256 * m:256 * m + 256])
        for m in range(4):
            wplan(w_ca_q[:, 256 * m:256 * m + 256])
        for cb in range(4):
            wplan(w_ca_o[:, 1024 * cb:1024 * cb + 1024])
        wstate["moe0"] = len(wstate["plan"])
        for ex in range(NE):
            wplan(w_exp_gate[ex][:, 0:256])
            wplan(w_exp_up[ex][:, 0:256])
            wplan(w_exp_gate[ex][:, 256:512])
            wplan(w_exp_up[ex][:, 256:512])
            wplan(w_exp_down[ex][:, 0:2048])
            wplan(w_exp_down[ex][:, 2048:4096])

        mix = contextlib.ExitStack()
        with mix:
            alT = sb("alT", [17, NT], BF16, mix)
            walb = sb("walb", [17, 1024], BF16, mix)
            ggl = sb("ggl", [128, 512], F32, mix)
            hT_halo = sb("hT_halo", [128, 32, 32], BF16, mix)
            dma("pool", walb[0:16, :], w_alpha_up, writes=["walb"])
            dma("pool", walb[16:17, :], b_alpha, writes=["walb"])
            load_gain(norm_mix_g)

            gl = contextlib.ExitStack()
            with gl:
                U1 = sb("U1", [128, 4608], F32, gl)
                bT = U1[:, 0:2 * NT].rearrange("p (c t) -> p c t", c=2)
                dfT = U1[:, 2 * NT:4 * NT].rearrange("p (c t) -> p c t", c=2)
                U1b = U1[:].bitcast(BF16)
                vh = U1b[:, 0:4608].rearrange("p (i v) -> p i v", i=9)
                gr = U1b[:, 4608:9216].rearrange("p (i v) -> p i v", i=9)
                xtb = xt[:].bitcast(BF16)
                E1 = xtb[:, 0:2 * NT].rearrange("p (c t) -> p c t", c=2)
                E2 = xtb[:, 2 * NT:4 * NT].rearrange("p (c t) -> p c t", c=2)
                E3 = xtb[:, 4 * NT:6 * NT].rearrange("p (c t) -> p c t", c=2)
                kdT = hb[:, 0:2 * NT].rearrange("p (c t) -> p c t", c=2)
                sT = hb[:, 2 * NT:2 * NT + 128]
                og = hb[:, 2304:2816]
                ogT = hb[:, 2816:3328].rearrange("p (a b) -> p a b", a=4)
                dec = sb("dec", [128, 2, 24], F32, gl)
                qeT = sb("qeT", [128, 2, NT], BF16, gl)
                keT = sb("keT", [128, 2, NT], BF16, gl)
                kd = sb("kd", [128, 9, 256], BF16, gl)
                of = sb("of", [128, 512], F32, gl)
                grf = of
                QMs = sb("QMs", [128, 2, 64], BF16, gl)
                Sf = sb("Sf", [128, 2, 512], F32, gl)
                Sb = sb("Sb", [128, 2, 512], BF16, gl)
                st6 = sb("st6", [128, 8], F32, gl)
                s0f = [sb("s0f%d" % i, [128, 512], F32, gl) for i in range(2)]
                s0b = [sb("s0b%d" % i, [128, 512], BF16, gl) for i in range(2)]
                sout = [sb("sout%d" % i, [128, 512], F32, gl) for i in range(2)]
                kdm = sb("kdm", [64, 256], BF16, gl)

                def gla_pass(main):
                    ntok = NT if main else NP
                    groups = NGRP if main else NGRP[:2]
                    tiles = TILES if main else TILES[:8]
                    for ti, (t0, rows) in enumerate(tiles):
                        if main:
                            src = x_main[t0:t0 + rows, :] if ti < 8 else x_s[:, :]
                        else:
                            src = x_pre[t0:t0 + rows, :]
                        norm_tile(src, rows, t0, hT, "hT")
                    if not main:
                        op("dve", lambda e: e.tensor_copy(out=hT_halo[:], in_=hT[:, :, NP - 32:NP]), ["hT"], ["hT_halo"])
                    wv, wk = wnext()
                    op("pool", lambda e: e.memset(alT[:], 1.0), [], ["alT"])

                    def ev_a(m, gi, t0, n, pap, pkey):
                        op("act", lambda e: e.activation(out=alT[0:16, t0:t0 + n], in_=pap[0:16, :], func=AF.Copy),
                           [pkey], ["alT"])
                    for gi, (t0, n) in enumerate(groups):
                        b = gi % 4

                        def mm(e, t0=t0, n=n, b=b):
                            for k in range(32):
                                ins = e.matmul(ps[b][0:16, 0:n], lhsT=wv[:, k, 0:16], rhs=hT[:, k, t0:t0 + n],
                                               start=(k == 0), stop=(k == 31))
                            return ins
                        op("pe", mm, [wk, "hT"], [PS(b)])
                        ev_a(0, gi, t0, n, ps[b][:, 0:n], PS(b))

                    for h in range(4):
                        for dc in range(2):
                            for gi, (t0, n) in enumerate(groups):
                                b = 4 + (dc * 3 + gi) % 4
                                c0 = (2 * h + dc) * 128
                                op("pe", lambda e, b=b, c0=c0, t0=t0, n=n: e.matmul(
                                    ps[b][:, 0:n], lhsT=walb[:, c0:c0 + 128], rhs=alT[:, t0:t0 + n], start=True, stop=True),
                                   ["walb", "alT"], [PS(b)])
                                op("act", lambda e, b=b, dc=dc, t0=t0, n=n: e.activation(
                                    out=dfT[:, dc, t0:t0 + n], in_=ps[b][:, 0:n], func=AF.Exp, scale=-1.0),
                                   [PS(b)], ["U1"])
                            op("act", lambda e, dc=dc: e.activation(out=dfT[:, dc, 0:ntok], in_=dfT[:, dc, 0:ntok],
                                                                     func=AF.Ln, bias=1.0), ["U1"], ["U1"])
                            op("dve", lambda e, dc=dc: e.tensor_scalar(out=dfT[:, dc, 0:ntok], in0=dfT[:, dc, 0:ntok],
                                                                        scalar1=-1.0 / 16.0, scalar2=None, op0=ALU.mult),
                               ["U1"], ["U1"])
                            op("dve", lambda e, dc=dc: e.tensor_tensor_scan(
                                out=bT[:, dc, 0:ntok], data0=rs_b[:, 0:ntok], data1=dfT[:, dc, 0:ntok],
                                initial=0.0, op0=ALU.mult, op1=ALU.add), ["U1", "rs_b"], ["U1"])
                        bTp = bT[:, :, 0:NP].rearrange("p c (n t) -> p c n t", t=128)
                        dfp = dfT[:, :, 0:NP].rearrange("p c (n t) -> p c n t", t=128)
                        for dc in range(2):
                            op("dve", lambda e, dc=dc: e.tensor_tensor(
                                out=dfp[:, dc], in0=bTp[:, dc, :, 127:128].to_broadcast([128, 8, 128]), in1=bTp[:, dc],
                                op=ALU.subtract), ["U1"], ["U1"])
                            op("act", lambda e, dc=dc: e.activation(out=dec[:, dc, 0:8], in_=bTp[:, dc, :, 127], func=AF.Exp),
                               ["U1"], ["dec"])
                        if main:
                            bTs = bT[:, :, NP:NT].rearrange("p c (n t) -> p c n t", t=4)
                            dfs = dfT[:, :, NP:NT].rearrange("p c (n t) -> p c n t", t=4)
                            for dc in range(2):
                                op("dve", lambda e, dc=dc: e.tensor_tensor(
                                    out=dfs[:, dc], in0=bTs[:, dc, :, 3:4].to_broadcast([128, 16, 4]), in1=bTs[:, dc],
                                    op=ALU.subtract), ["U1"], ["U1"])
                                op("act", lambda e, dc=dc: e.activation(out=dec[:, dc, 8:24], in_=bTs[:, dc, :, 3], func=AF.Exp),
                                   ["U1"], ["dec"])
                        op("act", lambda e: e.activation(out=E3[:, :, 0:ntok], in_=dfT[:, :, 0:ntok], func=AF.Exp),
                           ["U1"], ["xt_E3"])
                        if main:
                            op("act", lambda e: e.activation(out=E1[:, :, 0:ntok], in_=bT[:, :, 0:ntok], func=AF.Exp),
                               ["U1"], ["xt_E1"])
                            op("act", lambda e: e.activation(out=E2[:, :, 0:ntok], in_=bT[:, :, 0:ntok], func=AF.Exp,
                                                             scale=-1.0), ["U1"], ["xt_E2"])
                            wv, wk = wnext()

                            def ev_q(m, gi, t0, n, pap, pkey):
                                op("dve", lambda e: e.scalar_tensor_tensor(
                                    out=qeT[:, m, t0:t0 + n], in0=pap, scalar=1.0 / 16.0, in1=E1[:, m, t0:t0 + n],
                                    op0=ALU.mult, op1=ALU.mult), [pkey, "xt_E1"], ["qeT"])
                            gemm_ws(wv, wk, 32, 256, hT, "hT", groups, ev_q, [0, 1, 2, 3])
                        wv, wk = wnext()

                        def ev_k(m, gi, t0, n, pap, pkey):
                            if main:
                                op("dve", lambda e: e.tensor_tensor(out=keT[:, m, t0:t0 + n], in0=pap,
                                                                    in1=E2[:, m, t0:t0 + n], op=ALU.mult),
                                   [pkey, "xt_E2"], ["keT"])
                            op("dve", lambda e: e.tensor_tensor(out=kdT[:, m, t0:t0 + n], in0=pap,
                                                                in1=E3[:, m, t0:t0 + n], op=ALU.mult),
                               [pkey, "xt_E3"], ["hb_kdT"])
                        gemm_ws(wv, wk, 32, 256, hT, "hT", groups, ev_k, [0, 1, 2, 3])
                        for ti, (t0, rows) in enumerate(tiles):
                            b = 4 + ti % 2
                            pv = ps[b][:].bitcast(BF16)

                            def trk(e, t0=t0, rows=rows, pv=pv):
                                for dc in range(2):
                                    ins = e.transpose(out=pv[0:rows, dc * 128:(dc + 1) * 128], in_=kdT[:, dc, t0:t0 + rows],
                                                      identity=ident_b[:])
                                return ins
                            op("pe", trk, ["hb_kdT", "ident_b"], [PS(b)])
                            op("act", lambda e, ti=ti, rows=rows, pv=pv: e.activation(
                                out=kd[0:rows, ti, :], in_=pv[0:rows, 0:256], func=AF.Copy), [PS(b)], ["kd"])
                        for hf in range(2):
                            wv0, wk0 = wnext()

                            def ev_v(ti, t0, rows, pap, pkey, hf=hf):
                                op("act", lambda e: e.activation(out=vh[0:rows, ti, hf * 256:(hf + 1) * 256], in_=pap, func=AF.Copy),
                                   [pkey], ["U1"])
                            gemm_as([(wv0, 0)], [wk0], hT, "hT", tiles, 256, ev_v, [0, 1, 2, 3])
                        if main:
                            dma("sp", ggl[:], gla_norm_g[h * 512:(h + 1) * 512].partition_broadcast(128), writes=["ggl"])
                            for hf in range(2):
                                wv0, wk0 = wnext()

                                def ev_r(ti, t0, rows, pap, pkey, hf=hf):
                                    op("act", lambda e: e.activation(out=grf[0:rows, 0:256], in_=pap, func=AF.Silu), [pkey], ["of"])
                                    op("dve", lambda e: e.tensor_tensor(out=gr[0:rows, ti, hf * 256:(hf + 1) * 256], in0=grf[0:rows, 0:256],
                                                                        in1=ggl[0:rows, hf * 256:(hf + 1) * 256], op=ALU.mult),
                                       ["of", "ggl"], ["U1"])
                                gemm_as([(wv0, 0)], [wk0], hT, "hT", tiles, 256, ev_r, [0, 1, 2, 3])
                        if main:
                            dma("sp", Sf[:], s_state[h].rearrange("(c p) v -> p c v", p=128), reads=["s_state%d" % h], writes=["Sf"])
                        else:
                            op("pool", lambda e: e.memset(Sf[:], 0.0), [], ["Sf"])
                        op("act", lambda e: e.activation(out=Sb[:], in_=Sf[:], func=AF.Copy), ["Sf"], ["Sb"])
                        if main:
                            sample_pre(h)
                        for n_ in range(8):
                            t0 = n_ * 128
                            if main:
                                def mm_s(e, t0=t0):
                                    for dc in range(2):
                                        ins = e.matmul(ps[4][:, 0:128], lhsT=keT[:, dc, t0:t0 + 128], rhs=qeT[:, dc, t0:t0 + 128],
                                                       start=(dc == 0), stop=(dc == 1))
                                    return ins
                                op("pe", mm_s, ["keT", "qeT"], [PS(4)])
                                op("dve", lambda e: e.tensor_tensor(out=sT[:], in0=ps[4][:, 0:128], in1=cst[:, C_MU:C_MU + 128],
                                                                    op=ALU.mult), [PS(4), "cst"], ["hb_sT"])

                                def mm_o(e, t0=t0, n_=n_):
                                    e.matmul(ps[5][:, :], lhsT=sT[:], rhs=vh[:, n_, :], start=True, stop=False)
                                    for dc in range(2):
                                        ins = e.matmul(ps[5][:, :], lhsT=qeT[:, dc, t0:t0 + 128], rhs=Sb[:, dc, :],
                                                       start=False, stop=(dc == 1))
                                    return ins
                                op("pe", mm_o, ["hb_sT", "U1", "qeT", "Sb"], [PS(5)])
                                finish_o(h, n_, 128, t0)
                            for dc in range(2):
                                b = 6 + dc
                                op("pe", lambda e, dc=dc, b=b, n_=n_: e.matmul(
                                    ps[b][:, :], lhsT=kd[:, n_, dc * 128:(dc + 1) * 128], rhs=vh[:, n_, :], start=True, stop=True),
                                   ["kd", "U1"], [PS(b)])
                                op("dve", lambda e, dc=dc, b=b, n_=n_: e.scalar_tensor_tensor(
                                    out=Sf[:, dc, :], in0=Sf[:, dc, :], scalar=dec[:, dc, n_:n_ + 1], in1=ps[b][:, :],
                                    op0=ALU.mult, op1=ALU.add), ["Sf", "dec", PS(b)], ["Sf"])
                            if n_ < 7:
                                op("act", lambda e: e.activation(out=Sb[:], in_=Sf[:], func=AF.Copy), ["Sf"], ["Sb"])
                            if main:
                                sample_seq(h, 2 * n_)
                                sample_seq(h, 2 * n_ + 1)
                        if main:
                            dma("sp", gla_p[h].rearrange("(c p) v -> p c v", p=128), Sf[:], reads=["Sf"])
                            sample_post(h)
                        else:
                            dma("sp", s_state[h].rearrange("(c p) v -> p c v", p=128), Sf[:], reads=["Sf"],
                                writes=["s_state%d" % h])

                def finish_o(h, ti, rows, t0, ob=5):
                    op("dve", lambda e: e.bn_stats(out=st6[0:rows, 0:6], in_=ps[ob][0:rows, :]), [PS(ob)], ["st6"])
                    op("dve", lambda e: e.bn_aggr(out=st6[0:rows, 6:8], in_=st6[0:rows, 0:6]), ["st6"], ["st6"])
                    op("act", lambda e: e.activation(out=st6[0:rows, 0:1], in_=st6[0:rows, 7:8], func=AF.Sqrt, bias=EPS),
                       ["st6"], ["st6"])
                    op("dve", lambda e: e.reciprocal(out=st6[0:rows, 1:2], in_=st6[0:rows, 0:1]), ["st6"], ["st6"])
                    op("dve", lambda e: e.tensor_scalar(out=of[0:rows, :], in0=ps[ob][0:rows, :], scalar1=st6[0:rows, 6:7],
                                                        scalar2=st6[0:rows, 1:2], op0=ALU.subtract, op1=ALU.mult),
                       [PS(ob), "st6"], ["of"])
                    op("dve", lambda e: e.tensor_tensor(out=og[0:rows, :], in0=of[0:rows, :], in1=gr[0:rows, ti, :], op=ALU.mult),
                       ["of", "U1"], ["hb_og"])
                    pv = ps[4][:].bitcast(BF16).rearrange("p (a b) -> p a b", a=8)

                    def tr(e):
                        for j in range(4):
                            ins = e.transpose(out=pv[:, j, 0:rows], in_=og[0:rows, j * 128:(j + 1) * 128],
                                              identity=ident_b[0:rows, 0:rows])
                        return ins
                    op("pe", tr, ["hb_og", "ident_b"], [PS(4)])
                    op("act", lambda e: e.activation(out=ogT[:, :, 0:rows], in_=pv[:, 0:4, 0:rows], func=AF.Copy), [PS(4)], ["hb_ogT"])
                    dma("sp", s_ogT[h * 512:(h + 1) * 512, t0:t0 + rows].rearrange("(c p) t -> p c t", p=128),
                        ogT[:, :, 0:rows], reads=["hb_ogT"], writes=["s_ogT"])

                sTs = hb[:, 3328:3392]

                def sample_pre(h):
                    def mm_s(e):
                        for dc in range(2):
                            ins = e.matmul(ps[0][0:64, 0:64], lhsT=keT[:, dc, NP:NT], rhs=qeT[:, dc, NP:NT],
                                           start=(dc == 0), stop=(dc == 1))
                        return ins
                    op("pe", mm_s, ["keT", "qeT"], [PS(0)])
                    op("dve", lambda e: e.tensor_tensor(out=sTs[0:64, 0:64], in0=ps[0][0:64, 0:64], in1=cst[0:64, C_MS:C_MS + 64],
                                                        op=ALU.mult), [PS(0), "cst"], ["hb_sTs"])

                def sample_seq(h, sq):
                    op("dve", lambda e, sq=sq: e.tensor_tensor(
                        out=QMs[:], in0=qeT[:, :, NP:NT], in1=bm_b[:, sq, :].unsqueeze(1).to_broadcast([128, 2, 64]),
                        op=ALU.mult), ["qeT", "bm_b"], ["QMs"])
                    op("dve", lambda e, sq=sq: e.tensor_scalar(out=kdm[:], in0=kd[0:64, 8, :],
                                                                scalar1=cst[0:64, C_RM + sq:C_RM + sq + 1], scalar2=None,
                                                                op0=ALU.mult), ["kd", "cst"], ["kdm"])
                    for dc in range(2):
                        bi = dc
                        src = st_gla[sq, h, dc * 128:(dc + 1) * 128, :]
                        dma("sp", s0f[bi][:], src, writes=["s0f%d" % bi])
                        dma("pool", s0b[bi][:], src, writes=["s0b%d" % bi])

                        def mm_o(e, sq=sq, dc=dc, bi=bi):
                            if sq == 0 and dc == 0:
                                e.matmul(ps[1][0:64, :], lhsT=sTs[0:64, 0:64], rhs=vh[0:64, 8, :], start=True, stop=False)
                            return e.matmul(ps[1][0:64, :], lhsT=QMs[:, dc, :], rhs=s0b[bi][:, :],
                                            start=False, stop=(sq == NSEQ - 1 and dc == 1))
                        op("pe", mm_o, ["hb_sTs", "U1", "QMs", "s0b%d" % bi], [PS(1)])
                        b = 2 + dc
                        op("pe", lambda e, dc=dc, b=b: e.matmul(ps[b][:, :], lhsT=kdm[:, dc * 128:(dc + 1) * 128],
                                                                 rhs=vh[0:64, 8, :], start=True, stop=True),
                           ["kdm", "U1"], [PS(b)])
                        op("dve", lambda e, dc=dc, b=b, sq=sq, bi=bi: e.scalar_tensor_tensor(
                            out=sout[bi][:, :], in0=s0f[bi][:, :], scalar=dec[:, dc, 8 + sq:9 + sq], in1=ps[b][:, :],
                            op0=ALU.mult, op1=ALU.add), ["s0f%d" % bi, "dec", PS(b)], ["sout%d" % bi])
                        dma("sp", gla_s[sq, h, dc * 128:(dc + 1) * 128, :], sout[bi][:], reads=["sout%d" % bi])

                def sample_post(h):
                    finish_o(h, 8, 64, NP, ob=1)

                stage_end(0)
                gla_pass(False)
                stage_end(1)
                gla_pass(True)
                stage_end(2)

            with contextlib.ExitStack() as mg_es:
                ogTa = sb("ogTa", [128, 16, NT], BF16, mg_es)
                sga = sb("sga", [128, 512], F32, mg_es)
                mo = sb("mo", [128, 2, NT], BF16, mg_es)
                dma("sp", ogTa[:], s_ogT.rearrange("(c p) t -> p c t", p=128), reads=["s_ogT"], writes=["ogTa"])
                sgs = sb("sgs", [128, 2, NT], BF16, mg_es)
                for m in range(16):
                    wga, kga = wnext()

                    def ev_ga(mm_, gi, t0, n, pap, pkey):
                        op("act", lambda e: e.activation(out=sgs[:, mm_, t0:t0 + n], in_=pap, func=AF.Sigmoid), [pkey], ["sgs"])
                    gemm_ws(wga, kga, 32, 256, hT, "hT", NGRP, ev_ga, [0, 1, 2, 3])
                    wba, kba = wnext()

                    def ev_ba(mm_, gi, t0, n, pap, pkey):
                        op("dve", lambda e: e.tensor_tensor(out=mo[:, mm_, t0:t0 + n], in0=pap, in1=sgs[:, mm_, t0:t0 + n], op=ALU.mult),
                           [pkey, "sgs"], ["mo"])
                    gemm_ws(wba, kba, 16, 256, ogTa, "ogTa", NGRP, ev_ba, [4, 5, 6, 7])
                    dma("sp", s_mT[m * 256:(m + 1) * 256, :].rearrange("(c p) t -> p c t", p=128), mo[:], reads=["mo"],
                        writes=["s_mT"])

            stage_end(3)
            cv_es = contextlib.ExitStack()
            with cv_es:
                cT = sb("cT", [128, 16, NT], BF16, cv_es)
                cpar = sb("cpar", [128, 16, 34], F32, cv_es)
                extP = sb("extP", [128, 30 + NP], F32, cv_es)
                extS = sb("extS", [128, 16, 34], F32, cv_es)
                sig = xt[:, 3136:3648]
                acc = xt[:, 2048:2048 + NT]
                stc = sb("stc", [120, 4, 128], F32, cv_es)
                cvp_tm = sb("cvp_tm", [30, 128], F32, cv_es)
                cvs_tm = sb("cvs_tm", [64, 128], F32, cv_es)
                cpl = xt[0:34, 0:2048]
                dma("sp", cpl, convp_in, writes=[*XTK])
                for j in range(16):
                    b = 4 + j % 2
                    op("pe", lambda e, j=j, b=b: e.transpose(out=ps[b][:, 0:34], in_=xt[0:34, j * 128:(j + 1) * 128],
                                                             identity=ident_f[0:34, 0:34]), [*XTK, "cst"], [PS(b)])
                    op("dve", lambda e, j=j, b=b: e.tensor_copy(out=cpar[:, j, :], in_=ps[b][:, 0:34]), [PS(b)], ["cpar"])
                dma("sp", conv_s[:, 0:26, :], st_conv[:, 4:30, :])
                ugroups = [(0, 512), (512, 512), (1024, 64)]
                uscope = contextlib.ExitStack()
                u1s = sb("u1s", [128, 2, 32 + NT], F32, uscope)
                hbf32 = hb[:].bitcast(F32)
                extPs = [extP[:, :], hbf32[:, 0:30 + NP]]
                extSs = [extS[:, :, :], hbf32[:, 1056:1056 + 544].rearrange("p (s r) -> p s r", r=34)]
                extPk = ["extP", "extP1"]
                extSk = ["extS", "extS1"]
                sigs = [xt[:, 3136:3648], xt[:, 1088:1600]]
                sigk = ["sig0", "sig1"]
                accs = [xt[:, 2048:2048 + NT], xt[:, 0:NT]]
                acck = ["acc0", "acc1"]
                CBAR = [*XTK, *HBK, "extP1", "extS1", "sig0", "sig1", "acc0", "acc1"]
                op("pool", lambda e: e.memset(sm[:, 63:64], 0.0), [], CBAR)
                pending_tail = [None]

                def make_tail(j, par):
                    extPc, extSc, sigc = extPs[par], extSs[par], sigs[par]

                    def tail():
                        op("pe", lambda e: e.transpose(out=ps[7][0:30, 0:128], in_=extPc[:, NP:NP + 30], identity=ident_f),
                           [extPk[par], "cst"], [PS(7)])
                        op("act", lambda e: e.activation(out=cvp_tm[:, :], in_=ps[7][0:30, 0:128], func=AF.Copy), [PS(7)], ["cvp_tm"])
                        dma("sp", conv_p[:, j * 128:(j + 1) * 128], cvp_tm[:, :], reads=["cvp_tm"])
                        op("pool", lambda e: e.tensor_copy(out=sigc[:, 0:64].rearrange("p (s r) -> p s r", r=4), in_=extSc[:, :, 30:34]),
                           [extSk[par]], [sigk[par]])
                        op("pe", lambda e: e.transpose(out=ps[7][0:64, 128:256], in_=sigc[:, 0:64], identity=ident_f),
                           [sigk[par], "cst"], [PS(7)])
                        op("act", lambda e: e.activation(out=cvs_tm[:, :], in_=ps[7][0:64, 128:256], func=AF.Copy), [PS(7)], ["cvs_tm"])
                        for sq in range(NSEQ):
                            dma("sp", conv_s[sq, 26:30, j * 128:(j + 1) * 128], cvs_tm[4 * sq:4 * sq + 4, :], reads=["cvs_tm"])
                    return tail

                for m in range(8):
                    wu1, k1 = wnext()
                    for jj in range(2):
                        for gi, (t0, n) in enumerate([(-32, 32)] + ugroups):
                            src = hT_halo if t0 < 0 else hT
                            skey = "hT_halo" if t0 < 0 else "hT"
                            a0 = 0 if t0 < 0 else t0
                            b1 = (jj * 4 + gi) % 4

                            def mmu1(e, b=b1, a0=a0, n=n, src=src, jj=jj):
                                for k in range(32):
                                    ins = e.matmul(ps[b][:, 0:n], lhsT=wu1[:, k, jj * 128:(jj + 1) * 128], rhs=src[:, k, a0:a0 + n],
                                                   start=(k == 0), stop=(k == 31))
                                return ins
                            op("pe", mmu1, [k1, skey], [PS(b1)])
                            op("act", lambda e, b1=b1, jj=jj, t0=t0, n=n: e.activation(out=u1s[:, jj, 32 + t0:32 + t0 + n], in_=ps[b1][:, 0:n],
                                                                                        func=AF.Copy), [PS(b1)], ["u1s"])
                    wu2, k2 = wnext()
                    for jj in range(2):
                        j = 2 * m + jj
                        par = jj
                        extPc, extSc, sigc, accc = extPs[par], extSs[par], sigs[par], accs[par]
                        kP, kS, kG, kA = extPk[par], extSk[par], sigk[par], acck[par]
                        for g4 in range(4):
                            dma("sp", stc[:, g4, :], st_conv[4 * g4:4 * g4 + 4, :, j * 128:(j + 1) * 128].rearrange("s r c -> (s r) c"),
                                writes=["stc"])
                        for g4 in range(4):
                            op("pe", lambda e, g4=g4: e.transpose(out=ps[6][:, g4 * 120:(g4 + 1) * 120], in_=stc[:, g4, :],
                                                                  identity=ident_f[0:120, 0:120]), ["stc", "cst"], [PS(6)])
                        op("dve", lambda e: e.tensor_copy(out=extSc[:, :, 0:30],
                                                          in_=ps[6][:, 0:480].rearrange("p (s r) -> p s r", r=30)),
                           [PS(6)], [kS])
                        for gi, (t0, n) in enumerate([(-32, 32)] + ugroups):
                            src = hT_halo if t0 < 0 else hT
                            skey = "hT_halo" if t0 < 0 else "hT"
                            a0 = 0 if t0 < 0 else t0
                            b2 = gi % 4

                            def mmu(e, wv, b, a0=a0, n=n, src=src, jj=jj):
                                for k in range(32):
                                    ins = e.matmul(ps[b][:, 0:n], lhsT=wv[:, k, jj * 128:(jj + 1) * 128], rhs=src[:, k, a0:a0 + n],
                                                   start=(k == 0), stop=(k == 31))
                                return ins
                            op("pe", lambda e, b2=b2, f=mmu: f(e, wu2, b2), [k2, skey], [PS(b2)])
                            op("act", lambda e, b2=b2, n=n: e.activation(out=sigc[:, 0:n], in_=ps[b2][:, 0:n], func=AF.Sigmoid),
                               [PS(b2)], [kG])
                            if t0 < 0:
                                dst = extPc[:, 0:30]
                                i0 = u1s[:, jj, 2:32]
                                i1 = sigc[:, 2:32]
                                wk_ = kP
                            elif t0 < NP:
                                dst = extPc[:, 30 + t0:30 + t0 + n]
                                i0 = u1s[:, jj, 32 + t0:32 + t0 + n]
                                i1 = sigc[:, 0:n]
                                wk_ = kP
                            else:
                                dst = extSc[:, :, 30:34]
                                i0 = u1s[:, jj, 32 + NP:32 + NT].rearrange("p (s r) -> p s r", r=4)
                                i1 = sigc[:, 0:64].rearrange("p (s r) -> p s r", r=4)
                                wk_ = kS
                            op("dve", lambda e, dst=dst, i0=i0, i1=i1: e.tensor_tensor(out=dst, in0=i0, in1=i1, op=ALU.mult),
                               ["u1s", kG], [wk_])
                        if pending_tail[0] is not None:
                            pending_tail[0]()
                        pending_tail[0] = make_tail(j, par)
                        accS = accc[:, NP:NT].rearrange("p (s r) -> p s r", r=4)
                        op("dve", lambda e, j=j: e.tensor_scalar(out=accc[:, 0:NP], in0=extPc[:, 0:NP], scalar1=cpar[:, j, 0:1],
                                                                  scalar2=cpar[:, j, 31:32], op0=ALU.mult, op1=ALU.add),
                           [kP, "cpar"], [kA])
                        op("dve", lambda e, j=j: e.tensor_scalar(out=accS, in0=extSc[:, :, 0:4], scalar1=cpar[:, j, 0:1],
                                                                  scalar2=cpar[:, j, 31:32], op0=ALU.mult, op1=ALU.add),
                           [kS, "cpar"], [kA])
                        for tp in range(1, 31):
                            op("dve", lambda e, j=j, tp=tp: e.scalar_tensor_tensor(
                                out=accc[:, 0:NP], in0=extPc[:, tp:tp + NP], scalar=cpar[:, j, tp:tp + 1], in1=accc[:, 0:NP],
                                op0=ALU.mult, op1=ALU.add), [kP, "cpar", kA], [kA])
                            op("dve", lambda e, j=j, tp=tp: e.scalar_tensor_tensor(
                                out=accS, in0=extSc[:, :, tp:tp + 4], scalar=cpar[:, j, tp:tp + 1], in1=accS,
                                op0=ALU.mult, op1=ALU.add), [kS, "cpar", kA], [kA])
                        op("act", lambda e, j=j: e.activation(out=cT[:, j, :], in_=accc[:, :], func=AF.Copy), [kA], ["cT"])
                pending_tail[0]()
                op("pool", lambda e: e.memset(sm[:, 63:64], 0.0), [], CBAR)
                uscope.close()
                with contextlib.ExitStack() as ln_es:
                    sq_t = sb("sq_t", [128, 512], BF16, ln_es)
                    mu = sb("mu", [128, 512], F32, ln_es)
                    rs = sb("rs", [128, 512], F32, ln_es)
                    tmpf = sb("tmpf", [128, 512], F32, ln_es)
                    for gi, (t0, n) in enumerate(NGRP):
                        def mm1(e, t0=t0, n=n):
                            for j in range(16):
                                ins = e.matmul(ps[0][:, 0:n], lhsT=ones_b[:], rhs=cT[:, j, t0:t0 + n], start=(j == 0), stop=(j == 15))
                            return ins
                        op("pe", mm1, ["ones_b", "cT"], [PS(0)])
                        for j in range(16):
                            op("dve", lambda e, j=j, t0=t0, n=n: e.tensor_tensor(out=sq_t[:, 0:n], in0=cT[:, j, t0:t0 + n],
                                                                                  in1=cT[:, j, t0:t0 + n], op=ALU.mult),
                               ["cT"], ["sq_t"])
                            op("pe", lambda e, j=j, n=n: e.matmul(ps[1][:, 0:n], lhsT=ones_b[:], rhs=sq_t[:, 0:n],
                                                                  start=(j == 0), stop=(j == 15)), ["ones_b", "sq_t"], [PS(1)])
                        op("act", lambda e, n=n: e.activation(out=mu[:, 0:n], in_=ps[0][:, 0:n], func=AF.Copy, scale=1.0 / 2048),
                           [PS(0)], ["mu"])
                        op("dve", lambda e, n=n: e.tensor_tensor(out=tmpf[:, 0:n], in0=mu[:, 0:n], in1=mu[:, 0:n], op=ALU.mult),
                           ["mu"], ["tmpf"])
                        op("dve", lambda e, n=n: e.scalar_tensor_tensor(out=rs[:, 0:n], in0=ps[1][:, 0:n], scalar=1.0 / 2048,
                                                                         in1=tmpf[:, 0:n], op0=ALU.mult, op1=ALU.subtract),
                           [PS(1), "tmpf"], ["rs"])
                        op("act", lambda e, n=n: e.activation(out=rs[:, 0:n], in_=rs[:, 0:n], func=AF.Sqrt, bias=EPS), ["rs"], ["rs"])
                        op("dve", lambda e, n=n: e.reciprocal(out=rs[:, 0:n], in_=rs[:, 0:n]), ["rs"], ["rs"])
                        for j in range(16):
                            op("dve", lambda e, j=j, t0=t0, n=n: e.tensor_tensor(out=tmpf[:, 0:n], in0=cT[:, j, t0:t0 + n],
                                                                                  in1=mu[:, 0:n], op=ALU.subtract),
                               ["cT", "mu"], ["tmpf"])
                            op("dve", lambda e, n=n: e.tensor_tensor(out=tmpf[:, 0:n], in0=tmpf[:, 0:n], in1=rs[:, 0:n], op=ALU.mult),
                               ["tmpf", "rs"], ["tmpf"])
                            op("act", lambda e, j=j, t0=t0, n=n: e.activation(out=cT[:, j, t0:t0 + n], in_=tmpf[:, 0:n], func=AF.Silu,
                                                                               scale=cpar[:, j, 32:33], bias=cpar[:, j, 33:34]),
                               ["tmpf", "cpar"], ["cT"])
                stage_end(4)
                with contextlib.ExitStack() as mg_es:
                    xtb2 = xt[:].bitcast(BF16)
                    mab = xtb2[:, 0:2 * NT].rearrange("p (c t) -> p c t", c=2)
                    mo = xtb2[:, 2 * NT:4 * NT].rearrange("p (c t) -> p c t", c=2)
                    sgs2 = xt[:, 2304:2304 + NT].bitcast(BF16).rearrange("p (c t) -> p c t", c=2)
                    for m in range(16):
                        wgb, kgb = wnext()
                        dma("sp", mab[:], s_mT[m * 256:(m + 1) * 256, :].rearrange("(c p) t -> p c t", p=128), reads=["s_mT"],
                            writes=["xt_E1"])

                        def ev_gb(mm_, gi, t0, n, pap, pkey):
                            op("act", lambda e: e.activation(out=sgs2[:, mm_, t0:t0 + n], in_=pap, func=AF.Sigmoid), [pkey], ["xt_E3"])
                        gemm_ws(wgb, kgb, 32, 256, hT, "hT", NGRP, ev_gb, [0, 1, 2, 3])
                        wbb, kbb = wnext()

                        def ev_bb(mm_, gi, t0, n, pap, pkey):
                            op("dve", lambda e: e.tensor_tensor(out=sgs2[:, mm_, t0:t0 + n], in0=pap, in1=sgs2[:, mm_, t0:t0 + n], op=ALU.mult),
                               [pkey, "xt_E3"], ["xt_E3"])
                            op("dve", lambda e: e.tensor_tensor(out=mo[:, mm_, t0:t0 + n], in0=sgs2[:, mm_, t0:t0 + n], in1=mab[:, mm_, t0:t0 + n],
                                                                op=ALU.add), ["xt_E3", "xt_E1"], ["xt_E2"])
                        gemm_ws(wbb, kbb, 16, 256, cT, "cT", NGRP, ev_bb, [4, 5, 6, 7])
                        dma("sp", s_mT[m * 256:(m + 1) * 256, :].rearrange("(c p) t -> p c t", p=128), mo[:], reads=["xt_E2", "xt_E1"],
                            writes=["s_mT"])

        stage_end(5)
        late = contextlib.ExitStack()
        with late:
            xres = [sb("xres%d" % i, [128, 512], F32, late) for i in range(3)]
            dma("sp", hT[:], s_mT.rearrange("(c p) t -> p c t", p=128), reads=["s_mT"], writes=["hT"])
            rcount = [0]

            def resid_gemm(wview, wkey, actT, act_key, src_fn, src_key, dst, c0, ncols, dst_key):
                def ev(ti, t0, rows, pap, pkey):
                    bi = rcount[0] % 3
                    rcount[0] += 1
                    dma("sp", xres[bi][0:rows, 0:ncols], src_fn(t0, rows, c0, ncols), reads=[src_key], writes=["xres%d" % bi])
                    op("dve", lambda e: e.tensor_tensor(out=xres[bi][0:rows, 0:ncols], in0=pap, in1=xres[bi][0:rows, 0:ncols], op=ALU.add),
                       [pkey, "xres%d" % bi], ["xres%d" % bi])
                    dma("sp", dst[t0:t0 + rows, c0:c0 + ncols], xres[bi][0:rows, 0:ncols], reads=["xres%d" % bi],
                        writes=[dst_key])
                gemm_as([(wview, 0)], [wkey], actT, act_key, TILES, ncols, ev, [0, 1, 2, 3])

            def xsrc(t0, rows, c0, ncols):
                if t0 < NP:
                    return x_main[t0:t0 + rows, c0:c0 + ncols]
                return x_s[:, c0:c0 + ncols]
            for cb in range(16):
                w0, k0 = wnext()
                resid_gemm(w0, k0, hT, "hT", xsrc, "x_in", s_x1, cb * 256, 256, "s_x1")

            stage_end(6)
            ca = contextlib.ExitStack()
            with ca:
                mvb = sb("mvb", [128, 2, 1024], BF16, ca)
                mkT = sb("mkT", [128, 8, 256], BF16, ca)
                q2T = sb("q2T", [128, 8, NT], BF16, ca)
                oT = q2T
                kvf = sb("kvf", [128, 512], F32, ca)
                mscope = contextlib.ExitStack()
                mT = sb("mT", [128, 32, 256], BF16, mscope)
                load_gain(norm_mem_g)
                for i in range(2):
                    norm_tile(mem[i * 128:(i + 1) * 128, :], 128, i * 128, mT, "mT")
                stage_end(6.05)
                for which, dst_o in ((0, mk_o), (1, mv_o)):
                    for cb in range(4):
                        w0, k0 = wnext()

                        def ev_kv(ti, t0, rows, pap, pkey, which=which, cb=cb, dst_o=dst_o):
                            kb = "kvf%d" % (ti % 2)
                            kv_ = kvf[:, (ti % 2) * 256:(ti % 2) * 256 + 256]
                            op("act", lambda e: e.activation(out=kv_, in_=pap, func=AF.Copy), [pkey], [kb])
                            if which == 1:
                                op("dve", lambda e: e.tensor_copy(out=mvb[:, ti, cb * 256:(cb + 1) * 256], in_=kv_), [kb], ["mvb"])
                            dma("sp", dst_o[t0:t0 + 128, cb * 256:(cb + 1) * 256], kv_, reads=[kb])
                        gemm_as([(w0, 0)], [k0], mT, "mT", [(0, 128), (128, 128)], 256, ev_kv, [0, 1, 2, 3])
                stage_end(6.1)
                for m in range(4):
                    wv, wk = wnext()

                    def ev_mk(mm_, gi, t0, n, pap, pkey, m=m):
                        op("act", lambda e: e.activation(out=mkT[:, 2 * m + mm_, :], in_=pap, func=AF.Copy), [pkey], ["mkT"])
                    gemm_ws(wv, wk, 32, 256, mT, "mT", [(0, 256)], ev_mk, [0, 1, 2, 3])
                stage_end(6.2)
                mscope.close()
                load_gain(norm_ca_g)
                for ti, (t0, rows) in enumerate(TILES):
                    norm_tile(s_x1[t0:t0 + rows, :], rows, t0, hT, "hT", src_reads=["s_x1"])
                for m in range(4):
                    wv, wk = wnext()

                    def ev_q2(mm_, gi, t0, n, pap, pkey, m=m):
                        op("act", lambda e: e.activation(out=q2T[:, 2 * m + mm_, t0:t0 + n], in_=pap, func=AF.Copy), [pkey], ["q2T"])
                    gemm_ws(wv, wk, 32, 256, hT, "hT", NGRP, ev_q2, [0, 1, 2, 3])

                stage_end(6.4)
                at = contextlib.ExitStack()
                with at:
                    pex = sb("pex", [128, 4, 256], F32, at)
                    pbf = sb("pbf", [128, 4, 256], BF16, at)
                    pT = hb[:].rearrange("p (s h t) -> p s h t", s=2, h=4)
                    mx = sb("mx", [128, 16], F32, at)
                    xtb3 = xt[:].bitcast(BF16)
                    kc = [xtb3[:, i * 2048:(i + 1) * 2048].rearrange("p (c d) -> p c d", c=2) for i in range(2)]
                    vc = [xtb3[:, 4096 + i * 2048:4096 + (i + 1) * 2048].rearrange("p (c d) -> p c d", c=2) for i in range(2)]
                    kTs = sb("kTs", [128, 8, 256], BF16, at)
                    Q2M = sb("Q2M", [128, 8, 64], BF16, at)
                    pTm = sb("pTm", [128, 2, 4, 64], BF16, at)
                    osb = sb("osb", [64, 1024], BF16, at)

                    def softmax_rows(rows, banks):
                        for h in range(4):
                            sc = ps[banks[h // 2]][0:rows, (h % 2) * 256:(h % 2) * 256 + 256]
                            op("dve", lambda e, h=h, sc=sc: e.reduce_max(out=mx[0:rows, h:h + 1], in_=sc, axis=AX.X),
                               [PS(banks[h // 2])], ["mx"])
                        op("dve", lambda e: e.tensor_scalar(out=mx[0:rows, 4:8], in0=mx[0:rows, 0:4], scalar1=-1.0 / 16.0,
                                                            scalar2=None, op0=ALU.mult), ["mx"], ["mx"])
                        for h in range(4):
                            sc = ps[banks[h // 2]][0:rows, (h % 2) * 256:(h % 2) * 256 + 256]
                            op("act", lambda e, h=h, sc=sc: e.activation(out=pex[0:rows, h, :], in_=sc, func=AF.Exp, scale=1.0 / 16.0,
                                                                          bias=mx[0:rows, 4 + h:5 + h], accum_out=mx[0:rows, 8 + h:9 + h]),
                               [PS(banks[h // 2]), "mx"], ["pex", "mx"])
                        op("dve", lambda e: e.reciprocal(out=mx[0:rows, 12:16], in_=mx[0:rows, 8:12]), ["mx"], ["mx"])
                        for h in range(4):
                            op("dve", lambda e, h=h: e.tensor_scalar(out=pbf[0:rows, h, :], in0=pex[0:rows, h, :],
                                                                      scalar1=mx[0:rows, 12 + h:13 + h], scalar2=None, op0=ALU.mult),
                               ["pex", "mx"], ["pbf"])

                    for gq in range(2):
                        for tl in range(4):
                            t0 = gq * 512 + tl * 128

                            def mm_sc(e, t0=t0):
                                for h in range(4):
                                    for dc in range(2):
                                        ins = e.matmul(ps[h // 2][:, (h % 2) * 256:(h % 2) * 256 + 256], lhsT=q2T[:, 2 * h + dc, t0:t0 + 128],
                                                       rhs=mkT[:, 2 * h + dc, :], start=(dc == 0), stop=(dc == 1))
                                return ins
                            op("pe", mm_sc, ["q2T", "mkT"], [PS(0), PS(1)])
                            softmax_rows(128, [0, 1])
                            pv = ps[2][:].bitcast(BF16).rearrange("p (a b) -> p a b", a=8)

                            def trp(e):
                                for h in range(4):
                                    for sc_ in range(2):
                                        ins = e.transpose(out=pv[:, sc_ * 4 + h, :], in_=pbf[:, h, sc_ * 128:(sc_ + 1) * 128], identity=ident_b[:])
                                return ins
                            op("pe", trp, ["pbf", "ident_b"], [PS(2)])
                            op("act", lambda e, tl=tl: e.activation(out=pT[:, :, :, tl * 128:(tl + 1) * 128],
                                                                     in_=pv.rearrange("p (s h) t -> p s h t", s=2), func=AF.Copy),
                               [PS(2)], ["hb_kdT"])
                        for h in range(4):
                            for dc in range(2):
                                b = 4 + (h * 2 + dc) % 4

                                def mm_ov(e, h=h, dc=dc, b=b):
                                    for sc_ in range(2):
                                        ins = e.matmul(ps[b][:, :], lhsT=mvb[:, sc_, h * 256 + dc * 128:h * 256 + dc * 128 + 128],
                                                       rhs=pT[:, sc_, h, :], start=(sc_ == 0), stop=(sc_ == 1))
                                    return ins
                                op("pe", mm_ov, ["mvb", "hb_kdT"], [PS(b)])
                                op("act", lambda e, h=h, dc=dc, b=b, gq=gq: e.activation(
                                    out=oT[:, 2 * h + dc, gq * 512:(gq + 1) * 512], in_=ps[b][:, :], func=AF.Copy), [PS(b)], ["q2T"])
                    stage_end(6.6)
                    for c8 in range(8):
                        op("dve", lambda e, c8=c8: e.tensor_copy(out=Q2M[:, c8, :], in_=q2T[:, c8, NP:NT]), ["q2T"], ["Q2M"])
                    bmv = bm_b[:]
                    for sq in range(NSEQ):
                        bi = sq % 2
                        dma("pool", kc[bi][:], ck[sq].rearrange("(c p) d -> p c d", p=128), writes=[("xt_E1", "xt_E2")[bi]])
                        for half in range(2):
                            pv = ps[4 + half][:].bitcast(BF16).rearrange("p (a b) -> p a b", a=8)

                            def trk(e, half=half, pv=pv, bi=bi):
                                for c4 in range(4):
                                    c8 = half * 4 + c4
                                    for sc_ in range(2):
                                        ins = e.transpose(out=pv[:, c4 * 2 + sc_, :], in_=kc[bi][:, sc_, c8 * 128:(c8 + 1) * 128],
                                                          identity=ident_b[:])
                                return ins
                            op("pe", trk, [("xt_E1", "xt_E2")[bi], "ident_b"], [PS(4 + half)])
                            op("act", lambda e, half=half, pv=pv: e.activation(
                                out=kTs[:, half * 4:(half + 1) * 4, :].rearrange("p c (s t) -> p c s t", s=2),
                                in_=pv.rearrange("p (c s) t -> p c s t", s=2), func=AF.Copy), [PS(4 + half)], ["kTs"])
                        qm = sb if False else None

                        def mm_ss(e, sq=sq):
                            for h in range(4):
                                for dc in range(2):
                                    ins = e.matmul(ps[h][0:64, 0:256], lhsT=QMs[:, 2 * h + dc, :], rhs=kTs[:, 2 * h + dc, :],
                                                   start=(sq == 0 and dc == 0), stop=(sq == NSEQ - 1 and dc == 1))
                            return ins
                        QMs = sb("QMs%d" % sq, [128, 8, 64], BF16, at) if sq < 2 else QMs_l[sq % 2]
                        if sq < 2:
                            if sq == 0:
                                QMs_l = [QMs, None]
                            else:
                                QMs_l[1] = QMs
                        op("dve", lambda e, sq=sq, QMs=QMs: e.tensor_tensor(
                            out=QMs[:], in0=Q2M[:], in1=bmv[:, sq, :].unsqueeze(1).to_broadcast([128, 8, 64]), op=ALU.mult),
                           ["Q2M", "bm_b"], ["QMs%d" % (sq % 2)])
                        op("pe", mm_ss, ["QMs%d" % (sq % 2), "kTs"], [PS(0), PS(1), PS(2), PS(3)])
                    for h in range(4):
                        op("dve", lambda e, h=h: e.reduce_max(out=mx[0:64, h:h + 1], in_=ps[h][0:64, 0:256], axis=AX.X), [PS(h)], ["mx"])
                    op("dve", lambda e: e.tensor_scalar(out=mx[0:64, 4:8], in0=mx[0:64, 0:4], scalar1=-1.0 / 16.0, scalar2=None,
                                                        op0=ALU.mult), ["mx"], ["mx"])
                    for h in range(4):
                        op("act", lambda e, h=h: e.activation(out=pex[0:64, h, :], in_=ps[h][0:64, 0:256], func=AF.Exp, scale=1.0 / 16.0,
                                                               bias=mx[0:64, 4 + h:5 + h], accum_out=mx[0:64, 8 + h:9 + h]),
                           [PS(h), "mx"], ["pex", "mx"])
                    op("dve", lambda e: e.reciprocal(out=mx[0:64, 12:16], in_=mx[0:64, 8:12]), ["mx"], ["mx"])
                    for h in range(4):
                        op("dve", lambda e, h=h: e.tensor_scalar(out=pbf[0:64, h, :], in0=pex[0:64, h, :], scalar1=mx[0:64, 12 + h:13 + h],
                                                                  scalar2=None, op0=ALU.mult), ["pex", "mx"], ["pbf"])
                    pv = ps[4][:].bitcast(BF16).rearrange("p (a b) -> p a b", a=8)

                    def trps(e):
                        for h in range(4):
                            for sc_ in range(2):
                                ins = e.transpose(out=pv[:, sc_ * 4 + h, 0:64], in_=pbf[0:64, h, sc_ * 128:(sc_ + 1) * 128],
                                                  identity=ident_b[0:64, 0:64])
                        return ins
                    op("pe", trps, ["pbf", "ident_b"], [PS(4)])
                    pTs = sb("pTs", [128, 8, 64], BF16, at)
                    op("act", lambda e: e.activation(out=pTs[:], in_=pv[:, :, 0:64], func=AF.Copy), [PS(4)], ["pTs"])
                    for sq in range(NSEQ):
                        bi = sq % 2
                        dma("pool", vc[bi][:], cv[sq].rearrange("(c p) d -> p c d", p=128), writes=[("xt_E3", "xt")[bi]])
                        op("dve", lambda e, sq=sq: e.tensor_tensor(
                            out=pTm[:].rearrange("p s h t -> p (s h) t"), in0=pTs[:],
                            in1=bmv[:, sq, :].unsqueeze(1).to_broadcast([128, 8, 64]), op=ALU.mult), ["pTs", "bm_b"], ["pTm"])

                        def mm_os(e, sq=sq, bi=bi):
                            for h in range(4):
                                for sc_ in range(2):
                                    ins = e.matmul(ps[h][0:64, 0:256], lhsT=pTm[:, sc_, h, :], rhs=vc[bi][:, sc_, h * 256:(h + 1) * 256],
                                                   start=(sq == 0 and sc_ == 0), stop=(sq == NSEQ - 1 and sc_ == 1))
                            return ins
                        op("pe", mm_os, ["pTm", ("xt_E3", "xt")[bi]], [PS(0), PS(1), PS(2), PS(3)])
                    for h in range(4):
                        op("act", lambda e, h=h: e.activation(out=osb[:, h * 256:(h + 1) * 256], in_=ps[h][0:64, 0:256], func=AF.Copy),
                           [PS(h)], ["osb"])
                    pv = ps[5][:].bitcast(BF16).rearrange("p (a b) -> p a b", a=8)

                    def tro(e):
                        for c8 in range(8):
                            ins = e.transpose(out=pv[:, c8, 0:64], in_=osb[:, c8 * 128:(c8 + 1) * 128], identity=ident_b[0:64, 0:64])
                        return ins
                    op("pe", tro, ["osb", "ident_b"], [PS(5)])
                    op("act", lambda e: e.activation(out=oT[:, :, NP:NT], in_=pv[:, :, 0:64], func=AF.Copy), [PS(5)], ["q2T"])

                stage_end(6.8)
                def x1src(t0, rows, c0, ncols):
                    return s_x1[t0:t0 + rows, c0:c0 + ncols]
                for cb in range(4):
                    w0, k0 = wnext()
                    for sub in range(2):
                        resid_gemm(w0[:, :, sub * 512:(sub + 1) * 512], k0, oT, "q2T", x1src, "s_x1", s_x2, cb * 1024 + sub * 512, 512, "s_x2")

            stage_end(7)
            late.close()
            hscope.close()
            moe = contextlib.ExitStack()
            with moe:
                wsl.append(sb("wsl2", [128, 8192], BF16, moe))
                wsl.append(sb("wsl3", [128, 8192], BF16, moe))
                wr = sb("wr", [128, 32, 36], F32, moe)
                brt = sb("brt", [128, 36], F32, moe)
                xgTbuf = sb("xgTbuf", [128, D], F32, moe)
                hTf = xgTbuf[:].rearrange("p (k t) -> p k t", k=32)
                xgT = xgTbuf[:].bitcast(BF16).rearrange("p (k c) -> p k c", k=32)
                hbf = sb("hbf", [128, D], F32, moe)
                gbcf = sb("gbcf", [128, D], F32, moe)
                lg = sb("lg", [128, 9, 36], F32, moe)
                gate = sb("gate", [128, 9, 32], F32, moe)
                msk = sb("msk", [128, 9, 32], BF16, moe)
                m2f = sb("m2f", [128, 9, 32], F32, moe)
                rank = sb("rank", [128, 9, 32], F32, moe)
                tmp = sb("tmp", [128, 9, 40], F32, moe)
                rhs6 = sb("rhs6", [128, 9, NE, 6], BF16, moe)
                sel = sb("sel", [128, 9, CAP], BF16, moe)
                slot = sb("slot", [128, 2, 8], F32, moe)
                idxg = sb("idxg", [128, 2], U32, moe)
                idxs = sb("idxs", [128, 2], U32, moe)
                xg = sb("xg", [128, 2, D], BF16, moe)
                hidT = sb("hidT", [128, 4, CAP], BF16, moe)
                sgl = sb("sgl", [128, 2, CAP], F32, moe)
                ye = [sb("ye0", [128, D], F32, moe), hbf]
                yek = ["ye0", "hbf"]
                dma("sp", wr[:], w_router.rearrange("(k p) n -> p k n", p=128), writes=["wr"])
                dma("sp", brt[:], b_router.partition_broadcast(128), writes=["brt"])
                op("pool", lambda e: e.memset(hb[0:1, :], 0.0), [], [*HBK])
                dma("sp", s_h3[ZROW:ZROW + 1, :], hb[0:1, :], reads=[*HBK], writes=["s_h3"])
                dma("sp", gbcf[:], norm_ffn_g.partition_broadcast(128), writes=["gbcf"])

                for ti, (t0, rows) in enumerate(TILES):
                    def f32T(rows, ti=ti, t0=t0):
                        op("dve", lambda e: e.scalar_tensor_tensor(out=hbf[0:rows, :], in0=xt[0:rows, :], scalar=sm[0:rows, 2:3],
                                                                   in1=gbcf[0:rows, :], op0=ALU.mult, op1=ALU.mult),
                           [*XTK, "sm", "gbcf"], ["hbf"])
                        op("act", lambda e: e.activation(out=hb[0:rows, :], in_=hbf[0:rows, :], func=AF.Copy), ["hbf"], [*HBK])
                        dma("sp", s_h3[t0:t0 + rows, :], hb[0:rows, :], reads=[*HBK], writes=["s_h3"])
                        for g in range(8):
                            b = 4 + g % 4

                            def tr(e, g=g, b=b):
                                for j in range(4):
                                    c = g * 4 + j
                                    ins = e.transpose(out=ps[b][:, j * 128:j * 128 + rows], in_=hbf[0:rows, c * 128:(c + 1) * 128],
                                                      identity=ident_f[0:rows, 0:rows])
                                return ins
                            op("pe", tr, ["hbf", "cst"], [PS(b)])
                            op("act" if g % 2 == 0 else "dve",
                               (lambda e, g=g, b=b: e.activation(out=hTf[:, g * 4:(g + 1) * 4, 0:rows],
                                                                 in_=ps[b][:].rearrange("p (a t) -> p a t", a=4)[:, :, 0:rows], func=AF.Copy))
                               if g % 2 == 0 else
                               (lambda e, g=g, b=b: e.tensor_copy(out=hTf[:, g * 4:(g + 1) * 4, 0:rows],
                                                                  in_=ps[b][:].rearrange("p (a t) -> p a t", a=4)[:, :, 0:rows])),
                               [PS(b)], ["xgT"])

                        def mmr(e):
                            for k in range(32):
                                ins = e.matmul(ps[3][0:rows, 0:36], lhsT=hTf[:, k, 0:rows], rhs=wr[:, k, :], start=(k == 0), stop=(k == 31))
                            return ins
                        op("pe", mmr, ["xgT", "wr"], [PS(3)])
                        op("dve", lambda e: e.tensor_tensor(out=lg[0:rows, ti, :], in0=ps[3][0:rows, 0:36], in1=brt[0:rows, :], op=ALU.add),
                           [PS(3), "brt"], ["lg"])
                    norm_tile(s_x2[t0:t0 + rows, :], rows, t0, None, None, fp32_T=f32T, src_reads=["s_x2"])
                op("pool", lambda e: e.memset(lg[64:128, 8, :], -30000.0), [], ["lg"]) if False else None
                R_ = ["lg", "tmp", "gate", "msk", "m2f"]
                lgg = lg[:, :, 0:4]
                lge = lg[:, :, 4:36].rearrange("p t (g e) -> p t g e", g=4)
                T0, T1, T2, T3, T4, T5 = (tmp[:, :, i:i + 1] for i in range(6))
                gm = tmp[:, :, 8:12]
                op("dve", lambda e: e.tensor_reduce(out=tmp[:, :, 0:1], in_=lgg, axis=AX.X, op=ALU.max), ["lg"], ["tmp"])
                op("dve", lambda e: e.tensor_tensor(out=tmp[:, :, 12:16], in0=lgg, in1=T0.to_broadcast([128, 9, 4]), op=ALU.subtract),
                   ["lg", "tmp"], ["tmp"])
                op("dve", lambda e: e.tensor_single_scalar(out=gm, in_=tmp[:, :, 12:16], scalar=0.0, op=ALU.is_ge), ["tmp"], ["tmp"])
                op("act", lambda e: e.activation(out=tmp[:, :, 16:20], in_=tmp[:, :, 12:16], func=AF.Exp), ["tmp"], ["tmp"])
                op("dve", lambda e: e.tensor_reduce(out=tmp[:, :, 1:2], in_=tmp[:, :, 16:20], axis=AX.X, op=ALU.add), ["tmp"], ["tmp"])
                op("dve", lambda e: e.reciprocal(out=tmp[:, :, 2:3], in_=tmp[:, :, 1:2]), ["tmp"], ["tmp"])
                op("dve", lambda e: e.tensor_scalar(out=tmp[:, :, 20:24], in0=gm, scalar1=-1.0, scalar2=10000.0, op0=ALU.add, op1=ALU.mult),
                   ["tmp"], ["tmp"])
                lem = m2f[:].rearrange("p t (g e) -> p t g e", g=4)
                op("dve", lambda e: e.tensor_tensor(out=lem, in0=lge, in1=tmp[:, :, 20:24].unsqueeze(3).to_broadcast([128, 9, 4, 8]),
                                                    op=ALU.add), ["lg", "tmp"], ["m2f"])
                op("dve", lambda e: e.tensor_reduce(out=tmp[:, :, 3:4], in_=m2f[:], axis=AX.X, op=ALU.max), ["m2f"], ["tmp"])
                op("dve", lambda e: e.tensor_tensor(out=gate[:], in0=m2f[:], in1=T3.to_broadcast([128, 9, 32]), op=ALU.is_ge),
                   ["m2f", "tmp"], ["gate"])
                op("dve", lambda e: e.scalar_tensor_tensor(out=rank[:].rearrange("p t e -> p (t e)"), in0=gate[:].rearrange("p t e -> p (t e)"),
                                                           scalar=-20000.0, in1=m2f[:].rearrange("p t e -> p (t e)"), op0=ALU.mult, op1=ALU.add),
                   ["gate", "m2f"], ["rank"])
                op("dve", lambda e: e.tensor_reduce(out=tmp[:, :, 4:5], in_=rank[:], axis=AX.X, op=ALU.max), ["rank"], ["tmp"])
                op("dve", lambda e: e.tensor_tensor(out=m2f[:], in0=rank[:], in1=T4.to_broadcast([128, 9, 32]), op=ALU.is_ge),
                   ["rank", "tmp"], ["m2f"])
                op("dve", lambda e: e.tensor_tensor(out=tmp[:, :, 5:6], in0=T3, in1=T4, op=ALU.subtract), ["tmp"], ["tmp"])
                op("act", lambda e: e.activation(out=tmp[:, :, 6:7], in_=tmp[:, :, 5:6], func=AF.Sigmoid), ["tmp"], ["tmp"])
                op("dve", lambda e: e.tensor_tensor(out=tmp[:, :, 24:25], in0=tmp[:, :, 6:7], in1=tmp[:, :, 2:3], op=ALU.mult), ["tmp"], ["tmp"])
                op("dve", lambda e: e.tensor_tensor(out=tmp[:, :, 25:26], in0=tmp[:, :, 2:3], in1=tmp[:, :, 24:25], op=ALU.subtract),
                   ["tmp"], ["tmp"])
                op("dve", lambda e: e.tensor_tensor(out=msk[:], in0=gate[:], in1=m2f[:], op=ALU.add), ["gate", "m2f"], ["msk"])
                op("dve", lambda e: e.tensor_tensor(out=gate[:], in0=gate[:], in1=tmp[:, :, 24:25].to_broadcast([128, 9, 32]), op=ALU.mult),
                   ["gate", "tmp"], ["gate"])
                op("dve", lambda e: e.tensor_tensor(out=rank[:], in0=m2f[:], in1=tmp[:, :, 25:26].to_broadcast([128, 9, 32]), op=ALU.mult),
                   ["m2f", "tmp"], ["rank"])
                op("dve", lambda e: e.tensor_tensor(out=gate[:], in0=gate[:], in1=rank[:], op=ALU.add), ["gate", "rank"], ["gate"])
                op("pool", lambda e: e.memset(msk[64:128, 8, :], 0.0), [], ["msk"])
                tk = cst[:, C_TK:C_TK + 27].rearrange("p (t c) -> p t c", c=3)
                for c3 in range(3):
                    op("dve", lambda e, c3=c3: e.tensor_tensor(out=rhs6[:, :, :, c3], in0=msk[:],
                                                                in1=tk[:, :, c3:c3 + 1].to_broadcast([128, 9, 32]), op=ALU.mult),
                       ["msk", "cst"], ["rhs6"])
                op("dve", lambda e: e.tensor_tensor(out=rhs6[:, :, :, 3], in0=gate[:], in1=msk[:], op=ALU.mult), ["gate", "msk"], ["rhs6"])
                op("dve", lambda e: e.tensor_tensor(out=rank[:], in0=gate[:], in1=rhs6[:, :, :, 3], op=ALU.subtract), ["gate", "rhs6"], ["rank"])
                op("dve", lambda e: e.tensor_tensor(out=rhs6[:, :, :, 4], in0=rank[:], in1=msk[:], op=ALU.mult), ["rank", "msk"], ["rhs6"])
                op("dve", lambda e: e.tensor_tensor(out=rhs6[:, :, :, 5], in0=m2f[:], in1=msk[:], op=ALU.mult), ["m2f", "msk"], ["rhs6"])
                for ti in range(9):
                    def mmrk(e, ti=ti):
                        for tj in range(ti):
                            e.matmul(ps[4][:, 0:32], lhsT=ones_b[:], rhs=msk[:, tj, :], start=(tj == 0), stop=False)
                        return e.matmul(ps[4][:, 0:32], lhsT=ls_b[:], rhs=msk[:, ti, :], start=(ti == 0), stop=True)
                    op("pe", mmrk, ["ones_b", "ls_b", "msk"], [PS(4)])
                    op("dve", lambda e, ti=ti: e.tensor_copy(out=rank[:, ti, :], in_=ps[4][:, 0:32]), [PS(4)], ["rank"])
                op("dve", lambda e: e.tensor_copy(out=m2f[:], in_=msk[:]), ["msk"], ["m2f"])

                stage_end(8)
                for ex in range(NE):
                    for ti in range(9):
                        op("dve", lambda e, ti=ti, ex=ex: e.tensor_scalar(
                            out=sel[:, ti, :], in0=cst[:, C_IO:C_IO + CAP], scalar1=rank[:, ti, ex:ex + 1], scalar2=m2f[:, ti, ex:ex + 1],
                            op0=ALU.is_equal, op1=ALU.mult), ["cst", "rank", "m2f"], ["sel"])
                    for sg in range(CAP // 128):
                        def mmsl(e, sg=sg, ex=ex):
                            for ti in range(9):
                                ins = e.matmul(ps[4][:, sg * 8:sg * 8 + 6], lhsT=sel[:, ti, sg * 128:(sg + 1) * 128], rhs=rhs6[:, ti, ex, :],
                                               start=(ti == 0), stop=(ti == 8))
                            return ins
                        op("pe", mmsl, ["sel", "rhs6"], [PS(4)])
                    op("dve", lambda e: e.tensor_copy(out=slot[:].rearrange("p a b -> p (a b)"), in_=ps[4][:, 0:16]), [PS(4)], ["slot"])
                    op("dve", lambda e: e.scalar_tensor_tensor(out=slot[:, :, 6], in0=slot[:, :, 0], scalar=32.0, in1=slot[:, :, 1],
                                                               op0=ALU.mult, op1=ALU.add), ["slot"], ["slot"])
                    op("dve", lambda e: e.tensor_scalar(out=slot[:, :, 7], in0=slot[:, :, 2], scalar1=-1.0, scalar2=-float(ZROW),
                                                        op0=ALU.add, op1=ALU.mult), ["slot"], ["slot"])
                    op("dve", lambda e: e.tensor_tensor(out=slot[:, :, 0], in0=slot[:, :, 6], in1=slot[:, :, 7], op=ALU.add), ["slot"], ["slot"])
                    op("dve", lambda e: e.tensor_copy(out=idxg[:], in_=slot[:, :, 0]), ["slot"], ["idxg"])
                    op("dve", lambda e: e.scalar_tensor_tensor(out=slot[:, :, 1], in0=slot[:, :, 5], scalar=float(NT), in1=slot[:, :, 6],
                                                               op0=ALU.mult, op1=ALU.add), ["slot"], ["slot"])
                    op("dve", lambda e: e.tensor_scalar(out=slot[:, :, 7], in0=slot[:, :, 2], scalar1=-1.0, scalar2=-float(DUMP),
                                                        op0=ALU.add, op1=ALU.mult), ["slot"], ["slot"])
                    op("dve", lambda e: e.tensor_tensor(out=slot[:, :, 1], in0=slot[:, :, 1], in1=slot[:, :, 7], op=ALU.add), ["slot"], ["slot"])
                    op("dve", lambda e: e.tensor_copy(out=idxs[:], in_=slot[:, :, 1]), ["slot"], ["idxs"])
                    op("dve", lambda e: e.tensor_tensor(out=slot[:, :, 3], in0=slot[:, :, 3], in1=slot[:, :, 4], op=ALU.add), ["slot"], ["slot"])
                    for sg in range(CAP // 128):
                        dma("pool", None, None, reads=["idxg", "s_h3"], writes=["xg"],
                            fn=lambda e, sg=sg: e.indirect_dma_start(
                                out=xg[:, sg, :], out_offset=None, in_=s_h3[:, :],
                                in_offset=bass.IndirectOffsetOnAxis(ap=idxg[:, sg:sg + 1], axis=0)))
                    for sg in range(CAP // 128):
                        for g in range(4):
                            b = 4 + g % 4
                            pv = ps[b][:].bitcast(BF16).rearrange("p (a b) -> p a b", a=8)

                            def trx(e, sg=sg, g=g, pv=pv):
                                for j in range(8):
                                    c = g * 8 + j
                                    ins = e.transpose(out=pv[:, j, :], in_=xg[:, sg, c * 128:(c + 1) * 128], identity=ident_b[:])
                                return ins
                            op("pe", trx, ["xg", "ident_b"], [PS(b)])
                            if g % 2 == 0:
                                op("act", lambda e, sg=sg, g=g, pv=pv: e.activation(
                                    out=xgT[:, g * 8:(g + 1) * 8, sg * 128:(sg + 1) * 128], in_=pv, func=AF.Copy), [PS(b)], ["xgT"])
                            else:
                                op("dve", lambda e, sg=sg, g=g, pv=pv: e.tensor_copy(
                                    out=xgT[:, g * 8:(g + 1) * 8, sg * 128:(sg + 1) * 128], in_=pv), [PS(b)], ["xgT"])
                    for half in range(2):
                        for which in range(2):
                            wv_, kv_ = wnext()
                            for mm_ in range(2):
                                fcn = half * 2 + mm_
                                bq = (which * 2 + mm_) % 4

                                def mmg(e, b=bq, mm_=mm_, wv_=wv_):
                                    for k in range(32):
                                        ins = e.matmul(ps[b][:, 0:CAP], lhsT=wv_[:, k, mm_ * 128:(mm_ + 1) * 128], rhs=xgT[:, k, :],
                                                       start=(k == 0), stop=(k == 31))
                                    return ins
                                op("pe", mmg, [kv_, "xgT"], [PS(bq)])
                                if which == 0:
                                    op("act", lambda e, bq=bq, mm_=mm_: e.activation(out=sgl[:, mm_, :], in_=ps[bq][:, 0:CAP], func=AF.Silu),
                                       [PS(bq)], ["sgl"])
                                else:
                                    op("dve", lambda e, bq=bq, fcn=fcn, mm_=mm_: e.tensor_tensor(out=hidT[:, fcn, :], in0=ps[bq][:, 0:CAP],
                                                                                                 in1=sgl[:, mm_, :], op=ALU.mult),
                                       [PS(bq), "sgl"], ["hidT"])
                    for dh in range(2):
                      wdv, kdv = wnext()
                      for sg in range(CAP // 128):
                        yb = ye[sg % 2]
                        for cb in range(dh * 4, dh * 4 + 4):
                            wv, wk_ = wdv, kdv
                            b = 4 + cb % 4

                            def mmd(e, sg=sg, cb=cb, wv=wv, b=b):
                                for k in range(4):
                                    ins = e.matmul(ps[b][:, :], lhsT=hidT[:, k, sg * 128:(sg + 1) * 128],
                                                   rhs=wv[:, k, (cb % 4) * 512:(cb % 4) * 512 + 512], start=(k == 0), stop=(k == 3))
                                return ins
                            op("pe", mmd, ["hidT", wk_], [PS(b)])
                            if cb % 2 == 0:
                                op("act", lambda e, sg=sg, cb=cb, b=b, yb=yb: e.activation(
                                    out=yb[:, cb * 512:(cb + 1) * 512], in_=ps[b][:, :], func=AF.Copy, scale=slot[:, sg, 3:4]),
                                   [PS(b), "slot"], [yek[sg % 2]])
                            else:
                                op("dve", lambda e, sg=sg, cb=cb, b=b, yb=yb: e.tensor_scalar(
                                    out=yb[:, cb * 512:(cb + 1) * 512], in0=ps[b][:, :], scalar1=slot[:, sg, 3:4], scalar2=None, op0=ALU.mult),
                                   [PS(b), "slot"], [yek[sg % 2]])
                        if dh == 1:
                            dma("pool", None, None, reads=["idxs", yek[sg % 2]], writes=["s_o12"],
                                fn=lambda e, sg=sg, yb=yb: e.indirect_dma_start(
                                    out=s_o12[:, :], out_offset=bass.IndirectOffsetOnAxis(ap=idxs[:, sg:sg + 1], axis=0),
                                    in_=yb[:, :], in_offset=None))

                stage_end(9)
                dma("sp", gbcf[:], norm_final_g.partition_broadcast(128), writes=["gbcf"])
                fsets = [
                    [(xt[:, :], [*XTK]), (hbf[:, :], ["hbf"]), (ye[0][:, :], ["ye0"])],
                    [(xgTbuf[:, :], ["xgT"]), (xg[:].rearrange("p a d -> p (a d)").bitcast(F32), ["xg"]),
                     (wsl[2][:].bitcast(F32), ["wsl2"])],
                ]
                for ti, (t0, rows) in enumerate(TILES):
                    (A, kA), (Bf, kB), (Cf, kC) = fsets[ti % 2]
                    dma("sp", A[0:rows, :], s_x2[t0:t0 + rows, :], reads=["s_x2"], writes=kA)
                    dma("sp", Bf[0:rows, :], s_o12[t0:t0 + rows, :], reads=["s_o12"], writes=kB)
                    dma("sp", Cf[0:rows, :], s_o12[NT + t0:NT + t0 + rows, :], reads=["s_o12"], writes=kC)
                    op("dve", lambda e, rows=rows, A=A, Bf=Bf: e.tensor_tensor(out=A[0:rows, :], in0=A[0:rows, :], in1=Bf[0:rows, :], op=ALU.add),
                       [*kA, *kB], kA)
                    op("dve", lambda e, rows=rows, A=A, Cf=Cf: e.tensor_tensor(out=A[0:rows, :], in0=A[0:rows, :], in1=Cf[0:rows, :], op=ALU.add),
                       [*kA, *kC], kA)
                    sc0 = 8 + 4 * (ti % 2)
                    ksm = "smf%d" % (ti % 2)
                    op("act", lambda e, rows=rows, A=A, sc0=sc0: e.activation(out=hb[0:rows, :], in_=A[0:rows, :], func=AF.Square,
                                                                               accum_out=sm[0:rows, sc0:sc0 + 1]), kA, [*HBK, ksm])
                    op("act", lambda e, rows=rows, sc0=sc0: e.activation(out=sm[0:rows, sc0 + 1:sc0 + 2], in_=sm[0:rows, sc0:sc0 + 1], func=AF.Sqrt,
                                                                          scale=1.0 / D, bias=EPS), [ksm], [ksm])
                    op("dve", lambda e, rows=rows, sc0=sc0: e.reciprocal(out=sm[0:rows, sc0 + 2:sc0 + 3], in_=sm[0:rows, sc0 + 1:sc0 + 2]), [ksm], [ksm])
                    op("dve", lambda e, rows=rows, A=A, Bf=Bf, sc0=sc0: e.scalar_tensor_tensor(
                        out=Bf[0:rows, :], in0=A[0:rows, :], scalar=sm[0:rows, sc0 + 2:sc0 + 3],
                        in1=gbcf[0:rows, :], op0=ALU.mult, op1=ALU.mult), [*kA, ksm, "gbcf"], kB)
                    dst = y_main[t0:t0 + rows, :] if ti < 8 else y_s[:, :]
                    dma("sp", dst, Bf[0:rows, :], reads=kB)
      except _Stop:
        S.finish()
        es.pop_all()
        return nc
      S.finish()
    return nc


_NC_CACHE = {}
_RET_MAPS = [False]


def kernel(**inp):
    f = lambda k: np.ascontiguousarray(np.asarray(inp[k], dtype=np.float32))
    x_prompt = f("x_prompt")
    x_sample = f("x_sample")
    consts = make_consts()
    convp = np.ascontiguousarray(np.concatenate(
        [f("conv_dw_w")[0], f("conv_dw_b")[0][None], f("conv_ln_g")[0][None], f("conv_ln_b")[0][None]], axis=0))
    w_router = np.ascontiguousarray(np.concatenate([f("w_router_group")[0], f("w_router_expert")[0]], axis=1))
    b_router = np.ascontiguousarray(np.concatenate([f("b_router_group")[0], f("b_router_expert")[0]], axis=0))
    shared = {
        "consts": consts,
        "norm_mix_g": f("norm_mix_g")[0], "w_in": f("w_in")[0], "w_alpha_up": f("w_alpha_up")[0],
        "b_alpha": f("b_alpha"), "gla_norm_g": f("gla_norm_g")[0], "w_branch_a": f("w_branch_a")[0],
        "convp_in": convp, "w_branch_b": f("w_branch_b")[0], "w_out": f("w_out")[0],
        "norm_ca_g": f("norm_ca_g")[0], "norm_mem_g": f("norm_mem_g")[0], "w_ca_q": f("w_ca_q")[0],
        "w_ca_k": f("w_ca_k")[0], "w_ca_v": f("w_ca_v")[0], "w_ca_o": f("w_ca_o")[0],
        "norm_ffn_g": f("norm_ffn_g")[0], "w_router": w_router, "b_router": b_router,
        "w_exp_gate": f("w_exp_gate")[0], "w_exp_up": f("w_exp_up")[0], "w_exp_down": f("w_exp_down")[0],
        "norm_final_g": f("norm_final_g"),
    }
    st_gla = f("state_gla")[0]
    st_conv = f("state_conv")[0]
    ck = f("cache_mem_k")[0].reshape(128, 256, 1024)
    cv = f("cache_mem_v")[0].reshape(128, 256, 1024)
    mem = f("mem_prompt")
    zeros_pre = np.zeros((NP, D), np.float32)
    in_maps = []
    for c in range(NCORE):
        b, half = c // 2, c % 2
        m = dict(shared)
        m["x_main"] = x_prompt[b, half * NP:(half + 1) * NP]
        m["x_pre"] = x_prompt[b, 0:NP] if half == 1 else zeros_pre
        m["x_s"] = x_sample[c * NSEQ:(c + 1) * NSEQ].reshape(NS, D)
        m["mem"] = mem[b]
        m["st_gla"] = st_gla[c * NSEQ:(c + 1) * NSEQ]
        m["st_conv"] = st_conv[c * NSEQ:(c + 1) * NSEQ]
        m["ck"] = ck[c * NSEQ:(c + 1) * NSEQ]
        m["cv"] = cv[c * NSEQ:(c + 1) * NSEQ]
        in_maps.append(m)
    if _RET_MAPS[0]:
        return in_maps
    if "nc" not in _NC_CACHE:
        _NC_CACHE["nc"] = build()
    nc = _NC_CACHE["nc"]
    res = run_bass_kernel_spmd(nc, in_maps, core_ids=list(range(NCORE)))
    r = res.results
    y_prompt = np.stack([np.concatenate([r[2 * b]["y_main"], r[2 * b + 1]["y_main"]], axis=0) for b in range(4)])
    y_sample = np.concatenate([r[c]["y_s"].reshape(NSEQ, 4, D) for c in range(NCORE)], axis=0)
    gla_prompt = np.stack([r[2 * b + 1]["gla_p"] for b in range(4)])[None]
    conv_prompt = np.stack([r[2 * b + 1]["conv_p"] for b in range(4)])[None]
    mk = np.stack([r[2 * b]["mk_o"].reshape(256, 4, 256) for b in range(4)])[None]
    mv = np.stack([r[2 * b]["mv_o"].reshape(256, 4, 256) for b in range(4)])[None]
    gla_sample = np.concatenate([r[c]["gla_s"] for c in range(NCORE)], axis=0)[None]
    conv_sample = np.concatenate([r[c]["conv_s"] for c in range(NCORE)], axis=0)[None]
    return (y_prompt.astype(np.float32), y_sample.astype(np.float32), gla_prompt.astype(np.float32),
            conv_prompt.astype(np.float32), mk.astype(np.float32), mv.astype(np.float32),
            gla_sample.astype(np.float32), conv_sample.astype(np.float32))
```

```python
import contextlib
import numpy as np
import concourse.bass as bass
import concourse.mybir as mybir
from concourse.bass_utils import run_bass_kernel_spmd

F32 = mybir.dt.float32
BF16 = mybir.dt.bfloat16
U32 = mybir.dt.uint32
I32 = mybir.dt.int32
AF = mybir.ActivationFunctionType
ALU = mybir.AluOpType
AX = mybir.AxisListType

D = 4096
NCORE = 8
NP = 1024
NS = 64
NT = NP + NS
NSEQ = 16
IN_COLS = 18448
OFF_Q, OFF_K, OFF_V, OFF_R, OFF_A, OFF_U, OFF_G = 0, 1024, 2048, 4096, 6144, 6160, 10256
EPS = 1e-6
CAP = 256
NE = 32
ZROW = NT
DUMP = 2 * NT
NGRP = [(0, 512), (512, 512), (1024, 64)]
TILES = [(i * 128, 128) for i in range(8)] + [(1024, 64)]

C_ID, C_MU, C_MS, C_RM, C_IO, C_TK = 0, 128, 256, 320, 336, 592
CWP = 619
C_BM, C_RS, C_LS, C_ON = 619, 1643, 2731, 2859
CW = 2987


def make_consts():
    c = np.zeros((128, CW), np.float32)
    p = np.arange(128)
    c[:, C_ID:C_ID + 128] = np.eye(128)
    c[:, C_MU:C_MU + 128] = (p[:, None] <= p[None, :])
    q = np.arange(64)
    ms = ((q[:, None] // 4) == (q[None, :] // 4)) & (q[:, None] <= q[None, :])
    c[:64, C_MS:C_MS + 64] = ms
    bm = (np.arange(16)[:, None] == (q[None, :] // 4)).astype(np.float32)
    c[:, C_BM:C_BM + 1024] = bm.reshape(1, 1024)
    c[:64, C_RM:C_RM + 16] = ((q[:, None] // 4) == np.arange(16)[None, :])
    t = np.arange(NT)
    rs = np.ones(NT, np.float32)
    rs[:NP][t[:NP] % 128 == 0] = 0.0
    rs[NP:][(t[NP:] - NP) % 4 == 0] = 0.0
    c[:, C_RS:C_RS + NT] = rs[None, :]
    c[:, C_IO:C_IO + 256] = np.arange(256)[None, :]
    for i in range(9):
        tok = i * 128 + p
        c[:, C_TK + 3 * i + 0] = tok // 32
        c[:, C_TK + 3 * i + 1] = tok % 32
        c[:, C_TK + 3 * i + 2] = 1.0
    c[:, C_LS:C_LS + 128] = (p[:, None] < p[None, :])
    c[:, C_ON:C_ON + 128] = 1.0
    return c


class Sched:
    def __init__(self, nc, es, ndma=28):
        self.nc = nc
        self.E = dict(pe=nc.tensor, act=nc.scalar, dve=nc.vector, pool=nc.gpsimd, sp=nc.sync)
        self.sem = {k: es.enter_context(nc.semaphore("c_" + k)) for k in self.E}
        self.cnt = {k: 0 for k in self.E}
        self.seen = {k: {} for k in self.E}
        self.dsem = [es.enter_context(nc.semaphore("dm%d" % i)) for i in range(ndma)]
        self.dval = [0] * ndma
        self.dnext = {"sp": 0, "pool": 0, "act": 0}
        self.dhalf = ndma // 2
        self.res = {}

    def _semobj(self, key):
        return self.sem[key] if isinstance(key, str) else self.dsem[key]

    def _wait(self, eng, tok):
        key, val, src = tok
        if self.seen[eng].get(key, 0) >= val:
            return
        self.E[eng].wait_ge(self._semobj(key), val)
        self.seen[eng][key] = val

    def _deps(self, eng, reads, writes):
        for r in reads:
            st = self.res.get(r)
            if st and st["w"]:
                tok = st["w"]
                if not (tok[2] == eng and eng == "pe"):
                    self._wait(eng, tok)
        for w in writes:
            st = self.res.get(w)
            if st:
                if st["w"] and st["w"][2] != eng:
                    self._wait(eng, st["w"])
                for tok in st["r"]:
                    if tok[2] != eng:
                        self._wait(eng, tok)

    def _commit(self, tok, reads, writes):
        for r in reads:
            st = self.res.setdefault(r, {"w": None, "r": []})
            if tok[2] != "dma":
                st["r"] = [x for x in st["r"] if x[2] != tok[2]]
            st["r"].append(tok)
        for w in writes:
            self.res[w] = {"w": tok, "r": []}

    mute = False

    def op(self, eng, fn, reads=(), writes=()):
        if self.mute:
            return
        self._deps(eng, reads, writes)
        ins = fn(self.E[eng])
        self.cnt[eng] += 1
        ins.then_inc(self.sem[eng], 1)
        self._commit((eng, self.cnt[eng], eng), reads, writes)

    def dma(self, q, out, in_, reads=(), writes=(), fn=None):
        if self.mute:
            return
        base = 0 if q == "pool" else self.dhalf
        i = base + self.dnext[q]
        self.dnext[q] = (self.dnext[q] + 1) % self.dhalf
        if self.dval[i] > 0:
            self._wait(q, (i, self.dval[i], "dma"))
        self._deps(q, reads, writes)
        if fn is None:
            ins = self.E[q].dma_start(out=out, in_=in_)
        else:
            ins = fn(self.E[q])
        ins.then_inc(self.dsem[i], 16)
        self.dval[i] += 16
        self._commit((i, self.dval[i], "dma"), reads, writes)

    def finish(self):
        for i, v in enumerate(self.dval):
            if v > 0:
                self._wait("sp", (i, v, "dma"))
        for k in ("pe", "act", "dve", "pool"):
            if self.cnt[k] > 0:
                self._wait("sp", (k, self.cnt[k], k))


class _Stop(Exception):
    pass


_SB_MIN = [1 << 30]


def build(stage=99, mute_until=None, lite=None):
    nc = bass.Bass("TRN2", target_bir_lowering=False)

    def stage_end(k):
        if mute_until is not None and k >= mute_until:
            S.mute = False
        if stage <= k:
            raise _Stop()

    def din(name, shape, dt=F32):
        kind = "ExternalInput" if (lite is None or name in lite) else "Internal"
        return nc.dram_tensor(name, list(shape), dt, kind=kind).ap()

    def dout(name, shape, dt=F32):
        return nc.dram_tensor(name, list(shape), dt, kind="ExternalOutput").ap()

    def dscr(name, shape, dt):
        return nc.dram_tensor(name, list(shape), dt, kind="Internal").ap()

    x_main = din("x_main", [NP, D])
    x_pre = din("x_pre", [NP, D])
    x_s = din("x_s", [NS, D])
    mem = din("mem", [256, D])
    st_gla = din("st_gla", [NSEQ, 4, 256, 512])
    st_conv = din("st_conv", [NSEQ, 30, 2048])
    ck = din("ck", [NSEQ, 256, 1024])
    cv = din("cv", [NSEQ, 256, 1024])
    consts = din("consts", [128, CW])
    norm_mix_g = din("norm_mix_g", [D])
    w_in = din("w_in", [D, IN_COLS])
    w_alpha_up = din("w_alpha_up", [16, 1024])
    b_alpha = din("b_alpha", [1, 1024])
    gla_norm_g = din("gla_norm_g", [2048])
    w_branch_a = din("w_branch_a", [2048, D])
    convp_in = din("convp_in", [34, 2048])
    w_branch_b = din("w_branch_b", [2048, D])
    w_out = din("w_out", [D, D])
    norm_ca_g = din("norm_ca_g", [D])
    norm_mem_g = din("norm_mem_g", [D])
    w_ca_q = din("w_ca_q", [D, 1024])
    w_ca_k = din("w_ca_k", [D, 1024])
    w_ca_v = din("w_ca_v", [D, 1024])
    w_ca_o = din("w_ca_o", [1024, D])
    norm_ffn_g = din("norm_ffn_g", [D])
    w_router = din("w_router", [D, 36])
    b_router = din("b_router", [36])
    w_exp_gate = din("w_exp_gate", [NE, D, 512])
    w_exp_up = din("w_exp_up", [NE, D, 512])
    w_exp_down = din("w_exp_down", [NE, 512, D])
    norm_final_g = din("norm_final_g", [D])

    y_main = dout("y_main", [NP, D])
    y_s = dout("y_s", [NS, D])
    gla_p = dout("gla_p", [4, 256, 512])
    conv_p = dout("conv_p", [30, 2048])
    mk_o = dout("mk_o", [256, 1024])
    mv_o = dout("mv_o", [256, 1024])
    gla_s = dout("gla_s", [NSEQ, 4, 256, 512])
    conv_s = dout("conv_s", [NSEQ, 30, 2048])

    s_state = dscr("s_state", [4, 256, 512], F32)
    s_ogT = dscr("s_ogT", [2048, NT], BF16)
    s_mT = dscr("s_mT", [D, NT], BF16)
    s_x1 = dscr("s_x1", [NT, D], F32)
    s_x2 = dscr("s_x2", [NT, D], F32)
    s_h3 = dscr("s_h3", [NT + 1, D], BF16)
    s_o12 = dscr("s_o12", [2 * NT + 1, D], F32)

    es = contextlib.ExitStack()
    with es:
      S = Sched(nc, es)
      try:
        pass
        XTK = ["xt", "xt_E1", "xt_E2", "xt_E3"]
        HBK = ["hb", "hb_kdT", "hb_sT", "hb_og", "hb_ogT", "hb_sTs"]
        op, dma = S.op, S.dma

        def sb(name, shape, dt, stack=es):
            t = stack.enter_context(nc.sbuf_tensor(name, list(shape), dt))
            _SB_MIN[0] = min(_SB_MIN[0], nc.sbuf_bytes_remaining)
            return t

        ps = [es.enter_context(nc.psum_tensor("ps%d" % i, [128, 512], F32)) for i in range(8)]

        def PS(i):
            return "ps%d" % i

        cst = sb("cst", [128, CWP], F32)
        ident_b = sb("ident_b", [128, 128], BF16)
        bm_b = sb("bm_b", [128, 16, 64], BF16)
        rs_b = sb("rs_b", [128, NT], BF16)
        ls_b = sb("ls_b", [128, 128], BF16)
        ones_b = sb("ones_b", [128, 128], BF16)
        NSLOT = 2
        wsl = [sb("wsl%d" % i, [128, 8192], BF16) for i in range(NSLOT)]
        gbc = sb("gbc", [128, D], BF16)
        xt = sb("xt", [128, D], F32)
        hb = sb("hb", [128, D], BF16)
        sm = sb("sm", [128, 64], F32)

        dma("sp", cst[:], consts[:, 0:CWP], writes=["cst"])
        dma("sp", xt[:, 0:CW - CWP], consts[:, CWP:CW], writes=[*XTK])
        ident_f = cst[:, C_ID:C_ID + 128]
        op("dve", lambda e: e.tensor_copy(out=ident_b[:], in_=cst[:, C_ID:C_ID + 128]), ["cst"], ["ident_b"])
        op("dve", lambda e: e.tensor_copy(out=bm_b[:].rearrange("p a b -> p (a b)"), in_=xt[:, C_BM - CWP:C_BM - CWP + 1024]), [*XTK], ["bm_b"])
        op("dve", lambda e: e.tensor_copy(out=rs_b[:], in_=xt[:, C_RS - CWP:C_RS - CWP + NT]), [*XTK], ["rs_b"])
        op("dve", lambda e: e.tensor_copy(out=ls_b[:], in_=xt[:, C_LS - CWP:C_LS - CWP + 128]), [*XTK], ["ls_b"])
        op("dve", lambda e: e.tensor_copy(out=ones_b[:], in_=xt[:, C_ON - CWP:C_ON - CWP + 128]), [*XTK], ["ones_b"])
        hscope = contextlib.ExitStack()
        hT = sb("hT", [128, 32, NT], BF16, hscope)
        if mute_until is not None:
            S.mute = True

        wstate = {"n": 0, "plan": [], "issued": 0, "moe0": 10 ** 9}

        def wplan(ap2d):
            wstate["plan"].append(ap2d)

        def slot_of(i):
            m0 = wstate["moe0"]
            if i < m0:
                return i % 2
            return [m0 % 2, 1 - m0 % 2, 2][(i - m0) % 3]

        def look(i):
            return 2 if i < wstate["moe0"] else 3

        def _wissue(i):
            ap2d = wstate["plan"][i]
            K, ncols = ap2d.shape
            kc = K // 128
            slot = slot_of(i)
            view = wsl[slot][:, 0:kc * ncols].rearrange("p (k n) -> p k n", k=kc)
            dma("pool", view, ap2d.rearrange("(k p) n -> p k n", p=128), writes=["wsl%d" % slot])

        def wnext(ap2d_check=None):
            i = wstate["n"]
            wstate["n"] += 1
            if S.mute:
                wstate["issued"] = max(wstate["issued"], wstate["n"])
            while not S.mute and wstate["issued"] < min(len(wstate["plan"]), i + look(i)):
                _wissue(wstate["issued"])
                wstate["issued"] += 1
            ap2d = wstate["plan"][i]
            if ap2d_check is not None:
                assert ap2d.shape == ap2d_check.shape and ap2d.offset == ap2d_check.offset, (i, ap2d, ap2d_check)
            K, ncols = ap2d.shape
            kc = K // 128
            slot = slot_of(i)
            view = wsl[slot][:, 0:kc * ncols].rearrange("p (k n) -> p k n", k=kc)
            return view, "wsl%d" % slot

        def load_gain(g_ap):
            dma("pool", gbc[:], g_ap.partition_broadcast(128), writes=["gbc"])

        def norm_tile(src_ap, rows, col, dstT, dst_key, scratch_out=None, fp32_T=None, src_reads=()):
            dma("sp", xt[0:rows, :], src_ap, reads=list(src_reads), writes=[*XTK])
            op("act", lambda e: e.activation(out=hb[0:rows, :], in_=xt[0:rows, :], func=AF.Square,
                                             accum_out=sm[0:rows, 0:1]), [*XTK], [*HBK, "sm"])
            op("act", lambda e: e.activation(out=sm[0:rows, 1:2], in_=sm[0:rows, 0:1], func=AF.Sqrt,
                                             scale=1.0 / D, bias=EPS), ["sm"], ["sm"])
            op("dve", lambda e: e.reciprocal(out=sm[0:rows, 2:3], in_=sm[0:rows, 1:2]), ["sm"], ["sm"])
            if fp32_T is None:
                op("dve", lambda e: e.scalar_tensor_tensor(out=hb[0:rows, :], in0=xt[0:rows, :], scalar=sm[0:rows, 2:3],
                                                           in1=gbc[0:rows, :], op0=ALU.mult, op1=ALU.mult),
                   [*XTK, "sm", "gbc"], [*HBK])
                if scratch_out is not None:
                    dma("sp", scratch_out, hb[0:rows, :], reads=[*HBK])
                for g in range(4):
                    pv = ps[4 + g][:].bitcast(BF16).rearrange("p (a b) -> p a b", a=8)

                    def tr(e, g=g, pv=pv):
                        for j in range(8):
                            c = g * 8 + j
                            ins = e.transpose(out=pv[:, j, 0:rows], in_=hb[0:rows, c * 128:(c + 1) * 128],
                                              identity=ident_b[0:rows, 0:rows])
                        return ins
                    op("pe", tr, [*HBK, "ident_b"], [PS(4 + g)])
                    eng = "act" if g % 2 == 0 else "dve"
                    if eng == "act":
                        op("act", lambda e, g=g, pv=pv: e.activation(out=dstT[:, g * 8:(g + 1) * 8, col:col + rows],
                                                                      in_=pv[:, :, 0:rows], func=AF.Copy),
                           [PS(4 + g)], [dst_key])
                    else:
                        op("dve", lambda e, g=g, pv=pv: e.tensor_copy(out=dstT[:, g * 8:(g + 1) * 8, col:col + rows],
                                                                       in_=pv[:, :, 0:rows]),
                           [PS(4 + g)], [dst_key])
            else:
                fp32_T(rows)

        def gemm_ws(wview, wkey, kc, mcols, acts, act_key, groups, evac, psbanks, extra_reads=()):
            nb = 0
            for m in range(mcols // 128):
                for gi, (t0, n) in enumerate(groups):
                    b = psbanks[nb % len(psbanks)]
                    nb += 1

                    def mm(e, m=m, t0=t0, n=n, b=b):
                        for k in range(kc):
                            ins = e.matmul(ps[b][:, 0:n], lhsT=wview[:, k, m * 128:(m + 1) * 128],
                                           rhs=acts[:, k, t0:t0 + n], start=(k == 0), stop=(k == kc - 1))
                        return ins
                    op("pe", mm, [wkey, act_key] + list(extra_reads), [PS(b)])
                    evac(m, gi, t0, n, ps[b][:, 0:n], PS(b))

        def gemm_as(wviews, wkeys, actT, act_key, tiles, ncols, evac, psbanks):
            nb = 0
            for ti, (t0, rows) in enumerate(tiles):
                b = psbanks[nb % len(psbanks)]
                nb += 1
                ktot = sum(v.shape[1] for v, _ in wviews)

                def mm(e, t0=t0, rows=rows, b=b):
                    kk = 0
                    for v, koff in wviews:
                        for k in range(v.shape[1]):
                            ins = e.matmul(ps[b][0:rows, 0:ncols], lhsT=actT[:, koff + k, t0:t0 + rows],
                                           rhs=v[:, k, :], start=(kk == 0), stop=(kk == ktot - 1))
                            kk += 1
                    return ins
                op("pe", mm, list(wkeys) + [act_key], [PS(b)])
                evac(ti, t0, rows, ps[b][0:rows, 0:ncols], PS(b))

        def cols(a, n):
            return w_in[:, a:a + n]

        def plan_gla(with_q):
            wplan(cols(OFF_A, 16))
            for h in range(4):
                if with_q:
                    wplan(cols(OFF_Q + 256 * h, 256))
                wplan(cols(OFF_K + 256 * h, 256))
                wplan(cols(OFF_V + 512 * h, 256))
                wplan(cols(OFF_V + 512 * h + 256, 256))
                if with_q:
                    wplan(cols(OFF_R + 512 * h, 256))
                    wplan(cols(OFF_R + 512 * h + 256, 256))
        plan_gla(False)
        plan_gla(True)
        for m in range(16):
            wplan(cols(OFF_G + 256 * m, 256))
            wplan(w_branch_a[:, 256 * m:256 * m + 256])
        for m in range(8):
            wplan(cols(OFF_U + 256 * m, 256))
            wplan(cols(OFF_U + 2048 + 256 * m, 256))
        for m in range(16):
            wplan(cols(OFF_G + D + 256 * m, 256))
            wplan(w_branch_b[:, 256 * m:256 * m + 256])
        for cb in range(16):
            wplan(w_out[:, 256 * cb:256 * cb + 256])
        for wm in (w_ca_k, w_ca_v):
            for cb in range(4):
                wplan(wm[:, 256 * cb:256 * cb + 256])
        for m in range(4):
            wplan(w_ca_k[:, 256 * m:256 * m + 256])
        for m in range(4):
            wplan(w_ca_q[:, 256 * m:256 * m + 256])
        for cb in range(4):
            wplan(w_ca_o[:, 1024 * cb:1024 * cb + 1024])
        wstate["moe0"] = len(wstate["plan"])
        for ex in range(NE):
            wplan(w_exp_gate[ex][:, 0:256])
            wplan(w_exp_up[ex][:, 0:256])
            wplan(w_exp_gate[ex][:, 256:512])
            wplan(w_exp_up[ex][:, 256:512])
            wplan(w_exp_down[ex][:, 0:2048])
            wplan(w_exp_down[ex][:, 2048:4096])

        mix = contextlib.ExitStack()
        with mix:
            alT = sb("alT", [17, NT], BF16, mix)
            walb = sb("walb", [17, 1024], BF16, mix)
            ggl = sb("ggl", [128, 512], F32, mix)
            hT_halo = sb("hT_halo", [128, 32, 32], BF16, mix)
            dma("pool", walb[0:16, :], w_alpha_up, writes=["walb"])
            dma("pool", walb[16:17, :], b_alpha, writes=["walb"])
            load_gain(norm_mix_g)

            gl = contextlib.ExitStack()
            with gl:
                U1 = sb("U1", [128, 4608], F32, gl)
                bT = U1[:, 0:2 * NT].rearrange("p (c t) -> p c t", c=2)
                dfT = U1[:, 2 * NT:4 * NT].rearrange("p (c t) -> p c t", c=2)
                U1b = U1[:].bitcast(BF16)
                vh = U1b[:, 0:4608].rearrange("p (i v) -> p i v", i=9)
                gr = U1b[:, 4608:9216].rearrange("p (i v) -> p i v", i=9)
                xtb = xt[:].bitcast(BF16)
                E1 = xtb[:, 0:2 * NT].rearrange("p (c t) -> p c t", c=2)
                E2 = xtb[:, 2 * NT:4 * NT].rearrange("p (c t) -> p c t", c=2)
                E3 = xtb[:, 4 * NT:6 * NT].rearrange("p (c t) -> p c t", c=2)
                kdT = hb[:, 0:2 * NT].rearrange("p (c t) -> p c t", c=2)
                sT = hb[:, 2 * NT:2 * NT + 128]
                og = hb[:, 2304:2816]
                ogT = hb[:, 2816:3328].rearrange("p (a b) -> p a b", a=4)
                dec = sb("dec", [128, 2, 24], F32, gl)
                qeT = sb("qeT", [128, 2, NT], BF16, gl)
                keT = sb("keT", [128, 2, NT], BF16, gl)
                kd = sb("kd", [128, 9, 256], BF16, gl)
                of = sb("of", [128, 512], F32, gl)
                grf = of
                QMs = sb("QMs", [128, 2, 64], BF16, gl)
                Sf = sb("Sf", [128, 2, 512], F32, gl)
                Sb = sb("Sb", [128, 2, 512], BF16, gl)
                st6 = sb("st6", [128, 8], F32, gl)
                s0f = [sb("s0f%d" % i, [128, 512], F32, gl) for i in range(2)]
                s0b = [sb("s0b%d" % i, [128, 512], BF16, gl) for i in range(2)]
                sout = [sb("sout%d" % i, [128, 512], F32, gl) for i in range(2)]
                kdm = sb("kdm", [64, 256], BF16, gl)

                def gla_pass(main):
                    ntok = NT if main else NP
                    groups = NGRP if main else NGRP[:2]
                    tiles = TILES if main else TILES[:8]
                    for ti, (t0, rows) in enumerate(tiles):
                        if main:
                            src = x_main[t0:t0 + rows, :] if ti < 8 else x_s[:, :]
                        else:
                            src = x_pre[t0:t0 + rows, :]
                        norm_tile(src, rows, t0, hT, "hT")
                    if not main:
                        op("dve", lambda e: e.tensor_copy(out=hT_halo[:], in_=hT[:, :, NP - 32:NP]), ["hT"], ["hT_halo"])
                    wv, wk = wnext()
                    op("pool", lambda e: e.memset(alT[:], 1.0), [], ["alT"])

                    def ev_a(m, gi, t0, n, pap, pkey):
                        op("act", lambda e: e.activation(out=alT[0:16, t0:t0 + n], in_=pap[0:16, :], func=AF.Copy),
                           [pkey], ["alT"])
                    for gi, (t0, n) in enumerate(groups):
                        b = gi % 4

                        def mm(e, t0=t0, n=n, b=b):
                            for k in range(32):
                                ins = e.matmul(ps[b][0:16, 0:n], lhsT=wv[:, k, 0:16], rhs=hT[:, k, t0:t0 + n],
                                               start=(k == 0), stop=(k == 31))
                            return ins
                        op("pe", mm, [wk, "hT"], [PS(b)])
                        ev_a(0, gi, t0, n, ps[b][:, 0:n], PS(b))

                    for h in range(4):
                        for dc in range(2):
                            for gi, (t0, n) in enumerate(groups):
                                b = 4 + (dc * 3 + gi) % 4
                                c0 = (2 * h + dc) * 128
                                op("pe", lambda e, b=b, c0=c0, t0=t0, n=n: e.matmul(
                                    ps[b][:, 0:n], lhsT=walb[:, c0:c0 + 128], rhs=alT[:, t0:t0 + n], start=True, stop=True),
                                   ["walb", "alT"], [PS(b)])
                                op("act", lambda e, b=b, dc=dc, t0=t0, n=n: e.activation(
                                    out=dfT[:, dc, t0:t0 + n], in_=ps[b][:, 0:n], func=AF.Exp, scale=-1.0),
                                   [PS(b)], ["U1"])
                            op("act", lambda e, dc=dc: e.activation(out=dfT[:, dc, 0:ntok], in_=dfT[:, dc, 0:ntok],
                                                                     func=AF.Ln, bias=1.0), ["U1"], ["U1"])
                            op("dve", lambda e, dc=dc: e.tensor_scalar(out=dfT[:, dc, 0:ntok], in0=dfT[:, dc, 0:ntok],
                                                                        scalar1=-1.0 / 16.0, scalar2=None, op0=ALU.mult),
                               ["U1"], ["U1"])
                            op("dve", lambda e, dc=dc: e.tensor_tensor_scan(
                                out=bT[:, dc, 0:ntok], data0=rs_b[:, 0:ntok], data1=dfT[:, dc, 0:ntok],
                                initial=0.0, op0=ALU.mult, op1=ALU.add), ["U1", "rs_b"], ["U1"])
                        bTp = bT[:, :, 0:NP].rearrange("p c (n t) -> p c n t", t=128)
                        dfp = dfT[:, :, 0:NP].rearrange("p c (n t) -> p c n t", t=128)
                        for dc in range(2):
                            op("dve", lambda e, dc=dc: e.tensor_tensor(
                                out=dfp[:, dc], in0=bTp[:, dc, :, 127:128].to_broadcast([128, 8, 128]), in1=bTp[:, dc],
                                op=ALU.subtract), ["U1"], ["U1"])
                            op("act", lambda e, dc=dc: e.activation(out=dec[:, dc, 0:8], in_=bTp[:, dc, :, 127], func=AF.Exp),
                               ["U1"], ["dec"])
                        if main:
                            bTs = bT[:, :, NP:NT].rearrange("p c (n t) -> p c n t", t=4)
                            dfs = dfT[:, :, NP:NT].rearrange("p c (n t) -> p c n t", t=4)
                            for dc in range(2):
                                op("dve", lambda e, dc=dc: e.tensor_tensor(
                                    out=dfs[:, dc], in0=bTs[:, dc, :, 3:4].to_broadcast([128, 16, 4]), in1=bTs[:, dc],
                                    op=ALU.subtract), ["U1"], ["U1"])
                                op("act", lambda e, dc=dc: e.activation(out=dec[:, dc, 8:24], in_=bTs[:, dc, :, 3], func=AF.Exp),
                                   ["U1"], ["dec"])
                        op("act", lambda e: e.activation(out=E3[:, :, 0:ntok], in_=dfT[:, :, 0:ntok], func=AF.Exp),
                           ["U1"], ["xt_E3"])
                        if main:
                            op("act", lambda e: e.activation(out=E1[:, :, 0:ntok], in_=bT[:, :, 0:ntok], func=AF.Exp),
                               ["U1"], ["xt_E1"])
                            op("act", lambda e: e.activation(out=E2[:, :, 0:ntok], in_=bT[:, :, 0:ntok], func=AF.Exp,
                                                             scale=-1.0), ["U1"], ["xt_E2"])
                            wv, wk = wnext()

                            def ev_q(m, gi, t0, n, pap, pkey):
                                op("dve", lambda e: e.scalar_tensor_tensor(
                                    out=qeT[:, m, t0:t0 + n], in0=pap, scalar=1.0 / 16.0, in1=E1[:, m, t0:t0 + n],
                                    op0=ALU.mult, op1=ALU.mult), [pkey, "xt_E1"], ["qeT"])
                            gemm_ws(wv, wk, 32, 256, hT, "hT", groups, ev_q, [0, 1, 2, 3])
                        wv, wk = wnext()

                        def ev_k(m, gi, t0, n, pap, pkey):
                            if main:
                                op("dve", lambda e: e.tensor_tensor(out=keT[:, m, t0:t0 + n], in0=pap,
                                                                    in1=E2[:, m, t0:t0 + n], op=ALU.mult),
                                   [pkey, "xt_E2"], ["keT"])
                            op("dve", lambda e: e.tensor_tensor(out=kdT[:, m, t0:t0 + n], in0=pap,
                                                                in1=E3[:, m, t0:t0 + n], op=ALU.mult),
                               [pkey, "xt_E3"], ["hb_kdT"])
                        gemm_ws(wv, wk, 32, 256, hT, "hT", groups, ev_k, [0, 1, 2, 3])
                        for ti, (t0, rows) in enumerate(tiles):
                            b = 4 + ti % 2
                            pv = ps[b][:].bitcast(BF16)

                            def trk(e, t0=t0, rows=rows, pv=pv):
                                for dc in range(2):
                                    ins = e.transpose(out=pv[0:rows, dc * 128:(dc + 1) * 128], in_=kdT[:, dc, t0:t0 + rows],
                                                      identity=ident_b[:])
                                return ins
                            op("pe", trk, ["hb_kdT", "ident_b"], [PS(b)])
                            op("act", lambda e, ti=ti, rows=rows, pv=pv: e.activation(
                                out=kd[0:rows, ti, :], in_=pv[0:rows, 0:256], func=AF.Copy), [PS(b)], ["kd"])
                        for hf in range(2):
                            wv0, wk0 = wnext()

                            def ev_v(ti, t0, rows, pap, pkey, hf=hf):
                                op("act", lambda e: e.activation(out=vh[0:rows, ti, hf * 256:(hf + 1) * 256], in_=pap, func=AF.Copy),
                                   [pkey], ["U1"])
                            gemm_as([(wv0, 0)], [wk0], hT, "hT", tiles, 256, ev_v, [0, 1, 2, 3])
                        if main:
                            dma("sp", ggl[:], gla_norm_g[h * 512:(h + 1) * 512].partition_broadcast(128), writes=["ggl"])
                            for hf in range(2):
                                wv0, wk0 = wnext()

                                def ev_r(ti, t0, rows, pap, pkey, hf=hf):
                                    op("act", lambda e: e.activation(out=grf[0:rows, 0:256], in_=pap, func=AF.Silu), [pkey], ["of"])
                                    op("dve", lambda e: e.tensor_tensor(out=gr[0:rows, ti, hf * 256:(hf + 1) * 256], in0=grf[0:rows, 0:256],
                                                                        in1=ggl[0:rows, hf * 256:(hf + 1) * 256], op=ALU.mult),
                                       ["of", "ggl"], ["U1"])
                                gemm_as([(wv0, 0)], [wk0], hT, "hT", tiles, 256, ev_r, [0, 1, 2, 3])
                        if main:
                            dma("sp", Sf[:], s_state[h].rearrange("(c p) v -> p c v", p=128), reads=["s_state%d" % h], writes=["Sf"])
                        else:
                            op("pool", lambda e: e.memset(Sf[:], 0.0), [], ["Sf"])
                        op("act", lambda e: e.activation(out=Sb[:], in_=Sf[:], func=AF.Copy), ["Sf"], ["Sb"])
                        if main:
                            sample_pre(h)
                        for n_ in range(8):
                            t0 = n_ * 128
                            if main:
                                def mm_s(e, t0=t0):
                                    for dc in range(2):
                                        ins = e.matmul(ps[4][:, 0:128], lhsT=keT[:, dc, t0:t0 + 128], rhs=qeT[:, dc, t0:t0 + 128],
                                                       start=(dc == 0), stop=(dc == 1))
                                    return ins
                                op("pe", mm_s, ["keT", "qeT"], [PS(4)])
                                op("dve", lambda e: e.tensor_tensor(out=sT[:], in0=ps[4][:, 0:128], in1=cst[:, C_MU:C_MU + 128],
                                                                    op=ALU.mult), [PS(4), "cst"], ["hb_sT"])

                                def mm_o(e, t0=t0, n_=n_):
                                    e.matmul(ps[5][:, :], lhsT=sT[:], rhs=vh[:, n_, :], start=True, stop=False)
                                    for dc in range(2):
                                        ins = e.matmul(ps[5][:, :], lhsT=qeT[:, dc, t0:t0 + 128], rhs=Sb[:, dc, :],
                                                       start=False, stop=(dc == 1))
                                    return ins
                                op("pe", mm_o, ["hb_sT", "U1", "qeT", "Sb"], [PS(5)])
                                finish_o(h, n_, 128, t0)
                            for dc in range(2):
                                b = 6 + dc
                                op("pe", lambda e, dc=dc, b=b, n_=n_: e.matmul(
                                    ps[b][:, :], lhsT=kd[:, n_, dc * 128:(dc + 1) * 128], rhs=vh[:, n_, :], start=True, stop=True),
                                   ["kd", "U1"], [PS(b)])
                                op("dve", lambda e, dc=dc, b=b, n_=n_: e.scalar_tensor_tensor(
                                    out=Sf[:, dc, :], in0=Sf[:, dc, :], scalar=dec[:, dc, n_:n_ + 1], in1=ps[b][:, :],
                                    op0=ALU.mult, op1=ALU.add), ["Sf", "dec", PS(b)], ["Sf"])
                            if n_ < 7:
                                op("act", lambda e: e.activation(out=Sb[:], in_=Sf[:], func=AF.Copy), ["Sf"], ["Sb"])
                            if main:
                                sample_seq(h, 2 * n_)
                                sample_seq(h, 2 * n_ + 1)
                        if main:
                            dma("sp", gla_p[h].rearrange("(c p) v -> p c v", p=128), Sf[:], reads=["Sf"])
                            sample_post(h)
                        else:
                            dma("sp", s_state[h].rearrange("(c p) v -> p c v", p=128), Sf[:], reads=["Sf"],
                                writes=["s_state%d" % h])

                def finish_o(h, ti, rows, t0, ob=5):
                    op("dve", lambda e: e.bn_stats(out=st6[0:rows, 0:6], in_=ps[ob][0:rows, :]), [PS(ob)], ["st6"])
                    op("dve", lambda e: e.bn_aggr(out=st6[0:rows, 6:8], in_=st6[0:rows, 0:6]), ["st6"], ["st6"])
                    op("act", lambda e: e.activation(out=st6[0:rows, 0:1], in_=st6[0:rows, 7:8], func=AF.Sqrt, bias=EPS),
                       ["st6"], ["st6"])
                    op("dve", lambda e: e.reciprocal(out=st6[0:rows, 1:2], in_=st6[0:rows, 0:1]), ["st6"], ["st6"])
                    op("dve", lambda e: e.tensor_scalar(out=of[0:rows, :], in0=ps[ob][0:rows, :], scalar1=st6[0:rows, 6:7],
                                                        scalar2=st6[0:rows, 1:2], op0=ALU.subtract, op1=ALU.mult),
                       [PS(ob), "st6"], ["of"])
                    op("dve", lambda e: e.tensor_tensor(out=og[0:rows, :], in0=of[0:rows, :], in1=gr[0:rows, ti, :], op=ALU.mult),
                       ["of", "U1"], ["hb_og"])
                    pv = ps[4][:].bitcast(BF16).rearrange("p (a b) -> p a b", a=8)

                    def tr(e):
                        for j in range(4):
                            ins = e.transpose(out=pv[:, j, 0:rows], in_=og[0:rows, j * 128:(j + 1) * 128],
                                              identity=ident_b[0:rows, 0:rows])
                        return ins
                    op("pe", tr, ["hb_og", "ident_b"], [PS(4)])
                    op("act", lambda e: e.activation(out=ogT[:, :, 0:rows], in_=pv[:, 0:4, 0:rows], func=AF.Copy), [PS(4)], ["hb_ogT"])
                    dma("sp", s_ogT[h * 512:(h + 1) * 512, t0:t0 + rows].rearrange("(c p) t -> p c t", p=128),
                        ogT[:, :, 0:rows], reads=["hb_ogT"], writes=["s_ogT"])

                sTs = hb[:, 3328:3392]

                def sample_pre(h):
                    def mm_s(e):
                        for dc in range(2):
                            ins = e.matmul(ps[0][0:64, 0:64], lhsT=keT[:, dc, NP:NT], rhs=qeT[:, dc, NP:NT],
                                           start=(dc == 0), stop=(dc == 1))
                        return ins
                    op("pe", mm_s, ["keT", "qeT"], [PS(0)])
                    op("dve", lambda e: e.tensor_tensor(out=sTs[0:64, 0:64], in0=ps[0][0:64, 0:64], in1=cst[0:64, C_MS:C_MS + 64],
                                                        op=ALU.mult), [PS(0), "cst"], ["hb_sTs"])

                def sample_seq(h, sq):
                    op("dve", lambda e, sq=sq: e.tensor_tensor(
                        out=QMs[:], in0=qeT[:, :, NP:NT], in1=bm_b[:, sq, :].unsqueeze(1).to_broadcast([128, 2, 64]),
                        op=ALU.mult), ["qeT", "bm_b"], ["QMs"])
                    op("dve", lambda e, sq=sq: e.tensor_scalar(out=kdm[:], in0=kd[0:64, 8, :],
                                                                scalar1=cst[0:64, C_RM + sq:C_RM + sq + 1], scalar2=None,
                                                                op0=ALU.mult), ["kd", "cst"], ["kdm"])
                    for dc in range(2):
                        bi = dc
                        src = st_gla[sq, h, dc * 128:(dc + 1) * 128, :]
                        dma("sp", s0f[bi][:], src, writes=["s0f%d" % bi])
                        dma("pool", s0b[bi][:], src, writes=["s0b%d" % bi])

                        def mm_o(e, sq=sq, dc=dc, bi=bi):
                            if sq == 0 and dc == 0:
                                e.matmul(ps[1][0:64, :], lhsT=sTs[0:64, 0:64], rhs=vh[0:64, 8, :], start=True, stop=False)
                            return e.matmul(ps[1][0:64, :], lhsT=QMs[:, dc, :], rhs=s0b[bi][:, :],
                                            start=False, stop=(sq == NSEQ - 1 and dc == 1))
                        op("pe", mm_o, ["hb_sTs", "U1", "QMs", "s0b%d" % bi], [PS(1)])
                        b = 2 + dc
                        op("pe", lambda e, dc=dc, b=b: e.matmul(ps[b][:, :], lhsT=kdm[:, dc * 128:(dc + 1) * 128],
                                                                 rhs=vh[0:64, 8, :], start=True, stop=True),
                           ["kdm", "U1"], [PS(b)])
                        op("dve", lambda e, dc=dc, b=b, sq=sq, bi=bi: e.scalar_tensor_tensor(
                            out=sout[bi][:, :], in0=s0f[bi][:, :], scalar=dec[:, dc, 8 + sq:9 + sq], in1=ps[b][:, :],
                            op0=ALU.mult, op1=ALU.add), ["s0f%d" % bi, "dec", PS(b)], ["sout%d" % bi])
                        dma("sp", gla_s[sq, h, dc * 128:(dc + 1) * 128, :], sout[bi][:], reads=["sout%d" % bi])

                def sample_post(h):
                    finish_o(h, 8, 64, NP, ob=1)

                stage_end(0)
                gla_pass(False)
                stage_end(1)
                gla_pass(True)
                stage_end(2)

            with contextlib.ExitStack() as mg_es:
                ogTa = sb("ogTa", [128, 16, NT], BF16, mg_es)
                sga = sb("sga", [128, 512], F32, mg_es)
                mo = sb("mo", [128, 2, NT], BF16, mg_es)
                dma("sp", ogTa[:], s_ogT.rearrange("(c p) t -> p c t", p=128), reads=["s_ogT"], writes=["ogTa"])
                sgs = sb("sgs", [128, 2, NT], BF16, mg_es)
                for m in range(16):
                    wga, kga = wnext()

                    def ev_ga(mm_, gi, t0, n, pap, pkey):
                        op("act", lambda e: e.activation(out=sgs[:, mm_, t0:t0 + n], in_=pap, func=AF.Sigmoid), [pkey], ["sgs"])
                    gemm_ws(wga, kga, 32, 256, hT, "hT", NGRP, ev_ga, [0, 1, 2, 3])
                    wba, kba = wnext()

                    def ev_ba(mm_, gi, t0, n, pap, pkey):
                        op("dve", lambda e: e.tensor_tensor(out=mo[:, mm_, t0:t0 + n], in0=pap, in1=sgs[:, mm_, t0:t0 + n], op=ALU.mult),
                           [pkey, "sgs"], ["mo"])
                    gemm_ws(wba, kba, 16, 256, ogTa, "ogTa", NGRP, ev_ba, [4, 5, 6, 7])
                    dma("sp", s_mT[m * 256:(m + 1) * 256, :].rearrange("(c p) t -> p c t", p=128), mo[:], reads=["mo"],
                        writes=["s_mT"])

            stage_end(3)
            cv_es = contextlib.ExitStack()
            with cv_es:
                cT = sb("cT", [128, 16, NT], BF16, cv_es)
                cpar = sb("cpar", [128, 16, 34], F32, cv_es)
                extP = sb("extP", [128, 30 + NP], F32, cv_es)
                extS = sb("extS", [128, 16, 34], F32, cv_es)
                sig = xt[:, 3136:3648]
                acc = xt[:, 2048:2048 + NT]
                stc = sb("stc", [120, 4, 128], F32, cv_es)
                cvp_tm = sb("cvp_tm", [30, 128], F32, cv_es)
                cvs_tm = sb("cvs_tm", [64, 128], F32, cv_es)
                cpl = xt[0:34, 0:2048]
                dma("sp", cpl, convp_in, writes=[*XTK])
                for j in range(16):
                    b = 4 + j % 2
                    op("pe", lambda e, j=j, b=b: e.transpose(out=ps[b][:, 0:34], in_=xt[0:34, j * 128:(j + 1) * 128],
                                                             identity=ident_f[0:34, 0:34]), [*XTK, "cst"], [PS(b)])
                    op("dve", lambda e, j=j, b=b: e.tensor_copy(out=cpar[:, j, :], in_=ps[b][:, 0:34]), [PS(b)], ["cpar"])
                dma("sp", conv_s[:, 0:26, :], st_conv[:, 4:30, :])
                ugroups = [(0, 512), (512, 512), (1024, 64)]
                uscope = contextlib.ExitStack()
                u1s = sb("u1s", [128, 2, 32 + NT], F32, uscope)
                hbf32 = hb[:].bitcast(F32)
                extPs = [extP[:, :], hbf32[:, 0:30 + NP]]
                extSs = [extS[:, :, :], hbf32[:, 1056:1056 + 544].rearrange("p (s r) -> p s r", r=34)]
                extPk = ["extP", "extP1"]
                extSk = ["extS", "extS1"]
                sigs = [xt[:, 3136:3648], xt[:, 1088:1600]]
                sigk = ["sig0", "sig1"]
                accs = [xt[:, 2048:2048 + NT], xt[:, 0:NT]]
                acck = ["acc0", "acc1"]
                CBAR = [*XTK, *HBK, "extP1", "extS1", "sig0", "sig1", "acc0", "acc1"]
                op("pool", lambda e: e.memset(sm[:, 63:64], 0.0), [], CBAR)
                pending_tail = [None]

                def make_tail(j, par):
                    extPc, extSc, sigc = extPs[par], extSs[par], sigs[par]

                    def tail():
                        op("pe", lambda e: e.transpose(out=ps[7][0:30, 0:128], in_=extPc[:, NP:NP + 30], identity=ident_f),
                           [extPk[par], "cst"], [PS(7)])
                        op("act", lambda e: e.activation(out=cvp_tm[:, :], in_=ps[7][0:30, 0:128], func=AF.Copy), [PS(7)], ["cvp_tm"])
                        dma("sp", conv_p[:, j * 128:(j + 1) * 128], cvp_tm[:, :], reads=["cvp_tm"])
                        op("pool", lambda e: e.tensor_copy(out=sigc[:, 0:64].rearrange("p (s r) -> p s r", r=4), in_=extSc[:, :, 30:34]),
                           [extSk[par]], [sigk[par]])
                        op("pe", lambda e: e.transpose(out=ps[7][0:64, 128:256], in_=sigc[:, 0:64], identity=ident_f),
                           [sigk[par], "cst"], [PS(7)])
                        op("act", lambda e: e.activation(out=cvs_tm[:, :], in_=ps[7][0:64, 128:256], func=AF.Copy), [PS(7)], ["cvs_tm"])
                        for sq in range(NSEQ):
                            dma("sp", conv_s[sq, 26:30, j * 128:(j + 1) * 128], cvs_tm[4 * sq:4 * sq + 4, :], reads=["cvs_tm"])
                    return tail

                for m in range(8):
                    wu1, k1 = wnext()
                    for jj in range(2):
                        for gi, (t0, n) in enumerate([(-32, 32)] + ugroups):
                            src = hT_halo if t0 < 0 else hT
                            skey = "hT_halo" if t0 < 0 else "hT"
                            a0 = 0 if t0 < 0 else t0
                            b1 = (jj * 4 + gi) % 4

                            def mmu1(e, b=b1, a0=a0, n=n, src=src, jj=jj):
                                for k in range(32):
                                    ins = e.matmul(ps[b][:, 0:n], lhsT=wu1[:, k, jj * 128:(jj + 1) * 128], rhs=src[:, k, a0:a0 + n],
                                                   start=(k == 0), stop=(k == 31))
                                return ins
                            op("pe", mmu1, [k1, skey], [PS(b1)])
                            op("act", lambda e, b1=b1, jj=jj, t0=t0, n=n: e.activation(out=u1s[:, jj, 32 + t0:32 + t0 + n], in_=ps[b1][:, 0:n],
                                                                                        func=AF.Copy), [PS(b1)], ["u1s"])
                    wu2, k2 = wnext()
                    for jj in range(2):
                        j = 2 * m + jj
                        par = jj
                        extPc, extSc, sigc, accc = extPs[par], extSs[par], sigs[par], accs[par]
                        kP, kS, kG, kA = extPk[par], extSk[par], sigk[par], acck[par]
                        for g4 in range(4):
                            dma("sp", stc[:, g4, :], st_conv[4 * g4:4 * g4 + 4, :, j * 128:(j + 1) * 128].rearrange("s r c -> (s r) c"),
                                writes=["stc"])
                        for g4 in range(4):
                            op("pe", lambda e, g4=g4: e.transpose(out=ps[6][:, g4 * 120:(g4 + 1) * 120], in_=stc[:, g4, :],
                                                                  identity=ident_f[0:120, 0:120]), ["stc", "cst"], [PS(6)])
                        op("dve", lambda e: e.tensor_copy(out=extSc[:, :, 0:30],
                                                          in_=ps[6][:, 0:480].rearrange("p (s r) -> p s r", r=30)),
                           [PS(6)], [kS])
                        for gi, (t0, n) in enumerate([(-32, 32)] + ugroups):
                            src = hT_halo if t0 < 0 else hT
                            skey = "hT_halo" if t0 < 0 else "hT"
                            a0 = 0 if t0 < 0 else t0
                            b2 = gi % 4

                            def mmu(e, wv, b, a0=a0, n=n, src=src, jj=jj):
                                for k in range(32):
                                    ins = e.matmul(ps[b][:, 0:n], lhsT=wv[:, k, jj * 128:(jj + 1) * 128], rhs=src[:, k, a0:a0 + n],
                                                   start=(k == 0), stop=(k == 31))
                                return ins
                            op("pe", lambda e, b2=b2, f=mmu: f(e, wu2, b2), [k2, skey], [PS(b2)])
                            op("act", lambda e, b2=b2, n=n: e.activation(out=sigc[:, 0:n], in_=ps[b2][:, 0:n], func=AF.Sigmoid),
                               [PS(b2)], [kG])
                            if t0 < 0:
                                dst = extPc[:, 0:30]
                                i0 = u1s[:, jj, 2:32]
                                i1 = sigc[:, 2:32]
                                wk_ = kP
                            elif t0 < NP:
                                dst = extPc[:, 30 + t0:30 + t0 + n]
                                i0 = u1s[:, jj, 32 + t0:32 + t0 + n]
                                i1 = sigc[:, 0:n]
                                wk_ = kP
                            else:
                                dst = extSc[:, :, 30:34]
                                i0 = u1s[:, jj, 32 + NP:32 + NT].rearrange("p (s r) -> p s r", r=4)
                                i1 = sigc[:, 0:64].rearrange("p (s r) -> p s r", r=4)
                                wk_ = kS
                            op("dve", lambda e, dst=dst, i0=i0, i1=i1: e.tensor_tensor(out=dst, in0=i0, in1=i1, op=ALU.mult),
                               ["u1s", kG], [wk_])
                        if pending_tail[0] is not None:
                            pending_tail[0]()
                        pending_tail[0] = make_tail(j, par)
                        accS = accc[:, NP:NT].rearrange("p (s r) -> p s r", r=4)
                        op("dve", lambda e, j=j: e.tensor_scalar(out=accc[:, 0:NP], in0=extPc[:, 0:NP], scalar1=cpar[:, j, 0:1],
                                                                  scalar2=cpar[:, j, 31:32], op0=ALU.mult, op1=ALU.add),
                           [kP, "cpar"], [kA])
                        op("dve", lambda e, j=j: e.tensor_scalar(out=accS, in0=extSc[:, :, 0:4], scalar1=cpar[:, j, 0:1],
                                                                  scalar2=cpar[:, j, 31:32], op0=ALU.mult, op1=ALU.add),
                           [kS, "cpar"], [kA])
                        for tp in range(1, 31):
                            op("dve", lambda e, j=j, tp=tp: e.scalar_tensor_tensor(
                                out=accc[:, 0:NP], in0=extPc[:, tp:tp + NP], scalar=cpar[:, j, tp:tp + 1], in1=accc[:, 0:NP],
                                op0=ALU.mult, op1=ALU.add), [kP, "cpar", kA], [kA])
                            op("dve", lambda e, j=j, tp=tp: e.scalar_tensor_tensor(
                                out=accS, in0=extSc[:, :, tp:tp + 4], scalar=cpar[:, j, tp:tp + 1], in1=accS,
                                op0=ALU.mult, op1=ALU.add), [kS, "cpar", kA], [kA])
                        op("act", lambda e, j=j: e.activation(out=cT[:, j, :], in_=accc[:, :], func=AF.Copy), [kA], ["cT"])
                pending_tail[0]()
                op("pool", lambda e: e.memset(sm[:, 63:64], 0.0), [], CBAR)
                uscope.close()
                with contextlib.ExitStack() as ln_es:
                    sq_t = sb("sq_t", [128, 512], BF16, ln_es)
                    mu = sb("mu", [128, 512], F32, ln_es)
                    rs = sb("rs", [128, 512], F32, ln_es)
                    tmpf = sb("tmpf", [128, 512], F32, ln_es)
                    for gi, (t0, n) in enumerate(NGRP):
                        def mm1(e, t0=t0, n=n):
                            for j in range(16):
                                ins = e.matmul(ps[0][:, 0:n], lhsT=ones_b[:], rhs=cT[:, j, t0:t0 + n], start=(j == 0), stop=(j == 15))
                            return ins
                        op("pe", mm1, ["ones_b", "cT"], [PS(0)])
                        for j in range(16):
                            op("dve", lambda e, j=j, t0=t0, n=n: e.tensor_tensor(out=sq_t[:, 0:n], in0=cT[:, j, t0:t0 + n],
                                                                                  in1=cT[:, j, t0:t0 + n], op=ALU.mult),
                               ["cT"], ["sq_t"])
                            op("pe", lambda e, j=j, n=n: e.matmul(ps[1][:, 0:n], lhsT=ones_b[:], rhs=sq_t[:, 0:n],
                                                                  start=(j == 0), stop=(j == 15)), ["ones_b", "sq_t"], [PS(1)])
                        op("act", lambda e, n=n: e.activation(out=mu[:, 0:n], in_=ps[0][:, 0:n], func=AF.Copy, scale=1.0 / 2048),
                           [PS(0)], ["mu"])
                        op("dve", lambda e, n=n: e.tensor_tensor(out=tmpf[:, 0:n], in0=mu[:, 0:n], in1=mu[:, 0:n], op=ALU.mult),
                           ["mu"], ["tmpf"])
                        op("dve", lambda e, n=n: e.scalar_tensor_tensor(out=rs[:, 0:n], in0=ps[1][:, 0:n], scalar=1.0 / 2048,
                                                                         in1=tmpf[:, 0:n], op0=ALU.mult, op1=ALU.subtract),
                           [PS(1), "tmpf"], ["rs"])
                        op("act", lambda e, n=n: e.activation(out=rs[:, 0:n], in_=rs[:, 0:n], func=AF.Sqrt, bias=EPS), ["rs"], ["rs"])
                        op("dve", lambda e, n=n: e.reciprocal(out=rs[:, 0:n], in_=rs[:, 0:n]), ["rs"], ["rs"])
                        for j in range(16):
                            op("dve", lambda e, j=j, t0=t0, n=n: e.tensor_tensor(out=tmpf[:, 0:n], in0=cT[:, j, t0:t0 + n],
                                                                                  in1=mu[:, 0:n], op=ALU.subtract),
                               ["cT", "mu"], ["tmpf"])
                            op("dve", lambda e, n=n: e.tensor_tensor(out=tmpf[:, 0:n], in0=tmpf[:, 0:n], in1=rs[:, 0:n], op=ALU.mult),
                               ["tmpf", "rs"], ["tmpf"])
                            op("act", lambda e, j=j, t0=t0, n=n: e.activation(out=cT[:, j, t0:t0 + n], in_=tmpf[:, 0:n], func=AF.Silu,
                                                                               scale=cpar[:, j, 32:33], bias=cpar[:, j, 33:34]),
                               ["tmpf", "cpar"], ["cT"])
                stage_end(4)
                with contextlib.ExitStack() as mg_es:
                    xtb2 = xt[:].bitcast(BF16)
                    mab = xtb2[:, 0:2 * NT].rearrange("p (c t) -> p c t", c=2)
                    mo = xtb2[:, 2 * NT:4 * NT].rearrange("p (c t) -> p c t", c=2)
                    sgs2 = xt[:, 2304:2304 + NT].bitcast(BF16).rearrange("p (c t) -> p c t", c=2)
                    for m in range(16):
                        wgb, kgb = wnext()
                        dma("sp", mab[:], s_mT[m * 256:(m + 1) * 256, :].rearrange("(c p) t -> p c t", p=128), reads=["s_mT"],
                            writes=["xt_E1"])

                        def ev_gb(mm_, gi, t0, n, pap, pkey):
                            op("act", lambda e: e.activation(out=sgs2[:, mm_, t0:t0 + n], in_=pap, func=AF.Sigmoid), [pkey], ["xt_E3"])
                        gemm_ws(wgb, kgb, 32, 256, hT, "hT", NGRP, ev_gb, [0, 1, 2, 3])
                        wbb, kbb = wnext()

                        def ev_bb(mm_, gi, t0, n, pap, pkey):
                            op("dve", lambda e: e.tensor_tensor(out=sgs2[:, mm_, t0:t0 + n], in0=pap, in1=sgs2[:, mm_, t0:t0 + n], op=ALU.mult),
                               [pkey, "xt_E3"], ["xt_E3"])
                            op("dve", lambda e: e.tensor_tensor(out=mo[:, mm_, t0:t0 + n], in0=sgs2[:, mm_, t0:t0 + n], in1=mab[:, mm_, t0:t0 + n],
                                                                op=ALU.add), ["xt_E3", "xt_E1"], ["xt_E2"])
                        gemm_ws(wbb, kbb, 16, 256, cT, "cT", NGRP, ev_bb, [4, 5, 6, 7])
                        dma("sp", s_mT[m * 256:(m + 1) * 256, :].rearrange("(c p) t -> p c t", p=128), mo[:], reads=["xt_E2", "xt_E1"],
                            writes=["s_mT"])

        stage_end(5)
        late = contextlib.ExitStack()
        with late:
            xres = [sb("xres%d" % i, [128, 512], F32, late) for i in range(3)]
            dma("sp", hT[:], s_mT.rearrange("(c p) t -> p c t", p=128), reads=["s_mT"], writes=["hT"])
            rcount = [0]

            def resid_gemm(wview, wkey, actT, act_key, src_fn, src_key, dst, c0, ncols, dst_key):
                def ev(ti, t0, rows, pap, pkey):
                    bi = rcount[0] % 3
                    rcount[0] += 1
                    dma("sp", xres[bi][0:rows, 0:ncols], src_fn(t0, rows, c0, ncols), reads=[src_key], writes=["xres%d" % bi])
                    op("dve", lambda e: e.tensor_tensor(out=xres[bi][0:rows, 0:ncols], in0=pap, in1=xres[bi][0:rows, 0:ncols], op=ALU.add),
                       [pkey, "xres%d" % bi], ["xres%d" % bi])
                    dma("sp", dst[t0:t0 + rows, c0:c0 + ncols], xres[bi][0:rows, 0:ncols], reads=["xres%d" % bi],
                        writes=[dst_key])
                gemm_as([(wview, 0)], [wkey], actT, act_key, TILES, ncols, ev, [0, 1, 2, 3])

            def xsrc(t0, rows, c0, ncols):
                if t0 < NP:
                    return x_main[t0:t0 + rows, c0:c0 + ncols]
                return x_s[:, c0:c0 + ncols]
            for cb in range(16):
                w0, k0 = wnext()
                resid_gemm(w0, k0, hT, "hT", xsrc, "x_in", s_x1, cb * 256, 256, "s_x1")

            stage_end(6)
            ca = contextlib.ExitStack()
            with ca:
                mvb = sb("mvb", [128, 2, 1024], BF16, ca)
                mkT = sb("mkT", [128, 8, 256], BF16, ca)
                q2T = sb("q2T", [128, 8, NT], BF16, ca)
                oT = q2T
                kvf = sb("kvf", [128, 512], F32, ca)
                mscope = contextlib.ExitStack()
                mT = sb("mT", [128, 32, 256], BF16, mscope)
                load_gain(norm_mem_g)
                for i in range(2):
                    norm_tile(mem[i * 128:(i + 1) * 128, :], 128, i * 128, mT, "mT")
                stage_end(6.05)
                for which, dst_o in ((0, mk_o), (1, mv_o)):
                    for cb in range(4):
                        w0, k0 = wnext()

                        def ev_kv(ti, t0, rows, pap, pkey, which=which, cb=cb, dst_o=dst_o):
                            kb = "kvf%d" % (ti % 2)
                            kv_ = kvf[:, (ti % 2) * 256:(ti % 2) * 256 + 256]
                            op("act", lambda e: e.activation(out=kv_, in_=pap, func=AF.Copy), [pkey], [kb])
                            if which == 1:
                                op("dve", lambda e: e.tensor_copy(out=mvb[:, ti, cb * 256:(cb + 1) * 256], in_=kv_), [kb], ["mvb"])
                            dma("sp", dst_o[t0:t0 + 128, cb * 256:(cb + 1) * 256], kv_, reads=[kb])
                        gemm_as([(w0, 0)], [k0], mT, "mT", [(0, 128), (128, 128)], 256, ev_kv, [0, 1, 2, 3])
                stage_end(6.1)
                for m in range(4):
                    wv, wk = wnext()

                    def ev_mk(mm_, gi, t0, n, pap, pkey, m=m):
                        op("act", lambda e: e.activation(out=mkT[:, 2 * m + mm_, :], in_=pap, func=AF.Copy), [pkey], ["mkT"])
                    gemm_ws(wv, wk, 32, 256, mT, "mT", [(0, 256)], ev_mk, [0, 1, 2, 3])
                stage_end(6.2)
                mscope.close()
                load_gain(norm_ca_g)
                for ti, (t0, rows) in enumerate(TILES):
                    norm_tile(s_x1[t0:t0 + rows, :], rows, t0, hT, "hT", src_reads=["s_x1"])
                for m in range(4):
                    wv, wk = wnext()

                    def ev_q2(mm_, gi, t0, n, pap, pkey, m=m):
                        op("act", lambda e: e.activation(out=q2T[:, 2 * m + mm_, t0:t0 + n], in_=pap, func=AF.Copy), [pkey], ["q2T"])
                    gemm_ws(wv, wk, 32, 256, hT, "hT", NGRP, ev_q2, [0, 1, 2, 3])

                stage_end(6.4)
                at = contextlib.ExitStack()
                with at:
                    pex = sb("pex", [128, 4, 256], F32, at)
                    pbf = sb("pbf", [128, 4, 256], BF16, at)
                    pT = hb[:].rearrange("p (s h t) -> p s h t", s=2, h=4)
                    mx = sb("mx", [128, 16], F32, at)
                    xtb3 = xt[:].bitcast(BF16)
                    kc = [xtb3[:, i * 2048:(i + 1) * 2048].rearrange("p (c d) -> p c d", c=2) for i in range(2)]
                    vc = [xtb3[:, 4096 + i * 2048:4096 + (i + 1) * 2048].rearrange("p (c d) -> p c d", c=2) for i in range(2)]
                    kTs = sb("kTs", [128, 8, 256], BF16, at)
                    Q2M = sb("Q2M", [128, 8, 64], BF16, at)
                    pTm = sb("pTm", [128, 2, 4, 64], BF16, at)
                    osb = sb("osb", [64, 1024], BF16, at)

                    def softmax_rows(rows, banks):
                        for h in range(4):
                            sc = ps[banks[h // 2]][0:rows, (h % 2) * 256:(h % 2) * 256 + 256]
                            op("dve", lambda e, h=h, sc=sc: e.reduce_max(out=mx[0:rows, h:h + 1], in_=sc, axis=AX.X),
                               [PS(banks[h // 2])], ["mx"])
                        op("dve", lambda e: e.tensor_scalar(out=mx[0:rows, 4:8], in0=mx[0:rows, 0:4], scalar1=-1.0 / 16.0,
                                                            scalar2=None, op0=ALU.mult), ["mx"], ["mx"])
                        for h in range(4):
                            sc = ps[banks[h // 2]][0:rows, (h % 2) * 256:(h % 2) * 256 + 256]
                            op("act", lambda e, h=h, sc=sc: e.activation(out=pex[0:rows, h, :], in_=sc, func=AF.Exp, scale=1.0 / 16.0,
                                                                          bias=mx[0:rows, 4 + h:5 + h], accum_out=mx[0:rows, 8 + h:9 + h]),
                               [PS(banks[h // 2]), "mx"], ["pex", "mx"])
                        op("dve", lambda e: e.reciprocal(out=mx[0:rows, 12:16], in_=mx[0:rows, 8:12]), ["mx"], ["mx"])
                        for h in range(4):
                            op("dve", lambda e, h=h: e.tensor_scalar(out=pbf[0:rows, h, :], in0=pex[0:rows, h, :],
                                                                      scalar1=mx[0:rows, 12 + h:13 + h], scalar2=None, op0=ALU.mult),
                               ["pex", "mx"], ["pbf"])

                    for gq in range(2):
                        for tl in range(4):
                            t0 = gq * 512 + tl * 128

                            def mm_sc(e, t0=t0):
                                for h in range(4):
                                    for dc in range(2):
                                        ins = e.matmul(ps[h // 2][:, (h % 2) * 256:(h % 2) * 256 + 256], lhsT=q2T[:, 2 * h + dc, t0:t0 + 128],
                                                       rhs=mkT[:, 2 * h + dc, :], start=(dc == 0), stop=(dc == 1))
                                return ins
                            op("pe", mm_sc, ["q2T", "mkT"], [PS(0), PS(1)])
                            softmax_rows(128, [0, 1])
                            pv = ps[2][:].bitcast(BF16).rearrange("p (a b) -> p a b", a=8)

                            def trp(e):
                                for h in range(4):
                                    for sc_ in range(2):
                                        ins = e.transpose(out=pv[:, sc_ * 4 + h, :], in_=pbf[:, h, sc_ * 128:(sc_ + 1) * 128], identity=ident_b[:])
                                return ins
                            op("pe", trp, ["pbf", "ident_b"], [PS(2)])
                            op("act", lambda e, tl=tl: e.activation(out=pT[:, :, :, tl * 128:(tl + 1) * 128],
                                                                     in_=pv.rearrange("p (s h) t -> p s h t", s=2), func=AF.Copy),
                               [PS(2)], ["hb_kdT"])
                        for h in range(4):
                            for dc in range(2):
                                b = 4 + (h * 2 + dc) % 4

                                def mm_ov(e, h=h, dc=dc, b=b):
                                    for sc_ in range(2):
                                        ins = e.matmul(ps[b][:, :], lhsT=mvb[:, sc_, h * 256 + dc * 128:h * 256 + dc * 128 + 128],
                                                       rhs=pT[:, sc_, h, :], start=(sc_ == 0), stop=(sc_ == 1))
                                    return ins
                                op("pe", mm_ov, ["mvb", "hb_kdT"], [PS(b)])
                                op("act", lambda e, h=h, dc=dc, b=b, gq=gq: e.activation(
                                    out=oT[:, 2 * h + dc, gq * 512:(gq + 1) * 512], in_=ps[b][:, :], func=AF.Copy), [PS(b)], ["q2T"])
                    stage_end(6.6)
                    for c8 in range(8):
                        op("dve", lambda e, c8=c8: e.tensor_copy(out=Q2M[:, c8, :], in_=q2T[:, c8, NP:NT]), ["q2T"], ["Q2M"])
                    bmv = bm_b[:]
                    for sq in range(NSEQ):
                        bi = sq % 2
                        dma("pool", kc[bi][:], ck[sq].rearrange("(c p) d -> p c d", p=128), writes=[("xt_E1", "xt_E2")[bi]])
                        for half in range(2):
                            pv = ps[4 + half][:].bitcast(BF16).rearrange("p (a b) -> p a b", a=8)

                            def trk(e, half=half, pv=pv, bi=bi):
                                for c4 in range(4):
                                    c8 = half * 4 + c4
                                    for sc_ in range(2):
                                        ins = e.transpose(out=pv[:, c4 * 2 + sc_, :], in_=kc[bi][:, sc_, c8 * 128:(c8 + 1) * 128],
                                                          identity=ident_b[:])
                                return ins
                            op("pe", trk, [("xt_E1", "xt_E2")[bi], "ident_b"], [PS(4 + half)])
                            op("act", lambda e, half=half, pv=pv: e.activation(
                                out=kTs[:, half * 4:(half + 1) * 4, :].rearrange("p c (s t) -> p c s t", s=2),
                                in_=pv.rearrange("p (c s) t -> p c s t", s=2), func=AF.Copy), [PS(4 + half)], ["kTs"])
                        qm = sb if False else None

                        def mm_ss(e, sq=sq):
                            for h in range(4):
                                for dc in range(2):
                                    ins = e.matmul(ps[h][0:64, 0:256], lhsT=QMs[:, 2 * h + dc, :], rhs=kTs[:, 2 * h + dc, :],
                                                   start=(sq == 0 and dc == 0), stop=(sq == NSEQ - 1 and dc == 1))
                            return ins
                        QMs = sb("QMs%d" % sq, [128, 8, 64], BF16, at) if sq < 2 else QMs_l[sq % 2]
                        if sq < 2:
                            if sq == 0:
                                QMs_l = [QMs, None]
                            else:
                                QMs_l[1] = QMs
                        op("dve", lambda e, sq=sq, QMs=QMs: e.tensor_tensor(
                            out=QMs[:], in0=Q2M[:], in1=bmv[:, sq, :].unsqueeze(1).to_broadcast([128, 8, 64]), op=ALU.mult),
                           ["Q2M", "bm_b"], ["QMs%d" % (sq % 2)])
                        op("pe", mm_ss, ["QMs%d" % (sq % 2), "kTs"], [PS(0), PS(1), PS(2), PS(3)])
                    for h in range(4):
                        op("dve", lambda e, h=h: e.reduce_max(out=mx[0:64, h:h + 1], in_=ps[h][0:64, 0:256], axis=AX.X), [PS(h)], ["mx"])
                    op("dve", lambda e: e.tensor_scalar(out=mx[0:64, 4:8], in0=mx[0:64, 0:4], scalar1=-1.0 / 16.0, scalar2=None,
                                                        op0=ALU.mult), ["mx"], ["mx"])
                    for h in range(4):
                        op("act", lambda e, h=h: e.activation(out=pex[0:64, h, :], in_=ps[h][0:64, 0:256], func=AF.Exp, scale=1.0 / 16.0,
                                                               bias=mx[0:64, 4 + h:5 + h], accum_out=mx[0:64, 8 + h:9 + h]),
                           [PS(h), "mx"], ["pex", "mx"])
                    op("dve", lambda e: e.reciprocal(out=mx[0:64, 12:16], in_=mx[0:64, 8:12]), ["mx"], ["mx"])
                    for h in range(4):
                        op("dve", lambda e, h=h: e.tensor_scalar(out=pbf[0:64, h, :], in0=pex[0:64, h, :], scalar1=mx[0:64, 12 + h:13 + h],
                                                                  scalar2=None, op0=ALU.mult), ["pex", "mx"], ["pbf"])
                    pv = ps[4][:].bitcast(BF16).rearrange("p (a b) -> p a b", a=8)

                    def trps(e):
                        for h in range(4):
                            for sc_ in range(2):
                                ins = e.transpose(out=pv[:, sc_ * 4 + h, 0:64], in_=pbf[0:64, h, sc_ * 128:(sc_ + 1) * 128],
                                                  identity=ident_b[0:64, 0:64])
                        return ins
                    op("pe", trps, ["pbf", "ident_b"], [PS(4)])
                    pTs = sb("pTs", [128, 8, 64], BF16, at)
                    op("act", lambda e: e.activation(out=pTs[:], in_=pv[:, :, 0:64], func=AF.Copy), [PS(4)], ["pTs"])
                    for sq in range(NSEQ):
                        bi = sq % 2
                        dma("pool", vc[bi][:], cv[sq].rearrange("(c p) d -> p c d", p=128), writes=[("xt_E3", "xt")[bi]])
                        op("dve", lambda e, sq=sq: e.tensor_tensor(
                            out=pTm[:].rearrange("p s h t -> p (s h) t"), in0=pTs[:],
                            in1=bmv[:, sq, :].unsqueeze(1).to_broadcast([128, 8, 64]), op=ALU.mult), ["pTs", "bm_b"], ["pTm"])

                        def mm_os(e, sq=sq, bi=bi):
                            for h in range(4):
                                for sc_ in range(2):
                                    ins = e.matmul(ps[h][0:64, 0:256], lhsT=pTm[:, sc_, h, :], rhs=vc[bi][:, sc_, h * 256:(h + 1) * 256],
                                                   start=(sq == 0 and sc_ == 0), stop=(sq == NSEQ - 1 and sc_ == 1))
                            return ins
                        op("pe", mm_os, ["pTm", ("xt_E3", "xt")[bi]], [PS(0), PS(1), PS(2), PS(3)])
                    for h in range(4):
                        op("act", lambda e, h=h: e.activation(out=osb[:, h * 256:(h + 1) * 256], in_=ps[h][0:64, 0:256], func=AF.Copy),
                           [PS(h)], ["osb"])
                    pv = ps[5][:].bitcast(BF16).rearrange("p (a b) -> p a b", a=8)

                    def tro(e):
                        for c8 in range(8):
                            ins = e.transpose(out=pv[:, c8, 0:64], in_=osb[:, c8 * 128:(c8 + 1) * 128], identity=ident_b[0:64, 0:64])
                        return ins
                    op("pe", tro, ["osb", "ident_b"], [PS(5)])
                    op("act", lambda e: e.activation(out=oT[:, :, NP:NT], in_=pv[:, :, 0:64], func=AF.Copy), [PS(5)], ["q2T"])

                stage_end(6.8)
                def x1src(t0, rows, c0, ncols):
                    return s_x1[t0:t0 + rows, c0:c0 + ncols]
                for cb in range(4):
                    w0, k0 = wnext()
                    for sub in range(2):
                        resid_gemm(w0[:, :, sub * 512:(sub + 1) * 512], k0, oT, "q2T", x1src, "s_x1", s_x2, cb * 1024 + sub * 512, 512, "s_x2")

            stage_end(7)
            late.close()
            hscope.close()
            moe = contextlib.ExitStack()
            with moe:
                wsl.append(sb("wsl2", [128, 8192], BF16, moe))
                wr = sb("wr", [128, 32, 36], F32, moe)
                brt = sb("brt", [128, 36], F32, moe)
                xgTbuf = sb("xgTbuf", [128, D], F32, moe)
                hTf = xgTbuf[:].rearrange("p (k t) -> p k t", k=32)
                xgT = xgTbuf[:].bitcast(BF16).rearrange("p (k c) -> p k c", k=32)
                hbf = sb("hbf", [128, D], F32, moe)
                gbcf = sb("gbcf", [128, D], F32, moe)
                lg = sb("lg", [128, 9, 36], F32, moe)
                gate = sb("gate", [128, 9, 32], F32, moe)
                msk = sb("msk", [128, 9, 32], BF16, moe)
                m2f = sb("m2f", [128, 9, 32], F32, moe)
                rank = sb("rank", [128, 9, 32], F32, moe)
                tmp = sb("tmp", [128, 9, 40], F32, moe)
                rhs6 = sb("rhs6", [128, 9, NE, 6], BF16, moe)
                sel = sb("sel", [128, 9, CAP], BF16, moe)
                slot2 = [sb("slot%d" % i, [128, 2, 8], F32, moe) for i in range(2)]
                idxg2 = [sb("idxg%d" % i, [128, 2], U32, moe) for i in range(2)]
                idxs2 = [sb("idxs%d" % i, [128, 2], U32, moe) for i in range(2)]
                xg2 = [sb("xg%d" % i, [128, 2, D], BF16, moe) for i in range(2)]
                xg = xg2[0]
                hidT = sb("hidT", [128, 4, CAP], BF16, moe)
                sgl = sb("sgl", [128, 2, CAP], F32, moe)
                ye = [sb("ye0", [128, D], F32, moe), hbf]
                yek = ["ye0", "hbf"]
                dma("sp", wr[:], w_router.rearrange("(k p) n -> p k n", p=128), writes=["wr"])
                dma("sp", brt[:], b_router.partition_broadcast(128), writes=["brt"])
                op("pool", lambda e: e.memset(hb[0:1, :], 0.0), [], [*HBK])
                dma("sp", s_h3[ZROW:ZROW + 1, :], hb[0:1, :], reads=[*HBK], writes=["s_h3"])
                dma("sp", gbcf[:], norm_ffn_g.partition_broadcast(128), writes=["gbcf"])

                for ti, (t0, rows) in enumerate(TILES):
                    def f32T(rows, ti=ti, t0=t0):
                        op("dve", lambda e: e.scalar_tensor_tensor(out=hbf[0:rows, :], in0=xt[0:rows, :], scalar=sm[0:rows, 2:3],
                                                                   in1=gbcf[0:rows, :], op0=ALU.mult, op1=ALU.mult),
                           [*XTK, "sm", "gbcf"], ["hbf"])
                        op("act", lambda e: e.activation(out=hb[0:rows, :], in_=hbf[0:rows, :], func=AF.Copy), ["hbf"], [*HBK])
                        dma("sp", s_h3[t0:t0 + rows, :], hb[0:rows, :], reads=[*HBK], writes=["s_h3"])
                        for g in range(8):
                            b = 4 + g % 4

                            def tr(e, g=g, b=b):
                                for j in range(4):
                                    c = g * 4 + j
                                    ins = e.transpose(out=ps[b][:, j * 128:j * 128 + rows], in_=hbf[0:rows, c * 128:(c + 1) * 128],
                                                      identity=ident_f[0:rows, 0:rows])
                                return ins
                            op("pe", tr, ["hbf", "cst"], [PS(b)])
                            op("act" if g % 2 == 0 else "dve",
                               (lambda e, g=g, b=b: e.activation(out=hTf[:, g * 4:(g + 1) * 4, 0:rows],
                                                                 in_=ps[b][:].rearrange("p (a t) -> p a t", a=4)[:, :, 0:rows], func=AF.Copy))
                               if g % 2 == 0 else
                               (lambda e, g=g, b=b: e.tensor_copy(out=hTf[:, g * 4:(g + 1) * 4, 0:rows],
                                                                  in_=ps[b][:].rearrange("p (a t) -> p a t", a=4)[:, :, 0:rows])),
                               [PS(b)], ["xgT"])

                        def mmr(e):
                            for k in range(32):
                                ins = e.matmul(ps[3][0:rows, 0:36], lhsT=hTf[:, k, 0:rows], rhs=wr[:, k, :], start=(k == 0), stop=(k == 31))
                            return ins
                        op("pe", mmr, ["xgT", "wr"], [PS(3)])
                        op("dve", lambda e: e.tensor_tensor(out=lg[0:rows, ti, :], in0=ps[3][0:rows, 0:36], in1=brt[0:rows, :], op=ALU.add),
                           [PS(3), "brt"], ["lg"])
                    norm_tile(s_x2[t0:t0 + rows, :], rows, t0, None, None, fp32_T=f32T, src_reads=["s_x2"])
                op("pool", lambda e: e.memset(lg[64:128, 8, :], -30000.0), [], ["lg"]) if False else None
                R_ = ["lg", "tmp", "gate", "msk", "m2f"]
                lgg = lg[:, :, 0:4]
                lge = lg[:, :, 4:36].rearrange("p t (g e) -> p t g e", g=4)
                T0, T1, T2, T3, T4, T5 = (tmp[:, :, i:i + 1] for i in range(6))
                gm = tmp[:, :, 8:12]
                op("dve", lambda e: e.tensor_reduce(out=tmp[:, :, 0:1], in_=lgg, axis=AX.X, op=ALU.max), ["lg"], ["tmp"])
                op("dve", lambda e: e.tensor_tensor(out=tmp[:, :, 12:16], in0=lgg, in1=T0.to_broadcast([128, 9, 4]), op=ALU.subtract),
                   ["lg", "tmp"], ["tmp"])
                op("dve", lambda e: e.tensor_single_scalar(out=gm, in_=tmp[:, :, 12:16], scalar=0.0, op=ALU.is_ge), ["tmp"], ["tmp"])
                op("act", lambda e: e.activation(out=tmp[:, :, 16:20], in_=tmp[:, :, 12:16], func=AF.Exp), ["tmp"], ["tmp"])
                op("dve", lambda e: e.tensor_reduce(out=tmp[:, :, 1:2], in_=tmp[:, :, 16:20], axis=AX.X, op=ALU.add), ["tmp"], ["tmp"])
                op("dve", lambda e: e.reciprocal(out=tmp[:, :, 2:3], in_=tmp[:, :, 1:2]), ["tmp"], ["tmp"])
                op("dve", lambda e: e.tensor_scalar(out=tmp[:, :, 20:24], in0=gm, scalar1=-1.0, scalar2=10000.0, op0=ALU.add, op1=ALU.mult),
                   ["tmp"], ["tmp"])
                lem = m2f[:].rearrange("p t (g e) -> p t g e", g=4)
                op("dve", lambda e: e.tensor_tensor(out=lem, in0=lge, in1=tmp[:, :, 20:24].unsqueeze(3).to_broadcast([128, 9, 4, 8]),
                                                    op=ALU.add), ["lg", "tmp"], ["m2f"])
                op("dve", lambda e: e.tensor_reduce(out=tmp[:, :, 3:4], in_=m2f[:], axis=AX.X, op=ALU.max), ["m2f"], ["tmp"])
                op("dve", lambda e: e.tensor_tensor(out=gate[:], in0=m2f[:], in1=T3.to_broadcast([128, 9, 32]), op=ALU.is_ge),
                   ["m2f", "tmp"], ["gate"])
                op("dve", lambda e: e.scalar_tensor_tensor(out=rank[:].rearrange("p t e -> p (t e)"), in0=gate[:].rearrange("p t e -> p (t e)"),
                                                           scalar=-20000.0, in1=m2f[:].rearrange("p t e -> p (t e)"), op0=ALU.mult, op1=ALU.add),
                   ["gate", "m2f"], ["rank"])
                op("dve", lambda e: e.tensor_reduce(out=tmp[:, :, 4:5], in_=rank[:], axis=AX.X, op=ALU.max), ["rank"], ["tmp"])
                op("dve", lambda e: e.tensor_tensor(out=m2f[:], in0=rank[:], in1=T4.to_broadcast([128, 9, 32]), op=ALU.is_ge),
                   ["rank", "tmp"], ["m2f"])
                op("dve", lambda e: e.tensor_tensor(out=tmp[:, :, 5:6], in0=T3, in1=T4, op=ALU.subtract), ["tmp"], ["tmp"])
                op("act", lambda e: e.activation(out=tmp[:, :, 6:7], in_=tmp[:, :, 5:6], func=AF.Sigmoid), ["tmp"], ["tmp"])
                op("dve", lambda e: e.tensor_tensor(out=tmp[:, :, 24:25], in0=tmp[:, :, 6:7], in1=tmp[:, :, 2:3], op=ALU.mult), ["tmp"], ["tmp"])
                op("dve", lambda e: e.tensor_tensor(out=tmp[:, :, 25:26], in0=tmp[:, :, 2:3], in1=tmp[:, :, 24:25], op=ALU.subtract),
                   ["tmp"], ["tmp"])
                op("dve", lambda e: e.tensor_tensor(out=msk[:], in0=gate[:], in1=m2f[:], op=ALU.add), ["gate", "m2f"], ["msk"])
                op("dve", lambda e: e.tensor_tensor(out=gate[:], in0=gate[:], in1=tmp[:, :, 24:25].to_broadcast([128, 9, 32]), op=ALU.mult),
                   ["gate", "tmp"], ["gate"])
                op("dve", lambda e: e.tensor_tensor(out=rank[:], in0=m2f[:], in1=tmp[:, :, 25:26].to_broadcast([128, 9, 32]), op=ALU.mult),
                   ["m2f", "tmp"], ["rank"])
                op("dve", lambda e: e.tensor_tensor(out=gate[:], in0=gate[:], in1=rank[:], op=ALU.add), ["gate", "rank"], ["gate"])
                op("pool", lambda e: e.memset(msk[64:128, 8, :], 0.0), [], ["msk"])
                tk = cst[:, C_TK:C_TK + 27].rearrange("p (t c) -> p t c", c=3)
                for c3 in range(3):
                    op("dve", lambda e, c3=c3: e.tensor_tensor(out=rhs6[:, :, :, c3], in0=msk[:],
                                                                in1=tk[:, :, c3:c3 + 1].to_broadcast([128, 9, 32]), op=ALU.mult),
                       ["msk", "cst"], ["rhs6"])
                op("dve", lambda e: e.tensor_tensor(out=rhs6[:, :, :, 3], in0=gate[:], in1=msk[:], op=ALU.mult), ["gate", "msk"], ["rhs6"])
                op("dve", lambda e: e.tensor_tensor(out=rank[:], in0=gate[:], in1=rhs6[:, :, :, 3], op=ALU.subtract), ["gate", "rhs6"], ["rank"])
                op("dve", lambda e: e.tensor_tensor(out=rhs6[:, :, :, 4], in0=rank[:], in1=msk[:], op=ALU.mult), ["rank", "msk"], ["rhs6"])
                op("dve", lambda e: e.tensor_tensor(out=rhs6[:, :, :, 5], in0=m2f[:], in1=msk[:], op=ALU.mult), ["m2f", "msk"], ["rhs6"])
                for ti in range(9):
                    def mmrk(e, ti=ti):
                        for tj in range(ti):
                            e.matmul(ps[4][:, 0:32], lhsT=ones_b[:], rhs=msk[:, tj, :], start=(tj == 0), stop=False)
                        return e.matmul(ps[4][:, 0:32], lhsT=ls_b[:], rhs=msk[:, ti, :], start=(ti == 0), stop=True)
                    op("pe", mmrk, ["ones_b", "ls_b", "msk"], [PS(4)])
                    op("dve", lambda e, ti=ti: e.tensor_copy(out=rank[:, ti, :], in_=ps[4][:, 0:32]), [PS(4)], ["rank"])
                op("dve", lambda e: e.tensor_copy(out=m2f[:], in_=msk[:]), ["msk"], ["m2f"])

                stage_end(8)
                def prepA(ex):
                    slot, idxg, idxs, xg = slot2[ex % 2], idxg2[ex % 2], idxs2[ex % 2], xg2[ex % 2]
                    kS, kG, kI, kX = "slot%d" % (ex % 2), "idxg%d" % (ex % 2), "idxs%d" % (ex % 2), "xg%d" % (ex % 2)
                    for ti in range(9):
                        op("dve", lambda e, ti=ti, ex=ex: e.tensor_scalar(
                            out=sel[:, ti, :], in0=cst[:, C_IO:C_IO + CAP], scalar1=rank[:, ti, ex:ex + 1], scalar2=m2f[:, ti, ex:ex + 1],
                            op0=ALU.is_equal, op1=ALU.mult), ["cst", "rank", "m2f"], ["sel"])
                    for sg in range(CAP // 128):
                        def mmsl(e, sg=sg, ex=ex):
                            for ti in range(9):
                                ins = e.matmul(ps[4][:, sg * 8:sg * 8 + 6], lhsT=sel[:, ti, sg * 128:(sg + 1) * 128], rhs=rhs6[:, ti, ex, :],
                                               start=(ti == 0), stop=(ti == 8))
                            return ins
                        op("pe", mmsl, ["sel", "rhs6"], [PS(4)])
                    op("dve", lambda e: e.tensor_copy(out=slot[:].rearrange("p a b -> p (a b)"), in_=ps[4][:, 0:16]), [PS(4)], [kS])
                    op("dve", lambda e: e.scalar_tensor_tensor(out=slot[:, :, 6], in0=slot[:, :, 0], scalar=32.0, in1=slot[:, :, 1],
                                                               op0=ALU.mult, op1=ALU.add), [kS], [kS])
                    op("dve", lambda e: e.tensor_scalar(out=slot[:, :, 7], in0=slot[:, :, 2], scalar1=-1.0, scalar2=-float(ZROW),
                                                        op0=ALU.add, op1=ALU.mult), [kS], [kS])
                    op("dve", lambda e: e.tensor_tensor(out=slot[:, :, 0], in0=slot[:, :, 6], in1=slot[:, :, 7], op=ALU.add), [kS], [kS])
                    op("dve", lambda e: e.tensor_copy(out=idxg[:], in_=slot[:, :, 0]), [kS], [kG])
                    op("dve", lambda e: e.scalar_tensor_tensor(out=slot[:, :, 1], in0=slot[:, :, 5], scalar=float(NT), in1=slot[:, :, 6],
                                                               op0=ALU.mult, op1=ALU.add), [kS], [kS])
                    op("dve", lambda e: e.tensor_scalar(out=slot[:, :, 7], in0=slot[:, :, 2], scalar1=-1.0, scalar2=-float(DUMP),
                                                        op0=ALU.add, op1=ALU.mult), [kS], [kS])
                    op("dve", lambda e: e.tensor_tensor(out=slot[:, :, 1], in0=slot[:, :, 1], in1=slot[:, :, 7], op=ALU.add), [kS], [kS])
                    op("dve", lambda e: e.tensor_copy(out=idxs[:], in_=slot[:, :, 1]), [kS], [kI])
                    op("dve", lambda e: e.tensor_tensor(out=slot[:, :, 3], in0=slot[:, :, 3], in1=slot[:, :, 4], op=ALU.add), [kS], [kS])
                    for sg in range(CAP // 128):
                        dma("pool", None, None, reads=[kG, "s_h3"], writes=[kX],
                            fn=lambda e, sg=sg: e.indirect_dma_start(
                                out=xg[:, sg, :], out_offset=None, in_=s_h3[:, :],
                                in_offset=bass.IndirectOffsetOnAxis(ap=idxg[:, sg:sg + 1], axis=0)))

                prepA(0)
                for ex in range(NE):
                    if ex + 1 < NE:
                        prepA(ex + 1)
                    slot, idxg, idxs, xg = slot2[ex % 2], idxg2[ex % 2], idxs2[ex % 2], xg2[ex % 2]
                    kS, kG, kI, kX = "slot%d" % (ex % 2), "idxg%d" % (ex % 2), "idxs%d" % (ex % 2), "xg%d" % (ex % 2)
                    for sg in range(CAP // 128):
                        for g in range(4):
                            b = 4 + g % 4
                            pv = ps[b][:].bitcast(BF16).rearrange("p (a b) -> p a b", a=8)

                            def trx(e, sg=sg, g=g, pv=pv):
                                for j in range(8):
                                    c = g * 8 + j
                                    ins = e.transpose(out=pv[:, j, :], in_=xg[:, sg, c * 128:(c + 1) * 128], identity=ident_b[:])
                                return ins
                            op("pe", trx, [kX, "ident_b"], [PS(b)])
                            if g % 2 == 0:
                                op("act", lambda e, sg=sg, g=g, pv=pv: e.activation(
                                    out=xgT[:, g * 8:(g + 1) * 8, sg * 128:(sg + 1) * 128], in_=pv, func=AF.Copy), [PS(b)], ["xgT"])
                            else:
                                op("dve", lambda e, sg=sg, g=g, pv=pv: e.tensor_copy(
                                    out=xgT[:, g * 8:(g + 1) * 8, sg * 128:(sg + 1) * 128], in_=pv), [PS(b)], ["xgT"])
                    for half in range(2):
                        for which in range(2):
                            wv_, kv_ = wnext()
                            for mm_ in range(2):
                                fcn = half * 2 + mm_
                                bq = (which * 2 + mm_) % 4

                                def mmg(e, b=bq, mm_=mm_, wv_=wv_):
                                    for k in range(32):
                                        ins = e.matmul(ps[b][:, 0:CAP], lhsT=wv_[:, k, mm_ * 128:(mm_ + 1) * 128], rhs=xgT[:, k, :],
                                                       start=(k == 0), stop=(k == 31))
                                    return ins
                                op("pe", mmg, [kv_, "xgT"], [PS(bq)])
                                if which == 0:
                                    op("act", lambda e, bq=bq, mm_=mm_: e.activation(out=sgl[:, mm_, :], in_=ps[bq][:, 0:CAP], func=AF.Silu),
                                       [PS(bq)], ["sgl"])
                                else:
                                    op("dve", lambda e, bq=bq, fcn=fcn, mm_=mm_: e.tensor_tensor(out=hidT[:, fcn, :], in0=ps[bq][:, 0:CAP],
                                                                                                 in1=sgl[:, mm_, :], op=ALU.mult),
                                       [PS(bq), "sgl"], ["hidT"])
                    for dh in range(2):
                      wdv, kdv = wnext()
                      for sg in range(CAP // 128):
                        yb = ye[sg % 2]
                        for cb in range(dh * 4, dh * 4 + 4):
                            wv, wk_ = wdv, kdv
                            b = 4 + cb % 4

                            def mmd(e, sg=sg, cb=cb, wv=wv, b=b):
                                for k in range(4):
                                    ins = e.matmul(ps[b][:, :], lhsT=hidT[:, k, sg * 128:(sg + 1) * 128],
                                                   rhs=wv[:, k, (cb % 4) * 512:(cb % 4) * 512 + 512], start=(k == 0), stop=(k == 3))
                                return ins
                            op("pe", mmd, ["hidT", wk_], [PS(b)])
                            if cb % 2 == 0:
                                op("act", lambda e, sg=sg, cb=cb, b=b, yb=yb: e.activation(
                                    out=yb[:, cb * 512:(cb + 1) * 512], in_=ps[b][:, :], func=AF.Copy, scale=slot[:, sg, 3:4]),
                                   [PS(b), kS], [yek[sg % 2]])
                            else:
                                op("dve", lambda e, sg=sg, cb=cb, b=b, yb=yb: e.tensor_scalar(
                                    out=yb[:, cb * 512:(cb + 1) * 512], in0=ps[b][:, :], scalar1=slot[:, sg, 3:4], scalar2=None, op0=ALU.mult),
                                   [PS(b), kS], [yek[sg % 2]])
                        if dh == 1:
                            dma("pool", None, None, reads=[kI, yek[sg % 2]], writes=["s_o12"],
                                fn=lambda e, sg=sg, yb=yb: e.indirect_dma_start(
                                    out=s_o12[:, :], out_offset=bass.IndirectOffsetOnAxis(ap=idxs[:, sg:sg + 1], axis=0),
                                    in_=yb[:, :], in_offset=None))

                stage_end(9)
                dma("sp", gbcf[:], norm_final_g.partition_broadcast(128), writes=["gbcf"])
                fsets = [
                    [(xt[:, :], [*XTK]), (hbf[:, :], ["hbf"]), (ye[0][:, :], ["ye0"])],
                    [(xgTbuf[:, :], ["xgT"]), (xg2[0][:].rearrange("p a d -> p (a d)").bitcast(F32), ["xg0"]),
                     (wsl[2][:].bitcast(F32), ["wsl2"])],
                ]
                for ti, (t0, rows) in enumerate(TILES):
                    (A, kA), (Bf, kB), (Cf, kC) = fsets[ti % 2]
                    dma("sp", A[0:rows, :], s_x2[t0:t0 + rows, :], reads=["s_x2"], writes=kA)
                    dma("sp", Bf[0:rows, :], s_o12[t0:t0 + rows, :], reads=["s_o12"], writes=kB)
                    dma("sp", Cf[0:rows, :], s_o12[NT + t0:NT + t0 + rows, :], reads=["s_o12"], writes=kC)
                    op("dve", lambda e, rows=rows, A=A, Bf=Bf: e.tensor_tensor(out=A[0:rows, :], in0=A[0:rows, :], in1=Bf[0:rows, :], op=ALU.add),
                       [*kA, *kB], kA)
                    op("dve", lambda e, rows=rows, A=A, Cf=Cf: e.tensor_tensor(out=A[0:rows, :], in0=A[0:rows, :], in1=Cf[0:rows, :], op=ALU.add),
                       [*kA, *kC], kA)
                    sc0 = 8 + 4 * (ti % 2)
                    ksm = "smf%d" % (ti % 2)
                    op("act", lambda e, rows=rows, A=A, sc0=sc0: e.activation(out=hb[0:rows, :], in_=A[0:rows, :], func=AF.Square,
                                                                               accum_out=sm[0:rows, sc0:sc0 + 1]), kA, [*HBK, ksm])
                    op("act", lambda e, rows=rows, sc0=sc0: e.activation(out=sm[0:rows, sc0 + 1:sc0 + 2], in_=sm[0:rows, sc0:sc0 + 1], func=AF.Sqrt,
                                                                          scale=1.0 / D, bias=EPS), [ksm], [ksm])
                    op("dve", lambda e, rows=rows, sc0=sc0: e.reciprocal(out=sm[0:rows, sc0 + 2:sc0 + 3], in_=sm[0:rows, sc0 + 1:sc0 + 2]), [ksm], [ksm])
                    op("dve", lambda e, rows=rows, A=A, Bf=Bf, sc0=sc0: e.scalar_tensor_tensor(
                        out=Bf[0:rows, :], in0=A[0:rows, :], scalar=sm[0:rows, sc0 + 2:sc0 + 3],
                        in1=gbcf[0:rows, :], op0=ALU.mult, op1=ALU.mult), [*kA, ksm, "gbcf"], kB)
                    dst = y_main[t0:t0 + rows, :] if ti < 8 else y_s[:, :]
                    dma("sp", dst, Bf[0:rows, :], reads=kB)
      except _Stop:
        S.finish()
        es.pop_all()
        return nc
      S.finish()
    return nc


_NC_CACHE = {}
_RET_MAPS = [False]


def kernel(**inp):
    f = lambda k: np.ascontiguousarray(np.asarray(inp[k], dtype=np.float32))
    x_prompt = f("x_prompt")
    x_sample = f("x_sample")
    consts = make_consts()
    convp = np.ascontiguousarray(np.concatenate(
        [f("conv_dw_w")[0], f("conv_dw_b")[0][None], f("conv_ln_g")[0][None], f("conv_ln_b")[0][None]], axis=0))
    w_router = np.ascontiguousarray(np.concatenate([f("w_router_group")[0], f("w_router_expert")[0]], axis=1))
    b_router = np.ascontiguousarray(np.concatenate([f("b_router_group")[0], f("b_router_expert")[0]], axis=0))
    shared = {
        "consts": consts,
        "norm_mix_g": f("norm_mix_g")[0], "w_in": f("w_in")[0], "w_alpha_up": f("w_alpha_up")[0],
        "b_alpha": f("b_alpha"), "gla_norm_g": f("gla_norm_g")[0], "w_branch_a": f("w_branch_a")[0],
        "convp_in": convp, "w_branch_b": f("w_branch_b")[0], "w_out": f("w_out")[0],
        "norm_ca_g": f("norm_ca_g")[0], "norm_mem_g": f("norm_mem_g")[0], "w_ca_q": f("w_ca_q")[0],
        "w_ca_k": f("w_ca_k")[0], "w_ca_v": f("w_ca_v")[0], "w_ca_o": f("w_ca_o")[0],
        "norm_ffn_g": f("norm_ffn_g")[0], "w_router": w_router, "b_router": b_router,
        "w_exp_gate": f("w_exp_gate")[0], "w_exp_up": f("w_exp_up")[0], "w_exp_down": f("w_exp_down")[0],
        "norm_final_g": f("norm_final_g"),
    }
    st_gla = f("state_gla")[0]
    st_conv = f("state_conv")[0]
    ck = f("cache_mem_k")[0].reshape(128, 256, 1024)
    cv = f("cache_mem_v")[0].reshape(128, 256, 1024)
    mem = f("mem_prompt")
    zeros_pre = np.zeros((NP, D), np.float32)
    in_maps = []
    for c in range(NCORE):
        b, half = c // 2, c % 2
        m = dict(shared)
        m["x_main"] = x_prompt[b, half * NP:(half + 1) * NP]
        m["x_pre"] = x_prompt[b, 0:NP] if half == 1 else zeros_pre
        m["x_s"] = x_sample[c * NSEQ:(c + 1) * NSEQ].reshape(NS, D)
        m["mem"] = mem[b]
        m["st_gla"] = st_gla[c * NSEQ:(c + 1) * NSEQ]
        m["st_conv"] = st_conv[c * NSEQ:(c + 1) * NSEQ]
        m["ck"] = ck[c * NSEQ:(c + 1) * NSEQ]
        m["cv"] = cv[c * NSEQ:(c + 1) * NSEQ]
        in_maps.append(m)
    if _RET_MAPS[0]:
        return in_maps
    if "nc" not in _NC_CACHE:
        _NC_CACHE["nc"] = build()
    nc = _NC_CACHE["nc"]
    res = run_bass_kernel_spmd(nc, in_maps, core_ids=list(range(NCORE)))
    r = res.results
    y_prompt = np.stack([np.concatenate([r[2 * b]["y_main"], r[2 * b + 1]["y_main"]], axis=0) for b in range(4)])
    y_sample = np.concatenate([r[c]["y_s"].reshape(NSEQ, 4, D) for c in range(NCORE)], axis=0)
    gla_prompt = np.stack([r[2 * b + 1]["gla_p"] for b in range(4)])[None]
    conv_prompt = np.stack([r[2 * b + 1]["conv_p"] for b in range(4)])[None]
    mk = np.stack([r[2 * b]["mk_o"].reshape(256, 4, 256) for b in range(4)])[None]
    mv = np.stack([r[2 * b]["mv_o"].reshape(256, 4, 256) for b in range(4)])[None]
    gla_sample = np.concatenate([r[c]["gla_s"] for c in range(NCORE)], axis=0)[None]
    conv_sample = np.concatenate([r[c]["conv_s"] for c in range(NCORE)], axis=0)[None]
    return (y_prompt.astype(np.float32), y_sample.astype(np.float32), gla_prompt.astype(np.float32),
            conv_prompt.astype(np.float32), mk.astype(np.float32), mv.astype(np.float32),
            gla_sample.astype(np.float32), conv_sample.astype(np.float32))
```

```python
import contextlib
import numpy as np
import concourse.bass as bass
import concourse.mybir as mybir
from concourse.bass_utils import run_bass_kernel_spmd

F32 = mybir.dt.float32
BF16 = mybir.dt.bfloat16
U32 = mybir.dt.uint32
I32 = mybir.dt.int32
AF = mybir.ActivationFunctionType
ALU = mybir.AluOpType
AX = mybir.AxisListType

D = 4096
NCORE = 8
NP = 1024
NS = 64
NT = NP + NS
NSEQ = 16
IN_COLS = 18448
OFF_Q, OFF_K, OFF_V, OFF_R, OFF_A, OFF_U, OFF_G = 0, 1024, 2048, 4096, 6144, 6160, 10256
EPS = 1e-6
CAP = 256
NE = 32
ZROW = NT
DUMP = 2 * NT
NGRP = [(0, 512), (512, 512), (1024, 64)]
TILES = [(i * 128, 128) for i in range(8)] + [(1024, 64)]

C_ID, C_MU, C_MS, C_RM, C_IO, C_TK = 0, 128, 256, 320, 336, 592
CWP = 619
C_BM, C_RS, C_LS, C_ON = 619, 1643, 2731, 2859
CW = 2987


def make_consts():
    c = np.zeros((128, CW), np.float32)
    p = np.arange(128)
    c[:, C_ID:C_ID + 128] = np.eye(128)
    c[:, C_MU:C_MU + 128] = (p[:, None] <= p[None, :])
    q = np.arange(64)
    ms = ((q[:, None] // 4) == (q[None, :] // 4)) & (q[:, None] <= q[None, :])
    c[:64, C_MS:C_MS + 64] = ms
    bm = (np.arange(16)[:, None] == (q[None, :] // 4)).astype(np.float32)
    c[:, C_BM:C_BM + 1024] = bm.reshape(1, 1024)
    c[:64, C_RM:C_RM + 16] = ((q[:, None] // 4) == np.arange(16)[None, :])
    t = np.arange(NT)
    rs = np.ones(NT, np.float32)
    rs[:NP][t[:NP] % 128 == 0] = 0.0
    rs[NP:][(t[NP:] - NP) % 4 == 0] = 0.0
    c[:, C_RS:C_RS + NT] = rs[None, :]
    c[:, C_IO:C_IO + 256] = np.arange(256)[None, :]
    for i in range(9):
        tok = i * 128 + p
        c[:, C_TK + 3 * i + 0] = tok // 32
        c[:, C_TK + 3 * i + 1] = tok % 32
        c[:, C_TK + 3 * i + 2] = 1.0
    c[:, C_LS:C_LS + 128] = (p[:, None] < p[None, :])
    c[:, C_ON:C_ON + 128] = 1.0
    return c


class Sched:
    def __init__(self, nc, es, ndma=28):
        self.nc = nc
        self.E = dict(pe=nc.tensor, act=nc.scalar, dve=nc.vector, pool=nc.gpsimd, sp=nc.sync)
        self.sem = {k: es.enter_context(nc.semaphore("c_" + k)) for k in self.E}
        self.cnt = {k: 0 for k in self.E}
        self.seen = {k: {} for k in self.E}
        self.dsem = [es.enter_context(nc.semaphore("dm%d" % i)) for i in range(ndma)]
        self.dval = [0] * ndma
        self.dnext = {"sp": 0, "pool": 0, "act": 0}
        self.dhalf = ndma // 2
        self.res = {}

    def _semobj(self, key):
        return self.sem[key] if isinstance(key, str) else self.dsem[key]

    def _wait(self, eng, tok):
        key, val, src = tok
        if self.seen[eng].get(key, 0) >= val:
            return
        self.E[eng].wait_ge(self._semobj(key), val)
        self.seen[eng][key] = val

    def _deps(self, eng, reads, writes):
        for r in reads:
            st = self.res.get(r)
            if st and st["w"]:
                tok = st["w"]
                if not (tok[2] == eng and eng == "pe"):
                    self._wait(eng, tok)
        for w in writes:
            st = self.res.get(w)
            if st:
                if st["w"] and st["w"][2] != eng:
                    self._wait(eng, st["w"])
                for tok in st["r"]:
                    if tok[2] != eng:
                        self._wait(eng, tok)

    def _commit(self, tok, reads, writes):
        for r in reads:
            st = self.res.setdefault(r, {"w": None, "r": []})
            if tok[2] != "dma":
                st["r"] = [x for x in st["r"] if x[2] != tok[2]]
            st["r"].append(tok)
        for w in writes:
            self.res[w] = {"w": tok, "r": []}

    mute = False

    def op(self, eng, fn, reads=(), writes=()):
        if self.mute:
            return
        self._deps(eng, reads, writes)
        ins = fn(self.E[eng])
        self.cnt[eng] += 1
        ins.then_inc(self.sem[eng], 1)
        self._commit((eng, self.cnt[eng], eng), reads, writes)

    def dma(self, q, out, in_, reads=(), writes=(), fn=None):
        if self.mute:
            return
        base = 0 if q == "pool" else self.dhalf
        i = base + self.dnext[q]
        self.dnext[q] = (self.dnext[q] + 1) % self.dhalf
        if self.dval[i] > 0:
            self._wait(q, (i, self.dval[i], "dma"))
        self._deps(q, reads, writes)
        if fn is None:
            ins = self.E[q].dma_start(out=out, in_=in_)
        else:
            ins = fn(self.E[q])
        ins.then_inc(self.dsem[i], 16)
        self.dval[i] += 16
        self._commit((i, self.dval[i], "dma"), reads, writes)

    def finish(self):
        for i, v in enumerate(self.dval):
            if v > 0:
                self._wait("sp", (i, v, "dma"))
        for k in ("pe", "act", "dve", "pool"):
            if self.cnt[k] > 0:
                self._wait("sp", (k, self.cnt[k], k))


class _Stop(Exception):
    pass


_SB_MIN = [1 << 30]


def build(stage=99, mute_until=None, lite=None):
    nc = bass.Bass("TRN2", target_bir_lowering=False)

    def stage_end(k):
        if mute_until is not None and k >= mute_until:
            S.mute = False
        if stage <= k:
            raise _Stop()

    def din(name, shape, dt=F32):
        kind = "ExternalInput" if (lite is None or name in lite) else "Internal"
        return nc.dram_tensor(name, list(shape), dt, kind=kind).ap()

    def dout(name, shape, dt=F32):
        return nc.dram_tensor(name, list(shape), dt, kind="ExternalOutput").ap()

    def dscr(name, shape, dt):
        return nc.dram_tensor(name, list(shape), dt, kind="Internal").ap()

    x_main = din("x_main", [NP, D])
    x_pre = din("x_pre", [NP, D])
    x_s = din("x_s", [NS, D])
    mem = din("mem", [256, D])
    st_gla = din("st_gla", [NSEQ, 4, 256, 512])
    st_conv = din("st_conv", [NSEQ, 30, 2048])
    ck = din("ck", [NSEQ, 256, 1024])
    cv = din("cv", [NSEQ, 256, 1024])
    consts = din("consts", [128, CW])
    norm_mix_g = din("norm_mix_g", [D])
    w_in = din("w_in", [D, IN_COLS])
    w_alpha_up = din("w_alpha_up", [16, 1024])
    b_alpha = din("b_alpha", [1, 1024])
    gla_norm_g = din("gla_norm_g", [2048])
    w_branch_a = din("w_branch_a", [2048, D])
    convp_in = din("convp_in", [34, 2048])
    w_branch_b = din("w_branch_b", [2048, D])
    w_out = din("w_out", [D, D])
    norm_ca_g = din("norm_ca_g", [D])
    norm_mem_g = din("norm_mem_g", [D])
    w_ca_q = din("w_ca_q", [D, 1024])
    w_ca_k = din("w_ca_k", [D, 1024])
    w_ca_v = din("w_ca_v", [D, 1024])
    w_ca_o = din("w_ca_o", [1024, D])
    norm_ffn_g = din("norm_ffn_g", [D])
    w_router = din("w_router", [D, 36])
    b_router = din("b_router", [36])
    w_exp_gate = din("w_exp_gate", [NE, D, 512])
    w_exp_up = din("w_exp_up", [NE, D, 512])
    w_exp_down = din("w_exp_down", [NE, 512, D])
    norm_final_g = din("norm_final_g", [D])

    y_main = dout("y_main", [NP, D])
    y_s = dout("y_s", [NS, D])
    gla_p = dout("gla_p", [4, 256, 512])
    conv_p = dout("conv_p", [30, 2048])
    mk_o = dout("mk_o", [256, 1024])
    mv_o = dout("mv_o", [256, 1024])
    gla_s = dout("gla_s", [NSEQ, 4, 256, 512])
    conv_s = dout("conv_s", [NSEQ, 30, 2048])

    s_state = dscr("s_state", [4, 256, 512], F32)
    s_ogT = dscr("s_ogT", [2048, NT], BF16)
    s_mT = dscr("s_mT", [D, NT], BF16)
    s_x1 = dscr("s_x1", [NT, D], F32)
    s_x2 = dscr("s_x2", [NT, D], F32)
    s_h3 = dscr("s_h3", [NT + 1, D], BF16)
    s_o12 = dscr("s_o12", [2 * NT + 1, D], F32)

    es = contextlib.ExitStack()
    with es:
      S = Sched(nc, es)
      try:
        pass
        XTK = ["xt", "xt_E1", "xt_E2", "xt_E3"]
        HBK = ["hb", "hb_kdT", "hb_sT", "hb_og", "hb_ogT", "hb_sTs"]
        op, dma = S.op, S.dma

        def sb(name, shape, dt, stack=es):
            t = stack.enter_context(nc.sbuf_tensor(name, list(shape), dt))
            _SB_MIN[0] = min(_SB_MIN[0], nc.sbuf_bytes_remaining)
            return t

        ps = [es.enter_context(nc.psum_tensor("ps%d" % i, [128, 512], F32)) for i in range(8)]

        def PS(i):
            return "ps%d" % i

        cst = sb("cst", [128, CWP], F32)
        ident_b = sb("ident_b", [128, 128], BF16)
        bm_b = sb("bm_b", [128, 16, 64], BF16)
        rs_b = sb("rs_b", [128, NT], BF16)
        ls_b = sb("ls_b", [128, 128], BF16)
        ones_b = sb("ones_b", [128, 128], BF16)
        NSLOT = 2
        wsl = [sb("wsl%d" % i, [128, 8192], BF16) for i in range(NSLOT)]
        gbc = sb("gbc", [128, D], BF16)
        xt = sb("xt", [128, D], F32)
        hb = sb("hb", [128, D], BF16)
        sm = sb("sm", [128, 64], F32)

        dma("sp", cst[:], consts[:, 0:CWP], writes=["cst"])
        dma("sp", xt[:, 0:CW - CWP], consts[:, CWP:CW], writes=[*XTK])
        ident_f = cst[:, C_ID:C_ID + 128]
        op("dve", lambda e: e.tensor_copy(out=ident_b[:], in_=cst[:, C_ID:C_ID + 128]), ["cst"], ["ident_b"])
        op("dve", lambda e: e.tensor_copy(out=bm_b[:].rearrange("p a b -> p (a b)"), in_=xt[:, C_BM - CWP:C_BM - CWP + 1024]), [*XTK], ["bm_b"])
        op("dve", lambda e: e.tensor_copy(out=rs_b[:], in_=xt[:, C_RS - CWP:C_RS - CWP + NT]), [*XTK], ["rs_b"])
        op("dve", lambda e: e.tensor_copy(out=ls_b[:], in_=xt[:, C_LS - CWP:C_LS - CWP + 128]), [*XTK], ["ls_b"])
        op("dve", lambda e: e.tensor_copy(out=ones_b[:], in_=xt[:, C_ON - CWP:C_ON - CWP + 128]), [*XTK], ["ones_b"])
        hscope = contextlib.ExitStack()
        hT = sb("hT", [128, 32, NT], BF16, hscope)
        if mute_until is not None:
            S.mute = True

        wstate = {"n": 0, "plan": [], "issued": 0, "moe0": 10 ** 9}

        def wplan(ap2d):
            wstate["plan"].append(ap2d)

        def slot_of(i):
            m0 = wstate["moe0"]
            if i < m0:
                return i % 2
            return [m0 % 2, 1 - m0 % 2, 2][(i - m0) % 3]

        def look(i):
            return 2 if i < wstate["moe0"] else 3

        def _wissue(i):
            ap2d = wstate["plan"][i]
            K, ncols = ap2d.shape
            kc = K // 128
            slot = slot_of(i)
            view = wsl[slot][:, 0:kc * ncols].rearrange("p (k n) -> p k n", k=kc)
            dma("pool", view, ap2d.rearrange("(k p) n -> p k n", p=128), writes=["wsl%d" % slot])

        def wnext(ap2d_check=None):
            i = wstate["n"]
            wstate["n"] += 1
            if S.mute:
                wstate["issued"] = max(wstate["issued"], wstate["n"])
            while not S.mute and wstate["issued"] < min(len(wstate["plan"]), i + look(i)):
                _wissue(wstate["issued"])
                wstate["issued"] += 1
            ap2d = wstate["plan"][i]
            if ap2d_check is not None:
                assert ap2d.shape == ap2d_check.shape and ap2d.offset == ap2d_check.offset, (i, ap2d, ap2d_check)
            K, ncols = ap2d.shape
            kc = K // 128
            slot = slot_of(i)
            view = wsl[slot][:, 0:kc * ncols].rearrange("p (k n) -> p k n", k=kc)
            return view, "wsl%d" % slot

        def load_gain(g_ap):
            dma("pool", gbc[:], g_ap.partition_broadcast(128), writes=["gbc"])

        def norm_tile(src_ap, rows, col, dstT, dst_key, scratch_out=None, fp32_T=None, src_reads=()):
            dma("sp", xt[0:rows, :], src_ap, reads=list(src_reads), writes=[*XTK])
            op("act", lambda e: e.activation(out=hb[0:rows, :], in_=xt[0:rows, :], func=AF.Square,
                                             accum_out=sm[0:rows, 0:1]), [*XTK], [*HBK, "sm"])
            op("act", lambda e: e.activation(out=sm[0:rows, 1:2], in_=sm[0:rows, 0:1], func=AF.Sqrt,
                                             scale=1.0 / D, bias=EPS), ["sm"], ["sm"])
            op("dve", lambda e: e.reciprocal(out=sm[0:rows, 2:3], in_=sm[0:rows, 1:2]), ["sm"], ["sm"])
            if fp32_T is None:
                op("dve", lambda e: e.scalar_tensor_tensor(out=hb[0:rows, :], in0=xt[0:rows, :], scalar=sm[0:rows, 2:3],
                                                           in1=gbc[0:rows, :], op0=ALU.mult, op1=ALU.mult),
                   [*XTK, "sm", "gbc"], [*HBK])
                if scratch_out is not None:
                    dma("sp", scratch_out, hb[0:rows, :], reads=[*HBK])
                for g in range(4):
                    pv = ps[4 + g][:].bitcast(BF16).rearrange("p (a b) -> p a b", a=8)

                    def tr(e, g=g, pv=pv):
                        for j in range(8):
                            c = g * 8 + j
                            ins = e.transpose(out=pv[:, j, 0:rows], in_=hb[0:rows, c * 128:(c + 1) * 128],
                                              identity=ident_b[0:rows, 0:rows])
                        return ins
                    op("pe", tr, [*HBK, "ident_b"], [PS(4 + g)])
                    eng = "act" if g % 2 == 0 else "dve"
                    if eng == "act":
                        op("act", lambda e, g=g, pv=pv: e.activation(out=dstT[:, g * 8:(g + 1) * 8, col:col + rows],
                                                                      in_=pv[:, :, 0:rows], func=AF.Copy),
                           [PS(4 + g)], [dst_key])
                    else:
                        op("dve", lambda e, g=g, pv=pv: e.tensor_copy(out=dstT[:, g * 8:(g + 1) * 8, col:col + rows],
                                                                       in_=pv[:, :, 0:rows]),
                           [PS(4 + g)], [dst_key])
            else:
                fp32_T(rows)

        def gemm_ws(wview, wkey, kc, mcols, acts, act_key, groups, evac, psbanks, extra_reads=()):
            nb = 0
            for m in range(mcols // 128):
                for gi, (t0, n) in enumerate(groups):
                    b = psbanks[nb % len(psbanks)]
                    nb += 1

                    def mm(e, m=m, t0=t0, n=n, b=b):
                        for k in range(kc):
                            ins = e.matmul(ps[b][:, 0:n], lhsT=wview[:, k, m * 128:(m + 1) * 128],
                                           rhs=acts[:, k, t0:t0 + n], start=(k == 0), stop=(k == kc - 1))
                        return ins
                    op("pe", mm, [wkey, act_key] + list(extra_reads), [PS(b)])
                    evac(m, gi, t0, n, ps[b][:, 0:n], PS(b))

        def gemm_as(wviews, wkeys, actT, act_key, tiles, ncols, evac, psbanks):
            nb = 0
            for ti, (t0, rows) in enumerate(tiles):
                b = psbanks[nb % len(psbanks)]
                nb += 1
                ktot = sum(v.shape[1] for v, _ in wviews)

                def mm(e, t0=t0, rows=rows, b=b):
                    kk = 0
                    for v, koff in wviews:
                        for k in range(v.shape[1]):
                            ins = e.matmul(ps[b][0:rows, 0:ncols], lhsT=actT[:, koff + k, t0:t0 + rows],
                                           rhs=v[:, k, :], start=(kk == 0), stop=(kk == ktot - 1))
                            kk += 1
                    return ins
                op("pe", mm, list(wkeys) + [act_key], [PS(b)])
                evac(ti, t0, rows, ps[b][0:rows, 0:ncols], PS(b))

        def cols(a, n):
            return w_in[:, a:a + n]

        def plan_gla(with_q):
            wplan(cols(OFF_A, 16))
            for h in range(4):
                if with_q:
                    wplan(cols(OFF_Q + 256 * h, 256))
                wplan(cols(OFF_K + 256 * h, 256))
                wplan(cols(OFF_V + 512 * h, 256))
                wplan(cols(OFF_V + 512 * h + 256, 256))
                if with_q:
                    wplan(cols(OFF_R + 512 * h, 256))
                    wplan(cols(OFF_R + 512 * h + 256, 256))
        plan_gla(False)
        plan_gla(True)
        for m in range(16):
            wplan(cols(OFF_G + 256 * m, 256))
            wplan(w_branch_a[:, 256 * m:256 * m + 256])
        for m in range(8):
            wplan(cols(OFF_U + 256 * m, 256))
            wplan(cols(OFF_U + 2048 + 256 * m, 256))
        for m in range(16):
            wplan(cols(OFF_G + D + 256 * m, 256))
            wplan(w_branch_b[:, 256 * m:256 * m + 256])
        for cb in range(16):
            wplan(w_out[:, 256 * cb:256 * cb + 256])
        for wm in (w_ca_k, w_ca_v):
            for cb in range(4):
                wplan(wm[:, 256 * cb:256 * cb + 256])
        for m in range(4):
            wplan(w_ca_k[:, 256 * m:256 * m + 256])
        for m in range(4):
            wplan(w_ca_q[:, 256 * m:256 * m + 256])
        for cb in range(4):
            wplan(w_ca_o[:, 1024 * cb:1024 * cb + 1024])
        wstate["moe0"] = len(wstate["plan"])
        for ex in range(NE):
            wplan(w_exp_gate[ex][:, 0:256])
            wplan(w_exp_up[ex][:, 0:256])
            wplan(w_exp_gate[ex][:, 256:512])
            wplan(w_exp_up[ex][:, 256:512])
            wplan(w_exp_down[ex][:, 0:2048])
            wplan(w_exp_down[ex][:, 2048:4096])

        mix = contextlib.ExitStack()
        with mix:
            alT = sb("alT", [17, NT], BF16, mix)
            walb = sb("walb", [17, 1024], BF16, mix)
            ggl = sb("ggl", [128, 512], F32, mix)
            hT_halo = sb("hT_halo", [128, 32, 32], BF16, mix)
            dma("pool", walb[0:16, :], w_alpha_up, writes=["walb"])
            dma("pool", walb[16:17, :], b_alpha, writes=["walb"])
            load_gain(norm_mix_g)

            gl = contextlib.ExitStack()
            with gl:
                U1 = sb("U1", [128, 4608], F32, gl)
                bT = U1[:, 0:2 * NT].rearrange("p (c t) -> p c t", c=2)
                dfT = U1[:, 2 * NT:4 * NT].rearrange("p (c t) -> p c t", c=2)
                U1b = U1[:].bitcast(BF16)
                vh = U1b[:, 0:4608].rearrange("p (i v) -> p i v", i=9)
                gr = U1b[:, 4608:9216].rearrange("p (i v) -> p i v", i=9)
                xtb = xt[:].bitcast(BF16)
                E1 = xtb[:, 0:2 * NT].rearrange("p (c t) -> p c t", c=2)
                E2 = xtb[:, 2 * NT:4 * NT].rearrange("p (c t) -> p c t", c=2)
                E3 = xtb[:, 4 * NT:6 * NT].rearrange("p (c t) -> p c t", c=2)
                kdT = hb[:, 0:2 * NT].rearrange("p (c t) -> p c t", c=2)
                sT = hb[:, 2 * NT:2 * NT + 128]
                og = hb[:, 2304:2816]
                ogT = hb[:, 2816:3328].rearrange("p (a b) -> p a b", a=4)
                dec = sb("dec", [128, 2, 24], F32, gl)
                qeT = sb("qeT", [128, 2, NT], BF16, gl)
                keT = sb("keT", [128, 2, NT], BF16, gl)
                kd = sb("kd", [128, 9, 256], BF16, gl)
                of = sb("of", [128, 512], F32, gl)
                grf = of
                QMs = sb("QMs", [128, 2, 64], BF16, gl)
                Sf = sb("Sf", [128, 2, 512], F32, gl)
                Sb = sb("Sb", [128, 2, 512], BF16, gl)
                st6 = sb("st6", [128, 8], F32, gl)
                s0f = [sb("s0f%d" % i, [128, 512], F32, gl) for i in range(2)]
                s0b = [sb("s0b%d" % i, [128, 512], BF16, gl) for i in range(2)]
                sout = [sb("sout%d" % i, [128, 512], F32, gl) for i in range(2)]
                kdm = sb("kdm", [64, 256], BF16, gl)

                def gla_pass(main):
                    ntok = NT if main else NP
                    groups = NGRP if main else NGRP[:2]
                    tiles = TILES if main else TILES[:8]
                    for ti, (t0, rows) in enumerate(tiles):
                        if main:
                            src = x_main[t0:t0 + rows, :] if ti < 8 else x_s[:, :]
                        else:
                            src = x_pre[t0:t0 + rows, :]
                        norm_tile(src, rows, t0, hT, "hT")
                    if not main:
                        op("dve", lambda e: e.tensor_copy(out=hT_halo[:], in_=hT[:, :, NP - 32:NP]), ["hT"], ["hT_halo"])
                    wv, wk = wnext()
                    op("pool", lambda e: e.memset(alT[:], 1.0), [], ["alT"])

                    def ev_a(m, gi, t0, n, pap, pkey):
                        op("act", lambda e: e.activation(out=alT[0:16, t0:t0 + n], in_=pap[0:16, :], func=AF.Copy),
                           [pkey], ["alT"])
                    for gi, (t0, n) in enumerate(groups):
                        b = gi % 4

                        def mm(e, t0=t0, n=n, b=b):
                            for k in range(32):
                                ins = e.matmul(ps[b][0:16, 0:n], lhsT=wv[:, k, 0:16], rhs=hT[:, k, t0:t0 + n],
                                               start=(k == 0), stop=(k == 31))
                            return ins
                        op("pe", mm, [wk, "hT"], [PS(b)])
                        ev_a(0, gi, t0, n, ps[b][:, 0:n], PS(b))

                    for h in range(4):
                        for dc in range(2):
                            for gi, (t0, n) in enumerate(groups):
                                b = 4 + (dc * 3 + gi) % 4
                                c0 = (2 * h + dc) * 128
                                op("pe", lambda e, b=b, c0=c0, t0=t0, n=n: e.matmul(
                                    ps[b][:, 0:n], lhsT=walb[:, c0:c0 + 128], rhs=alT[:, t0:t0 + n], start=True, stop=True),
                                   ["walb", "alT"], [PS(b)])
                                op("act", lambda e, b=b, dc=dc, t0=t0, n=n: e.activation(
                                    out=dfT[:, dc, t0:t0 + n], in_=ps[b][:, 0:n], func=AF.Exp, scale=-1.0),
                                   [PS(b)], ["U1"])
                            op("act", lambda e, dc=dc: e.activation(out=dfT[:, dc, 0:ntok], in_=dfT[:, dc, 0:ntok],
                                                                     func=AF.Ln, bias=1.0), ["U1"], ["U1"])
                            op("dve", lambda e, dc=dc: e.tensor_scalar(out=dfT[:, dc, 0:ntok], in0=dfT[:, dc, 0:ntok],
                                                                        scalar1=-1.0 / 16.0, scalar2=None, op0=ALU.mult),
                               ["U1"], ["U1"])
                            op("dve", lambda e, dc=dc: e.tensor_tensor_scan(
                                out=bT[:, dc, 0:ntok], data0=rs_b[:, 0:ntok], data1=dfT[:, dc, 0:ntok],
                                initial=0.0, op0=ALU.mult, op1=ALU.add), ["U1", "rs_b"], ["U1"])
                        bTp = bT[:, :, 0:NP].rearrange("p c (n t) -> p c n t", t=128)
                        dfp = dfT[:, :, 0:NP].rearrange("p c (n t) -> p c n t", t=128)
                        for dc in range(2):
                            op("dve", lambda e, dc=dc: e.tensor_tensor(
                                out=dfp[:, dc], in0=bTp[:, dc, :, 127:128].to_broadcast([128, 8, 128]), in1=bTp[:, dc],
                                op=ALU.subtract), ["U1"], ["U1"])
                            op("act", lambda e, dc=dc: e.activation(out=dec[:, dc, 0:8], in_=bTp[:, dc, :, 127], func=AF.Exp),
                               ["U1"], ["dec"])
                        if main:
                            bTs = bT[:, :, NP:NT].rearrange("p c (n t) -> p c n t", t=4)
                            dfs = dfT[:, :, NP:NT].rearrange("p c (n t) -> p c n t", t=4)
                            for dc in range(2):
                                op("dve", lambda e, dc=dc: e.tensor_tensor(
                                    out=dfs[:, dc], in0=bTs[:, dc, :, 3:4].to_broadcast([128, 16, 4]), in1=bTs[:, dc],
                                    op=ALU.subtract), ["U1"], ["U1"])
                                op("act", lambda e, dc=dc: e.activation(out=dec[:, dc, 8:24], in_=bTs[:, dc, :, 3], func=AF.Exp),
                                   ["U1"], ["dec"])
                        op("act", lambda e: e.activation(out=E3[:, :, 0:ntok], in_=dfT[:, :, 0:ntok], func=AF.Exp),
                           ["U1"], ["xt_E3"])
                        if main:
                            op("act", lambda e: e.activation(out=E1[:, :, 0:ntok], in_=bT[:, :, 0:ntok], func=AF.Exp),
                               ["U1"], ["xt_E1"])
                            op("act", lambda e: e.activation(out=E2[:, :, 0:ntok], in_=bT[:, :, 0:ntok], func=AF.Exp,
                                                             scale=-1.0), ["U1"], ["xt_E2"])
                            wv, wk = wnext()

                            def ev_q(m, gi, t0, n, pap, pkey):
                                op("dve", lambda e: e.scalar_tensor_tensor(
                                    out=qeT[:, m, t0:t0 + n], in0=pap, scalar=1.0 / 16.0, in1=E1[:, m, t0:t0 + n],
                                    op0=ALU.mult, op1=ALU.mult), [pkey, "xt_E1"], ["qeT"])
                            gemm_ws(wv, wk, 32, 256, hT, "hT", groups, ev_q, [0, 1, 2, 3])
                        wv, wk = wnext()

                        def ev_k(m, gi, t0, n, pap, pkey):
                            if main:
                                op("dve", lambda e: e.tensor_tensor(out=keT[:, m, t0:t0 + n], in0=pap,
                                                                    in1=E2[:, m, t0:t0 + n], op=ALU.mult),
                                   [pkey, "xt_E2"], ["keT"])
                            op("dve", lambda e: e.tensor_tensor(out=kdT[:, m, t0:t0 + n], in0=pap,
                                                                in1=E3[:, m, t0:t0 + n], op=ALU.mult),
                               [pkey, "xt_E3"], ["hb_kdT"])
                        gemm_ws(wv, wk, 32, 256, hT, "hT", groups, ev_k, [0, 1, 2, 3])
                        for ti, (t0, rows) in enumerate(tiles):
                            b = 4 + ti % 2
                            pv = ps[b][:].bitcast(BF16)

                            def trk(e, t0=t0, rows=rows, pv=pv):
                                for dc in range(2):
                                    ins = e.transpose(out=pv[0:rows, dc * 128:(dc + 1) * 128], in_=kdT[:, dc, t0:t0 + rows],
                                                      identity=ident_b[:])
                                return ins
                            op("pe", trk, ["hb_kdT", "ident_b"], [PS(b)])
                            op("act", lambda e, ti=ti, rows=rows, pv=pv: e.activation(
                                out=kd[0:rows, ti, :], in_=pv[0:rows, 0:256], func=AF.Copy), [PS(b)], ["kd"])
                        for hf in range(2):
                            wv0, wk0 = wnext()

                            def ev_v(ti, t0, rows, pap, pkey, hf=hf):
                                op("act", lambda e: e.activation(out=vh[0:rows, ti, hf * 256:(hf + 1) * 256], in_=pap, func=AF.Copy),
                                   [pkey], ["U1"])
                            gemm_as([(wv0, 0)], [wk0], hT, "hT", tiles, 256, ev_v, [0, 1, 2, 3])
                        if main:
                            dma("sp", ggl[:], gla_norm_g[h * 512:(h + 1) * 512].partition_broadcast(128), writes=["ggl"])
                            for hf in range(2):
                                wv0, wk0 = wnext()

                                def ev_r(ti, t0, rows, pap, pkey, hf=hf):
                                    op("act", lambda e: e.activation(out=grf[0:rows, 0:256], in_=pap, func=AF.Silu), [pkey], ["of"])
                                    op("dve", lambda e: e.tensor_tensor(out=gr[0:rows, ti, hf * 256:(hf + 1) * 256], in0=grf[0:rows, 0:256],
                                                                        in1=ggl[0:rows, hf * 256:(hf + 1) * 256], op=ALU.mult),
                                       ["of", "ggl"], ["U1"])
                                gemm_as([(wv0, 0)], [wk0], hT, "hT", tiles, 256, ev_r, [0, 1, 2, 3])
                        if main:
                            dma("sp", Sf[:], s_state[h].rearrange("(c p) v -> p c v", p=128), reads=["s_state%d" % h], writes=["Sf"])
                        else:
                            op("pool", lambda e: e.memset(Sf[:], 0.0), [], ["Sf"])
                        op("act", lambda e: e.activation(out=Sb[:], in_=Sf[:], func=AF.Copy), ["Sf"], ["Sb"])
                        if main:
                            sample_pre(h)
                        for n_ in range(8):
                            t0 = n_ * 128
                            if main:
                                def mm_s(e, t0=t0):
                                    for dc in range(2):
                                        ins = e.matmul(ps[4][:, 0:128], lhsT=keT[:, dc, t0:t0 + 128], rhs=qeT[:, dc, t0:t0 + 128],
                                                       start=(dc == 0), stop=(dc == 1))
                                    return ins
                                op("pe", mm_s, ["keT", "qeT"], [PS(4)])
                                op("dve", lambda e: e.tensor_tensor(out=sT[:], in0=ps[4][:, 0:128], in1=cst[:, C_MU:C_MU + 128],
                                                                    op=ALU.mult), [PS(4), "cst"], ["hb_sT"])

                                def mm_o(e, t0=t0, n_=n_):
                                    e.matmul(ps[5][:, :], lhsT=sT[:], rhs=vh[:, n_, :], start=True, stop=False)
                                    for dc in range(2):
                                        ins = e.matmul(ps[5][:, :], lhsT=qeT[:, dc, t0:t0 + 128], rhs=Sb[:, dc, :],
                                                       start=False, stop=(dc == 1))
                                    return ins
                                op("pe", mm_o, ["hb_sT", "U1", "qeT", "Sb"], [PS(5)])
                                finish_o(h, n_, 128, t0)
                            for dc in range(2):
                                b = 6 + dc
                                op("pe", lambda e, dc=dc, b=b, n_=n_: e.matmul(
                                    ps[b][:, :], lhsT=kd[:, n_, dc * 128:(dc + 1) * 128], rhs=vh[:, n_, :], start=True, stop=True),
                                   ["kd", "U1"], [PS(b)])
                                op("dve", lambda e, dc=dc, b=b, n_=n_: e.scalar_tensor_tensor(
                                    out=Sf[:, dc, :], in0=Sf[:, dc, :], scalar=dec[:, dc, n_:n_ + 1], in1=ps[b][:, :],
                                    op0=ALU.mult, op1=ALU.add), ["Sf", "dec", PS(b)], ["Sf"])
                            if n_ < 7:
                                op("act", lambda e: e.activation(out=Sb[:], in_=Sf[:], func=AF.Copy), ["Sf"], ["Sb"])
                            if main:
                                sample_seq(h, 2 * n_)
                                sample_seq(h, 2 * n_ + 1)
                        if main:
                            dma("sp", gla_p[h].rearrange("(c p) v -> p c v", p=128), Sf[:], reads=["Sf"])
                            sample_post(h)
                        else:
                            dma("sp", s_state[h].rearrange("(c p) v -> p c v", p=128), Sf[:], reads=["Sf"],
                                writes=["s_state%d" % h])

                def finish_o(h, ti, rows, t0, ob=5):
                    op("dve", lambda e: e.bn_stats(out=st6[0:rows, 0:6], in_=ps[ob][0:rows, :]), [PS(ob)], ["st6"])
                    op("dve", lambda e: e.bn_aggr(out=st6[0:rows, 6:8], in_=st6[0:rows, 0:6]), ["st6"], ["st6"])
                    op("act", lambda e: e.activation(out=st6[0:rows, 0:1], in_=st6[0:rows, 7:8], func=AF.Sqrt, bias=EPS),
                       ["st6"], ["st6"])
                    op("dve", lambda e: e.reciprocal(out=st6[0:rows, 1:2], in_=st6[0:rows, 0:1]), ["st6"], ["st6"])
                    op("dve", lambda e: e.tensor_scalar(out=of[0:rows, :], in0=ps[ob][0:rows, :], scalar1=st6[0:rows, 6:7],
                                                        scalar2=st6[0:rows, 1:2], op0=ALU.subtract, op1=ALU.mult),
                       [PS(ob), "st6"], ["of"])
                    op("dve", lambda e: e.tensor_tensor(out=og[0:rows, :], in0=of[0:rows, :], in1=gr[0:rows, ti, :], op=ALU.mult),
                       ["of", "U1"], ["hb_og"])
                    pv = ps[4][:].bitcast(BF16).rearrange("p (a b) -> p a b", a=8)

                    def tr(e):
                        for j in range(4):
                            ins = e.transpose(out=pv[:, j, 0:rows], in_=og[0:rows, j * 128:(j + 1) * 128],
                                              identity=ident_b[0:rows, 0:rows])
                        return ins
                    op("pe", tr, ["hb_og", "ident_b"], [PS(4)])
                    op("act", lambda e: e.activation(out=ogT[:, :, 0:rows], in_=pv[:, 0:4, 0:rows], func=AF.Copy), [PS(4)], ["hb_ogT"])
                    dma("sp", s_ogT[h * 512:(h + 1) * 512, t0:t0 + rows].rearrange("(c p) t -> p c t", p=128),
                        ogT[:, :, 0:rows], reads=["hb_ogT"], writes=["s_ogT"])

                sTs = hb[:, 3328:3392]

                def sample_pre(h):
                    def mm_s(e):
                        for dc in range(2):
                            ins = e.matmul(ps[0][0:64, 0:64], lhsT=keT[:, dc, NP:NT], rhs=qeT[:, dc, NP:NT],
                                           start=(dc == 0), stop=(dc == 1))
                        return ins
                    op("pe", mm_s, ["keT", "qeT"], [PS(0)])
                    op("dve", lambda e: e.tensor_tensor(out=sTs[0:64, 0:64], in0=ps[0][0:64, 0:64], in1=cst[0:64, C_MS:C_MS + 64],
                                                        op=ALU.mult), [PS(0), "cst"], ["hb_sTs"])

                def sample_seq(h, sq):
                    op("dve", lambda e, sq=sq: e.tensor_tensor(
                        out=QMs[:], in0=qeT[:, :, NP:NT], in1=bm_b[:, sq, :].unsqueeze(1).to_broadcast([128, 2, 64]),
                        op=ALU.mult), ["qeT", "bm_b"], ["QMs"])
                    op("dve", lambda e, sq=sq: e.tensor_scalar(out=kdm[:], in0=kd[0:64, 8, :],
                                                                scalar1=cst[0:64, C_RM + sq:C_RM + sq + 1], scalar2=None,
                                                                op0=ALU.mult), ["kd", "cst"], ["kdm"])
                    for dc in range(2):
                        bi = dc
                        src = st_gla[sq, h, dc * 128:(dc + 1) * 128, :]
                        dma("sp", s0f[bi][:], src, writes=["s0f%d" % bi])
                        dma("pool", s0b[bi][:], src, writes=["s0b%d" % bi])

                        def mm_o(e, sq=sq, dc=dc, bi=bi):
                            if sq == 0 and dc == 0:
                                e.matmul(ps[1][0:64, :], lhsT=sTs[0:64, 0:64], rhs=vh[0:64, 8, :], start=True, stop=False)
                            return e.matmul(ps[1][0:64, :], lhsT=QMs[:, dc, :], rhs=s0b[bi][:, :],
                                            start=False, stop=(sq == NSEQ - 1 and dc == 1))
                        op("pe", mm_o, ["hb_sTs", "U1", "QMs", "s0b%d" % bi], [PS(1)])
                        b = 2 + dc
                        op("pe", lambda e, dc=dc, b=b: e.matmul(ps[b][:, :], lhsT=kdm[:, dc * 128:(dc + 1) * 128],
                                                                 rhs=vh[0:64, 8, :], start=True, stop=True),
                           ["kdm", "U1"], [PS(b)])
                        op("dve", lambda e, dc=dc, b=b, sq=sq, bi=bi: e.scalar_tensor_tensor(
                            out=sout[bi][:, :], in0=s0f[bi][:, :], scalar=dec[:, dc, 8 + sq:9 + sq], in1=ps[b][:, :],
                            op0=ALU.mult, op1=ALU.add), ["s0f%d" % bi, "dec", PS(b)], ["sout%d" % bi])
                        dma("sp", gla_s[sq, h, dc * 128:(dc + 1) * 128, :], sout[bi][:], reads=["sout%d" % bi])

                def sample_post(h):
                    finish_o(h, 8, 64, NP, ob=1)

                stage_end(0)
                gla_pass(False)
                stage_end(1)
                gla_pass(True)
                stage_end(2)

            with contextlib.ExitStack() as mg_es:
                ogTa = sb("ogTa", [128, 16, NT], BF16, mg_es)
                sga = sb("sga", [128, 512], F32, mg_es)
                mo = sb("mo", [128, 2, NT], BF16, mg_es)
                dma("sp", ogTa[:], s_ogT.rearrange("(c p) t -> p c t", p=128), reads=["s_ogT"], writes=["ogTa"])
                sgs = sb("sgs", [128, 2, NT], BF16, mg_es)
                for m in range(16):
                    wga, kga = wnext()

                    def ev_ga(mm_, gi, t0, n, pap, pkey):
                        op("act", lambda e: e.activation(out=sgs[:, mm_, t0:t0 + n], in_=pap, func=AF.Sigmoid), [pkey], ["sgs"])
                    gemm_ws(wga, kga, 32, 256, hT, "hT", NGRP, ev_ga, [0, 1, 2, 3])
                    wba, kba = wnext()

                    def ev_ba(mm_, gi, t0, n, pap, pkey):
                        op("dve", lambda e: e.tensor_tensor(out=mo[:, mm_, t0:t0 + n], in0=pap, in1=sgs[:, mm_, t0:t0 + n], op=ALU.mult),
                           [pkey, "sgs"], ["mo"])
                    gemm_ws(wba, kba, 16, 256, ogTa, "ogTa", NGRP, ev_ba, [4, 5, 6, 7])
                    dma("sp", s_mT[m * 256:(m + 1) * 256, :].rearrange("(c p) t -> p c t", p=128), mo[:], reads=["mo"],
                        writes=["s_mT"])

            stage_end(3)
            cv_es = contextlib.ExitStack()
            with cv_es:
                cT = sb("cT", [128, 16, NT], BF16, cv_es)
                cpar = sb("cpar", [128, 16, 34], F32, cv_es)
                extP = sb("extP", [128, 30 + NP], F32, cv_es)
                extS = sb("extS", [128, 16, 34], F32, cv_es)
                sig = xt[:, 3136:3648]
                acc = xt[:, 2048:2048 + NT]
                stc = sb("stc", [120, 4, 128], F32, cv_es)
                cvp_tm = sb("cvp_tm", [30, 128], F32, cv_es)
                cvs_tm = sb("cvs_tm", [64, 128], F32, cv_es)
                cpl = xt[0:34, 0:2048]
                dma("sp", cpl, convp_in, writes=[*XTK])
                for j in range(16):
                    b = 4 + j % 2
                    op("pe", lambda e, j=j, b=b: e.transpose(out=ps[b][:, 0:34], in_=xt[0:34, j * 128:(j + 1) * 128],
                                                             identity=ident_f[0:34, 0:34]), [*XTK, "cst"], [PS(b)])
                    op("dve", lambda e, j=j, b=b: e.tensor_copy(out=cpar[:, j, :], in_=ps[b][:, 0:34]), [PS(b)], ["cpar"])
                dma("sp", conv_s[:, 0:26, :], st_conv[:, 4:30, :])
                ugroups = [(0, 512), (512, 512), (1024, 64)]
                uscope = contextlib.ExitStack()
                u1s = sb("u1s", [128, 2, 32 + NT], F32, uscope)
                hbf32 = hb[:].bitcast(F32)
                extPs = [extP[:, :], hbf32[:, 0:30 + NP]]
                extSs = [extS[:, :, :], hbf32[:, 1056:1056 + 544].rearrange("p (s r) -> p s r", r=34)]
                extPk = ["extP", "extP1"]
                extSk = ["extS", "extS1"]
                sigs = [xt[:, 3136:3648], xt[:, 1088:1600]]
                sigk = ["sig0", "sig1"]
                accs = [xt[:, 2048:2048 + NT], xt[:, 0:NT]]
                acck = ["acc0", "acc1"]
                CBAR = [*XTK, *HBK, "extP1", "extS1", "sig0", "sig1", "acc0", "acc1"]
                op("pool", lambda e: e.memset(sm[:, 63:64], 0.0), [], CBAR)
                pending_tail = [None]

                def make_tail(j, par):
                    extPc, extSc, sigc = extPs[par], extSs[par], sigs[par]

                    def tail():
                        op("pe", lambda e: e.transpose(out=ps[7][0:30, 0:128], in_=extPc[:, NP:NP + 30], identity=ident_f),
                           [extPk[par], "cst"], [PS(7)])
                        op("act", lambda e: e.activation(out=cvp_tm[:, :], in_=ps[7][0:30, 0:128], func=AF.Copy), [PS(7)], ["cvp_tm"])
                        dma("sp", conv_p[:, j * 128:(j + 1) * 128], cvp_tm[:, :], reads=["cvp_tm"])
                        op("pool", lambda e: e.tensor_copy(out=sigc[:, 0:64].rearrange("p (s r) -> p s r", r=4), in_=extSc[:, :, 30:34]),
                           [extSk[par]], [sigk[par]])
                        op("pe", lambda e: e.transpose(out=ps[7][0:64, 128:256], in_=sigc[:, 0:64], identity=ident_f),
                           [sigk[par], "cst"], [PS(7)])
                        op("act", lambda e: e.activation(out=cvs_tm[:, :], in_=ps[7][0:64, 128:256], func=AF.Copy), [PS(7)], ["cvs_tm"])
                        for sq in range(NSEQ):
                            dma("sp", conv_s[sq, 26:30, j * 128:(j + 1) * 128], cvs_tm[4 * sq:4 * sq + 4, :], reads=["cvs_tm"])
                    return tail

                for m in range(8):
                    wu1, k1 = wnext()
                    for jj in range(2):
                        for gi, (t0, n) in enumerate([(-32, 32)] + ugroups):
                            src = hT_halo if t0 < 0 else hT
                            skey = "hT_halo" if t0 < 0 else "hT"
                            a0 = 0 if t0 < 0 else t0
                            b1 = (jj * 4 + gi) % 4

                            def mmu1(e, b=b1, a0=a0, n=n, src=src, jj=jj):
                                for k in range(32):
                                    ins = e.matmul(ps[b][:, 0:n], lhsT=wu1[:, k, jj * 128:(jj + 1) * 128], rhs=src[:, k, a0:a0 + n],
                                                   start=(k == 0), stop=(k == 31))
                                return ins
                            op("pe", mmu1, [k1, skey], [PS(b1)])
                            op("act", lambda e, b1=b1, jj=jj, t0=t0, n=n: e.activation(out=u1s[:, jj, 32 + t0:32 + t0 + n], in_=ps[b1][:, 0:n],
                                                                                        func=AF.Copy), [PS(b1)], ["u1s"])
                    wu2, k2 = wnext()
                    for jj in range(2):
                        j = 2 * m + jj
                        par = jj
                        extPc, extSc, sigc, accc = extPs[par], extSs[par], sigs[par], accs[par]
                        kP, kS, kG, kA = extPk[par], extSk[par], sigk[par], acck[par]
                        for g4 in range(4):
                            dma("sp", stc[:, g4, :], st_conv[4 * g4:4 * g4 + 4, :, j * 128:(j + 1) * 128].rearrange("s r c -> (s r) c"),
                                writes=["stc"])
                        for g4 in range(4):
                            op("pe", lambda e, g4=g4: e.transpose(out=ps[6][:, g4 * 120:(g4 + 1) * 120], in_=stc[:, g4, :],
                                                                  identity=ident_f[0:120, 0:120]), ["stc", "cst"], [PS(6)])
                        op("dve", lambda e: e.tensor_copy(out=extSc[:, :, 0:30],
                                                          in_=ps[6][:, 0:480].rearrange("p (s r) -> p s r", r=30)),
                           [PS(6)], [kS])
                        for gi, (t0, n) in enumerate([(-32, 32)] + ugroups):
                            src = hT_halo if t0 < 0 else hT
                            skey = "hT_halo" if t0 < 0 else "hT"
                            a0 = 0 if t0 < 0 else t0
                            b2 = gi % 4

                            def mmu(e, wv, b, a0=a0, n=n, src=src, jj=jj):
                                for k in range(32):
                                    ins = e.matmul(ps[b][:, 0:n], lhsT=wv[:, k, jj * 128:(jj + 1) * 128], rhs=src[:, k, a0:a0 + n],
                                                   start=(k == 0), stop=(k == 31))
                                return ins
                            op("pe", lambda e, b2=b2, f=mmu: f(e, wu2, b2), [k2, skey], [PS(b2)])
                            op("act", lambda e, b2=b2, n=n: e.activation(out=sigc[:, 0:n], in_=ps[b2][:, 0:n], func=AF.Sigmoid),
                               [PS(b2)], [kG])
                            if t0 < 0:
                                dst = extPc[:, 0:30]
                                i0 = u1s[:, jj, 2:32]
                                i1 = sigc[:, 2:32]
                                wk_ = kP
                            elif t0 < NP:
                                dst = extPc[:, 30 + t0:30 + t0 + n]
                                i0 = u1s[:, jj, 32 + t0:32 + t0 + n]
                                i1 = sigc[:, 0:n]
                                wk_ = kP
                            else:
                                dst = extSc[:, :, 30:34]
                                i0 = u1s[:, jj, 32 + NP:32 + NT].rearrange("p (s r) -> p s r", r=4)
                                i1 = sigc[:, 0:64].rearrange("p (s r) -> p s r", r=4)
                                wk_ = kS
                            op("dve", lambda e, dst=dst, i0=i0, i1=i1: e.tensor_tensor(out=dst, in0=i0, in1=i1, op=ALU.mult),
                               ["u1s", kG], [wk_])
                        if pending_tail[0] is not None:
                            pending_tail[0]()
                        pending_tail[0] = make_tail(j, par)
                        accS = accc[:, NP:NT].rearrange("p (s r) -> p s r", r=4)
                        op("dve", lambda e, j=j: e.tensor_scalar(out=accc[:, 0:NP], in0=extPc[:, 0:NP], scalar1=cpar[:, j, 0:1],
                                                                  scalar2=cpar[:, j, 31:32], op0=ALU.mult, op1=ALU.add),
                           [kP, "cpar"], [kA])
                        op("dve", lambda e, j=j: e.tensor_scalar(out=accS, in0=extSc[:, :, 0:4], scalar1=cpar[:, j, 0:1],
                                                                  scalar2=cpar[:, j, 31:32], op0=ALU.mult, op1=ALU.add),
                           [kS, "cpar"], [kA])
                        for tp in range(1, 31):
                            op("dve", lambda e, j=j, tp=tp: e.scalar_tensor_tensor(
                                out=accc[:, 0:NP], in0=extPc[:, tp:tp + NP], scalar=cpar[:, j, tp:tp + 1], in1=accc[:, 0:NP],
                                op0=ALU.mult, op1=ALU.add), [kP, "cpar", kA], [kA])
                            op("dve", lambda e, j=j, tp=tp: e.scalar_tensor_tensor(
                                out=accS, in0=extSc[:, :, tp:tp + 4], scalar=cpar[:, j, tp:tp + 1], in1=accS,
                                op0=ALU.mult, op1=ALU.add), [kS, "cpar", kA], [kA])
                        op("act", lambda e, j=j: e.activation(out=cT[:, j, :], in_=accc[:, :], func=AF.Copy), [kA], ["cT"])
                pending_tail[0]()
                op("pool", lambda e: e.memset(sm[:, 63:64], 0.0), [], CBAR)
                uscope.close()
                with contextlib.ExitStack() as ln_es:
                    sq_t = sb("sq_t", [128, 512], BF16, ln_es)
                    mu = sb("mu", [128, 512], F32, ln_es)
                    rs = sb("rs", [128, 512], F32, ln_es)
                    tmpf = sb("tmpf", [128, 512], F32, ln_es)
                    for gi, (t0, n) in enumerate(NGRP):
                        def mm1(e, t0=t0, n=n):
                            for j in range(16):
                                ins = e.matmul(ps[0][:, 0:n], lhsT=ones_b[:], rhs=cT[:, j, t0:t0 + n], start=(j == 0), stop=(j == 15))
                            return ins
                        op("pe", mm1, ["ones_b", "cT"], [PS(0)])
                        for j in range(16):
                            op("dve", lambda e, j=j, t0=t0, n=n: e.tensor_tensor(out=sq_t[:, 0:n], in0=cT[:, j, t0:t0 + n],
                                                                                  in1=cT[:, j, t0:t0 + n], op=ALU.mult),
                               ["cT"], ["sq_t"])
                            op("pe", lambda e, j=j, n=n: e.matmul(ps[1][:, 0:n], lhsT=ones_b[:], rhs=sq_t[:, 0:n],
                                                                  start=(j == 0), stop=(j == 15)), ["ones_b", "sq_t"], [PS(1)])
                        op("act", lambda e, n=n: e.activation(out=mu[:, 0:n], in_=ps[0][:, 0:n], func=AF.Copy, scale=1.0 / 2048),
                           [PS(0)], ["mu"])
                        op("dve", lambda e, n=n: e.tensor_tensor(out=tmpf[:, 0:n], in0=mu[:, 0:n], in1=mu[:, 0:n], op=ALU.mult),
                           ["mu"], ["tmpf"])
                        op("dve", lambda e, n=n: e.scalar_tensor_tensor(out=rs[:, 0:n], in0=ps[1][:, 0:n], scalar=1.0 / 2048,
                                                                         in1=tmpf[:, 0:n], op0=ALU.mult, op1=ALU.subtract),
                           [PS(1), "tmpf"], ["rs"])
                        op("act", lambda e, n=n: e.activation(out=rs[:, 0:n], in_=rs[:, 0:n], func=AF.Sqrt, bias=EPS), ["rs"], ["rs"])
                        op("dve", lambda e, n=n: e.reciprocal(out=rs[:, 0:n], in_=rs[:, 0:n]), ["rs"], ["rs"])
                        for j in range(16):
                            op("dve", lambda e, j=j, t0=t0, n=n: e.tensor_tensor(out=tmpf[:, 0:n], in0=cT[:, j, t0:t0 + n],
                                                                                  in1=mu[:, 0:n], op=ALU.subtract),
                               ["cT", "mu"], ["tmpf"])
                            op("dve", lambda e, n=n: e.tensor_tensor(out=tmpf[:, 0:n], in0=tmpf[:, 0:n], in1=rs[:, 0:n], op=ALU.mult),
                               ["tmpf", "rs"], ["tmpf"])
                            op("act", lambda e, j=j, t0=t0, n=n: e.activation(out=cT[:, j, t0:t0 + n], in_=tmpf[:, 0:n], func=AF.Silu,
                                                                               scale=cpar[:, j, 32:33], bias=cpar[:, j, 33:34]),
                               ["tmpf", "cpar"], ["cT"])
                stage_end(4)
                with contextlib.ExitStack() as mg_es:
                    xtb2 = xt[:].bitcast(BF16)
                    mab = xtb2[:, 0:2 * NT].rearrange("p (c t) -> p c t", c=2)
                    mo = xtb2[:, 2 * NT:4 * NT].rearrange("p (c t) -> p c t", c=2)
                    sgs2 = xt[:, 2304:2304 + NT].bitcast(BF16).rearrange("p (c t) -> p c t", c=2)
                    for m in range(16):
                        wgb, kgb = wnext()
                        dma("sp", mab[:], s_mT[m * 256:(m + 1) * 256, :].rearrange("(c p) t -> p c t", p=128), reads=["s_mT"],
                            writes=["xt_E1"])

                        def ev_gb(mm_, gi, t0, n, pap, pkey):
                            op("act", lambda e: e.activation(out=sgs2[:, mm_, t0:t0 + n], in_=pap, func=AF.Sigmoid), [pkey], ["xt_E3"])
                        gemm_ws(wgb, kgb, 32, 256, hT, "hT", NGRP, ev_gb, [0, 1, 2, 3])
                        wbb, kbb = wnext()

                        def ev_bb(mm_, gi, t0, n, pap, pkey):
                            op("dve", lambda e: e.tensor_tensor(out=sgs2[:, mm_, t0:t0 + n], in0=pap, in1=sgs2[:, mm_, t0:t0 + n], op=ALU.mult),
                               [pkey, "xt_E3"], ["xt_E3"])
                            op("dve", lambda e: e.tensor_tensor(out=mo[:, mm_, t0:t0 + n], in0=sgs2[:, mm_, t0:t0 + n], in1=mab[:, mm_, t0:t0 + n],
                                                                op=ALU.add), ["xt_E3", "xt_E1"], ["xt_E2"])
                        gemm_ws(wbb, kbb, 16, 256, cT, "cT", NGRP, ev_bb, [4, 5, 6, 7])
                        dma("sp", s_mT[m * 256:(m + 1) * 256, :].rearrange("(c p) t -> p c t", p=128), mo[:], reads=["xt_E2", "xt_E1"],
                            writes=["s_mT"])

        stage_end(5)
        late = contextlib.ExitStack()
        with late:
            xres = [sb("xres%d" % i, [128, 512], F32, late) for i in range(3)]
            dma("sp", hT[:], s_mT.rearrange("(c p) t -> p c t", p=128), reads=["s_mT"], writes=["hT"])
            rcount = [0]

            def resid_gemm(wview, wkey, actT, act_key, src_fn, src_key, dst, c0, ncols, dst_key):
                def ev(ti, t0, rows, pap, pkey):
                    bi = rcount[0] % 3
                    rcount[0] += 1
                    dma("sp", xres[bi][0:rows, 0:ncols], src_fn(t0, rows, c0, ncols), reads=[src_key], writes=["xres%d" % bi])
                    op("dve", lambda e: e.tensor_tensor(out=xres[bi][0:rows, 0:ncols], in0=pap, in1=xres[bi][0:rows, 0:ncols], op=ALU.add),
                       [pkey, "xres%d" % bi], ["xres%d" % bi])
                    dma("sp", dst[t0:t0 + rows, c0:c0 + ncols], xres[bi][0:rows, 0:ncols], reads=["xres%d" % bi],
                        writes=[dst_key])
                gemm_as([(wview, 0)], [wkey], actT, act_key, TILES, ncols, ev, [0, 1, 2, 3])

            def xsrc(t0, rows, c0, ncols):
                if t0 < NP:
                    return x_main[t0:t0 + rows, c0:c0 + ncols]
                return x_s[:, c0:c0 + ncols]
            for cb in range(16):
                w0, k0 = wnext()
                resid_gemm(w0, k0, hT, "hT", xsrc, "x_in", s_x1, cb * 256, 256, "s_x1")

            stage_end(6)
            ca = contextlib.ExitStack()
            with ca:
                mvb = sb("mvb", [128, 2, 1024], BF16, ca)
                mkT = sb("mkT", [128, 8, 256], BF16, ca)
                q2T = sb("q2T", [128, 8, NT], BF16, ca)
                oT = q2T
                kvf = sb("kvf", [128, 512], F32, ca)
                mscope = contextlib.ExitStack()
                mT = sb("mT", [128, 32, 256], BF16, mscope)
                load_gain(norm_mem_g)
                for i in range(2):
                    norm_tile(mem[i * 128:(i + 1) * 128, :], 128, i * 128, mT, "mT")
                stage_end(6.05)
                for which, dst_o in ((0, mk_o), (1, mv_o)):
                    for cb in range(4):
                        w0, k0 = wnext()

                        def ev_kv(ti, t0, rows, pap, pkey, which=which, cb=cb, dst_o=dst_o):
                            kb = "kvf%d" % (ti % 2)
                            kv_ = kvf[:, (ti % 2) * 256:(ti % 2) * 256 + 256]
                            op("act", lambda e: e.activation(out=kv_, in_=pap, func=AF.Copy), [pkey], [kb])
                            if which == 1:
                                op("dve", lambda e: e.tensor_copy(out=mvb[:, ti, cb * 256:(cb + 1) * 256], in_=kv_), [kb], ["mvb"])
                            dma("sp", dst_o[t0:t0 + 128, cb * 256:(cb + 1) * 256], kv_, reads=[kb])
                        gemm_as([(w0, 0)], [k0], mT, "mT", [(0, 128), (128, 128)], 256, ev_kv, [0, 1, 2, 3])
                stage_end(6.1)
                for m in range(4):
                    wv, wk = wnext()

                    def ev_mk(mm_, gi, t0, n, pap, pkey, m=m):
                        op("act", lambda e: e.activation(out=mkT[:, 2 * m + mm_, :], in_=pap, func=AF.Copy), [pkey], ["mkT"])
                    gemm_ws(wv, wk, 32, 256, mT, "mT", [(0, 256)], ev_mk, [0, 1, 2, 3])
                stage_end(6.2)
                mscope.close()
                load_gain(norm_ca_g)
                for ti, (t0, rows) in enumerate(TILES):
                    norm_tile(s_x1[t0:t0 + rows, :], rows, t0, hT, "hT", src_reads=["s_x1"])
                for m in range(4):
                    wv, wk = wnext()

                    def ev_q2(mm_, gi, t0, n, pap, pkey, m=m):
                        op("act", lambda e: e.activation(out=q2T[:, 2 * m + mm_, t0:t0 + n], in_=pap, func=AF.Copy), [pkey], ["q2T"])
                    gemm_ws(wv, wk, 32, 256, hT, "hT", NGRP, ev_q2, [0, 1, 2, 3])

                stage_end(6.4)
                at = contextlib.ExitStack()
                with at:
                    pex = sb("pex", [128, 4, 256], F32, at)
                    pbf = sb("pbf", [128, 4, 256], BF16, at)
                    pT = hb[:].rearrange("p (s h t) -> p s h t", s=2, h=4)
                    mx = sb("mx", [128, 16], F32, at)
                    xtb3 = xt[:].bitcast(BF16)
                    kc = [xtb3[:, i * 2048:(i + 1) * 2048].rearrange("p (c d) -> p c d", c=2) for i in range(2)]
                    vc = [xtb3[:, 4096 + i * 2048:4096 + (i + 1) * 2048].rearrange("p (c d) -> p c d", c=2) for i in range(2)]
                    kTs = sb("kTs", [128, 8, 256], BF16, at)
                    Q2M = sb("Q2M", [128, 8, 64], BF16, at)
                    pTm = sb("pTm", [128, 2, 4, 64], BF16, at)
                    osb = sb("osb", [64, 1024], BF16, at)

                    def softmax_rows(rows, banks):
                        for h in range(4):
                            sc = ps[banks[h // 2]][0:rows, (h % 2) * 256:(h % 2) * 256 + 256]
                            op("dve", lambda e, h=h, sc=sc: e.reduce_max(out=mx[0:rows, h:h + 1], in_=sc, axis=AX.X),
                               [PS(banks[h // 2])], ["mx"])
                        op("dve", lambda e: e.tensor_scalar(out=mx[0:rows, 4:8], in0=mx[0:rows, 0:4], scalar1=-1.0 / 16.0,
                                                            scalar2=None, op0=ALU.mult), ["mx"], ["mx"])
                        for h in range(4):
                            sc = ps[banks[h // 2]][0:rows, (h % 2) * 256:(h % 2) * 256 + 256]
                            op("act", lambda e, h=h, sc=sc: e.activation(out=pex[0:rows, h, :], in_=sc, func=AF.Exp, scale=1.0 / 16.0,
                                                                          bias=mx[0:rows, 4 + h:5 + h], accum_out=mx[0:rows, 8 + h:9 + h]),
                               [PS(banks[h // 2]), "mx"], ["pex", "mx"])
                        op("dve", lambda e: e.reciprocal(out=mx[0:rows, 12:16], in_=mx[0:rows, 8:12]), ["mx"], ["mx"])
                        for h in range(4):
                            op("dve", lambda e, h=h: e.tensor_scalar(out=pbf[0:rows, h, :], in0=pex[0:rows, h, :],
                                                                      scalar1=mx[0:rows, 12 + h:13 + h], scalar2=None, op0=ALU.mult),
                               ["pex", "mx"], ["pbf"])

                    for gq in range(2):
                        for tl in range(4):
                            t0 = gq * 512 + tl * 128

                            def mm_sc(e, t0=t0):
                                for h in range(4):
                                    for dc in range(2):
                                        ins = e.matmul(ps[h // 2][:, (h % 2) * 256:(h % 2) * 256 + 256], lhsT=q2T[:, 2 * h + dc, t0:t0 + 128],
                                                       rhs=mkT[:, 2 * h + dc, :], start=(dc == 0), stop=(dc == 1))
                                return ins
                            op("pe", mm_sc, ["q2T", "mkT"], [PS(0), PS(1)])
                            softmax_rows(128, [0, 1])
                            pv = ps[2][:].bitcast(BF16).rearrange("p (a b) -> p a b", a=8)

                            def trp(e):
                                for h in range(4):
                                    for sc_ in range(2):
                                        ins = e.transpose(out=pv[:, sc_ * 4 + h, :], in_=pbf[:, h, sc_ * 128:(sc_ + 1) * 128], identity=ident_b[:])
                                return ins
                            op("pe", trp, ["pbf", "ident_b"], [PS(2)])
                            op("act", lambda e, tl=tl: e.activation(out=pT[:, :, :, tl * 128:(tl + 1) * 128],
                                                                     in_=pv.rearrange("p (s h) t -> p s h t", s=2), func=AF.Copy),
                               [PS(2)], ["hb_kdT"])
                        for h in range(4):
                            for dc in range(2):
                                b = 4 + (h * 2 + dc) % 4

                                def mm_ov(e, h=h, dc=dc, b=b):
                                    for sc_ in range(2):
                                        ins = e.matmul(ps[b][:, :], lhsT=mvb[:, sc_, h * 256 + dc * 128:h * 256 + dc * 128 + 128],
                                                       rhs=pT[:, sc_, h, :], start=(sc_ == 0), stop=(sc_ == 1))
                                    return ins
                                op("pe", mm_ov, ["mvb", "hb_kdT"], [PS(b)])
                                op("act", lambda e, h=h, dc=dc, b=b, gq=gq: e.activation(
                                    out=oT[:, 2 * h + dc, gq * 512:(gq + 1) * 512], in_=ps[b][:, :], func=AF.Copy), [PS(b)], ["q2T"])
                    stage_end(6.6)
                    for c8 in range(8):
                        op("dve", lambda e, c8=c8: e.tensor_copy(out=Q2M[:, c8, :], in_=q2T[:, c8, NP:NT]), ["q2T"], ["Q2M"])
                    bmv = bm_b[:]
                    for sq in range(NSEQ):
                        bi = sq % 2
                        dma("pool", kc[bi][:], ck[sq].rearrange("(c p) d -> p c d", p=128), writes=[("xt_E1", "xt_E2")[bi]])
                        for half in range(2):
                            pv = ps[4 + half][:].bitcast(BF16).rearrange("p (a b) -> p a b", a=8)

                            def trk(e, half=half, pv=pv, bi=bi):
                                for c4 in range(4):
                                    c8 = half * 4 + c4
                                    for sc_ in range(2):
                                        ins = e.transpose(out=pv[:, c4 * 2 + sc_, :], in_=kc[bi][:, sc_, c8 * 128:(c8 + 1) * 128],
                                                          identity=ident_b[:])
                                return ins
                            op("pe", trk, [("xt_E1", "xt_E2")[bi], "ident_b"], [PS(4 + half)])
                            op("act", lambda e, half=half, pv=pv: e.activation(
                                out=kTs[:, half * 4:(half + 1) * 4, :].rearrange("p c (s t) -> p c s t", s=2),
                                in_=pv.rearrange("p (c s) t -> p c s t", s=2), func=AF.Copy), [PS(4 + half)], ["kTs"])
                        qm = sb if False else None

                        def mm_ss(e, sq=sq):
                            for h in range(4):
                                for dc in range(2):
                                    ins = e.matmul(ps[h][0:64, 0:256], lhsT=QMs[:, 2 * h + dc, :], rhs=kTs[:, 2 * h + dc, :],
                                                   start=(sq == 0 and dc == 0), stop=(sq == NSEQ - 1 and dc == 1))
                            return ins
                        QMs = sb("QMs%d" % sq, [128, 8, 64], BF16, at) if sq < 2 else QMs_l[sq % 2]
                        if sq < 2:
                            if sq == 0:
                                QMs_l = [QMs, None]
                            else:
                                QMs_l[1] = QMs
                        op("dve", lambda e, sq=sq, QMs=QMs: e.tensor_tensor(
                            out=QMs[:], in0=Q2M[:], in1=bmv[:, sq, :].unsqueeze(1).to_broadcast([128, 8, 64]), op=ALU.mult),
                           ["Q2M", "bm_b"], ["QMs%d" % (sq % 2)])
                        op("pe", mm_ss, ["QMs%d" % (sq % 2), "kTs"], [PS(0), PS(1), PS(2), PS(3)])
                    for h in range(4):
                        op("dve", lambda e, h=h: e.reduce_max(out=mx[0:64, h:h + 1], in_=ps[h][0:64, 0:256], axis=AX.X), [PS(h)], ["mx"])
                    op("dve", lambda e: e.tensor_scalar(out=mx[0:64, 4:8], in0=mx[0:64, 0:4], scalar1=-1.0 / 16.0, scalar2=None,
                                                        op0=ALU.mult), ["mx"], ["mx"])
                    for h in range(4):
                        op("act", lambda e, h=h: e.activation(out=pex[0:64, h, :], in_=ps[h][0:64, 0:256], func=AF.Exp, scale=1.0 / 16.0,
                                                               bias=mx[0:64, 4 + h:5 + h], accum_out=mx[0:64, 8 + h:9 + h]),
                           [PS(h), "mx"], ["pex", "mx"])
                    op("dve", lambda e: e.reciprocal(out=mx[0:64, 12:16], in_=mx[0:64, 8:12]), ["mx"], ["mx"])
                    for h in range(4):
                        op("dve", lambda e, h=h: e.tensor_scalar(out=pbf[0:64, h, :], in0=pex[0:64, h, :], scalar1=mx[0:64, 12 + h:13 + h],
                                                                  scalar2=None, op0=ALU.mult), ["pex", "mx"], ["pbf"])
                    pv = ps[4][:].bitcast(BF16).rearrange("p (a b) -> p a b", a=8)

                    def trps(e):
                        for h in range(4):
                            for sc_ in range(2):
                                ins = e.transpose(out=pv[:, sc_ * 4 + h, 0:64], in_=pbf[0:64, h, sc_ * 128:(sc_ + 1) * 128],
                                                  identity=ident_b[0:64, 0:64])
                        return ins
                    op("pe", trps, ["pbf", "ident_b"], [PS(4)])
                    pTs = sb("pTs", [128, 8, 64], BF16, at)
                    op("act", lambda e: e.activation(out=pTs[:], in_=pv[:, :, 0:64], func=AF.Copy), [PS(4)], ["pTs"])
                    for sq in range(NSEQ):
                        bi = sq % 2
                        dma("pool", vc[bi][:], cv[sq].rearrange("(c p) d -> p c d", p=128), writes=[("xt_E3", "xt")[bi]])
                        op("dve", lambda e, sq=sq: e.tensor_tensor(
                            out=pTm[:].rearrange("p s h t -> p (s h) t"), in0=pTs[:],
                            in1=bmv[:, sq, :].unsqueeze(1).to_broadcast([128, 8, 64]), op=ALU.mult), ["pTs", "bm_b"], ["pTm"])

                        def mm_os(e, sq=sq, bi=bi):
                            for h in range(4):
                                for sc_ in range(2):
                                    ins = e.matmul(ps[h][0:64, 0:256], lhsT=pTm[:, sc_, h, :], rhs=vc[bi][:, sc_, h * 256:(h + 1) * 256],
                                                   start=(sq == 0 and sc_ == 0), stop=(sq == NSEQ - 1 and sc_ == 1))
                            return ins
                        op("pe", mm_os, ["pTm", ("xt_E3", "xt")[bi]], [PS(0), PS(1), PS(2), PS(3)])
                    for h in range(4):
                        op("act", lambda e, h=h: e.activation(out=osb[:, h * 256:(h + 1) * 256], in_=ps[h][0:64, 0:256], func=AF.Copy),
                           [PS(h)], ["osb"])
                    pv = ps[5][:].bitcast(BF16).rearrange("p (a b) -> p a b", a=8)

                    def tro(e):
                        for c8 in range(8):
                            ins = e.transpose(out=pv[:, c8, 0:64], in_=osb[:, c8 * 128:(c8 + 1) * 128], identity=ident_b[0:64, 0:64])
                        return ins
                    op("pe", tro, ["osb", "ident_b"], [PS(5)])
                    op("act", lambda e: e.activation(out=oT[:, :, NP:NT], in_=pv[:, :, 0:64], func=AF.Copy), [PS(5)], ["q2T"])

                stage_end(6.8)
                def x1src(t0, rows, c0, ncols):
                    return s_x1[t0:t0 + rows, c0:c0 + ncols]
                for cb in range(4):
                    w0, k0 = wnext()
                    for sub in range(2):
                        resid_gemm(w0[:, :, sub * 512:(sub + 1) * 512], k0, oT, "q2T", x1src, "s_x1", s_x2, cb * 1024 + sub * 512, 512, "s_x2")

            stage_end(7)
            late.close()
            hscope.close()
            moe = contextlib.ExitStack()
            with moe:
                wsl.append(sb("wsl2", [128, 8192], BF16, moe))
                wr = sb("wr", [128, 32, 36], F32, moe)
                brt = sb("brt", [128, 36], F32, moe)
                xgTbuf = sb("xgTbuf", [128, D], F32, moe)
                hTf = xgTbuf[:].rearrange("p (k t) -> p k t", k=32)
                xgT = xgTbuf[:].bitcast(BF16).rearrange("p (k c) -> p k c", k=32)
                hbf = sb("hbf", [128, D], F32, moe)
                gbcf = sb("gbcf", [128, D], F32, moe)
                lg = sb("lg", [128, 9, 36], F32, moe)
                gate = sb("gate", [128, 9, 32], F32, moe)
                msk = sb("msk", [128, 9, 32], BF16, moe)
                m2f = sb("m2f", [128, 9, 32], F32, moe)
                rank = sb("rank", [128, 9, 32], F32, moe)
                tmp = sb("tmp", [128, 9, 40], F32, moe)
                rhs6 = sb("rhs6", [128, 9, NE, 6], BF16, moe)
                sel = sb("sel", [128, 9, CAP], BF16, moe)
                slot2 = [sb("slot%d" % i, [128, 2, 8], F32, moe) for i in range(2)]
                idxg2 = [sb("idxg%d" % i, [128, 2], U32, moe) for i in range(2)]
                idxs2 = [sb("idxs%d" % i, [128, 2], U32, moe) for i in range(2)]
                xg2 = [sb("xg%d" % i, [128, 2, D], BF16, moe) for i in range(2)]
                xg = xg2[0]
                hidT = sb("hidT", [128, 4, CAP], BF16, moe)
                sgl = sb("sgl", [128, 2, CAP], F32, moe)
                ye = [sb("ye0", [128, D], F32, moe), hbf]
                yek = ["ye0", "hbf"]
                dma("sp", wr[:], w_router.rearrange("(k p) n -> p k n", p=128), writes=["wr"])
                dma("sp", brt[:], b_router.partition_broadcast(128), writes=["brt"])
                op("pool", lambda e: e.memset(hb[0:1, :], 0.0), [], [*HBK])
                dma("sp", s_h3[ZROW:ZROW + 1, :], hb[0:1, :], reads=[*HBK], writes=["s_h3"])
                dma("sp", gbcf[:], norm_ffn_g.partition_broadcast(128), writes=["gbcf"])

                for ti, (t0, rows) in enumerate(TILES):
                    def f32T(rows, ti=ti, t0=t0):
                        op("dve", lambda e: e.scalar_tensor_tensor(out=hbf[0:rows, :], in0=xt[0:rows, :], scalar=sm[0:rows, 2:3],
                                                                   in1=gbcf[0:rows, :], op0=ALU.mult, op1=ALU.mult),
                           [*XTK, "sm", "gbcf"], ["hbf"])
                        op("act", lambda e: e.activation(out=hb[0:rows, :], in_=hbf[0:rows, :], func=AF.Copy), ["hbf"], [*HBK])
                        dma("sp", s_h3[t0:t0 + rows, :], hb[0:rows, :], reads=[*HBK], writes=["s_h3"])
                        for g in range(8):
                            b = 4 + g % 4

                            def tr(e, g=g, b=b):
                                for j in range(4):
                                    c = g * 4 + j
                                    ins = e.transpose(out=ps[b][:, j * 128:j * 128 + rows], in_=hbf[0:rows, c * 128:(c + 1) * 128],
                                                      identity=ident_f[0:rows, 0:rows])
                                return ins
                            op("pe", tr, ["hbf", "cst"], [PS(b)])
                            op("act" if g % 2 == 0 else "dve",
                               (lambda e, g=g, b=b: e.activation(out=hTf[:, g * 4:(g + 1) * 4, 0:rows],
                                                                 in_=ps[b][:].rearrange("p (a t) -> p a t", a=4)[:, :, 0:rows], func=AF.Copy))
                               if g % 2 == 0 else
                               (lambda e, g=g, b=b: e.tensor_copy(out=hTf[:, g * 4:(g + 1) * 4, 0:rows],
                                                                  in_=ps[b][:].rearrange("p (a t) -> p a t", a=4)[:, :, 0:rows])),
                               [PS(b)], ["xgT"])

                        def mmr(e):
                            for k in range(32):
                                ins = e.matmul(ps[3][0:rows, 0:36], lhsT=hTf[:, k, 0:rows], rhs=wr[:, k, :], start=(k == 0), stop=(k == 31))
                            return ins
                        op("pe", mmr, ["xgT", "wr"], [PS(3)])
                        op("dve", lambda e: e.tensor_tensor(out=lg[0:rows, ti, :], in0=ps[3][0:rows, 0:36], in1=brt[0:rows, :], op=ALU.add),
                           [PS(3), "brt"], ["lg"])
                    norm_tile(s_x2[t0:t0 + rows, :], rows, t0, None, None, fp32_T=f32T, src_reads=["s_x2"])
                op("pool", lambda e: e.memset(lg[64:128, 8, :], -30000.0), [], ["lg"]) if False else None
                R_ = ["lg", "tmp", "gate", "msk", "m2f"]
                lgg = lg[:, :, 0:4]
                lge = lg[:, :, 4:36].rearrange("p t (g e) -> p t g e", g=4)
                T0, T1, T2, T3, T4, T5 = (tmp[:, :, i:i + 1] for i in range(6))
                gm = tmp[:, :, 8:12]
                op("dve", lambda e: e.tensor_reduce(out=tmp[:, :, 0:1], in_=lgg, axis=AX.X, op=ALU.max), ["lg"], ["tmp"])
                op("dve", lambda e: e.tensor_tensor(out=tmp[:, :, 12:16], in0=lgg, in1=T0.to_broadcast([128, 9, 4]), op=ALU.subtract),
                   ["lg", "tmp"], ["tmp"])
                op("dve", lambda e: e.tensor_single_scalar(out=gm, in_=tmp[:, :, 12:16], scalar=0.0, op=ALU.is_ge), ["tmp"], ["tmp"])
                op("act", lambda e: e.activation(out=tmp[:, :, 16:20], in_=tmp[:, :, 12:16], func=AF.Exp), ["tmp"], ["tmp"])
                op("dve", lambda e: e.tensor_reduce(out=tmp[:, :, 1:2], in_=tmp[:, :, 16:20], axis=AX.X, op=ALU.add), ["tmp"], ["tmp"])
                op("dve", lambda e: e.reciprocal(out=tmp[:, :, 2:3], in_=tmp[:, :, 1:2]), ["tmp"], ["tmp"])
                op("dve", lambda e: e.tensor_scalar(out=tmp[:, :, 20:24], in0=gm, scalar1=-1.0, scalar2=10000.0, op0=ALU.add, op1=ALU.mult),
                   ["tmp"], ["tmp"])
                lem = m2f[:].rearrange("p t (g e) -> p t g e", g=4)
                op("dve", lambda e: e.tensor_tensor(out=lem, in0=lge, in1=tmp[:, :, 20:24].unsqueeze(3).to_broadcast([128, 9, 4, 8]),
                                                    op=ALU.add), ["lg", "tmp"], ["m2f"])
                op("dve", lambda e: e.tensor_reduce(out=tmp[:, :, 3:4], in_=m2f[:], axis=AX.X, op=ALU.max), ["m2f"], ["tmp"])
                op("dve", lambda e: e.tensor_tensor(out=gate[:], in0=m2f[:], in1=T3.to_broadcast([128, 9, 32]), op=ALU.is_ge),
                   ["m2f", "tmp"], ["gate"])
                op("dve", lambda e: e.scalar_tensor_tensor(out=rank[:].rearrange("p t e -> p (t e)"), in0=gate[:].rearrange("p t e -> p (t e)"),
                                                           scalar=-20000.0, in1=m2f[:].rearrange("p t e -> p (t e)"), op0=ALU.mult, op1=ALU.add),
                   ["gate", "m2f"], ["rank"])
                op("dve", lambda e: e.tensor_reduce(out=tmp[:, :, 4:5], in_=rank[:], axis=AX.X, op=ALU.max), ["rank"], ["tmp"])
                op("dve", lambda e: e.tensor_tensor(out=m2f[:], in0=rank[:], in1=T4.to_broadcast([128, 9, 32]), op=ALU.is_ge),
                   ["rank", "tmp"], ["m2f"])
                op("dve", lambda e: e.tensor_tensor(out=tmp[:, :, 5:6], in0=T3, in1=T4, op=ALU.subtract), ["tmp"], ["tmp"])
                op("act", lambda e: e.activation(out=tmp[:, :, 6:7], in_=tmp[:, :, 5:6], func=AF.Sigmoid), ["tmp"], ["tmp"])
                op("dve", lambda e: e.tensor_tensor(out=tmp[:, :, 24:25], in0=tmp[:, :, 6:7], in1=tmp[:, :, 2:3], op=ALU.mult), ["tmp"], ["tmp"])
                op("dve", lambda e: e.tensor_tensor(out=tmp[:, :, 25:26], in0=tmp[:, :, 2:3], in1=tmp[:, :, 24:25], op=ALU.subtract),
                   ["tmp"], ["tmp"])
                op("dve", lambda e: e.tensor_tensor(out=msk[:], in0=gate[:], in1=m2f[:], op=ALU.add), ["gate", "m2f"], ["msk"])
                op("dve", lambda e: e.tensor_tensor(out=gate[:], in0=gate[:], in1=tmp[:, :, 24:25].to_broadcast([128, 9, 32]), op=ALU.mult),
                   ["gate", "tmp"], ["gate"])
                op("dve", lambda e: e.tensor_tensor(out=rank[:], in0=m2f[:], in1=tmp[:, :, 25:26].to_broadcast([128, 9, 32]), op=ALU.mult),
                   ["m2f", "tmp"], ["rank"])
                op("dve", lambda e: e.tensor_tensor(out=gate[:], in0=gate[:], in1=rank[:], op=ALU.add), ["gate", "rank"], ["gate"])
                op("pool", lambda e: e.memset(msk[64:128, 8, :], 0.0), [], ["msk"])
                tk = cst[:, C_TK:C_TK + 27].rearrange("p (t c) -> p t c", c=3)
                for c3 in range(3):
                    op("dve", lambda e, c3=c3: e.tensor_tensor(out=rhs6[:, :, :, c3], in0=msk[:],
                                                                in1=tk[:, :, c3:c3 + 1].to_broadcast([128, 9, 32]), op=ALU.mult),
                       ["msk", "cst"], ["rhs6"])
                op("dve", lambda e: e.tensor_tensor(out=rhs6[:, :, :, 3], in0=gate[:], in1=msk[:], op=ALU.mult), ["gate", "msk"], ["rhs6"])
                op("dve", lambda e: e.tensor_tensor(out=rank[:], in0=gate[:], in1=rhs6[:, :, :, 3], op=ALU.subtract), ["gate", "rhs6"], ["rank"])
                op("dve", lambda e: e.tensor_tensor(out=rhs6[:, :, :, 4], in0=rank[:], in1=msk[:], op=ALU.mult), ["rank", "msk"], ["rhs6"])
                op("dve", lambda e: e.tensor_tensor(out=rhs6[:, :, :, 5], in0=m2f[:], in1=msk[:], op=ALU.mult), ["m2f", "msk"], ["rhs6"])
                for ti in range(9):
                    def mmrk(e, ti=ti):
                        for tj in range(ti):
                            e.matmul(ps[4][:, 0:32], lhsT=ones_b[:], rhs=msk[:, tj, :], start=(tj == 0), stop=False)
                        return e.matmul(ps[4][:, 0:32], lhsT=ls_b[:], rhs=msk[:, ti, :], start=(ti == 0), stop=True)
                    op("pe", mmrk, ["ones_b", "ls_b", "msk"], [PS(4)])
                    op("dve", lambda e, ti=ti: e.tensor_copy(out=rank[:, ti, :], in_=ps[4][:, 0:32]), [PS(4)], ["rank"])
                op("dve", lambda e: e.tensor_copy(out=m2f[:], in_=msk[:]), ["msk"], ["m2f"])

                stage_end(8)
                def prepA(ex):
                    slot, idxg, idxs, xg = slot2[ex % 2], idxg2[ex % 2], idxs2[ex % 2], xg2[ex % 2]
                    kS, kG, kI, kX = "slot%d" % (ex % 2), "idxg%d" % (ex % 2), "idxs%d" % (ex % 2), "xg%d" % (ex % 2)
                    for ti in range(9):
                        op("dve", lambda e, ti=ti, ex=ex: e.tensor_scalar(
                            out=sel[:, ti, :], in0=cst[:, C_IO:C_IO + CAP], scalar1=rank[:, ti, ex:ex + 1], scalar2=m2f[:, ti, ex:ex + 1],
                            op0=ALU.is_equal, op1=ALU.mult), ["cst", "rank", "m2f"], ["sel"])
                    for sg in range(CAP // 128):
                        def mmsl(e, sg=sg, ex=ex):
                            for ti in range(9):
                                ins = e.matmul(ps[4][:, sg * 8:sg * 8 + 6], lhsT=sel[:, ti, sg * 128:(sg + 1) * 128], rhs=rhs6[:, ti, ex, :],
                                               start=(ti == 0), stop=(ti == 8))
                            return ins
                        op("pe", mmsl, ["sel", "rhs6"], [PS(4)])
                    op("dve", lambda e: e.tensor_copy(out=slot[:].rearrange("p a b -> p (a b)"), in_=ps[4][:, 0:16]), [PS(4)], [kS])
                    op("dve", lambda e: e.scalar_tensor_tensor(out=slot[:, :, 6], in0=slot[:, :, 0], scalar=32.0, in1=slot[:, :, 1],
                                                               op0=ALU.mult, op1=ALU.add), [kS], [kS])
                    op("dve", lambda e: e.tensor_scalar(out=slot[:, :, 7], in0=slot[:, :, 2], scalar1=-1.0, scalar2=-float(ZROW),
                                                        op0=ALU.add, op1=ALU.mult), [kS], [kS])
                    op("dve", lambda e: e.tensor_tensor(out=slot[:, :, 0], in0=slot[:, :, 6], in1=slot[:, :, 7], op=ALU.add), [kS], [kS])
                    op("dve", lambda e: e.tensor_copy(out=idxg[:], in_=slot[:, :, 0]), [kS], [kG])
                    op("dve", lambda e: e.scalar_tensor_tensor(out=slot[:, :, 1], in0=slot[:, :, 5], scalar=float(NT), in1=slot[:, :, 6],
                                                               op0=ALU.mult, op1=ALU.add), [kS], [kS])
                    op("dve", lambda e: e.tensor_scalar(out=slot[:, :, 7], in0=slot[:, :, 2], scalar1=-1.0, scalar2=-float(DUMP),
                                                        op0=ALU.add, op1=ALU.mult), [kS], [kS])
                    op("dve", lambda e: e.tensor_tensor(out=slot[:, :, 1], in0=slot[:, :, 1], in1=slot[:, :, 7], op=ALU.add), [kS], [kS])
                    op("dve", lambda e: e.tensor_copy(out=idxs[:], in_=slot[:, :, 1]), [kS], [kI])
                    op("dve", lambda e: e.tensor_tensor(out=slot[:, :, 3], in0=slot[:, :, 3], in1=slot[:, :, 4], op=ALU.add), [kS], [kS])
                    for sg in range(CAP // 128):
                        dma("pool", None, None, reads=[kG, "s_h3"], writes=[kX],
                            fn=lambda e, sg=sg: e.indirect_dma_start(
                                out=xg[:, sg, :], out_offset=None, in_=s_h3[:, :],
                                in_offset=bass.IndirectOffsetOnAxis(ap=idxg[:, sg:sg + 1], axis=0)))

                prepA(0)
                for ex in range(NE):
                    if ex + 1 < NE:
                        prepA(ex + 1)
                    slot, idxg, idxs, xg = slot2[ex % 2], idxg2[ex % 2], idxs2[ex % 2], xg2[ex % 2]
                    kS, kG, kI, kX = "slot%d" % (ex % 2), "idxg%d" % (ex % 2), "idxs%d" % (ex % 2), "xg%d" % (ex % 2)
                    for sg in range(CAP // 128):
                        for g in range(4):
                            b = 4 + g % 4
                            pv = ps[b][:].bitcast(BF16).rearrange("p (a b) -> p a b", a=8)

                            def trx(e, sg=sg, g=g, pv=pv):
                                for j in range(8):
                                    c = g * 8 + j
                                    ins = e.transpose(out=pv[:, j, :], in_=xg[:, sg, c * 128:(c + 1) * 128], identity=ident_b[:])
                                return ins
                            op("pe", trx, [kX, "ident_b"], [PS(b)])
                            if g % 2 == 0:
                                op("act", lambda e, sg=sg, g=g, pv=pv: e.activation(
                                    out=xgT[:, g * 8:(g + 1) * 8, sg * 128:(sg + 1) * 128], in_=pv, func=AF.Copy), [PS(b)], ["xgT"])
                            else:
                                op("dve", lambda e, sg=sg, g=g, pv=pv: e.tensor_copy(
                                    out=xgT[:, g * 8:(g + 1) * 8, sg * 128:(sg + 1) * 128], in_=pv), [PS(b)], ["xgT"])
                    for half in range(2):
                        for which in range(2):
                            wv_, kv_ = wnext()
                            for mm_ in range(2):
                                fcn = half * 2 + mm_
                                bq = (which * 2 + mm_) % 4

                                def mmg(e, b=bq, mm_=mm_, wv_=wv_):
                                    for k in range(32):
                                        ins = e.matmul(ps[b][:, 0:CAP], lhsT=wv_[:, k, mm_ * 128:(mm_ + 1) * 128], rhs=xgT[:, k, :],
                                                       start=(k == 0), stop=(k == 31))
                                    return ins
                                op("pe", mmg, [kv_, "xgT"], [PS(bq)])
                                if which == 0:
                                    op("act", lambda e, bq=bq, mm_=mm_: e.activation(out=sgl[:, mm_, :], in_=ps[bq][:, 0:CAP], func=AF.Silu),
                                       [PS(bq)], ["sgl"])
                                else:
                                    op("dve", lambda e, bq=bq, fcn=fcn, mm_=mm_: e.tensor_tensor(out=hidT[:, fcn, :], in0=ps[bq][:, 0:CAP],
                                                                                                 in1=sgl[:, mm_, :], op=ALU.mult),
                                       [PS(bq), "sgl"], ["hidT"])
                    for dh in range(2):
                      wdv, kdv = wnext()
                      for sg in range(CAP // 128):
                        yb = ye[sg % 2]
                        for cb in range(dh * 4, dh * 4 + 4):
                            wv, wk_ = wdv, kdv
                            b = 4 + cb % 4

                            def mmd(e, sg=sg, cb=cb, wv=wv, b=b):
                                for k in range(4):
                                    ins = e.matmul(ps[b][:, :], lhsT=hidT[:, k, sg * 128:(sg + 1) * 128],
                                                   rhs=wv[:, k, (cb % 4) * 512:(cb % 4) * 512 + 512], start=(k == 0), stop=(k == 3))
                                return ins
                            op("pe", mmd, ["hidT", wk_], [PS(b)])
                            if cb % 2 == 0:
                                op("act", lambda e, sg=sg, cb=cb, b=b, yb=yb: e.activation(
                                    out=yb[:, cb * 512:(cb + 1) * 512], in_=ps[b][:, :], func=AF.Copy, scale=slot[:, sg, 3:4]),
                                   [PS(b), kS], [yek[sg % 2]])
                            else:
                                op("dve", lambda e, sg=sg, cb=cb, b=b, yb=yb: e.tensor_scalar(
                                    out=yb[:, cb * 512:(cb + 1) * 512], in0=ps[b][:, :], scalar1=slot[:, sg, 3:4], scalar2=None, op0=ALU.mult),
                                   [PS(b), kS], [yek[sg % 2]])
                        if dh == 1:
                            dma("pool", None, None, reads=[kI, yek[sg % 2]], writes=["s_o12"],
                                fn=lambda e, sg=sg, yb=yb: e.indirect_dma_start(
                                    out=s_o12[:, :], out_offset=bass.IndirectOffsetOnAxis(ap=idxs[:, sg:sg + 1], axis=0),
                                    in_=yb[:, :], in_offset=None))

                stage_end(9)
                dma("sp", gbcf[:], norm_final_g.partition_broadcast(128), writes=["gbcf"])
                fsets = [
                    [(xt[:, :], [*XTK]), (hbf[:, :], ["hbf"]), (ye[0][:, :], ["ye0"])],
                    [(xgTbuf[:, :], ["xgT"]), (xg2[0][:].rearrange("p a d -> p (a d)").bitcast(F32), ["xg0"]),
                     (wsl[2][:].bitcast(F32), ["wsl2"])],
                ]
                def fin_loads(ti):
                    t0, rows = TILES[ti]
                    (A, kA), (Bf, kB), (Cf, kC) = fsets[ti % 2]
                    dma("sp", A[0:rows, :], s_x2[t0:t0 + rows, :], reads=["s_x2"], writes=kA)
                    dma("sp", Bf[0:rows, :], s_o12[t0:t0 + rows, :], reads=["s_o12"], writes=kB)
                    dma("sp", Cf[0:rows, :], s_o12[NT + t0:NT + t0 + rows, :], reads=["s_o12"], writes=kC)
                fin_loads(0)
                for ti, (t0, rows) in enumerate(TILES):
                    (A, kA), (Bf, kB), (Cf, kC) = fsets[ti % 2]
                    if ti + 1 < len(TILES):
                        fin_loads(ti + 1)
                    op("dve", lambda e, rows=rows, A=A, Bf=Bf: e.tensor_tensor(out=A[0:rows, :], in0=A[0:rows, :], in1=Bf[0:rows, :], op=ALU.add),
                       [*kA, *kB], kA)
                    op("dve", lambda e, rows=rows, A=A, Cf=Cf: e.tensor_tensor(out=A[0:rows, :], in0=A[0:rows, :], in1=Cf[0:rows, :], op=ALU.add),
                       [*kA, *kC], kA)
                    sc0 = 8 + 4 * (ti % 2)
                    ksm = "smf%d" % (ti % 2)
                    op("act", lambda e, rows=rows, A=A, sc0=sc0: e.activation(out=hb[0:rows, :], in_=A[0:rows, :], func=AF.Square,
                                                                               accum_out=sm[0:rows, sc0:sc0 + 1]), kA, [*HBK, ksm])
                    op("act", lambda e, rows=rows, sc0=sc0: e.activation(out=sm[0:rows, sc0 + 1:sc0 + 2], in_=sm[0:rows, sc0:sc0 + 1], func=AF.Sqrt,
                                                                          scale=1.0 / D, bias=EPS), [ksm], [ksm])
                    op("dve", lambda e, rows=rows, sc0=sc0: e.reciprocal(out=sm[0:rows, sc0 + 2:sc0 + 3], in_=sm[0:rows, sc0 + 1:sc0 + 2]), [ksm], [ksm])
                    op("dve", lambda e, rows=rows, A=A, Bf=Bf, sc0=sc0: e.scalar_tensor_tensor(
                        out=Bf[0:rows, :], in0=A[0:rows, :], scalar=sm[0:rows, sc0 + 2:sc0 + 3],
                        in1=gbcf[0:rows, :], op0=ALU.mult, op1=ALU.mult), [*kA, ksm, "gbcf"], kB)
                    dst = y_main[t0:t0 + rows, :] if ti < 8 else y_s[:, :]
                    dma("sp", dst, Bf[0:rows, :], reads=kB)
      except _Stop:
        S.finish()
        es.pop_all()
        return nc
      S.finish()
    return nc


_NC_CACHE = {}
_RET_MAPS = [False]


def kernel(**inp):
    f = lambda k: np.ascontiguousarray(np.asarray(inp[k], dtype=np.float32))
    x_prompt = f("x_prompt")
    x_sample = f("x_sample")
    consts = make_consts()
    convp = np.ascontiguousarray(np.concatenate(
        [f("conv_dw_w")[0], f("conv_dw_b")[0][None], f("conv_ln_g")[0][None], f("conv_ln_b")[0][None]], axis=0))
    w_router = np.ascontiguousarray(np.concatenate([f("w_router_group")[0], f("w_router_expert")[0]], axis=1))
    b_router = np.ascontiguousarray(np.concatenate([f("b_router_group")[0], f("b_router_expert")[0]], axis=0))
    shared = {
        "consts": consts,
        "norm_mix_g": f("norm_mix_g")[0], "w_in": f("w_in")[0], "w_alpha_up": f("w_alpha_up")[0],
        "b_alpha": f("b_alpha"), "gla_norm_g": f("gla_norm_g")[0], "w_branch_a": f("w_branch_a")[0],
        "convp_in": convp, "w_branch_b": f("w_branch_b")[0], "w_out": f("w_out")[0],
        "norm_ca_g": f("norm_ca_g")[0], "norm_mem_g": f("norm_mem_g")[0], "w_ca_q": f("w_ca_q")[0],
        "w_ca_k": f("w_ca_k")[0], "w_ca_v": f("w_ca_v")[0], "w_ca_o": f("w_ca_o")[0],
        "norm_ffn_g": f("norm_ffn_g")[0], "w_router": w_router, "b_router": b_router,
        "w_exp_gate": f("w_exp_gate")[0], "w_exp_up": f("w_exp_up")[0], "w_exp_down": f("w_exp_down")[0],
        "norm_final_g": f("norm_final_g"),
    }
    st_gla = f("state_gla")[0]
    st_conv = f("state_conv")[0]
    ck = f("cache_mem_k")[0].reshape(128, 256, 1024)
    cv = f("cache_mem_v")[0].reshape(128, 256, 1024)
    mem = f("mem_prompt")
    zeros_pre = np.zeros((NP, D), np.float32)
    in_maps = []
    for c in range(NCORE):
        b, half = c // 2, c % 2
        m = dict(shared)
        m["x_main"] = x_prompt[b, half * NP:(half + 1) * NP]
        m["x_pre"] = x_prompt[b, 0:NP] if half == 1 else zeros_pre
        m["x_s"] = x_sample[c * NSEQ:(c + 1) * NSEQ].reshape(NS, D)
        m["mem"] = mem[b]
        m["st_gla"] = st_gla[c * NSEQ:(c + 1) * NSEQ]
        m["st_conv"] = st_conv[c * NSEQ:(c + 1) * NSEQ]
        m["ck"] = ck[c * NSEQ:(c + 1) * NSEQ]
        m["cv"] = cv[c * NSEQ:(c + 1) * NSEQ]
        in_maps.append(m)
    if _RET_MAPS[0]:
        return in_maps
    if "nc" not in _NC_CACHE:
        _NC_CACHE["nc"] = build()
    nc = _NC_CACHE["nc"]
    res = run_bass_kernel_spmd(nc, in_maps, core_ids=list(range(NCORE)))
    r = res.results
    y_prompt = np.stack([np.concatenate([r[2 * b]["y_main"], r[2 * b + 1]["y_main"]], axis=0) for b in range(4)])
    y_sample = np.concatenate([r[c]["y_s"].reshape(NSEQ, 4, D) for c in range(NCORE)], axis=0)
    gla_prompt = np.stack([r[2 * b + 1]["gla_p"] for b in range(4)])[None]
    conv_prompt = np.stack([r[2 * b + 1]["conv_p"] for b in range(4)])[None]
    mk = np.stack([r[2 * b]["mk_o"].reshape(256, 4, 256) for b in range(4)])[None]
    mv = np.stack([r[2 * b]["mv_o"].reshape(256, 4, 256) for b in range(4)])[None]
    gla_sample = np.concatenate([r[c]["gla_s"] for c in range(NCORE)], axis=0)[None]
    conv_sample = np.concatenate([r[c]["conv_s"] for c in range(NCORE)], axis=0)[None]
    return (y_prompt.astype(np.float32), y_sample.astype(np.float32), gla_prompt.astype(np.float32),
            conv_prompt.astype(np.float32), mk.astype(np.float32), mv.astype(np.float32),
            gla_sample.astype(np.float32), conv_sample.astype(np.float32))
```
